# Optimizing a Trainium2 kernel written in Bass

```python
import math
import jax, jax.numpy as jnp
from jax import lax
import numpy as np

D_MODEL = 1024
BATCH = 8
SEQ = 2048
DEPTH = 4

GRID_W = 64
CTX_LEN = 256

MLA_HEADS = 8
MLA_Q_LORA = 384
MLA_KV_LORA = 256
MLA_NOPE = 64
MLA_ROPE = 32
MLA_V = 64
MLA_SCALE = (MLA_NOPE + MLA_ROPE) ** -0.5
ROPE_BASE = 10000.0
ROPE_FREQS = MLA_ROPE // 4
Q_BLOCK = 128

HG_HEADS = 4
HG_DK = 128
HG_DV = 128
HG_KW = HG_HEADS * HG_DK
HG_VW = HG_HEADS * HG_DV

GDN_HEADS = 4
GDN_DK = 128
GDN_DV = 128
GDN_KW = GDN_HEADS * GDN_DK
GDN_VW = GDN_HEADS * GDN_DV
GDN_CONV_W = 2 * GDN_KW + GDN_VW
CONV_K = 5

CHUNK = 64

N_BRANCH = 3
BRANCH_W = MLA_HEADS * MLA_V

N_EXPERTS = 64
TOP_K = 8
N_GROUPS = 8
TOPK_GROUPS = 4
EXPERT_FF = 256
SHARED_FF = 256
ROUTED_SCALE = 2.5
MOE_BLOCK = 128

DEEPNORM_ALPHA = (2 * DEPTH) ** 0.25
DEEPNORM_BETA = (8 * DEPTH) ** -0.25
LN_EPS = 1e-6
RMS_EPS = 1e-6

IN_SIZES = (MLA_Q_LORA, MLA_KV_LORA + MLA_ROPE,
            HG_KW, HG_VW, HG_KW, HG_KW, HG_VW,
            GDN_CONV_W, GDN_VW, 2 * GDN_HEADS, 2 * GDN_HEADS,
            N_BRANCH * D_MODEL)
IN_WIDTH = sum(IN_SIZES)

kernel_name = "hybrid_mla_hgrn2_gdn_moe_dit"


def in_offsets():
    return [int(o) for o in np.cumsum(IN_SIZES)[:-1]]


def layer_norm(x, g, b):
    xf = x.astype(jnp.float32)
    mu = jnp.mean(xf, -1, keepdims=True)
    var = jnp.mean(jnp.square(xf - mu), -1, keepdims=True)
    return ((xf - mu) * lax.rsqrt(var + LN_EPS) * g + b).astype(x.dtype)


def rms_norm(x, g):
    xf = x.astype(jnp.float32)
    return (xf * lax.rsqrt(jnp.mean(xf * xf, -1, keepdims=True) + RMS_EPS) * g).astype(x.dtype)


def l2norm(x):
    xf = x.astype(jnp.float32)
    return xf * lax.rsqrt(jnp.sum(xf * xf, -1, keepdims=True) + 1e-6)


def modulate(x, shift, scale):
    return x * (1 + scale) + shift


def axial_rope_tables(n_tokens):
    rows = n_tokens // GRID_W
    row = jnp.broadcast_to(jnp.arange(rows, dtype=jnp.float32)[:, None], (rows, GRID_W)).reshape(-1)
    col = jnp.broadcast_to(jnp.arange(GRID_W, dtype=jnp.float32)[None, :], (rows, GRID_W)).reshape(-1)
    inv = ROPE_BASE ** (-jnp.arange(ROPE_FREQS, dtype=jnp.float32) / ROPE_FREQS)
    ang = jnp.stack([row[:, None] * inv, col[:, None] * inv], axis=1)
    return jnp.cos(ang), jnp.sin(ang)


def rope2d(x, cos, sin):
    xr = x.reshape(x.shape[:-1] + (2, 2, ROPE_FREQS))
    x1, x2 = xr[..., 0, :], xr[..., 1, :]
    cos = cos.astype(x.dtype)
    sin = sin.astype(x.dtype)
    return jnp.stack([x1 * cos - x2 * sin, x2 * cos + x1 * sin], axis=-2).reshape(x.shape)


def centred_dwconv(x, w):
    return lax.conv_general_dilated(x, w[:, None, :], window_strides=(1,),
                                    padding=[(CONV_K // 2, CONV_K // 2)],
                                    dimension_numbers=('NWC', 'WIO', 'NWC'),
                                    feature_group_count=x.shape[-1])


def to_chunks(t):
    B, L, H, d = t.shape
    return t.reshape(B, L // CHUNK, CHUNK, H, d).transpose(1, 0, 3, 2, 4)


def from_chunks(t):
    N, B, H, C, d = t.shape
    return t.transpose(1, 0, 3, 2, 4).reshape(B, N * C, H, d)


def gla_scan(q, k, v, log_f, s0):
    f32 = jnp.float32
    idx = jnp.arange(CHUNK)
    incl = (idx[:, None] >= idx[None, :])[:, :, None]

    def step(S, inp):
        qc, kc, vc, gc = inp
        cg = jnp.cumsum(gc, axis=-2)
        gl = cg[:, :, -1:, :]
        decay = jnp.exp(jnp.where(incl, cg[:, :, :, None, :] - cg[:, :, None, :, :], -jnp.inf))
        a = jnp.einsum('bhid,bhjd,bhijd->bhij', qc, kc, decay)
        o = jnp.einsum('bhcd,bhde->bhce', qc * jnp.exp(cg), S) + jnp.einsum('bhij,bhje->bhie', a, vc)
        S = jnp.exp(gl[:, :, 0, :, None]) * S + jnp.einsum('bhcd,bhce->bhde', kc * jnp.exp(gl - cg), vc)
        return S, o

    xs = (to_chunks(q.astype(f32)), to_chunks(k.astype(f32)), to_chunks(v.astype(f32)), to_chunks(log_f.astype(f32)))
    S, o = lax.scan(step, s0, xs)
    return from_chunks(o), S


def gated_delta_scan(q, k, v, g, beta, s0):
    f32 = jnp.float32
    qc = to_chunks(q.astype(f32)) * GDN_DK ** -0.5
    kc = to_chunks(k.astype(f32))
    vc = to_chunks(v.astype(f32))
    dv = vc.shape[-1]
    gc = jnp.cumsum(to_chunks(g.astype(f32)[..., None])[..., 0], axis=-1)
    bc = to_chunks(beta.astype(f32)[..., None])
    idx = jnp.arange(CHUNK)
    incl = idx[:, None] >= idx[None, :]
    strict = idx[:, None] > idx[None, :]
    decay = jnp.exp(jnp.where(incl, gc[..., :, None] - gc[..., None, :], -jnp.inf))
    kb = kc * bc
    lmat = jnp.where(strict, jnp.einsum('nbhid,nbhjd->nbhij', kb, kc) * decay, 0.0)
    rhs = jnp.concatenate([vc * bc, kb * jnp.exp(gc)[..., None]], axis=-1)
    sol = lax.linalg.triangular_solve(jnp.eye(CHUNK, dtype=f32) + lmat, rhs,
                                      left_side=True, lower=True, unit_diagonal=True)
    u, w = sol[..., :dv], sol[..., dv:]
    aqk = jnp.einsum('nbhid,nbhjd->nbhij', qc, kc) * decay
    qd = qc * jnp.exp(gc)[..., None]
    kd = kc * jnp.exp(gc[..., -1:] - gc)[..., None]
    gl = jnp.exp(gc[..., -1])

    def step(S, inp):
        u_n, w_n, qd_n, kd_n, a_n, gl_n = inp
        v_new = u_n - jnp.einsum('bhcd,bhde->bhce', w_n, S)
        o = jnp.einsum('bhcd,bhde->bhce', qd_n, S) + jnp.einsum('bhij,bhje->bhie', a_n, v_new)
        S = gl_n[..., None, None] * S + jnp.einsum('bhcd,bhce->bhde', kd_n, v_new)
        return S, o

    S, o = lax.scan(step, s0, (u, w, qd, kd, aqk, gl))
    return from_chunks(o), S


def two_stream_scan(scan_fn, ctx_args, lat_args, s0, reverse):
    flip = (lambda t: jnp.flip(t, axis=1)) if reverse else (lambda t: t)
    o_ctx, s_ctx = scan_fn(*[flip(t) for t in ctx_args], s0)
    o_lat, _ = scan_fn(*[flip(t) for t in lat_args], s_ctx)
    return flip(o_ctx), flip(o_lat)


def gated_head_norm(o, gate, w):
    B, L, H, d = o.shape
    n = o * lax.rsqrt(jnp.mean(o * o, -1, keepdims=True) + RMS_EPS) * w.astype(jnp.float32)
    return (n.reshape(B, L, H * d) * jax.nn.silu(gate.astype(jnp.float32))).astype(gate.dtype)


def mla_attend(q_nope, q_rope, k_nope, k_rope, v):
    s = (jnp.einsum('bqhd,bkhd->bhqk', q_nope, k_nope)
         + jnp.einsum('bqhd,bkd->bhqk', q_rope, k_rope)).astype(jnp.float32) * MLA_SCALE
    p = jax.nn.softmax(s, axis=-1).astype(v.dtype)
    return jnp.einsum('bhqk,bkhd->bqhd', p, v)


def stream_proj(h, lp, lb):
    f32 = jnp.float32
    B, L, _ = h.shape
    z = h @ lp['w_in']
    (qa, kva, hq, hi, hf_f, hf_b, hgate, gqkv, ggate, ga, gb, gates) = jnp.split(z, in_offsets(), axis=-1)
    q = (rms_norm(qa, lp['q_a_norm']) @ lp['w_q_b']).reshape(B, L, MLA_HEADS, MLA_NOPE + MLA_ROPE)
    kv = (rms_norm(kva[..., :MLA_KV_LORA], lp['kv_a_norm']) @ lp['w_kv_b']).reshape(B, L, MLA_HEADS, MLA_NOPE + MLA_V)
    f_fwd = lb[0] + (1.0 - lb[0]) * jax.nn.sigmoid(hf_f.astype(f32))
    f_bwd = lb[1] + (1.0 - lb[1]) * jax.nn.sigmoid(hf_b.astype(f32))
    hgh = lambda t: t.reshape(B, L, HG_HEADS, -1)
    hq_feat = hgh(jax.nn.silu(hq))
    hv = hgh(hi)
    qkv = jax.nn.silu(centred_dwconv(gqkv, lp['gdn_conv']))
    gq, gk, gv = jnp.split(qkv, [GDN_KW, 2 * GDN_KW], axis=-1)
    gdh = lambda t: t.reshape(B, L, GDN_HEADS, -1)
    gq, gk, gv = l2norm(gdh(gq)), l2norm(gdh(gk)), gdh(gv)
    a = ga.astype(f32).reshape(B, L, 2, GDN_HEADS)
    decay = -jnp.exp(lp['gdn_a_log'].astype(f32)) * jax.nn.softplus(a + lp['gdn_dt_bias'].astype(f32))
    beta = jax.nn.sigmoid(gb.astype(f32)).reshape(B, L, 2, GDN_HEADS)
    return dict(
        q_nope=q[..., :MLA_NOPE], q_rope=q[..., MLA_NOPE:],
        k_nope=kv[..., :MLA_NOPE], v=kv[..., MLA_NOPE:], k_rope=kva[..., MLA_KV_LORA:],
        hg_fwd=(hq_feat, hgh(1.0 - f_fwd), hv, hgh(jnp.log(f_fwd))),
        hg_bwd=(hq_feat, hgh(1.0 - f_bwd), hv, hgh(jnp.log(f_bwd))),
        hg_gate=hgate,
        gdn_fwd=(gq, gk, gv, decay[:, :, 0], beta[:, :, 0]),
        gdn_bwd=(gq, gk, gv, decay[:, :, 1], beta[:, :, 1]),
        gdn_gate=ggate, gates=gates)


def merge_branches(gates, outs, w_branch, w_out):
    o = jnp.stack(outs, axis=2)
    proj = jnp.einsum('blnw,nwd->blnd', o, w_branch)
    y = jnp.einsum('blnd,blnd->bld', jax.nn.sigmoid(gates).reshape(proj.shape), proj)
    return y @ w_out


def mixer(h_lat, h_ctx, cos, sin, lp, lb, want_ctx):
    pl = stream_proj(h_lat, lp, lb)
    pc = stream_proj(h_ctx, lp, lb)
    B, L, _ = h_lat.shape
    q_rope = rope2d(pl['q_rope'], cos[:, None], sin[:, None])
    k_nope = jnp.concatenate([pc['k_nope'], pl['k_nope']], axis=1)
    k_rope = jnp.concatenate([pc['k_rope'], rope2d(pl['k_rope'], cos, sin)], axis=1)
    v = jnp.concatenate([pc['v'], pl['v']], axis=1)
    nb = L // Q_BLOCK
    blocks = lambda t: t.reshape((B, nb, Q_BLOCK) + t.shape[2:]).swapaxes(0, 1)
    o = lax.map(lambda qs: mla_attend(qs[0], qs[1], k_nope, k_rope, v), (blocks(pl['q_nope']), blocks(q_rope)))
    mla_lat = o.swapaxes(0, 1).reshape(B, L, BRANCH_W)
    s0h = jnp.zeros((B, HG_HEADS, HG_DK, HG_DV), jnp.float32)
    hc_f, hl_f = two_stream_scan(gla_scan, pc['hg_fwd'], pl['hg_fwd'], s0h, False)
    hc_b, hl_b = two_stream_scan(gla_scan, pc['hg_bwd'], pl['hg_bwd'], s0h, True)
    s0g = jnp.zeros((B, GDN_HEADS, GDN_DK, GDN_DV), jnp.float32)
    gc_f, gl_f = two_stream_scan(gated_delta_scan, pc['gdn_fwd'], pl['gdn_fwd'], s0g, False)
    gc_b, gl_b = two_stream_scan(gated_delta_scan, pc['gdn_bwd'], pl['gdn_bwd'], s0g, True)
    hg_lat = gated_head_norm(hl_f + hl_b, pl['hg_gate'], lp['hg_norm'])
    gdn_lat = gated_head_norm(gl_f + gl_b, pl['gdn_gate'], lp['gdn_norm'])
    y_lat = merge_branches(pl['gates'], (mla_lat, hg_lat, gdn_lat), lp['w_branch'], lp['w_out'])
    if not want_ctx:
        return y_lat, None
    mla_ctx = mla_attend(pc['q_nope'], pc['q_rope'], pc['k_nope'], pc['k_rope'], pc['v'])
    mla_ctx = mla_ctx.reshape(B, -1, BRANCH_W)
    hg_ctx = gated_head_norm(hc_f + hc_b, pc['hg_gate'], lp['hg_norm'])
    gdn_ctx = gated_head_norm(gc_f + gc_b, pc['gdn_gate'], lp['gdn_norm'])
    y_ctx = merge_branches(pc['gates'], (mla_ctx, hg_ctx, gdn_ctx), lp['w_branch'], lp['w_out'])
    return y_lat, y_ctx


def moe_ffn(h, lp):
    f32 = jnp.float32
    T, D = h.shape
    scores = jax.nn.sigmoid((h @ lp['w_router']).astype(f32))
    sel = scores + lp['router_bias'].astype(f32)
    grp_score = lax.top_k(sel.reshape(T, N_GROUPS, N_EXPERTS // N_GROUPS), 2)[0].sum(-1)
    _, gidx = lax.top_k(grp_score, TOPK_GROUPS)
    rows = jnp.arange(T)[:, None]
    gmask = jnp.zeros((T, N_GROUPS), bool).at[rows, gidx].set(True)
    sel = jnp.where(jnp.repeat(gmask, N_EXPERTS // N_GROUPS, axis=1), sel, -jnp.inf)
    _, eidx = lax.top_k(sel, TOP_K)
    w = jnp.take_along_axis(scores, eidx, axis=1)
    w = w / jnp.sum(w, -1, keepdims=True) * ROUTED_SCALE
    comb = jnp.zeros((T, N_EXPERTS), f32).at[rows, eidx].set(w).astype(h.dtype)

    def block(args):
        hb, cb = args
        gate, up = jnp.split(jnp.einsum('td,edf->etf', hb, lp['w_gu']), 2, axis=-1)
        act = jax.nn.silu(gate) * up * cb.T[:, :, None]
        return jnp.einsum('etf,efd->td', act, lp['w_down'])

    nb = T // MOE_BLOCK
    routed = lax.map(block, (h.reshape(nb, MOE_BLOCK, D), comb.reshape(nb, MOE_BLOCK, N_EXPERTS))).reshape(T, D)
    gs, us = jnp.split(h @ lp['w_sh_gu'], 2, axis=-1)
    return routed + (jax.nn.silu(gs) * us) @ lp['w_sh_down']


def setup_inputs(seed: int = 0) -> dict:
    key = jax.random.key(seed)
    ks = jax.random.split(key, 32)
    D = D_MODEL
    nrm = lambda k, shape, s: jax.random.normal(k, shape, jnp.float32) * s
    dt = jnp.exp(jax.random.uniform(ks[15], (DEPTH, 2, GDN_HEADS), jnp.float32,
                                    minval=math.log(1e-3), maxval=math.log(1e-1)))
    return {
        "x": nrm(ks[0], (BATCH, SEQ, D), 1.0),
        "c": nrm(ks[1], (BATCH, D), 1.0),
        "ctx": nrm(ks[2], (BATCH, CTX_LEN, D), 1.0),
        "c_ctx": nrm(ks[3], (D,), 1.0),
        "w_mod": nrm(ks[4], (DEPTH, D, 6 * D), 0.5 * D ** -0.5),
        "b_mod": nrm(ks[5], (DEPTH, 6 * D), 0.02),
        "w_in": nrm(ks[6], (DEPTH, D, IN_WIDTH), D ** -0.5),
        "q_a_norm": 1.0 + nrm(ks[7], (DEPTH, MLA_Q_LORA), 0.02),
        "w_q_b": nrm(ks[8], (DEPTH, MLA_Q_LORA, MLA_HEADS * (MLA_NOPE + MLA_ROPE)), MLA_Q_LORA ** -0.5),
        "kv_a_norm": 1.0 + nrm(ks[9], (DEPTH, MLA_KV_LORA), 0.02),
        "w_kv_b": nrm(ks[10], (DEPTH, MLA_KV_LORA, MLA_HEADS * (MLA_NOPE + MLA_V)), MLA_KV_LORA ** -0.5),
        "hg_lb_logits": nrm(ks[11], (DEPTH, 2, HG_KW), 0.5),
        "hg_norm": 1.0 + nrm(ks[12], (DEPTH, HG_DV), 0.02),
        "gdn_conv": nrm(ks[13], (DEPTH, CONV_K, GDN_CONV_W), CONV_K ** -0.5),
        "gdn_a_log": jnp.log(jax.random.uniform(ks[14], (DEPTH, 2, GDN_HEADS), jnp.float32, minval=1.0, maxval=16.0)),
        "gdn_dt_bias": dt + jnp.log(-jnp.expm1(-dt)),
        "gdn_norm": 1.0 + nrm(ks[16], (DEPTH, GDN_DV), 0.02),
        "w_branch": nrm(ks[17], (DEPTH, N_BRANCH, BRANCH_W, D), BRANCH_W ** -0.5 * DEEPNORM_BETA),
        "w_out": nrm(ks[18], (DEPTH, D, D), D ** -0.5 * DEEPNORM_BETA),
        "ln1_g": 1.0 + nrm(ks[19], (DEPTH, D), 0.02),
        "ln1_b": nrm(ks[20], (DEPTH, D), 0.02),
        "ln2_g": 1.0 + nrm(ks[21], (DEPTH, D), 0.02),
        "ln2_b": nrm(ks[22], (DEPTH, D), 0.02),
        "w_router": nrm(ks[23], (DEPTH, D, N_EXPERTS), D ** -0.5),
        "router_bias": nrm(ks[24], (DEPTH, N_EXPERTS), 0.01),
        "w_gu": nrm(ks[25], (DEPTH, N_EXPERTS, D, 2 * EXPERT_FF), D ** -0.5),
        "w_down": nrm(ks[26], (DEPTH, N_EXPERTS, EXPERT_FF, D), EXPERT_FF ** -0.5 * DEEPNORM_BETA),
        "w_sh_gu": nrm(ks[27], (DEPTH, D, 2 * SHARED_FF), D ** -0.5),
        "w_sh_down": nrm(ks[28], (DEPTH, SHARED_FF, D), SHARED_FF ** -0.5 * DEEPNORM_BETA),
    }


def reference(x, c, ctx, c_ctx, w_mod, b_mod, w_in, q_a_norm, w_q_b, kv_a_norm, w_kv_b, hg_lb_logits, hg_norm,
              gdn_conv, gdn_a_log, gdn_dt_bias, gdn_norm, w_branch, w_out, ln1_g, ln1_b, ln2_g, ln2_b,
              w_router, router_bias, w_gu, w_down, w_sh_gu, w_sh_down):
    B, L, D = x.shape
    cos, sin = axial_rope_tables(L)
    lb_soft = jax.nn.softmax(hg_lb_logits.astype(jnp.float32), axis=0)
    lower_bounds = jnp.cumsum(lb_soft, axis=0) - lb_soft[0]
    silu_c = jax.nn.silu(c)
    silu_cc = jax.nn.silu(c_ctx)
    xc = ctx
    for l in range(DEPTH):
        last = l == DEPTH - 1
        lp = dict(w_in=w_in[l], q_a_norm=q_a_norm[l], w_q_b=w_q_b[l], kv_a_norm=kv_a_norm[l], w_kv_b=w_kv_b[l],
                  hg_norm=hg_norm[l], gdn_conv=gdn_conv[l], gdn_a_log=gdn_a_log[l], gdn_dt_bias=gdn_dt_bias[l],
                  gdn_norm=gdn_norm[l], w_branch=w_branch[l], w_out=w_out[l], w_router=w_router[l],
                  router_bias=router_bias[l], w_gu=w_gu[l], w_down=w_down[l], w_sh_gu=w_sh_gu[l],
                  w_sh_down=w_sh_down[l])
        mod = jnp.split((silu_c @ w_mod[l] + b_mod[l])[:, None, :], 6, axis=-1)
        modc = jnp.split((silu_cc @ w_mod[l] + b_mod[l])[None, None, :], 6, axis=-1)
        y, yc = mixer(modulate(x, mod[0], mod[1]), modulate(xc, modc[0], modc[1]), cos, sin, lp,
                      lower_bounds[l], not last)
        x = layer_norm(DEEPNORM_ALPHA * x + mod[2] * y, ln1_g[l], ln1_b[l])
        h = modulate(x, mod[3], mod[4])
        if last:
            f = moe_ffn(h.reshape(-1, D), lp).reshape(h.shape)
        else:
            xc = layer_norm(DEEPNORM_ALPHA * xc + modc[2] * yc, ln1_g[l], ln1_b[l])
            hc = modulate(xc, modc[3], modc[4])
            ff = moe_ffn(jnp.concatenate([h.reshape(-1, D), hc.reshape(-1, D)], axis=0), lp)
            f = ff[:B * L].reshape(h.shape)
            xc = layer_norm(DEEPNORM_ALPHA * xc + modc[5] * ff[B * L:].reshape(hc.shape), ln2_g[l], ln2_b[l])
        x = layer_norm(DEEPNORM_ALPHA * x + mod[5] * f, ln2_g[l], ln2_b[l])
    return x
```

```python
import contextlib
import numpy as np
import concourse.bass as bass
import concourse.mybir as mybir
from concourse.bass_utils import run_bass_kernel_spmd

F32 = mybir.dt.float32
BF16 = mybir.dt.bfloat16
AF = mybir.ActivationFunctionType
ALU = mybir.AluOpType
AX = mybir.AxisListType

EPOCH = 30000
NRING = 40

DEPTH = 4
D = 1024
KC = 8
LAT = 2048
CTX = 256
T = LAT + CTX
NT = T // 128
GROUPS = [(0, 256), (256, 512), (768, 512), (1280, 512), (1792, 512)]
NCH = T // 64
IN_W = 8368
MLA_SCALE = 96 ** -0.5
ALPHA = (2 * DEPTH) ** 0.25
O_HG = 672
O_GDN = O_HG + 2560
O_GG = O_GDN + 1536
O_GA = O_GG + 512
O_GB = O_GA + 8
O_GATES = O_GB + 8


class Buf:
    __slots__ = ("name", "w", "r", "ex")

    def __init__(self, name="", ex=False):
        self.name = name
        self.w = None
        self.r = []
        self.ex = ex


class Sched:
    def __init__(self, nc, es, self_sync=True):
        self.nc = nc
        self.es = es
        self.eng = {"pe": nc.tensor, "act": nc.scalar, "dve": nc.vector, "pool": nc.gpsimd, "sp": nc.sync}
        self.seq = {e: 0 for e in self.eng}
        self.sems = {e: [] for e in self.eng}
        self.known = {e: {} for e in self.eng}
        self.known_dma = {e: set() for e in self.eng}
        self.ring = [es.enter_context(nc.semaphore("dr%d" % i)) for i in range(NRING)]
        self.ring_cnt = [0] * NRING
        self.ring_next = 0
        self.self_sync = self_sync
        self.ninstr = 0

    def _sem(self, e, ep):
        while len(self.sems[e]) <= ep:
            self.sems[e].append(self.es.enter_context(self.nc.semaphore("s_%s%d" % (e, len(self.sems[e])))))
        return self.sems[e][ep]

    def _wait(self, e, tok):
        if tok is None:
            return
        if tok[0] == "dma":
            _, k, val = tok
            key = (k, val)
            if key in self.known_dma[e]:
                return
            self.eng[e].wait_ge(self.ring[k], val)
            self.known_dma[e].add(key)
            return
        _, e2, s = tok
        if e2 == e and (e == "pe" or not self.self_sync):
            return
        if self.known[e].get(e2, 0) >= s:
            return
        ep = (s - 1) // EPOCH
        self.eng[e].wait_ge(self._sem(e2, ep), s - ep * EPOCH)
        self.known[e][e2] = s

    def _deps(self, e, reads, writes):
        for b in reads:
            if b.w is not None:
                self._wait(e, b.w)
            if b.ex:
                for t in b.r:
                    if t[0] == "eng" and t[1] != e:
                        self._wait(e, t)
        for b in writes:
            if b.w is not None:
                self._wait(e, b.w)
            for t in b.r:
                self._wait(e, t)

    def _mark(self, tok, reads, writes):
        for b in reads:
            b.r.append(tok)
            if len(b.r) > 24:
                b.r = self._prune(b.r)
        for b in writes:
            b.w = tok
            b.r = []

    def _prune(self, r):
        best = {}
        out = []
        for t in r:
            if t[0] == "dma":
                out.append(t)
            elif t[1] not in best or best[t[1]][2] < t[2]:
                best[t[1]] = t
        return out + list(best.values())

    def op(self, e, fn, reads=(), writes=()):
        self._deps(e, reads, writes)
        ins = fn()
        self.seq[e] += 1
        s = self.seq[e]
        ep = (s - 1) // EPOCH
        ins.then_inc(self._sem(e, ep), 1)
        tok = ("eng", e, s)
        self._mark(tok, reads, writes)
        self.ninstr += 1
        return tok

    def dma(self, out, in_, reads=(), writes=(), q="sp", **kw):
        k = self.ring_next
        self.ring_next = (self.ring_next + 1) % NRING
        prev = self.ring_cnt[k]
        if prev > 0:
            self._wait(q, ("dma", k, 16 * prev))
        self._deps(q, reads, writes)
        self.eng[q].dma_start(out=out, in_=in_, **kw).then_inc(self.ring[k], 16)
        self.ring_cnt[k] = prev + 1
        tok = ("dma", k, 16 * (prev + 1))
        self._mark(tok, reads, writes)
        self.ninstr += 1
        return tok

    def barrier(self, engines=None):
        for e in (engines or self.eng):
            for e2 in self.eng:
                if self.seq[e2] > 0 and not (e2 == e and e == "pe"):
                    self._wait(e, ("eng", e2, self.seq[e2]))
            for k in range(NRING):
                if self.ring_cnt[k] > 0:
                    self._wait(e, ("dma", k, 16 * self.ring_cnt[k]))

    def finish(self):
        self.barrier(["sp"])


class Ctx:
    pass


def build_program(cfg):
    nlayers = cfg.get("nlayers", DEPTH)
    stop_after = cfg.get("stop_after", None)
    dbg = cfg.get("debug", [])
    nc = bass.Bass("TRN2", target_bir_lowering=False)
    K = Ctx()
    K.nc = nc

    def din(name, shape, dt=F32):
        return nc.dram_tensor(name, list(shape), dt, kind="ExternalInput").ap()

    def dscr(name, shape, dt=F32):
        return nc.dram_tensor(name, list(shape), dt, kind="Internal").ap()

    I = {}
    I["xin"] = din("xin", [T, D])
    I["cvecT"] = din("cvecT", [D, 2])
    for nm, shp in WEIGHT_SHAPES.items():
        I[nm] = din(nm, shp)
    for nm, shp in CONST_SHAPES.items():
        I[nm] = din(nm, shp)
    for nm, shp in DERIVED_SHAPES.items():
        I[nm] = din(nm, shp)
    out_d = nc.dram_tensor("out", [LAT, D], F32, kind="ExternalOutput").ap()
    DBG = {}
    for nm, shp in dbg:
        DBG[nm] = nc.dram_tensor("dbg_" + nm, list(shp), F32, kind="ExternalOutput").ap()

    modv_d = dscr("modv_d", [DEPTH, 2, 6 * D])
    xres_d = dscr("xres_d", [T, D])
    mla_o_d = dscr("mla_o_d", [8, 64, T], BF16)
    hg_o_d = dscr("hg_o_d", [4, 128, T], BF16)
    gdn_o_d = dscr("gdn_o_d", [4, 128, T], BF16)
    gdn_raw_d = dscr("gdn_raw_d", [2, T, 512])

    es = contextlib.ExitStack()
    with es:
        S = Sched(nc, es)
        K.S = S

        tlc = [0]

        def tl(st, name, shape, dt):
            tlc[0] += 1
            t = st.enter_context(nc.sbuf_tensor("sb%d_%s" % (tlc[0], name), list(shape), dt))
            return t, Buf(name)

        PS = []
        PB = []
        for i in range(8):
            PS.append(es.enter_context(nc.psum_tensor("ps%d" % i, [128, 512], F32)))
            PB.append(Buf("ps%d" % i, ex=True))

        identf, b_identf = tl(es, "identf", [128, 128], F32)
        identb, b_identb = tl(es, "identb", [128, 128], BF16)
        onesb, b_onesb = tl(es, "onesb", [128, 128], BF16)
        onesf, b_onesf = tl(es, "onesf", [128, 128], F32)
        S.dma(identf[:], I["ident"][:, :], writes=[b_identf])
        S.dma(identb[:], I["ident"][:, :], writes=[b_identb], q="pool")
        S.op("dve", lambda: nc.vector.memset(onesb[:], 1.0), writes=[b_onesb])
        S.op("dve", lambda: nc.vector.memset(onesf[:], 1.0), writes=[b_onesf])
        h_fm, b_hfm = tl(es, "h_fm", [128, KC, T], BF16)
        epsb, b_eps = tl(es, "epsb", [128, 1], F32)
        S.op("dve", lambda: nc.vector.memset(epsb[:], 1e-6), writes=[b_eps])

        def dbg_out(name, ap_sb, buf, dram_ap=None):
            if name in DBG:
                S.dma(dram_ap if dram_ap is not None else DBG[name], ap_sb, reads=[buf])

        with contextlib.ExitStack() as st:
            cv, b_cv = tl(st, "cv", [128, KC, 2], F32)
            scv, b_scv = tl(st, "scv", [128, KC, 2], F32)
            S.dma(cv[:], I["cvecT"].rearrange("(kc p) s -> p kc s", p=128), writes=[b_cv])
            S.op("act", lambda: nc.scalar.activation(out=scv[:], in_=cv[:], func=AF.Silu), reads=[b_cv], writes=[b_scv])
            wm = [tl(st, "wm%d" % i, [128, KC, 512], F32) for i in range(2)]
            bm, b_bm = tl(st, "bm", [1, 6 * D], F32)
            mv = [tl(st, "mv%d" % i, [2, 6 * D], F32) for i in range(2)]
            ones2, b_ones2 = tl(st, "ones2", [1, 2], F32)
            S.op("dve", lambda: nc.vector.memset(ones2[:], 1.0), writes=[b_ones2])
            cnt = 0
            for l in range(nlayers):
                mvt, b_mv = mv[l % 2]
                S.dma(bm[:], I["b_mod"][l:l + 1, :], writes=[b_bm])
                for cg in range(12):
                    wt, b_wt = wm[cnt % 2]
                    cnt += 1
                    S.dma(wt[:], I["w_mod"][l].rearrange("(kc p) n -> p kc n", p=128)[:, :, cg * 512:(cg + 1) * 512], writes=[b_wt])
                    pb = cnt % 2
                    for kc in range(KC):
                        S.op("pe", lambda kc=kc, wt=wt, pb=pb: nc.tensor.matmul(PS[pb][0:2, :], lhsT=scv[:, kc, :], rhs=wt[:, kc, :], start=(kc == 0), stop=False),
                             reads=[b_scv, b_wt], writes=[PB[pb]])
                    S.op("pe", lambda cg=cg, pb=pb: nc.tensor.matmul(PS[pb][0:2, :], lhsT=ones2[:], rhs=bm[:, cg * 512:(cg + 1) * 512], start=False, stop=True),
                         reads=[b_ones2, b_bm], writes=[PB[pb]])
                    S.op("act", lambda cg=cg, pb=pb, mvt=mvt: nc.scalar.copy(out=mvt[:, cg * 512:(cg + 1) * 512], in_=PS[pb][0:2, :]), reads=[PB[pb]], writes=[b_mv])
                S.dma(modv_d[l], mvt[:], reads=[b_mv], writes=[])
                K.modv_tok = None
            S.barrier()
        b_modv = Buf("modv_d")
        b_modv.w = None

        def load_bc(st, name, l, stream, j, plus_one=False):
            t, b = tl(st, name, [128, D], F32)
            S.dma(t[:], modv_d[l, stream:stream + 1, j * D:(j + 1) * D].partition_broadcast(128), writes=[b])
            if plus_one:
                S.op("pool", lambda: nc.gpsimd.tensor_scalar_add(out=t[:], in0=t[:], scalar1=1.0), reads=[b], writes=[b])
            return t, b

        def load_vec_bc(st, name, dram_row_ap, n=D):
            t, b = tl(st, name, [128, n], F32)
            S.dma(t[:], dram_row_ap.partition_broadcast(128), writes=[b])
            return t, b

        def to_fm(src_bf, b_src, i, psb):
            pv = PS[psb][:].bitcast(BF16)
            for kc in range(KC):
                S.op("pe", lambda kc=kc: nc.tensor.transpose(out=pv[:, kc * 128:(kc + 1) * 128], in_=src_bf[:, kc * 128:(kc + 1) * 128], identity=identb[:]),
                     reads=[b_src, b_identb], writes=[PB[psb]])
            S.op("act", lambda: nc.scalar.copy(out=h_fm[:, :, i * 128:(i + 1) * 128], in_=pv.rearrange("p (k t) -> p k t", k=KC)),
                 reads=[PB[psb]], writes=[b_hfm])

        def stage_entry(l):
            with contextlib.ExitStack() as st:
                bc = {}
                for s in range(2):
                    bc[(s, 0)] = load_bc(st, "bsh%d" % s, l, s, 0)
                    bc[(s, 1)] = load_bc(st, "bsc%d" % s, l, s, 1, plus_one=True)
                xt = [tl(st, "xt%d" % i, [128, D], F32) for i in range(2)]
                ht = [tl(st, "ht%d" % i, [128, D], BF16) for i in range(2)]
                for i in range(NT):
                    s = 1 if i < 2 else 0
                    x_t, b_x = xt[i % 2]
                    h_t, b_h = ht[i % 2]
                    S.dma(x_t[:], I["xin"][i * 128:(i + 1) * 128, :], writes=[b_x])
                    S.dma(xres_d[i * 128:(i + 1) * 128, :], x_t[:], reads=[b_x])
                    S.op("dve", lambda x_t=x_t, s=s: nc.vector.tensor_tensor(out=x_t[:], in0=x_t[:], in1=bc[(s, 1)][0][:], op=ALU.mult),
                         reads=[b_x, bc[(s, 1)][1]], writes=[b_x])
                    S.op("dve", lambda x_t=x_t, h_t=h_t, s=s: nc.vector.tensor_tensor(out=h_t[:], in0=x_t[:], in1=bc[(s, 0)][0][:], op=ALU.add),
                         reads=[b_x, bc[(s, 0)][1]], writes=[b_h])
                    to_fm(h_t, b_h, i, i % 2)
                S.barrier()


        lbT, b_lbT = tl(es, "lbT", [128, DEPTH, 8], F32)
        omlbT, b_omlbT = tl(es, "omlbT", [128, DEPTH, 8], F32)
        rmask, b_rmask = tl(es, "rmask", [128, T], BF16)
        triu, b_triu = tl(es, "triu", [64, 64], F32)
        tril, b_tril = tl(es, "tril", [64, 64], F32)
        S.dma(rmask[:], I["rmask"][:, :], writes=[b_rmask], q="pool")
        S.dma(triu[:], I["triu"][:, :], writes=[b_triu])
        S.dma(tril[:], I["tril"][:, :], writes=[b_tril])
        with contextlib.ExitStack() as st:
            lg, b_lg = tl(st, "lg", [32, 128], F32)
            eT, b_eT = tl(st, "eT", [128, DEPTH, 8], F32)
            tot, b_tot = tl(st, "lbtot", [128, 8], F32)
            S.dma(lg[:], I["hg_lb_logits"].rearrange("l s (h p) -> (l s h) p", p=128), writes=[b_lg])
            S.op("act", lambda: nc.scalar.activation(out=lg[:], in_=lg[:], func=AF.Exp), reads=[b_lg], writes=[b_lg])
            S.op("pe", lambda: nc.tensor.transpose(out=PS[0][:, 0:32], in_=lg[:], identity=identf[0:32, 0:32]), reads=[b_lg, b_identf], writes=[PB[0]])
            S.op("act", lambda: nc.scalar.copy(out=eT[:].rearrange("p l x -> p (l x)"), in_=PS[0][:, 0:32]), reads=[PB[0]], writes=[b_eT])
            S.op("dve", lambda: nc.vector.tensor_tensor(out=tot[:], in0=eT[:, 0, :], in1=eT[:, 1, :], op=ALU.add), reads=[b_eT], writes=[b_tot])
            S.op("dve", lambda: nc.vector.tensor_tensor(out=tot[:], in0=tot[:], in1=eT[:, 2, :], op=ALU.add), reads=[b_eT, b_tot], writes=[b_tot])
            S.op("dve", lambda: nc.vector.tensor_tensor(out=tot[:], in0=tot[:], in1=eT[:, 3, :], op=ALU.add), reads=[b_eT, b_tot], writes=[b_tot])
            S.op("dve", lambda: nc.vector.reciprocal(out=tot[:], in_=tot[:]), reads=[b_tot], writes=[b_tot])
            S.op("dve", lambda: nc.vector.memset(lbT[:, 0, :], 0.0), writes=[b_lbT])
            S.op("dve", lambda: nc.vector.tensor_copy(out=lbT[:, 1, :], in_=eT[:, 1, :]), reads=[b_eT, b_lbT], writes=[b_lbT])
            S.op("dve", lambda: nc.vector.tensor_tensor(out=lbT[:, 2, :], in0=lbT[:, 1, :], in1=eT[:, 2, :], op=ALU.add), reads=[b_eT, b_lbT], writes=[b_lbT])
            S.op("dve", lambda: nc.vector.tensor_tensor(out=lbT[:, 3, :], in0=lbT[:, 2, :], in1=eT[:, 3, :], op=ALU.add), reads=[b_eT, b_lbT], writes=[b_lbT])
            for l in range(1, DEPTH):
                S.op("dve", lambda l=l: nc.vector.tensor_tensor(out=lbT[:, l, :], in0=lbT[:, l, :], in1=tot[:], op=ALU.mult), reads=[b_tot, b_lbT], writes=[b_lbT])
            S.op("dve", lambda: nc.vector.tensor_scalar(out=omlbT[:], in0=lbT[:], scalar1=-1.0, scalar2=1.0, op0=ALU.mult, op1=ALU.add), reads=[b_lbT], writes=[b_omlbT])
            S.barrier()

        def stage_hgrn(l):
            winv = I["w_in"][l].rearrange("(kc p) n -> p kc n", p=128)
            with contextlib.ExitStack() as st:
                wh = [tl(st, "wh%d" % i, [128, KC, 5, 128], BF16) for i in range(2)]
                hgn4, b_hgn = tl(st, "hgn", [128, DEPTH], F32)
                S.dma(hgn4[:], I["hg_norm_t"][:, :], writes=[b_hgn])
                hgn = hgn4[:, l:l + 1]
                q_bf, b_q = tl(st, "hq_bf", [128, T], BF16)
                gate_sb, b_gate = tl(st, "hgate", [128, T], BF16)
                v_tm, b_v = tl(st, "hv_tm", [64, NCH, 128], BF16)
                A, b_A = tl(st, "hA", [128, T], F32)
                B, b_B = tl(st, "hB", [128, T], F32)
                Cc, b_C = tl(st, "hC", [128, T], F32)
                qt, b_qt = tl(st, "hqt", [128, T], BF16)
                kt, b_kt = tl(st, "hkt", [128, T], BF16)
                qh, b_qh = tl(st, "hqh", [128, T], BF16)
                kh, b_kh = tl(st, "hkh", [128, T], BF16)
                khT, b_khT = tl(st, "hkhT", [64, NCH, 128], BF16)
                aT, b_aT = tl(st, "haT", [64, NCH, 64], BF16)
                o_d = [tl(st, "ho%d" % i, [128, T], F32) for i in range(2)]
                tot, b_tot = tl(st, "htot", [128, NCH], F32)
                rmid, b_rmid = tl(st, "hrmid", [128, NCH], F32)
                egl, b_egl = tl(st, "hegl", [128, NCH], F32)
                Sst, b_S = tl(st, "hS", [128, 128], F32)
                Sb = [tl(st, "hSb%d" % i, [128, 128], BF16) for i in range(2)]
                rs0, b_rs0 = tl(st, "hrs0", [128, 512], F32)
                rs1, b_rs1 = tl(st, "hrs1", [128, 512], F32)
                og = [tl(st, "hog%d" % i, [128, 512], BF16) for i in range(2)]
                b_ho = Buf("hg_o")
                C3 = Cc[:].rearrange("p (c k) -> p c k", k=64)
                B3 = B[:].rearrange("p (c k) -> p c k", k=64)
                pcnt = [0]

                def proj(col, wt, b_wt, fn_evac):
                    for (t0, n) in GROUPS:
                        pb = pcnt[0] % 2
                        pcnt[0] += 1
                        for kc in range(KC):
                            S.op("pe", lambda kc=kc, pb=pb: nc.tensor.matmul(PS[pb][:, 0:n], lhsT=wt[:, kc, col, :], rhs=h_fm[:, kc, t0:t0 + n], start=(kc == 0), stop=(kc == KC - 1)),
                                 reads=[b_wt, b_hfm], writes=[PB[pb]])
                        fn_evac(pb, t0, n)

                ocnt = 0
                for hd in range(4):
                    wt, b_wt = wh[hd % 2]
                    for ci in range(5):
                        c0 = O_HG + ci * 512 + hd * 128
                        S.dma(wt[:, :, ci, :], winv[:, :, c0:c0 + 128], writes=[b_wt], q="pool")
                    proj(0, wt, b_wt, lambda pb, t0, n: S.op("act", lambda: nc.scalar.activation(out=q_bf[:, t0:t0 + n], in_=PS[pb][:, 0:n], func=AF.Silu), reads=[PB[pb]], writes=[b_q]))
                    proj(4, wt, b_wt, lambda pb, t0, n: S.op("act", lambda: nc.scalar.activation(out=gate_sb[:, t0:t0 + n], in_=PS[pb][:, 0:n], func=AF.Silu), reads=[PB[pb]], writes=[b_gate]))
                    for c4 in range(NCH // 4):
                        pb = pcnt[0] % 2
                        pcnt[0] += 1
                        for j in range(4):
                            c = c4 * 4 + j
                            for kc in range(KC):
                                S.op("pe", lambda kc=kc, c=c, j=j, pb=pb: nc.tensor.matmul(PS[pb][0:64, j * 128:(j + 1) * 128], lhsT=h_fm[:, kc, c * 64:(c + 1) * 64], rhs=wt[:, kc, 1, :],
                                                                                         start=(kc == 0), stop=(kc == KC - 1)), reads=[b_wt, b_hfm], writes=[PB[pb]])
                        S.op("act", lambda c4=c4, pb=pb: nc.scalar.copy(out=v_tm[:, c4 * 4:(c4 + 1) * 4, :], in_=PS[pb][0:64, :].rearrange("p (j x) -> p j x", j=4)),
                             reads=[PB[pb]], writes=[b_v])
                    for s in range(2):
                        o_t, b_o = o_d[s]
                        lbc = lbT[:, l, s * 4 + hd:s * 4 + hd + 1]
                        omc = omlbT[:, l, s * 4 + hd:s * 4 + hd + 1]
                        proj(2 + s, wt, b_wt, lambda pb, t0, n: S.op("act", lambda: nc.scalar.activation(out=A[:, t0:t0 + n], in_=PS[pb][:, 0:n], func=AF.Sigmoid), reads=[PB[pb]], writes=[b_A]))
                        S.op("dve", lambda: nc.vector.tensor_scalar(out=A[:], in0=A[:], scalar1=omc, scalar2=lbc, op0=ALU.mult, op1=ALU.add), reads=[b_A, b_lbT, b_omlbT], writes=[b_A])
                        S.op("act", lambda: nc.scalar.activation(out=B[:], in_=A[:], func=AF.Ln), reads=[b_A], writes=[b_B])
                        S.op("pool", lambda: nc.gpsimd.tensor_scalar(out=A[:], in0=A[:], scalar1=-1.0, scalar2=1.0, op0=ALU.mult, op1=ALU.add), reads=[b_A, b_B], writes=[b_A])
                        S.op("dve", lambda: nc.vector.tensor_tensor_scan(out=Cc[:], data0=rmask[:], data1=B[:], initial=0.0, op0=ALU.mult, op1=ALU.add), reads=[b_rmask, b_B], writes=[b_C])
                        S.op("dve", lambda: nc.vector.tensor_copy(out=tot[:], in_=C3[:, :, 63]), reads=[b_C], writes=[b_tot])
                        totb = tot[:].unsqueeze(2).to_broadcast([128, NCH, 64])
                        if s == 1:
                            S.op("dve", lambda: nc.vector.scalar_tensor_tensor(out=C3, in0=C3, scalar=-1.0, in1=totb, op0=ALU.mult, op1=ALU.add), reads=[b_C, b_tot], writes=[b_C])
                            S.op("dve", lambda: nc.vector.tensor_tensor(out=Cc[:], in0=Cc[:], in1=B[:], op=ALU.add), reads=[b_C, b_B], writes=[b_C])
                        S.op("dve", lambda: nc.vector.tensor_copy(out=rmid[:], in_=C3[:, :, 31 + s]), reads=[b_C], writes=[b_rmid])
                        S.op("act", lambda: nc.scalar.activation(out=egl[:], in_=tot[:], func=AF.Exp), reads=[b_tot], writes=[b_egl])
                        S.op("dve", lambda: nc.vector.tensor_tensor(out=B3, in0=C3, in1=rmid[:].unsqueeze(2).to_broadcast([128, NCH, 64]), op=ALU.subtract), reads=[b_C, b_rmid, b_B], writes=[b_B])
                        S.op("act", lambda: nc.scalar.activation(out=B[:], in_=B[:], func=AF.Exp), reads=[b_B], writes=[b_B])
                        S.op("pool", lambda: nc.gpsimd.tensor_tensor(out=qt[:], in0=q_bf[:], in1=B[:], op=ALU.mult), reads=[b_q, b_B], writes=[b_qt])
                        S.op("dve", lambda: nc.vector.reciprocal(out=B[:], in_=B[:]), reads=[b_B, b_qt], writes=[b_B])
                        S.op("dve", lambda: nc.vector.tensor_tensor(out=kt[:], in0=A[:], in1=B[:], op=ALU.mult), reads=[b_A, b_B], writes=[b_kt])
                        S.op("act", lambda: nc.scalar.activation(out=B[:], in_=Cc[:], func=AF.Exp), reads=[b_C, b_kt], writes=[b_B])
                        S.op("pool", lambda: nc.gpsimd.tensor_tensor(out=qh[:], in0=q_bf[:], in1=B[:], op=ALU.mult), reads=[b_q, b_B], writes=[b_qh])
                        S.op("dve", lambda: nc.vector.scalar_tensor_tensor(out=B3, in0=C3, scalar=-1.0, in1=totb, op0=ALU.mult, op1=ALU.add), reads=[b_C, b_tot, b_qh], writes=[b_B])
                        S.op("act", lambda: nc.scalar.activation(out=B[:], in_=B[:], func=AF.Exp), reads=[b_B], writes=[b_B])
                        S.op("dve", lambda: nc.vector.tensor_tensor(out=kh[:], in0=A[:], in1=B[:], op=ALU.mult), reads=[b_A, b_B], writes=[b_kh])
                        pvb = PS[2][:].bitcast(BF16)
                        msk = triu if s == 0 else tril
                        b_msk = b_triu if s == 0 else b_tril
                        fr = 0 if s == 0 else 32
                        dr = 32 - fr
                        S.op("dve", lambda: nc.vector.memset(aT[dr:dr + 32, :, fr:fr + 32], 0.0), writes=[b_aT])
                        for c8 in range(0, NCH, 8):
                            nb = min(8, NCH - c8)
                            for j in range(nb):
                                c = c8 + j
                                S.op("pe", lambda c=c, j=j: nc.tensor.transpose(out=pvb[0:64, j * 128:(j + 1) * 128], in_=kh[:, c * 64:(c + 1) * 64], identity=identb[:]),
                                     reads=[b_kh, b_identb], writes=[PB[2]])
                            S.op("act", lambda c8=c8, nb=nb: nc.scalar.copy(out=khT[:, c8:c8 + nb, :], in_=pvb[0:64, 0:nb * 128].rearrange("p (j x) -> p j x", j=nb)),
                                 reads=[PB[2]], writes=[b_khT])
                            for j in range(nb):
                                c = c8 + j
                                S.op("pe", lambda c=c, j=j: nc.tensor.matmul(PS[3][fr:fr + 32, j * 64:(j + 1) * 64], lhsT=kt[:, c * 64 + fr:c * 64 + fr + 32], rhs=qt[:, c * 64:(c + 1) * 64], start=True, stop=True),
                                     reads=[b_kt, b_qt], writes=[PB[3]])
                                S.op("pe", lambda c=c, j=j: nc.tensor.matmul(PS[3][dr:dr + 32, j * 64 + dr:j * 64 + dr + 32], lhsT=kt[:, c * 64 + dr:c * 64 + dr + 32], rhs=qt[:, c * 64 + dr:c * 64 + dr + 32], start=True, stop=True),
                                     reads=[b_kt, b_qt], writes=[PB[3]])
                            pv3 = PS[3][:, 0:nb * 64].rearrange("p (j x) -> p j x", j=nb)
                            S.op("dve", lambda c8=c8, nb=nb, pv3=pv3: nc.vector.tensor_tensor(out=aT[fr:fr + 32, c8:c8 + nb, :], in0=pv3[fr:fr + 32, :, :],
                                                                                     in1=msk[fr:fr + 32, :].unsqueeze(1).to_broadcast([32, nb, 64]), op=ALU.mult),
                                 reads=[PB[3], b_msk], writes=[b_aT])
                            S.op("dve", lambda c8=c8, nb=nb, pv3=pv3: nc.vector.tensor_tensor(out=aT[dr:dr + 32, c8:c8 + nb, dr:dr + 32], in0=pv3[dr:dr + 32, :, dr:dr + 32],
                                                                                     in1=msk[dr:dr + 32, dr:dr + 32].unsqueeze(1).to_broadcast([32, nb, 32]), op=ALU.mult),
                                 reads=[PB[3], b_msk], writes=[b_aT])
                        order = list(range(NCH)) if s == 0 else [3, 2, 1, 0] + list(range(NCH - 1, 3, -1))
                        for idx, c in enumerate(order):
                            po = 4 + idx % 2
                            pS = 6 + idx % 2
                            if idx > 0:
                                sb_t, b_sb = Sb[idx % 2]
                                S.op("pe", lambda c=c, po=po, sb_t=sb_t: nc.tensor.matmul(PS[po][:, 0:64], lhsT=sb_t[:], rhs=qh[:, c * 64:(c + 1) * 64], start=True, stop=False),
                                     reads=[b_sb, b_qh], writes=[PB[po]])
                            S.op("pe", lambda c=c, po=po, idx=idx: nc.tensor.matmul(PS[po][:, 0:64], lhsT=v_tm[:, c, :], rhs=aT[:, c, :], start=(idx == 0), stop=True),
                                 reads=[b_v, b_aT], writes=[PB[po]])
                            S.op("act", lambda c=c, po=po: nc.scalar.copy(out=o_t[:, c * 64:(c + 1) * 64], in_=PS[po][:, 0:64]), reads=[PB[po]], writes=[b_o])
                            if idx < NCH - 1:
                                S.op("pe", lambda c=c, pS=pS: nc.tensor.matmul(PS[pS][:, 0:128], lhsT=khT[:, c, :], rhs=v_tm[:, c, :], start=True, stop=True),
                                     reads=[b_khT, b_v], writes=[PB[pS]])
                                if idx == 0:
                                    S.op("dve", lambda pS=pS: nc.vector.tensor_copy(out=Sst[:], in_=PS[pS][:, 0:128]), reads=[PB[pS]], writes=[b_S])
                                else:
                                    S.op("dve", lambda c=c, pS=pS: nc.vector.scalar_tensor_tensor(out=Sst[:], in0=Sst[:], scalar=egl[:, c:c + 1], in1=PS[pS][:, 0:128], op0=ALU.mult, op1=ALU.add),
                                         reads=[b_S, b_egl, PB[pS]], writes=[b_S])
                                nsb, b_nsb = Sb[(idx + 1) % 2]
                                S.op("act", lambda nsb=nsb: nc.scalar.copy(out=nsb[:], in_=Sst[:]), reads=[b_S], writes=[b_nsb])
                    o_f, b_of = o_d[0]
                    o_b, b_ob = o_d[1]
                    S.op("dve", lambda: nc.vector.tensor_tensor(out=o_f[:], in0=o_f[:], in1=o_b[:], op=ALU.add), reads=[b_of, b_ob], writes=[b_of])
                    S.op("act", lambda: nc.scalar.activation(out=qt[:], in_=o_f[:], func=AF.Square), reads=[b_of, b_qt], writes=[b_qt])
                    for (t0, n) in GROUPS:
                        pb = pcnt[0] % 2
                        pcnt[0] += 1
                        og_t, b_og = og[ocnt % 2]
                        ocnt += 1
                        S.op("pe", lambda pb=pb: nc.tensor.matmul(PS[pb][:, 0:n], lhsT=onesb[:], rhs=qt[:, t0:t0 + n], start=True, stop=True), reads=[b_onesb, b_qt], writes=[PB[pb]])
                        S.op("act", lambda pb=pb: nc.scalar.activation(out=rs0[:, 0:n], in_=PS[pb][:, 0:n], func=AF.Sqrt, scale=1.0 / 128, bias=epsb[:, 0:1]), reads=[PB[pb], b_eps], writes=[b_rs0])
                        S.op("dve", lambda: nc.vector.reciprocal(out=rs1[:, 0:n], in_=rs0[:, 0:n]), reads=[b_rs0], writes=[b_rs1])
                        S.op("dve", lambda: nc.vector.scalar_tensor_tensor(out=rs0[:, 0:n], in0=o_f[:, t0:t0 + n], scalar=hgn, in1=rs1[:, 0:n], op0=ALU.mult, op1=ALU.mult),
                             reads=[b_of, b_hgn, b_rs1, b_rs0], writes=[b_rs0])
                        S.op("dve", lambda og_t=og_t: nc.vector.tensor_tensor(out=og_t[:, 0:n], in0=rs0[:, 0:n], in1=gate_sb[:, t0:t0 + n], op=ALU.mult), reads=[b_rs0, b_gate], writes=[b_og])
                        S.dma(hg_o_d[hd, :, t0:t0 + n], og_t[:, 0:n], reads=[b_og], writes=[b_ho])
                S.barrier()


        def stage_gdn(l):
            winv = I["w_in"][l].rearrange("(kc p) n -> p kc n", p=128)
            with contextlib.ExitStack() as st0:
                mist, b_mist = tl(st0, "mist", [64, 2, 64], F32)
                mast, b_mast = tl(st0, "mast", [64, 2, 64], F32)
                S.dma(mist[:], I["mist"][:, :, :], writes=[b_mist])
                S.dma(mast[:], I["mast"][:, :, :], writes=[b_mast])
                g_t, b_g = tl(st0, "g_g", [64, NCH, 8], F32)
                beta, b_beta = tl(st0, "g_beta", [64, NCH, 8], F32)
                nbeta, b_nbeta = tl(st0, "g_nbeta", [64, NCH, 8], F32)
                egc, b_egc = tl(st0, "g_egc", [64, NCH, 8], F32)
                negc, b_negc = tl(st0, "g_negc", [64, NCH, 8], F32)
                ekd, b_ekd = tl(st0, "g_ekd", [64, NCH, 8], F32)
                egl, b_egl = tl(st0, "g_egl", [128, NCH, 8], F32)
                cw, b_cw = tl(st0, "g_cw", [128, 12, 5], F32)
                S.dma(cw[:], I["gdn_conv_t"][l], writes=[b_cw])
                with contextlib.ExitStack() as st:
                    wg, b_wg = tl(st, "g_wg", [128, KC, 16], BF16)
                    S.dma(wg[:], winv[:, :, O_GA:O_GA + 16], writes=[b_wg], q="pool")
                    gab, b_gab = tl(st, "g_gab", [64, NCH, 16], F32)
                    alog, b_alog = tl(st, "g_alog", [64, 8], F32)
                    dtb, b_dtb = tl(st, "g_dtb", [64, 8], F32)
                    S.dma(alog[:], I["gdn_a_log"][l:l + 1].rearrange("o s h -> o (s h)").partition_broadcast(64), writes=[b_alog])
                    S.dma(dtb[:], I["gdn_dt_bias"][l:l + 1].rearrange("o s h -> o (s h)").partition_broadcast(64), writes=[b_dtb])
                    for c in range(NCH):
                        pb = c // 32
                        cc = c % 32
                        for kc in range(KC):
                            S.op("pe", lambda c=c, kc=kc, pb=pb, cc=cc: nc.tensor.matmul(PS[pb][0:64, cc * 16:(cc + 1) * 16], lhsT=h_fm[:, kc, c * 64:(c + 1) * 64], rhs=wg[:, kc, :],
                                                                                     start=(kc == 0), stop=(kc == KC - 1)), reads=[b_hfm, b_wg], writes=[PB[pb]])
                    S.op("act", lambda: nc.scalar.copy(out=gab[:, 0:32, :], in_=PS[0][0:64, :].rearrange("p (c x) -> p c x", x=16)), reads=[PB[0]], writes=[b_gab])
                    S.op("act", lambda: nc.scalar.copy(out=gab[:, 32:36, :], in_=PS[1][0:64, 0:64].rearrange("p (c x) -> p c x", x=16)), reads=[PB[1]], writes=[b_gab])
                    S.op("act", lambda: nc.scalar.activation(out=alog[:], in_=alog[:], func=AF.Exp), reads=[b_alog], writes=[b_alog])
                    S.op("dve", lambda: nc.vector.tensor_tensor(out=g_t[:], in0=gab[:, :, 0:8], in1=dtb[:].unsqueeze(1).to_broadcast([64, NCH, 8]), op=ALU.add), reads=[b_gab, b_dtb], writes=[b_g])
                    S.op("act", lambda: nc.scalar.activation(out=g_t[:], in_=g_t[:], func=AF.Exp), reads=[b_g], writes=[b_g])
                    S.op("act", lambda: nc.scalar.activation(out=g_t[:], in_=g_t[:], func=AF.Ln, bias=onesf[0:64, 0:1]), reads=[b_g, b_onesf], writes=[b_g])
                    S.op("dve", lambda: nc.vector.scalar_tensor_tensor(out=g_t[:], in0=g_t[:], scalar=-1.0, in1=alog[:].unsqueeze(1).to_broadcast([64, NCH, 8]), op0=ALU.mult, op1=ALU.mult),
                         reads=[b_g, b_alog], writes=[b_g])
                    S.op("act", lambda: nc.scalar.activation(out=beta[:], in_=gab[:, :, 8:16], func=AF.Sigmoid), reads=[b_gab], writes=[b_beta])
                    S.op("dve", lambda: nc.vector.tensor_scalar(out=nbeta[:], in0=beta[:], scalar1=-1.0, scalar2=None, op0=ALU.mult), reads=[b_beta], writes=[b_nbeta])
                    for c in range(NCH):
                        for sd in range(2):
                            S.op("pe", lambda c=c, sd=sd: nc.tensor.matmul(PS[2][0:64, c * 8 + sd * 4:c * 8 + sd * 4 + 4], lhsT=mist[:, sd, :], rhs=g_t[:, c, sd * 4:sd * 4 + 4], start=True, stop=True),
                                 reads=[b_mist, b_g], writes=[PB[2]])
                            S.op("pe", lambda c=c, sd=sd: nc.tensor.matmul(PS[3][0:64, c * 8 + sd * 4:c * 8 + sd * 4 + 4], lhsT=mast[:, sd, :], rhs=g_t[:, c, sd * 4:sd * 4 + 4], start=True, stop=True),
                                 reads=[b_mast, b_g], writes=[PB[3]])
                        S.op("pe", lambda c=c: nc.tensor.matmul(PS[4][:, c * 8:c * 8 + 8], lhsT=onesf[0:64, :], rhs=g_t[:, c, :], start=True, stop=True), reads=[b_onesf, b_g], writes=[PB[4]])
                    S.op("act", lambda: nc.scalar.activation(out=egc[:].rearrange("p c x -> p (c x)"), in_=PS[2][0:64, 0:NCH * 8], func=AF.Exp), reads=[PB[2]], writes=[b_egc])
                    S.op("act", lambda: nc.scalar.activation(out=ekd[:].rearrange("p c x -> p (c x)"), in_=PS[3][0:64, 0:NCH * 8], func=AF.Exp), reads=[PB[3]], writes=[b_ekd])
                    S.op("act", lambda: nc.scalar.activation(out=egl[:].rearrange("p c x -> p (c x)"), in_=PS[4][:, 0:NCH * 8], func=AF.Exp), reads=[PB[4]], writes=[b_egl])
                    S.op("dve", lambda: nc.vector.tensor_scalar(out=negc[:], in0=egc[:], scalar1=-1.0, scalar2=None, op0=ALU.mult), reads=[b_egc], writes=[b_negc])
                    S.barrier()
                if cfg.get("gdn_stop") == 1:
                    S.barrier()
                    return

                for pr in range(2):
                    with contextlib.ExitStack() as st:
                        q_fm = [tl(st, "g_q%d" % i, [128, T], BF16) for i in range(2)]
                        k_fm = [tl(st, "g_k%d" % i, [128, T], BF16) for i in range(2)]
                        k_tm = [tl(st, "g_ktm%d" % i, [64, NCH, 128], BF16) for i in range(2)]
                        v_tm = [tl(st, "g_vtm%d" % i, [64, NCH, 128], BF16) for i in range(2)]
                        aqkT, b_aqkT = tl(st, "g_aqkT", [64, NCH, 4, 64], BF16)
                        R5b, b_R5b = tl(st, "g_R5b", [64, NCH, 4, 64], BF16)
                        with contextlib.ExitStack() as st2:
                            wc = [tl(st2, "g_wc%d" % i, [128, KC, 128], BF16) for i in range(2)]
                            zpad, b_zp = tl(st2, "g_zpad", [128, T + 8], F32)
                            acc, b_acc = tl(st2, "g_acc", [128, T + 8], F32)
                            xs, b_xs = tl(st2, "g_xs", [128, T], F32)
                            sqb, b_sqb = tl(st2, "g_sq", [128, T], BF16)
                            vfm, b_vfm = tl(st2, "g_vfm", [128, T], BF16)
                            r0, b_r0 = tl(st2, "g_r0", [128, 512], F32)
                            r1, b_r1 = tl(st2, "g_r1", [128, 512], F32)
                            S.op("dve", lambda: nc.vector.memset(zpad[:], 0.0), writes=[b_zp])
                            wcnt = 0
                            for hh in range(2):
                                hd = pr * 2 + hh
                                for part in range(3):
                                    ch = part * 4 + hd
                                    wct, b_wc = wc[wcnt % 2]
                                    wcnt += 1
                                    S.dma(wct[:], winv[:, :, O_GDN + ch * 128:O_GDN + (ch + 1) * 128], writes=[b_wc], q="pool")
                                    for gi, (t0, n) in enumerate(GROUPS):
                                        pb = gi % 2
                                        for kc in range(KC):
                                            S.op("pe", lambda kc=kc, pb=pb, wct=wct: nc.tensor.matmul(PS[pb][:, 0:n], lhsT=wct[:, kc, :], rhs=h_fm[:, kc, t0:t0 + n], start=(kc == 0), stop=(kc == KC - 1)),
                                                 reads=[b_wc, b_hfm], writes=[PB[pb]])
                                        z0 = 2 + t0 if gi == 0 else 6 + t0
                                        S.op("act", lambda pb=pb, z0=z0: nc.scalar.copy(out=zpad[:, z0:z0 + n], in_=PS[pb][:, 0:n]), reads=[PB[pb]], writes=[b_zp])
                                    NW = T + 4
                                    S.op("dve", lambda ch=ch: nc.vector.tensor_scalar(out=acc[:, 2:2 + NW], in0=zpad[:, 0:NW], scalar1=cw[:, ch, 0:1], scalar2=None, op0=ALU.mult),
                                         reads=[b_zp, b_cw], writes=[b_acc])
                                    for tau in range(1, 5):
                                        S.op("dve", lambda ch=ch, tau=tau: nc.vector.scalar_tensor_tensor(out=acc[:, 2:2 + NW], in0=zpad[:, tau:tau + NW], scalar=cw[:, ch, tau:tau + 1], in1=acc[:, 2:2 + NW],
                                                                                                        op0=ALU.mult, op1=ALU.add), reads=[b_zp, b_cw, b_acc], writes=[b_acc])
                                    if part == 2:
                                        S.op("act", lambda: nc.scalar.activation(out=vfm[:, 0:CTX], in_=acc[:, 2:2 + CTX], func=AF.Silu), reads=[b_acc], writes=[b_vfm])
                                        S.op("act", lambda: nc.scalar.activation(out=vfm[:, CTX:T], in_=acc[:, 6 + CTX:6 + T], func=AF.Silu), reads=[b_acc], writes=[b_vfm])
                                        srcs = [(vfm, b_vfm, v_tm[hh])]
                                    else:
                                        S.op("act", lambda: nc.scalar.activation(out=xs[:, 0:CTX], in_=acc[:, 2:2 + CTX], func=AF.Silu), reads=[b_acc], writes=[b_xs])
                                        S.op("act", lambda: nc.scalar.activation(out=xs[:, CTX:T], in_=acc[:, 6 + CTX:6 + T], func=AF.Silu), reads=[b_acc], writes=[b_xs])
                                        S.op("act", lambda: nc.scalar.activation(out=sqb[:], in_=xs[:], func=AF.Square), reads=[b_xs], writes=[b_sqb])
                                        dst, b_dst = (q_fm if part == 0 else k_fm)[hh]
                                        for gi, (t0, n) in enumerate(GROUPS):
                                            pb = 2 + gi % 2
                                            S.op("pe", lambda pb=pb: nc.tensor.matmul(PS[pb][:, 0:n], lhsT=onesb[:], rhs=sqb[:, t0:t0 + n], start=True, stop=True), reads=[b_onesb, b_sqb], writes=[PB[pb]])
                                            S.op("act", lambda pb=pb: nc.scalar.activation(out=r0[:, 0:n], in_=PS[pb][:, 0:n], func=AF.Sqrt, bias=epsb[:, 0:1]), reads=[PB[pb], b_eps], writes=[b_r0])
                                            S.op("dve", lambda: nc.vector.reciprocal(out=r1[:, 0:n], in_=r0[:, 0:n]), reads=[b_r0], writes=[b_r1])
                                            S.op("dve", lambda dst=dst: nc.vector.scalar_tensor_tensor(out=dst[:, t0:t0 + n], in0=xs[:, t0:t0 + n], scalar=(128 ** -0.5 if part == 0 else 1.0), in1=r1[:, 0:n],
                                                                                                   op0=ALU.mult, op1=ALU.mult), reads=[b_xs, b_r1], writes=[b_dst])
                                        srcs = [(dst, b_dst, k_tm[hh])] if part == 1 else []
                                    for (src, b_src, (dtm, b_dtm)) in srcs:
                                        pvb = PS[4][:].bitcast(BF16)
                                        pvb2 = PS[5][:].bitcast(BF16)
                                        for c8 in range(0, NCH, 8):
                                            nb = min(8, NCH - c8)
                                            pv = pvb if (c8 // 8) % 2 == 0 else pvb2
                                            pbi = 4 + (c8 // 8) % 2
                                            for j in range(nb):
                                                c = c8 + j
                                                S.op("pe", lambda c=c, j=j, pv=pv, src=src: nc.tensor.transpose(out=pv[0:64, j * 128:(j + 1) * 128], in_=src[:, c * 64:(c + 1) * 64], identity=identb[:]),
                                                     reads=[b_src, b_identb], writes=[PB[pbi]])
                                            S.op("act", lambda c8=c8, nb=nb, pv=pv, dtm=dtm: nc.scalar.copy(out=dtm[:, c8:c8 + nb, :], in_=pv[0:64, 0:nb * 128].rearrange("p (j x) -> p j x", j=nb)),
                                                 reads=[PB[pbi]], writes=[b_dtm])
                            S.barrier()
                        if cfg.get("gdn_stop") == 2:
                            S.barrier()
                            return
                        with contextlib.ExitStack() as st2:
                            lI, b_lI = tl(st2, "g_lI", [64, 4, 64], F32)
                            lA, b_lA = tl(st2, "g_lA", [64, 4, 64], F32)
                            Dec, b_Dec = tl(st2, "g_Dec", [64, 4, 64], F32)
                            DecT, b_DecT = tl(st2, "g_DecT", [64, 4, 64], F32)
                            t1, b_t1 = tl(st2, "g_t1", [64, 4, 64], F32)
                            Pm = [tl(st2, "g_P%d" % i, [64, 4, 64], F32) for i in range(2)]
                            Qm = [tl(st2, "g_Q%d" % i, [64, 4, 64], F32) for i in range(2)]
                            Rm = [tl(st2, "g_R%d" % i, [64, 4, 64], F32) for i in range(2)]
                            def gcols(tile_, c):
                                return tile_[:, c, :].rearrange("p (s h) -> p s h", s=2)[:, :, pr * 2:pr * 2 + 2]
                            sub = cfg.get("gdn_sub", 99)
                            for c in range(cfg.get("gdn_nch", NCH)):
                                cs = slice(c * 64, (c + 1) * 64)
                                for hh in range(2):
                                    kf, b_kf = k_fm[hh]
                                    qf, b_qf = q_fm[hh]
                                    S.op("pe", lambda hh=hh, kf=kf: nc.tensor.matmul(PS[0][0:64, hh * 64:(hh + 1) * 64], lhsT=kf[:, cs], rhs=kf[:, cs], start=True, stop=True), reads=[b_kf], writes=[PB[0]])
                                    S.op("pe", lambda hh=hh, kf=kf, qf=qf: nc.tensor.matmul(PS[0][0:64, 128 + hh * 64:128 + (hh + 1) * 64], lhsT=kf[:, cs], rhs=qf[:, cs], start=True, stop=True),
                                         reads=[b_kf, b_qf], writes=[PB[0]])
                                if sub < 1:
                                    continue
                                g4 = gcols(g_t, c).unsqueeze(3).to_broadcast([64, 2, 2, 64])
                                S.op("dve", lambda g4=g4: nc.vector.tensor_tensor(out=lI[:].rearrange("p (s h) x -> p s h x", s=2), in0=mist[:].unsqueeze(2).to_broadcast([64, 2, 2, 64]), in1=g4, op=ALU.mult),
                                     reads=[b_mist, b_g], writes=[b_lI])
                                S.op("dve", lambda g4=g4: nc.vector.tensor_tensor(out=lA[:].rearrange("p (s h) x -> p s h x", s=2), in0=mast[:].unsqueeze(2).to_broadcast([64, 2, 2, 64]), in1=g4, op=ALU.mult),
                                     reads=[b_mast, b_g], writes=[b_lA])
                                if sub < 2:
                                    continue
                                for b in range(4):
                                    sd = b // 2
                                    S.op("pe", lambda b=b, sd=sd: nc.tensor.matmul(PS[1][0:64, b * 64:(b + 1) * 64], lhsT=lI[:, b, :], rhs=mast[:, sd, :], start=True, stop=True), reads=[b_lI, b_mast], writes=[PB[1]])
                                    S.op("pe", lambda b=b, sd=sd: nc.tensor.matmul(PS[2][0:64, b * 64:(b + 1) * 64], lhsT=lA[:, b, :], rhs=mist[:, sd, :], start=True, stop=True), reads=[b_lA, b_mist], writes=[PB[2]])
                                S.op("act", lambda: nc.scalar.activation(out=Dec[:].rearrange("p b x -> p (b x)"), in_=PS[1][0:64, 0:256], func=AF.Exp), reads=[PB[1]], writes=[b_Dec])
                                S.op("act", lambda: nc.scalar.activation(out=DecT[:].rearrange("p b x -> p (b x)"), in_=PS[2][0:64, 0:256], func=AF.Exp), reads=[PB[2]], writes=[b_DecT])
                                if sub < 3:
                                    continue
                                KKv = PS[0][0:64, 0:128].rearrange("p (h x) -> p h x", h=2).unsqueeze(1).to_broadcast([64, 2, 2, 64])
                                QKv = PS[0][0:64, 128:256].rearrange("p (h x) -> p h x", h=2).unsqueeze(1).to_broadcast([64, 2, 2, 64])
                                v4 = lambda t_: t_[:].rearrange("p (s h) x -> p s h x", s=2)
                                P0, b_P0 = Pm[0]
                                Q0, b_Q0 = Qm[0]
                                R0, b_R0 = Rm[0]
                                S.op("dve", lambda: nc.vector.tensor_tensor(out=v4(t1), in0=KKv, in1=v4(Dec), op=ALU.mult), reads=[PB[0], b_Dec], writes=[b_t1])
                                S.op("dve", lambda: nc.vector.tensor_tensor(out=v4(t1), in0=v4(t1), in1=mast[:].unsqueeze(2).to_broadcast([64, 2, 2, 64]), op=ALU.mult), reads=[b_t1, b_mast], writes=[b_t1])
                                S.op("dve", lambda: nc.vector.tensor_tensor(out=v4(P0), in0=v4(t1), in1=gcols(nbeta, c).unsqueeze(3).to_broadcast([64, 2, 2, 64]), op=ALU.mult),
                                     reads=[b_t1, b_nbeta], writes=[b_P0])
                                S.op("dve", lambda: nc.vector.tensor_tensor(out=v4(DecT), in0=QKv, in1=v4(DecT), op=ALU.mult), reads=[PB[0], b_DecT], writes=[b_DecT])
                                S.op("dve", lambda c=c: nc.vector.tensor_tensor(out=aqkT[:, c, :, :].rearrange("p (s h) x -> p s h x", s=2), in0=v4(DecT), in1=mist[:].unsqueeze(2).to_broadcast([64, 2, 2, 64]), op=ALU.mult),
                                     reads=[b_DecT, b_mist], writes=[b_aqkT])
                                if sub < 4:
                                    continue
                                for b in range(4):
                                    S.op("pe", lambda b=b: nc.tensor.transpose(out=PS[3][0:64, b * 64:(b + 1) * 64], in_=P0[:, b, :], identity=identf[0:64, 0:64]), reads=[b_P0, b_identf], writes=[PB[3]])
                                S.op("act", lambda: nc.scalar.copy(out=Q0[:].rearrange("p b x -> p (b x)"), in_=PS[3][0:64, 0:256]), reads=[PB[3]], writes=[b_Q0])
                                S.op("dve", lambda: nc.vector.tensor_tensor(out=R0[:], in0=Q0[:], in1=identf[0:64, 0:64].unsqueeze(1).to_broadcast([64, 4, 64]), op=ALU.add),
                                     reads=[b_Q0, b_identf], writes=[b_R0])
                                if sub < 5:
                                    continue
                                for k in range(1, 6):
                                    Pp, b_Pp = Pm[(k - 1) % 2]
                                    Qp, b_Qp = Qm[(k - 1) % 2]
                                    Rp, b_Rp = Rm[(k - 1) % 2]
                                    Pn, b_Pn = Pm[k % 2]
                                    Qn, b_Qn = Qm[k % 2]
                                    Rn, b_Rn = Rm[k % 2]
                                    for b in range(4):
                                        S.op("pe", lambda b=b: nc.tensor.matmul(PS[4][0:64, b * 64:(b + 1) * 64], lhsT=Qp[:, b, :], rhs=Pp[:, b, :], start=True, stop=True), reads=[b_Qp, b_Pp], writes=[PB[4]])
                                    S.op("act", lambda: nc.scalar.copy(out=Pn[:].rearrange("p b x -> p (b x)"), in_=PS[4][0:64, 0:256]), reads=[PB[4]], writes=[b_Pn])
                                    if k < 5:
                                        for b in range(4):
                                            S.op("pe", lambda b=b: nc.tensor.matmul(PS[5][0:64, b * 64:(b + 1) * 64], lhsT=Pp[:, b, :], rhs=Qp[:, b, :], start=True, stop=True), reads=[b_Qp, b_Pp], writes=[PB[5]])
                                        S.op("act", lambda: nc.scalar.copy(out=Qn[:].rearrange("p b x -> p (b x)"), in_=PS[5][0:64, 0:256]), reads=[PB[5]], writes=[b_Qn])
                                    for b in range(4):
                                        S.op("pe", lambda b=b: nc.tensor.matmul(PS[6][0:64, b * 64:(b + 1) * 64], lhsT=Pn[:, b, :], rhs=Rp[:, b, :], start=True, stop=True), reads=[b_Pn, b_Rp], writes=[PB[6]])
                                    if k < 5:
                                        S.op("dve", lambda: nc.vector.tensor_tensor(out=Rn[:], in0=Rp[:], in1=PS[6][0:64, 0:256].rearrange("p (b x) -> p b x", b=4), op=ALU.add), reads=[b_Rp, PB[6]], writes=[b_Rn])
                                    else:
                                        S.op("dve", lambda: nc.vector.tensor_tensor(out=Rn[:], in0=Rp[:], in1=PS[6][0:64, 0:256].rearrange("p (b x) -> p b x", b=4), op=ALU.add), reads=[b_Rp, PB[6]], writes=[b_Rn])
                                        S.op("dve", lambda c=c: nc.vector.tensor_tensor(out=R5b[:, c, :, :].rearrange("p (s h) x -> p s h x", s=2), in0=v4(Rn), in1=gcols(beta, c).unsqueeze(3).to_broadcast([64, 2, 2, 64]), op=ALU.mult),
                                             reads=[b_Rn, b_beta], writes=[b_R5b])
                            S.barrier()
                        if cfg.get("gdn_stop") == 3:
                            S.barrier()
                            return
                        with contextlib.ExitStack() as st2:
                            Sst = [tl(st2, "g_S%d" % i, [128, 128], F32) for i in range(4)]
                            Sbb = [[tl(st2, "g_Sb%d_%d" % (i, j), [128, 128], BF16) for j in range(2)] for i in range(4)]
                            Xs = [tl(st2, "g_X%d" % i, [64, 128], BF16) for i in range(4)]
                            vnb = [tl(st2, "g_vn%d" % i, [64, 128], BF16) for i in range(4)]
                            vnk = [tl(st2, "g_vk%d" % i, [64, 128], BF16) for i in range(4)]
                            tmpo = [tl(st2, "g_to%d" % i, [64, 128], F32) for i in range(4)]
                            oo = [[tl(st2, "g_oo%d_%d" % (i, j), [64, 128], F32) for j in range(2)] for i in range(4)]
                            b_raw = Buf("gdn_raw")
                            orders = [list(range(NCH)), [3, 2, 1, 0] + list(range(NCH - 1, 3, -1))]
                            for idx in range(NCH):
                                for b in range(4):
                                    sd, hh = b // 2, b % 2
                                    hd = pr * 2 + hh
                                    col = sd * 4 + hd
                                    c = orders[sd][idx]
                                    cs = slice(c * 64, (c + 1) * 64)
                                    pa, pc_ = 2 * b, 2 * b + 1
                                    kf, b_kf = k_fm[hh]
                                    qf, b_qf = q_fm[hh]
                                    vt, b_vt = v_tm[hh]
                                    ktm, b_ktm = k_tm[hh]
                                    X_t, b_X = Xs[b]
                                    vn_t, b_vn = vnb[b]
                                    vk_t, b_vk = vnk[b]
                                    to_t, b_to = tmpo[b]
                                    o_t, b_o = oo[b][idx % 2]
                                    S_t, b_S = Sst[b]
                                    if idx > 0:
                                        sb_t, b_sb = Sbb[b][idx % 2]
                                        S.op("pe", lambda: nc.tensor.matmul(PS[pa][0:64, 0:128], lhsT=kf[:, cs], rhs=sb_t[:], start=True, stop=True), reads=[b_kf, b_sb], writes=[PB[pa]])
                                        S.op("pe", lambda: nc.tensor.matmul(PS[pa][0:64, 128:256], lhsT=qf[:, cs], rhs=sb_t[:], start=True, stop=True), reads=[b_qf, b_sb], writes=[PB[pa]])
                                        S.op("dve", lambda: nc.vector.scalar_tensor_tensor(out=X_t[:], in0=PS[pa][0:64, 0:128], scalar=negc[:, c, col:col + 1], in1=vt[:, c, :], op0=ALU.mult, op1=ALU.add),
                                             reads=[PB[pa], b_negc, b_vt], writes=[b_X])
                                        xin_ap = X_t[:]
                                        xr = [b_X]
                                    else:
                                        xin_ap = vt[:, c, :]
                                        xr = [b_vt]
                                    S.op("pe", lambda: nc.tensor.matmul(PS[pa][0:64, 256:384], lhsT=R5b[:, c, b, :], rhs=xin_ap, start=True, stop=True), reads=[b_R5b] + xr, writes=[PB[pa]])
                                    S.op("act", lambda: nc.scalar.copy(out=vn_t[:], in_=PS[pa][0:64, 256:384]), reads=[PB[pa]], writes=[b_vn])
                                    S.op("dve", lambda: nc.vector.tensor_scalar(out=vk_t[:], in0=PS[pa][0:64, 256:384], scalar1=ekd[:, c, col:col + 1], scalar2=None, op0=ALU.mult), reads=[PB[pa], b_ekd], writes=[b_vk])
                                    S.op("pe", lambda: nc.tensor.matmul(PS[pa][0:64, 384:512], lhsT=aqkT[:, c, b, :], rhs=vn_t[:], start=True, stop=True), reads=[b_aqkT, b_vn], writes=[PB[pa]])
                                    if idx > 0:
                                        S.op("act", lambda: nc.scalar.copy(out=to_t[:], in_=PS[pa][0:64, 384:512]), reads=[PB[pa]], writes=[b_to])
                                        S.op("dve", lambda: nc.vector.scalar_tensor_tensor(out=o_t[:], in0=PS[pa][0:64, 128:256], scalar=egc[:, c, col:col + 1], in1=to_t[:], op0=ALU.mult, op1=ALU.add),
                                             reads=[PB[pa], b_egc, b_to], writes=[b_o])
                                    else:
                                        S.op("act", lambda: nc.scalar.copy(out=o_t[:], in_=PS[pa][0:64, 384:512]), reads=[PB[pa]], writes=[b_o])
                                    S.dma(gdn_raw_d[sd, c * 64:(c + 1) * 64, hd * 128:(hd + 1) * 128], o_t[:], reads=[b_o], writes=[b_raw])
                                    if idx < NCH - 1:
                                        S.op("pe", lambda: nc.tensor.matmul(PS[pc_][:, 0:128], lhsT=ktm[:, c, :], rhs=vk_t[:], start=True, stop=True), reads=[b_ktm, b_vk], writes=[PB[pc_]])
                                        if idx == 0:
                                            S.op("dve", lambda: nc.vector.tensor_copy(out=S_t[:], in_=PS[pc_][:, 0:128]), reads=[PB[pc_]], writes=[b_S])
                                        else:
                                            S.op("dve", lambda: nc.vector.scalar_tensor_tensor(out=S_t[:], in0=S_t[:], scalar=egl[:, c, col:col + 1], in1=PS[pc_][:, 0:128], op0=ALU.mult, op1=ALU.add),
                                                 reads=[b_S, b_egl, PB[pc_]], writes=[b_S])
                                        nsb, b_nsb = Sbb[b][(idx + 1) % 2]
                                        S.op("act", lambda: nc.scalar.copy(out=nsb[:], in_=S_t[:]), reads=[b_S], writes=[b_nsb])
                            S.barrier()
                if cfg.get("gdn_stop") == 4:
                    S.barrier()
                    return
                with contextlib.ExitStack() as st:
                    wgg, b_wgg = tl(st, "g_wgg", [128, KC, 512], BF16)
                    S.dma(wgg[:], winv[:, :, O_GG:O_GG + 512], writes=[b_wgg], q="pool")
                    gnw, b_gnw = tl(st, "g_gnw", [128, 128], F32)
                    S.dma(gnw[:], I["gdn_norm"][l:l + 1, :].partition_broadcast(128), writes=[b_gnw])
                    of_ = [tl(st, "g_of%d" % i, [128, 512], F32) for i in range(2)]
                    ob_ = [tl(st, "g_ob%d" % i, [128, 512], F32) for i in range(2)]
                    sqt = [tl(st, "g_sqt%d" % i, [128, 512], F32) for i in range(2)]
                    gt_ = [tl(st, "g_gt%d" % i, [128, 512], F32) for i in range(2)]
                    ms = [tl(st, "g_ms%d" % i, [128, 4], F32) for i in range(2)]
                    obf = [tl(st, "g_obf%d" % i, [128, 512], BF16) for i in range(2)]
                    ofm = [tl(st, "g_ofm%d" % i, [128, 4, 128], BF16) for i in range(2)]
                    b_go = Buf("gdn_o")
                    hsub = cfg.get("gdn_hsub", 99)
                    for i in range(cfg.get("gdn_hnt", NT)):
                        a, b_a = of_[i % 2]
                        bb, b_bb = ob_[i % 2]
                        sq_t, b_sq = sqt[i % 2]
                        g_tl, b_gt = gt_[i % 2]
                        ms_t, b_ms = ms[i % 2]
                        obf_t, b_obf = obf[i % 2]
                        ofm_t, b_ofm = ofm[i % 2]
                        ts_ = slice(i * 128, (i + 1) * 128)
                        S.dma(a[:], gdn_raw_d[0, ts_, :], writes=[b_a])
                        S.dma(bb[:], gdn_raw_d[1, ts_, :], writes=[b_bb])
                        pb = i % 2
                        for kc in range(KC):
                            S.op("pe", lambda kc=kc, pb=pb: nc.tensor.matmul(PS[pb][:, :], lhsT=h_fm[:, kc, ts_], rhs=wgg[:, kc, :], start=(kc == 0), stop=(kc == KC - 1)), reads=[b_hfm, b_wgg], writes=[PB[pb]])
                        S.op("act", lambda: nc.scalar.activation(out=g_tl[:], in_=PS[pb][:, :], func=AF.Silu), reads=[PB[pb]], writes=[b_gt])
                        if hsub < 1:
                            continue
                        S.op("dve", lambda: nc.vector.tensor_tensor(out=a[:], in0=a[:], in1=bb[:], op=ALU.add), reads=[b_a, b_bb], writes=[b_a])
                        S.op("act", lambda: nc.scalar.activation(out=sq_t[:], in_=a[:], func=AF.Square), reads=[b_a], writes=[b_sq])
                        S.op("dve", lambda: nc.vector.tensor_reduce(out=ms_t[:], in_=sq_t[:].rearrange("p (h x) -> p h x", h=4), axis=AX.X, op=ALU.add), reads=[b_sq], writes=[b_ms])
                        S.op("act", lambda: nc.scalar.activation(out=ms_t[:], in_=ms_t[:], func=AF.Sqrt, scale=1.0 / 128, bias=epsb[:, 0:1]), reads=[b_ms, b_eps], writes=[b_ms])
                        S.op("dve", lambda: nc.vector.reciprocal(out=ms_t[:], in_=ms_t[:]), reads=[b_ms], writes=[b_ms])
                        if hsub < 2:
                            continue
                        a3 = a[:].rearrange("p (h x) -> p h x", h=4)
                        S.op("dve", lambda: nc.vector.tensor_tensor(out=a3, in0=a3, in1=ms_t[:].unsqueeze(2).to_broadcast([128, 4, 128]), op=ALU.mult), reads=[b_a, b_ms], writes=[b_a])
                        S.op("dve", lambda: nc.vector.tensor_tensor(out=a3, in0=a3, in1=gnw[:].unsqueeze(1).to_broadcast([128, 4, 128]), op=ALU.mult), reads=[b_a, b_gnw], writes=[b_a])
                        S.op("dve", lambda: nc.vector.tensor_tensor(out=obf_t[:], in0=a[:], in1=g_tl[:], op=ALU.mult), reads=[b_a, b_gt], writes=[b_obf])
                        if hsub < 3:
                            continue
                        pv = PS[2 + pb][:].bitcast(BF16)
                        for hd in range(4):
                            S.op("pe", lambda hd=hd: nc.tensor.transpose(out=pv[:, hd * 128:(hd + 1) * 128], in_=obf_t[:, hd * 128:(hd + 1) * 128], identity=identb[:]), reads=[b_obf, b_identb], writes=[PB[2 + pb]])
                        S.op("act", lambda: nc.scalar.copy(out=ofm_t[:], in_=pv[:, 0:512].rearrange("p (h x) -> p h x", h=4)), reads=[PB[2 + pb]], writes=[b_ofm])
                        if hsub < 4:
                            continue
                        S.dma(gdn_o_d[:, :, ts_].rearrange("h d t -> d h t"), ofm_t[:], reads=[b_ofm], writes=[b_go])
                    S.barrier()


        def ln_tile(st_tiles, i, f_halves, f_bufs, prm, out_final):
            s = 1 if i < 2 else 0
            x_t, b_x = st_tiles["x"][i % 2]
            t_t, b_t = st_tiles["t"][i % 2]
            h_t, b_h = st_tiles["h"][i % 2]
            stt, b_st = st_tiles["st"][i % 2]
            mv, b_mv = st_tiles["mv"][i % 2]
            ts_ = slice(i * 128, (i + 1) * 128)
            S.dma(x_t[:], xres_d[ts_, :], reads=[b_xres[i]], writes=[b_x])
            gate_t, b_gate = prm["gate"][s]
            for hf in range(2):
                hs = slice(hf * 512, (hf + 1) * 512)
                S.op("dve", lambda hf=hf, hs=hs: nc.vector.tensor_tensor(out=t_t[:, hs], in0=f_halves[hf], in1=gate_t[:, hs], op=ALU.mult), reads=[f_bufs[hf], b_gate], writes=[b_t])
            S.op("dve", lambda: nc.vector.scalar_tensor_tensor(out=x_t[:], in0=x_t[:], scalar=ALPHA, in1=t_t[:], op0=ALU.mult, op1=ALU.add), reads=[b_x, b_t], writes=[b_x])
            for hf in range(2):
                S.op("dve", lambda hf=hf: nc.vector.bn_stats(out=stt[:, hf, :], in_=x_t[:, hf * 512:(hf + 1) * 512]), reads=[b_x], writes=[b_st])
            S.op("dve", lambda: nc.vector.bn_aggr(out=mv[:, 0:2], in_=stt[:].rearrange("p a b -> p (a b)")), reads=[b_st], writes=[b_mv])
            S.op("act", lambda: nc.scalar.activation(out=mv[:, 2:3], in_=mv[:, 1:2], func=AF.Sqrt, bias=epsb[:, 0:1]), reads=[b_mv, b_eps], writes=[b_mv])
            S.op("dve", lambda: nc.vector.reciprocal(out=mv[:, 3:4], in_=mv[:, 2:3]), reads=[b_mv], writes=[b_mv])
            S.op("dve", lambda: nc.vector.tensor_scalar(out=x_t[:], in0=x_t[:], scalar1=mv[:, 0:1], scalar2=mv[:, 3:4], op0=ALU.subtract, op1=ALU.mult), reads=[b_x, b_mv], writes=[b_x])
            S.op("pool", lambda: nc.gpsimd.tensor_tensor(out=x_t[:], in0=x_t[:], in1=prm["g"][0][:], op=ALU.mult), reads=[b_x, prm["g"][1]], writes=[b_x])
            S.op("pool", lambda: nc.gpsimd.tensor_tensor(out=x_t[:], in0=x_t[:], in1=prm["b"][0][:], op=ALU.add), reads=[b_x, prm["b"][1]], writes=[b_x])
            if out_final:
                S.dma(out_d[(i - 2) * 128:(i - 1) * 128, :], x_t[:], reads=[b_x])
                return
            S.dma(xres_d[ts_, :], x_t[:], reads=[b_x], writes=[b_xres[i]])
            sc_t, b_sc = prm["sc"][s]
            sh_t, b_sh = prm["sh"][s]
            S.op("dve", lambda: nc.vector.tensor_tensor(out=t_t[:], in0=x_t[:], in1=sc_t[:], op=ALU.mult), reads=[b_x, b_sc], writes=[b_t])
            S.op("pool", lambda: nc.gpsimd.tensor_tensor(out=h_t[:], in0=t_t[:], in1=sh_t[:], op=ALU.add), reads=[b_t, b_sh], writes=[b_h])
            to_fm(h_t, b_h, i, 6 + i % 2)

        def ln_setup(st, l_mod, jgate, ln_g, ln_b, l, jsh, jsc, need_mod):
            tiles = {
                "x": [tl(st, "ln_x%d" % i, [128, D], F32) for i in range(2)],
                "t": [tl(st, "ln_t%d" % i, [128, D], F32) for i in range(2)],
                "h": [tl(st, "ln_h%d" % i, [128, D], BF16) for i in range(2)],
                "st": [tl(st, "ln_st%d" % i, [128, 2, 6], F32) for i in range(2)],
                "mv": [tl(st, "ln_mv%d" % i, [128, 4], F32) for i in range(2)],
            }
            prm = {"gate": [load_bc(st, "ln_gate%d" % s_, l, s_, jgate) for s_ in range(2)],
                   "g": load_vec_bc(st, "ln_g", I[ln_g][l:l + 1, :]),
                   "b": load_vec_bc(st, "ln_b", I[ln_b][l:l + 1, :])}
            if need_mod:
                prm["sh"] = [load_bc(st, "ln_sh%d" % s_, l_mod, s_, jsh) for s_ in range(2)]
                prm["sc"] = [load_bc(st, "ln_sc%d" % s_, l_mod, s_, jsc, plus_one=True) for s_ in range(2)]
            return tiles, prm

        def stage_merge(l, last):
            winv = I["w_in"][l].rearrange("(kc p) n -> p kc n", p=128)
            groups = GROUPS[1:] if last else GROUPS
            tiles_i = range(2, NT) if last else range(NT)
            with contextlib.ExitStack() as st:
                y_fm, b_y = tl(st, "y_fm", [128, KC, T], BF16)
                with contextlib.ExitStack() as st2:
                    mo, b_mo = tl(st2, "m_mo", [64, 8, T], BF16)
                    ho, b_ho = tl(st2, "m_ho", [128, 4, T], BF16)
                    go, b_go = tl(st2, "m_go", [128, 4, T], BF16)
                    S.dma(mo[:], mla_o_d.rearrange("h d t -> d h t"), writes=[b_mo])
                    S.dma(ho[:], hg_o_d.rearrange("h d t -> d h t"), writes=[b_ho])
                    S.dma(go[:], gdn_o_d.rearrange("h d t -> d h t"), writes=[b_go])
                    wgt = [tl(st2, "m_wg%d" % i, [128, KC, 3, 128], BF16) for i in range(2)]
                    wbr = [tl(st2, "m_wbr%d" % i, [128, 2, 4, 128], BF16) for i in range(2)]
                    wbm = [tl(st2, "m_wbm%d" % i, [64, 8, 128], BF16) for i in range(2)]
                    sg = [tl(st2, "m_sg%d" % i, [128, 512], F32) for i in range(3)]
                    ta, b_ta = tl(st2, "m_ta", [128, 512], F32)
                    tb, b_tb = tl(st2, "m_tb", [128, 512], F32)
                    for dc in range(KC):
                        wg_t, b_wg = wgt[dc % 2]
                        wbr_t, b_wbr = wbr[dc % 2]
                        wbm_t, b_wbm = wbm[dc % 2]
                        for n_ in range(3):
                            c0 = O_GATES + n_ * D + dc * 128
                            S.dma(wg_t[:, :, n_, :], winv[:, :, c0:c0 + 128], writes=[b_wg], q="pool")
                        for n_ in range(2):
                            S.dma(wbr_t[:, n_, :, :], I["w_branch"][l, n_ + 1].rearrange("(kc p) n -> p kc n", p=128)[:, :, dc * 128:(dc + 1) * 128], writes=[b_wbr], q="pool")
                        S.dma(wbm_t[:], I["w_branch"][l, 0].rearrange("(h p) n -> p h n", p=64)[:, :, dc * 128:(dc + 1) * 128], writes=[b_wbm], q="pool")
                        for (t0, n) in groups:
                            for n_ in range(3):
                                for kc in range(KC):
                                    S.op("pe", lambda kc=kc, n_=n_: nc.tensor.matmul(PS[n_][:, 0:n], lhsT=wg_t[:, kc, n_, :], rhs=h_fm[:, kc, t0:t0 + n], start=(kc == 0), stop=(kc == KC - 1)),
                                         reads=[b_wg, b_hfm], writes=[PB[n_]])
                                S.op("act", lambda n_=n_: nc.scalar.activation(out=sg[n_][0][:, 0:n], in_=PS[n_][:, 0:n], func=AF.Sigmoid), reads=[PB[n_]], writes=[sg[n_][1]])
                            for h in range(8):
                                S.op("pe", lambda h=h: nc.tensor.matmul(PS[3][:, 0:n], lhsT=wbm_t[:, h, :], rhs=mo[:, h, t0:t0 + n], start=(h == 0), stop=(h == 7)), reads=[b_wbm, b_mo], writes=[PB[3]])
                            for kc in range(4):
                                S.op("pe", lambda kc=kc: nc.tensor.matmul(PS[4][:, 0:n], lhsT=wbr_t[:, 0, kc, :], rhs=ho[:, kc, t0:t0 + n], start=(kc == 0), stop=(kc == 3)), reads=[b_wbr, b_ho], writes=[PB[4]])
                            for kc in range(4):
                                S.op("pe", lambda kc=kc: nc.tensor.matmul(PS[5][:, 0:n], lhsT=wbr_t[:, 1, kc, :], rhs=go[:, kc, t0:t0 + n], start=(kc == 0), stop=(kc == 3)), reads=[b_wbr, b_go], writes=[PB[5]])
                            S.op("dve", lambda: nc.vector.tensor_tensor(out=ta[:, 0:n], in0=sg[0][0][:, 0:n], in1=PS[3][:, 0:n], op=ALU.mult), reads=[sg[0][1], PB[3]], writes=[b_ta])
                            S.op("dve", lambda: nc.vector.tensor_tensor(out=tb[:, 0:n], in0=sg[1][0][:, 0:n], in1=PS[4][:, 0:n], op=ALU.mult), reads=[sg[1][1], PB[4]], writes=[b_tb])
                            S.op("pool", lambda: nc.gpsimd.tensor_tensor(out=ta[:, 0:n], in0=ta[:, 0:n], in1=tb[:, 0:n], op=ALU.add), reads=[b_ta, b_tb], writes=[b_ta])
                            S.op("dve", lambda: nc.vector.tensor_tensor(out=tb[:, 0:n], in0=sg[2][0][:, 0:n], in1=PS[5][:, 0:n], op=ALU.mult), reads=[sg[2][1], PB[5], b_ta], writes=[b_tb])
                            S.op("dve", lambda dc=dc: nc.vector.tensor_tensor(out=y_fm[:, dc, t0:t0 + n], in0=ta[:, 0:n], in1=tb[:, 0:n], op=ALU.add), reads=[b_ta, b_tb], writes=[b_y])
                    S.barrier()
                if "y_fm" in DBG:
                    S.dma(DBG["y_fm"].rearrange("(kc p) t -> p kc t", p=128), y_fm[:], reads=[b_y], q="pool")
                wo, b_wo = tl(st, "m_wo", [128, KC, D], BF16)
                S.dma(wo[:], I["w_out"][l].rearrange("(kc p) n -> p kc n", p=128), writes=[b_wo], q="pool")
                tiles, prm = ln_setup(st, l, 2, "ln1_g", "ln1_b", l, 3, 4, True)
                for i in tiles_i:
                    ts_ = slice(i * 128, (i + 1) * 128)
                    for hf in range(2):
                        pb = 4 + hf
                        for kc in range(KC):
                            S.op("pe", lambda kc=kc, hf=hf, pb=pb: nc.tensor.matmul(PS[pb][:, :], lhsT=y_fm[:, kc, ts_], rhs=wo[:, kc, hf * 512:(hf + 1) * 512], start=(kc == 0), stop=(kc == KC - 1)),
                                 reads=[b_y, b_wo], writes=[PB[pb]])
                    ln_tile(tiles, i, [PS[4][:, :], PS[5][:, :]], [PB[4], PB[5]], prm, False)
                S.barrier()

        def stage_moe(l, last):
            groups = GROUPS[1:] if last else GROUPS
            tiles_i = list(range(2, NT)) if last else list(range(NT))
            with contextlib.ExitStack() as st:
                acc, b_acc = tl(st, "acc", [128, NT, D], F32)
                comb, b_comb = tl(st, "comb", [128, NT, 65], F32)
                S.op("dve", lambda: nc.vector.memset(comb[:, :, 64:65], 1.0), writes=[b_comb])
                with contextlib.ExitStack() as st2:
                    wr, b_wr = tl(st2, "wr", [128, KC, 64], BF16)
                    S.dma(wr[:], I["w_router"][l].rearrange("(kc p) n -> p kc n", p=128), writes=[b_wr], q="pool")
                    rb, b_rb = load_vec_bc(st2, "rb", I["router_bias"][l:l + 1, :], n=64)
                    sc_ = [tl(st2, "r_sc%d" % i, [128, 64], F32) for i in range(2)]
                    sel = [tl(st2, "r_sel%d" % i, [128, 64], F32) for i in range(2)]
                    selm = [tl(st2, "r_selm%d" % i, [128, 64], F32) for i in range(2)]
                    m8 = [tl(st2, "r_m8%d" % i, [128, 8, 8], F32) for i in range(2)]
                    sm = [tl(st2, "r_sm%d" % i, [128, 40], F32) for i in range(2)]
                    for i in tiles_i:
                        ts_ = slice(i * 128, (i + 1) * 128)
                        pb = i % 2
                        sc_t, b_sc = sc_[i % 2]
                        sel_t, b_sel = sel[i % 2]
                        selm_t, b_selm = selm[i % 2]
                        m8_t, b_m8 = m8[i % 2]
                        sm_t, b_sm = sm[i % 2]
                        for kc in range(KC):
                            S.op("pe", lambda kc=kc: nc.tensor.matmul(PS[pb][:, 0:64], lhsT=h_fm[:, kc, ts_], rhs=wr[:, kc, :], start=(kc == 0), stop=(kc == KC - 1)), reads=[b_hfm, b_wr], writes=[PB[pb]])
                        S.op("act", lambda: nc.scalar.activation(out=sc_t[:], in_=PS[pb][:, 0:64], func=AF.Sigmoid), reads=[PB[pb]], writes=[b_sc])
                        S.op("dve", lambda: nc.vector.tensor_tensor(out=sel_t[:], in0=sc_t[:], in1=rb[:], op=ALU.add), reads=[b_sc, b_rb], writes=[b_sel])
                        for g8 in range(8):
                            S.op("dve", lambda g8=g8: nc.vector.max(out=m8_t[:, g8, :], in_=sel_t[:, g8 * 8:(g8 + 1) * 8]), reads=[b_sel], writes=[b_m8])
                        gs = sm_t[:, 0:8]
                        gm8 = sm_t[:, 8:16]
                        gmask = sm_t[:, 16:24]
                        pen = sm_t[:, 24:32]
                        t8 = sm_t[:, 32:40]
                        S.op("dve", lambda: nc.vector.tensor_tensor(out=gs, in0=m8_t[:, :, 0], in1=m8_t[:, :, 1], op=ALU.add), reads=[b_m8], writes=[b_sm])
                        S.op("dve", lambda: nc.vector.max(out=gm8, in_=gs), reads=[b_sm], writes=[b_sm])
                        S.op("dve", lambda: nc.vector.tensor_scalar(out=gmask, in0=gs, scalar1=sm_t[:, 11:12], scalar2=None, op0=ALU.is_ge), reads=[b_sm], writes=[b_sm])
                        S.op("dve", lambda: nc.vector.tensor_scalar(out=pen, in0=gmask, scalar1=10.0, scalar2=-10.0, op0=ALU.mult, op1=ALU.add), reads=[b_sm], writes=[b_sm])
                        sel3 = sel_t[:].rearrange("p (g x) -> p g x", g=8)
                        selm3 = selm_t[:].rearrange("p (g x) -> p g x", g=8)
                        S.op("dve", lambda: nc.vector.tensor_tensor(out=selm3, in0=sel3, in1=gmask.unsqueeze(2).to_broadcast([128, 8, 8]), op=ALU.mult), reads=[b_sel, b_sm], writes=[b_selm])
                        S.op("dve", lambda: nc.vector.tensor_tensor(out=selm3, in0=selm3, in1=pen.unsqueeze(2).to_broadcast([128, 8, 8]), op=ALU.add), reads=[b_selm, b_sm], writes=[b_selm])
                        S.op("dve", lambda: nc.vector.max(out=t8, in_=selm_t[:]), reads=[b_selm], writes=[b_sm])
                        S.op("dve", lambda: nc.vector.tensor_scalar(out=selm_t[:], in0=selm_t[:], scalar1=sm_t[:, 39:40], scalar2=None, op0=ALU.is_ge), reads=[b_selm, b_sm], writes=[b_selm])
                        S.op("dve", lambda: nc.vector.tensor_tensor(out=sel_t[:], in0=sc_t[:], in1=selm_t[:], op=ALU.mult), reads=[b_sc, b_selm], writes=[b_sel])
                        S.op("dve", lambda: nc.vector.tensor_reduce(out=sm_t[:, 0:1], in_=sel_t[:], axis=AX.X, op=ALU.add), reads=[b_sel], writes=[b_sm])
                        S.op("dve", lambda: nc.vector.reciprocal(out=sm_t[:, 1:2], in_=sm_t[:, 0:1]), reads=[b_sm], writes=[b_sm])
                        S.op("dve", lambda i=i: nc.vector.tensor_scalar(out=comb[:, i, 0:64], in0=sel_t[:], scalar1=sm_t[:, 1:2], scalar2=2.5, op0=ALU.mult, op1=ALU.mult), reads=[b_sel, b_sm], writes=[b_comb])
                    S.barrier()
                if "comb" in DBG:
                    S.dma(DBG["comb"].rearrange("(i p) e -> p i e", p=128), comb[:], reads=[b_comb])
                with contextlib.ExitStack() as st2:
                    wgu = [tl(st2, "wgu%d" % i, [128, KC, 512], BF16) for i in range(3)]
                    wdn = [tl(st2, "wdn%d" % i, [128, 2, D], BF16) for i in range(3)]
                    sgt = [tl(st2, "e_sg%d" % i, [128, 512], F32) for i in range(2)]
                    act = [tl(st2, "e_act%d" % i, [128, 2, 512], BF16) for i in range(2)]
                    ne = cfg.get("n_experts", 65)

                    def load_e(e):
                        wg_t, b_wg = wgu[e % 3]
                        wd_t, b_wd = wdn[e % 3]
                        if e < 64:
                            S.dma(wg_t[:], I["w_gu"][l, e].rearrange("(kc p) n -> p kc n", p=128), writes=[b_wg], q="pool")
                            S.dma(wd_t[:], I["w_down"][l, e].rearrange("(kc p) n -> p kc n", p=128), writes=[b_wd], q="pool")
                        else:
                            S.dma(wg_t[:], I["w_sh_gu"][l].rearrange("(kc p) n -> p kc n", p=128), writes=[b_wg], q="pool")
                            S.dma(wd_t[:], I["w_sh_down"][l].rearrange("(kc p) n -> p kc n", p=128), writes=[b_wd], q="pool")

                    elist = list(range(64 - (ne - 1), 65)) if ne < 65 else list(range(65))
                    load_e(elist[0])
                    if len(elist) > 1:
                        load_e(elist[1])
                    gcnt = 0
                    dcnt = 0
                    for ei, e in enumerate(elist):
                        if ei + 2 < len(elist):
                            load_e(elist[ei + 2])
                        wg_t, b_wg = wgu[e % 3]
                        wd_t, b_wd = wdn[e % 3]
                        for (t0, n) in groups:
                            act_t, b_act = act[gcnt % 2]
                            gcnt += 1
                            for c in range(2):
                                pg, pu = 2 * c, 2 * c + 1
                                for kc in range(KC):
                                    S.op("pe", lambda kc=kc, c=c, pg=pg: nc.tensor.matmul(PS[pg][:, 0:n], lhsT=wg_t[:, kc, c * 128:(c + 1) * 128], rhs=h_fm[:, kc, t0:t0 + n], start=(kc == 0), stop=(kc == KC - 1)),
                                         reads=[b_wg, b_hfm], writes=[PB[pg]])
                                for kc in range(KC):
                                    S.op("pe", lambda kc=kc, c=c, pu=pu: nc.tensor.matmul(PS[pu][:, 0:n], lhsT=wg_t[:, kc, 256 + c * 128:256 + (c + 1) * 128], rhs=h_fm[:, kc, t0:t0 + n], start=(kc == 0), stop=(kc == KC - 1)),
                                         reads=[b_wg, b_hfm], writes=[PB[pu]])
                                sg_t, b_sg = sgt[c]
                                S.op("act", lambda pg=pg, sg_t=sg_t: nc.scalar.activation(out=sg_t[:, 0:n], in_=PS[pg][:, 0:n], func=AF.Silu), reads=[PB[pg]], writes=[b_sg])
                                S.op("dve", lambda c=c, pu=pu, sg_t=sg_t, act_t=act_t: nc.vector.tensor_tensor(out=act_t[:, c, 0:n], in0=sg_t[:, 0:n], in1=PS[pu][:, 0:n], op=ALU.mult), reads=[b_sg, PB[pu]], writes=[b_act])
                            for tt in range(n // 128):
                                i = t0 // 128 + tt
                                for hf in range(2):
                                    pd = 4 + dcnt % 4
                                    dcnt += 1
                                    for c in range(2):
                                        S.op("pe", lambda c=c, pd=pd, tt=tt, hf=hf, act_t=act_t: nc.tensor.matmul(PS[pd][:, :], lhsT=act_t[:, c, tt * 128:(tt + 1) * 128], rhs=wd_t[:, c, hf * 512:(hf + 1) * 512], start=(c == 0), stop=(c == 1)),
                                             reads=[b_act, b_wd], writes=[PB[pd]])
                                    if ei == 0:
                                        S.op("dve", lambda pd=pd, i=i, hf=hf, e=e: nc.vector.tensor_scalar(out=acc[:, i, hf * 512:(hf + 1) * 512], in0=PS[pd][:, :], scalar1=comb[:, i, e:e + 1], scalar2=None, op0=ALU.mult),
                                             reads=[PB[pd], b_comb], writes=[b_acc])
                                    else:
                                        S.op("dve", lambda pd=pd, i=i, hf=hf, e=e: nc.vector.scalar_tensor_tensor(out=acc[:, i, hf * 512:(hf + 1) * 512], in0=PS[pd][:, :], scalar=comb[:, i, e:e + 1], in1=acc[:, i, hf * 512:(hf + 1) * 512], op0=ALU.mult, op1=ALU.add),
                                             reads=[PB[pd], b_comb, b_acc], writes=[b_acc])
                    S.barrier()
                if "ff" in DBG:
                    S.dma(DBG["ff"].rearrange("(i p) d -> p i d", p=128), acc[:], reads=[b_acc])
                final = (l == nlayers - 1)
                tiles, prm = ln_setup(st, l + 1, 5, "ln2_g", "ln2_b", l, 0, 1, not final)
                for i in tiles_i:
                    ln_tile(tiles, i, [acc[:, i, 0:512], acc[:, i, 512:1024]], [b_acc, b_acc], prm, final)
                S.barrier()

        def stage_mla(l, last):
            with contextlib.ExitStack() as st:
                winv = I["w_in"][l].rearrange("(kc p) n -> p kc n", p=128)
                wA, b_wA = tl(st, "wA", [128, KC, 672], BF16)
                S.dma(wA[:], winv[:, :, 0:672], writes=[b_wA], q="pool")
                wKs, b_wKs = tl(st, "wKs", [128, KC, 32], BF16)
                S.dma(wKs[:], I["w_kr_sw"][l].rearrange("(kc p) n -> p kc n", p=128), writes=[b_wKs], q="pool")
                wQ, b_wQ = tl(st, "wQ", [128, 3, 768], BF16)
                S.dma(wQ[:], I["w_q_b"][l].rearrange("(kc p) n -> p kc n", p=128), writes=[b_wQ], q="pool")
                wQs, b_wQs = tl(st, "wQs", [128, 3, 256], BF16)
                S.dma(wQs[:], I["w_qr_sw"][l].rearrange("(kc p) n -> p kc n", p=128), writes=[b_wQs], q="pool")
                wKV, b_wKV = tl(st, "wKV", [128, 2, 1024], BF16)
                S.dma(wKV[:], I["w_kv_b"][l].rearrange("(kc p) n -> p kc n", p=128), writes=[b_wKV], q="pool")
                gq, b_gq = tl(st, "gq", [128, 3], F32)
                S.dma(gq[:], I["q_a_norm_t"][l], writes=[b_gq])
                gkv, b_gkv = tl(st, "gkv", [128, 2], F32)
                S.dma(gkv[:], I["kv_a_norm_t"][l], writes=[b_gkv])
                ropeC, b_rC = tl(st, "ropeC", [32, LAT], F32)
                ropeS, b_rS = tl(st, "ropeS", [32, LAT], F32)
                S.dma(ropeC[:], I["ropeC"][:, :], writes=[b_rC])
                S.dma(ropeS[:], I["ropeS"][:, :], writes=[b_rS])
                qan, b_qan = tl(st, "qan", [128, 3, T], BF16)
                kvan, b_kvan = tl(st, "kvan", [128, 2, T], BF16)
                kr, b_kr = tl(st, "kr", [32, T], BF16)
                raw = [tl(st, "raw%d" % i, [128, 512], F32) for i in range(5)]
                sq = [tl(st, "sq%d" % i, [128, 512], BF16) for i in range(5)]
                rs = [tl(st, "rs%d" % i, [128, 512], F32) for i in range(2)]
                rt = [tl(st, "rt%d" % i, [32, 512], F32) for i in range(2)]

                def rope_or_copy(dst, b_dst, t0, n, pa, pb, isctx):
                    if isctx:
                        S.op("act", lambda: nc.scalar.copy(out=dst[:, t0:t0 + n], in_=PS[pa][0:32, 0:n]), reads=[PB[pa]], writes=[b_dst])
                        return
                    l0 = t0 - CTX
                    S.op("dve", lambda: nc.vector.tensor_tensor(out=rt[0][0][:, 0:n], in0=PS[pa][0:32, 0:n], in1=ropeC[:, l0:l0 + n], op=ALU.mult),
                         reads=[PB[pa], b_rC], writes=[rt[0][1]])
                    S.op("dve", lambda: nc.vector.tensor_tensor(out=rt[1][0][:, 0:n], in0=PS[pb][0:32, 0:n], in1=ropeS[:, l0:l0 + n], op=ALU.mult),
                         reads=[PB[pb], b_rS], writes=[rt[1][1]])
                    S.op("dve", lambda: nc.vector.tensor_tensor(out=dst[:, t0:t0 + n], in0=rt[0][0][:, 0:n], in1=rt[1][0][:, 0:n], op=ALU.add),
                         reads=[rt[0][1], rt[1][1]], writes=[b_dst])

                def rmsnorm_group(col0, nchunk, gvec, b_gvec, dst, b_dst, t0, n, pbase, ri):
                    for c in range(nchunk):
                        pb = pbase + c
                        for kc in range(KC):
                            S.op("pe", lambda kc=kc, c=c, pb=pb: nc.tensor.matmul(PS[pb][:, 0:n], lhsT=wA[:, kc, col0 + c * 128:col0 + (c + 1) * 128],
                                                                                  rhs=h_fm[:, kc, t0:t0 + n], start=(kc == 0), stop=(kc == KC - 1)),
                                 reads=[b_wA, b_hfm], writes=[PB[pb]])
                        rw, b_rw = raw[ri + c]
                        sqt, b_sq = sq[ri + c]
                        S.op("act", lambda rw=rw, pb=pb: nc.scalar.copy(out=rw[:, 0:n], in_=PS[pb][:, 0:n]), reads=[PB[pb]], writes=[b_rw])
                        S.op("act", lambda sqt=sqt, pb=pb: nc.scalar.activation(out=sqt[:, 0:n], in_=PS[pb][:, 0:n], func=AF.Square), reads=[PB[pb]], writes=[b_sq])
                    pss = pbase + nchunk
                    for c in range(nchunk):
                        S.op("pe", lambda c=c: nc.tensor.matmul(PS[pss][:, 0:n], lhsT=onesb[:], rhs=sq[ri + c][0][:, 0:n], start=(c == 0), stop=(c == nchunk - 1)),
                             reads=[b_onesb, sq[ri + c][1]], writes=[PB[pss]])
                    r0, b_r0 = rs[0]
                    r1, b_r1 = rs[1]
                    S.op("act", lambda: nc.scalar.activation(out=r0[:, 0:n], in_=PS[pss][:, 0:n], func=AF.Sqrt, scale=1.0 / (128 * nchunk), bias=epsb[:, 0:1]),
                         reads=[PB[pss], b_eps], writes=[b_r0])
                    S.op("dve", lambda: nc.vector.reciprocal(out=r1[:, 0:n], in_=r0[:, 0:n]), reads=[b_r0], writes=[b_r1])
                    for c in range(nchunk):
                        S.op("dve", lambda c=c: nc.vector.scalar_tensor_tensor(out=dst[:, c, t0:t0 + n], in0=raw[ri + c][0][:, 0:n], scalar=gvec[:, c:c + 1],
                                                                                 in1=r1[:, 0:n], op0=ALU.mult, op1=ALU.mult),
                             reads=[raw[ri + c][1], b_gvec, b_r1], writes=[b_dst])

                for gi, (t0, n) in enumerate(GROUPS):
                    rmsnorm_group(0, 3, gq, b_gq, qan, b_qan, t0, n, 0, 0)
                    rmsnorm_group(384, 2, gkv, b_gkv, kvan, b_kvan, t0, n, 4, 3)
                    for kc in range(KC):
                        S.op("pe", lambda kc=kc: nc.tensor.matmul(PS[7][0:32, 0:n], lhsT=wA[:, kc, 640:672], rhs=h_fm[:, kc, t0:t0 + n], start=(kc == 0), stop=(kc == KC - 1)),
                             reads=[b_wA, b_hfm], writes=[PB[7]])
                    for kc in range(KC):
                        S.op("pe", lambda kc=kc: nc.tensor.matmul(PS[3][0:32, 0:n], lhsT=wKs[:, kc, :], rhs=h_fm[:, kc, t0:t0 + n], start=(kc == 0), stop=(kc == KC - 1)),
                             reads=[b_wKs, b_hfm], writes=[PB[3]])
                    rope_or_copy(kr, b_kr, t0, n, 7, 3, gi == 0)

                v_aug, b_va = tl(st, "v_aug", [128, NT, 8, 65], BF16)
                S.op("dve", lambda: nc.vector.memset(v_aug[:, :, :, 64:65], 1.0), writes=[b_va])
                wKVh = wKV[:].rearrange("p c (h x) -> p c h x", h=8)
                for i in range(NT):
                    pb = i % 2
                    for c in range(2):
                        S.op("pe", lambda c=c, i=i, pb=pb: nc.tensor.matmul(PS[pb][:, :].rearrange("p (h x) -> p h x", h=8), lhsT=kvan[:, c, i * 128:(i + 1) * 128],
                                                                            rhs=wKVh[:, c, :, 64:128], start=(c == 0), stop=(c == 1)),
                             reads=[b_kvan, b_wKV], writes=[PB[pb]])
                    S.op("act", lambda i=i, pb=pb: nc.scalar.copy(out=v_aug[:, i, :, 0:64], in_=PS[pb][:, :].rearrange("p (h x) -> p h x", h=8)),
                         reads=[PB[pb]], writes=[b_va])

                qn = [tl(st, "qn%d" % i, [64, T], BF16) for i in range(2)]
                qr = [tl(st, "qr%d" % i, [32, T], BF16) for i in range(2)]
                kn = [tl(st, "kn%d" % i, [64, T], BF16) for i in range(2)]
                Et = [tl(st, "Et%d" % i, [128, 512], BF16) for i in range(3)]
                rc, b_rc = tl(st, "rc", [65, 512], F32)
                numt = [tl(st, "numt%d" % i, [64, 512], F32) for i in range(2)]
                ot = [tl(st, "ot%d" % i, [64, 512], BF16) for i in range(2)]
                b_mo = Buf("mla_o")
                ecnt = 0
                ocnt = 0
                for h in range(8):
                    qn_t, b_qn = qn[h % 2]
                    qr_t, b_qr = qr[h % 2]
                    kn_t, b_kn = kn[h % 2]
                    for gi, (t0, n) in enumerate(GROUPS):
                        if not (last and gi == 0):
                            for c in range(3):
                                S.op("pe", lambda c=c: nc.tensor.matmul(PS[0][0:64, 0:n], lhsT=wQ[:, c, 96 * h:96 * h + 64], rhs=qan[:, c, t0:t0 + n], start=(c == 0), stop=(c == 2)),
                                     reads=[b_wQ, b_qan], writes=[PB[0]])
                            S.op("act", lambda: nc.scalar.copy(out=qn_t[:, t0:t0 + n], in_=PS[0][0:64, 0:n]), reads=[PB[0]], writes=[b_qn])
                            for c in range(3):
                                S.op("pe", lambda c=c: nc.tensor.matmul(PS[1][0:32, 0:n], lhsT=wQ[:, c, 96 * h + 64:96 * h + 96], rhs=qan[:, c, t0:t0 + n], start=(c == 0), stop=(c == 2)),
                                     reads=[b_wQ, b_qan], writes=[PB[1]])
                            for c in range(3):
                                S.op("pe", lambda c=c: nc.tensor.matmul(PS[2][0:32, 0:n], lhsT=wQs[:, c, 32 * h:32 * h + 32], rhs=qan[:, c, t0:t0 + n], start=(c == 0), stop=(c == 2)),
                                     reads=[b_wQs, b_qan], writes=[PB[2]])
                            rope_or_copy(qr_t, b_qr, t0, n, 1, 2, gi == 0)
                        for c in range(2):
                            S.op("pe", lambda c=c: nc.tensor.matmul(PS[3][0:64, 0:n], lhsT=wKV[:, c, 128 * h:128 * h + 64], rhs=kvan[:, c, t0:t0 + n], start=(c == 0), stop=(c == 1)),
                                 reads=[b_wKV, b_kvan], writes=[PB[3]])
                        S.op("act", lambda: nc.scalar.copy(out=kn_t[:, t0:t0 + n], in_=PS[3][0:64, 0:n]), reads=[PB[3]], writes=[b_kn])
                    for gi, (t0, n) in enumerate(GROUPS):
                        if last and gi == 0:
                            continue
                        kts = list(range(2)) if gi == 0 else list(range(NT))
                        for ki, kt in enumerate(kts):
                            psb = 4 + (ecnt % 2)
                            E_t, b_E = Et[ecnt % 3]
                            ecnt += 1
                            S.op("pe", lambda kt=kt, psb=psb: nc.tensor.matmul(PS[psb][:, 0:n], lhsT=kn_t[:, kt * 128:(kt + 1) * 128], rhs=qn_t[:, t0:t0 + n], start=True, stop=False),
                                 reads=[b_kn, b_qn], writes=[PB[psb]])
                            S.op("pe", lambda kt=kt, psb=psb: nc.tensor.matmul(PS[psb][:, 0:n], lhsT=kr[:, kt * 128:(kt + 1) * 128], rhs=qr_t[:, t0:t0 + n], start=False, stop=True),
                                 reads=[b_kr, b_qr], writes=[PB[psb]])
                            S.op("act", lambda psb=psb, E_t=E_t: nc.scalar.activation(out=E_t[:, 0:n], in_=PS[psb][:, 0:n], func=AF.Exp, scale=MLA_SCALE),
                                 reads=[PB[psb]], writes=[b_E])
                            S.op("pe", lambda kt=kt, E_t=E_t, ki=ki: nc.tensor.matmul(PS[6][0:65, 0:n], lhsT=v_aug[:, kt, h, :], rhs=E_t[:, 0:n], start=(ki == 0), stop=(ki == len(kts) - 1)),
                                 reads=[b_va, b_E], writes=[PB[6]])
                        nm_t, b_nm = numt[ocnt % 2]
                        o_t, b_o = ot[ocnt % 2]
                        ocnt += 1
                        S.op("dve", lambda: nc.vector.reciprocal(out=rc[64:65, 0:n], in_=PS[6][64:65, 0:n]), reads=[PB[6]], writes=[b_rc])
                        S.op("act", lambda nm_t=nm_t: nc.scalar.copy(out=nm_t[:, 0:n], in_=PS[6][0:64, 0:n]), reads=[PB[6]], writes=[b_nm])
                        S.op("pe", lambda: nc.tensor.matmul(PS[7][0:64, 0:n], lhsT=onesf[64:65, 0:64], rhs=rc[64:65, 0:n], start=True, stop=True),
                             reads=[b_onesf, b_rc], writes=[PB[7]])
                        S.op("dve", lambda nm_t=nm_t, o_t=o_t: nc.vector.tensor_tensor(out=o_t[:, 0:n], in0=nm_t[:, 0:n], in1=PS[7][0:64, 0:n], op=ALU.mult),
                             reads=[b_nm, PB[7]], writes=[b_o])
                        S.dma(mla_o_d[h, :, t0:t0 + n], o_t[:, 0:n], reads=[b_o], writes=[b_mo])
                S.barrier()

        b_xres = [Buf("xres%d" % i) for i in range(NT)]
        stage_entry(0)
        for l in range(nlayers):
            last = (l == nlayers - 1)
            if not cfg.get("skip_mla"):
                stage_mla(l, last)
            if not cfg.get("skip_hg"):
                stage_hgrn(l)
            if not cfg.get("skip_gdn"):
                stage_gdn(l)
            if cfg.get("stop_after") == "mixers":
                break
            stage_merge(l, last)
            if cfg.get("stop_after") == "merge":
                break
            stage_moe(l, last)
        if "h_fm" in DBG:
            S.dma(DBG["h_fm"].rearrange("(kc p) t -> p kc t", p=128), h_fm[:], reads=[b_hfm], q="pool")
        if "xres" in DBG:
            S.dma(DBG["xres"], xres_d[:, :])
        if "mla_o" in DBG:
            S.dma(DBG["mla_o"], mla_o_d.rearrange("h d t -> (h d) t"), q="pool")
        if "hg_o" in DBG:
            S.dma(DBG["hg_o"], hg_o_d.rearrange("h d t -> (h d) t"), q="pool")
        if "gdn_o" in DBG:
            S.dma(DBG["gdn_o"], gdn_o_d.rearrange("h d t -> (h d) t"), q="pool")
        K.PS, K.PB = PS, PB

        S.finish()
    K.ninstr = S.ninstr
    return nc, K


WEIGHT_SHAPES = {
    "w_mod": [DEPTH, D, 6 * D], "b_mod": [DEPTH, 6 * D], "w_in": [DEPTH, D, IN_W],
    "w_q_b": [DEPTH, 384, 768], "w_kv_b": [DEPTH, 256, 1024],
    "hg_lb_logits": [DEPTH, 2, 512],
    "gdn_a_log": [DEPTH, 2, 4], "gdn_dt_bias": [DEPTH, 2, 4], "gdn_norm": [DEPTH, 128],
    "w_branch": [DEPTH, 3, 512, D], "w_out": [DEPTH, D, D],
    "ln1_g": [DEPTH, D], "ln1_b": [DEPTH, D], "ln2_g": [DEPTH, D], "ln2_b": [DEPTH, D],
    "w_router": [DEPTH, D, 64], "router_bias": [DEPTH, 64],
    "w_gu": [DEPTH, 64, D, 512], "w_down": [DEPTH, 64, 256, D], "w_sh_gu": [DEPTH, D, 512], "w_sh_down": [DEPTH, 256, D],
}
DERIVED_SHAPES = {
    "w_kr_sw": [DEPTH, D, 32], "w_qr_sw": [DEPTH, 384, 256],
    "q_a_norm_t": [DEPTH, 128, 3], "kv_a_norm_t": [DEPTH, 128, 2],
    "hg_norm_t": [128, DEPTH], "gdn_conv_t": [DEPTH, 128, 12, 5],
}
CONST_SHAPES = {
    "ident": [128, 128], "ropeC": [32, LAT], "ropeS": [32, LAT],
    "rmask": [128, T], "triu": [64, 64], "tril": [64, 64],
    "mist": [64, 2, 64], "mast": [64, 2, 64],
}


def host_consts():
    c = {}
    c["ident"] = np.eye(128, dtype=np.float32)
    pos = np.arange(LAT)
    row = (pos // 64).astype(np.float32)
    col = (pos % 64).astype(np.float32)
    inv = (np.float32(10000.0) ** (-np.arange(8, dtype=np.float32) / np.float32(8))).astype(np.float32)
    C = np.zeros((32, LAT), np.float32)
    Sg = np.zeros((32, LAT), np.float32)
    for ax, p in enumerate((row, col)):
        ang = (p[None, :] * inv[:, None]).astype(np.float32)
        for half in range(2):
            r0 = ax * 16 + half * 8
            C[r0:r0 + 8] = np.cos(ang)
            Sg[r0:r0 + 8] = np.sin(ang) * (-1.0 if half == 0 else 1.0)
    c["ropeC"] = C
    rm = np.ones((128, T), np.float32)
    rm[:, ::64] = 0.0
    c["rmask"] = rm
    c["triu"] = np.triu(np.ones((64, 64), np.float32))
    c["tril"] = np.tril(np.ones((64, 64), np.float32))
    c["mist"] = np.ascontiguousarray(np.stack([c["triu"], c["tril"]], axis=1))
    c["mast"] = np.ascontiguousarray(np.stack([c["tril"] - np.eye(64, dtype=np.float32), c["triu"] - np.eye(64, dtype=np.float32)], axis=1))
    c["ropeS"] = Sg
    return c


def prep_inputs(inputs):
    x = np.asarray(inputs["x"], np.float32)
    ctx = np.asarray(inputs["ctx"], np.float32)
    c = np.asarray(inputs["c"], np.float32)
    c_ctx = np.asarray(inputs["c_ctx"], np.float32)
    shared = {}
    for nm in WEIGHT_SHAPES:
        shared[nm] = np.ascontiguousarray(np.asarray(inputs[nm], np.float32)).reshape(WEIGHT_SHAPES[nm])
    shared.update(host_consts())
    perm = np.arange(32) ^ 8
    w_in = shared["w_in"]
    shared["w_kr_sw"] = np.ascontiguousarray(w_in[:, :, 640:672][:, :, perm])
    wqb = shared["w_q_b"].reshape(DEPTH, 384, 8, 96)
    shared["w_qr_sw"] = np.ascontiguousarray(wqb[:, :, :, 64:96][:, :, :, perm].reshape(DEPTH, 384, 256))
    shared["q_a_norm_t"] = np.ascontiguousarray(np.asarray(inputs["q_a_norm"], np.float32).reshape(DEPTH, 3, 128).transpose(0, 2, 1))
    shared["gdn_conv_t"] = np.ascontiguousarray(np.asarray(inputs["gdn_conv"], np.float32).reshape(DEPTH, 5, 12, 128).transpose(0, 3, 2, 1))
    shared["hg_norm_t"] = np.ascontiguousarray(np.asarray(inputs["hg_norm"], np.float32).T)
    shared["kv_a_norm_t"] = np.ascontiguousarray(np.asarray(inputs["kv_a_norm"], np.float32).reshape(DEPTH, 2, 128).transpose(0, 2, 1))
    maps = []
    for b in range(x.shape[0]):
        m = dict(shared)
        m["xin"] = np.ascontiguousarray(np.concatenate([ctx[b], x[b]], axis=0))
        m["cvecT"] = np.ascontiguousarray(np.stack([c[b], c_ctx], axis=1))
        maps.append(m)
    return maps


def kernel(**inputs):
    maps = prep_inputs(inputs)
    nc, K = build_program({})
    res = run_bass_kernel_spmd(nc, maps, core_ids=list(range(8)))
    out = np.stack([np.asarray(r["out"], np.float32) for r in res.results], axis=0)
    return out
```

```python
import contextlib
import numpy as np
import concourse.bass as bass
import concourse.mybir as mybir
from concourse.bass_utils import run_bass_kernel_spmd

F32 = mybir.dt.float32
BF16 = mybir.dt.bfloat16
AF = mybir.ActivationFunctionType
ALU = mybir.AluOpType
AX = mybir.AxisListType

EPOCH = 30000
NRING = 40

DEPTH = 4
D = 1024
KC = 8
LAT = 2048
CTX = 256
T = LAT + CTX
NT = T // 128
GROUPS = [(0, 256), (256, 512), (768, 512), (1280, 512), (1792, 512)]
NCH = T // 64
IN_W = 8368
MLA_SCALE = 96 ** -0.5
ALPHA = (2 * DEPTH) ** 0.25
O_HG = 672
O_GDN = O_HG + 2560
O_GG = O_GDN + 1536
O_GA = O_GG + 512
O_GB = O_GA + 8
O_GATES = O_GB + 8


class Buf:
    __slots__ = ("name", "w", "r", "ex")

    def __init__(self, name="", ex=False):
        self.name = name
        self.w = None
        self.r = []
        self.ex = ex


class Sched:
    def __init__(self, nc, es, self_sync=True):
        self.nc = nc
        self.es = es
        self.eng = {"pe": nc.tensor, "act": nc.scalar, "dve": nc.vector, "pool": nc.gpsimd, "sp": nc.sync}
        self.seq = {e: 0 for e in self.eng}
        self.sems = {e: [] for e in self.eng}
        self.known = {e: {} for e in self.eng}
        self.known_dma = {e: set() for e in self.eng}
        self.ring = [es.enter_context(nc.semaphore("dr%d" % i)) for i in range(NRING)]
        self.ring_cnt = [0] * NRING
        self.ring_next = 0
        self.self_sync = self_sync
        self.ninstr = 0

    def _sem(self, e, ep):
        while len(self.sems[e]) <= ep:
            self.sems[e].append(self.es.enter_context(self.nc.semaphore("s_%s%d" % (e, len(self.sems[e])))))
        return self.sems[e][ep]

    def _wait(self, e, tok):
        if tok is None:
            return
        if tok[0] == "dma":
            _, k, val = tok
            key = (k, val)
            if key in self.known_dma[e]:
                return
            self.eng[e].wait_ge(self.ring[k], val)
            self.known_dma[e].add(key)
            return
        _, e2, s = tok
        if e2 == e and (e == "pe" or not self.self_sync):
            return
        if self.known[e].get(e2, 0) >= s:
            return
        ep = (s - 1) // EPOCH
        self.eng[e].wait_ge(self._sem(e2, ep), s - ep * EPOCH)
        self.known[e][e2] = s

    def _deps(self, e, reads, writes):
        for b in reads:
            if b.w is not None:
                self._wait(e, b.w)
            if b.ex:
                for t in b.r:
                    if t[0] == "eng" and t[1] != e:
                        self._wait(e, t)
        for b in writes:
            if b.w is not None:
                self._wait(e, b.w)
            for t in b.r:
                self._wait(e, t)

    def _mark(self, tok, reads, writes):
        for b in reads:
            b.r.append(tok)
            if len(b.r) > 24:
                b.r = self._prune(b.r)
        for b in writes:
            b.w = tok
            b.r = []

    def _prune(self, r):
        best = {}
        out = []
        for t in r:
            if t[0] == "dma":
                out.append(t)
            elif t[1] not in best or best[t[1]][2] < t[2]:
                best[t[1]] = t
        return out + list(best.values())

    def op(self, e, fn, reads=(), writes=()):
        self._deps(e, reads, writes)
        ins = fn()
        self.seq[e] += 1
        s = self.seq[e]
        ep = (s - 1) // EPOCH
        ins.then_inc(self._sem(e, ep), 1)
        tok = ("eng", e, s)
        self._mark(tok, reads, writes)
        self.ninstr += 1
        return tok

    def dma(self, out, in_, reads=(), writes=(), q="sp", **kw):
        k = self.ring_next
        self.ring_next = (self.ring_next + 1) % NRING
        prev = self.ring_cnt[k]
        if prev > 0:
            self._wait(q, ("dma", k, 16 * prev))
        self._deps(q, reads, writes)
        self.eng[q].dma_start(out=out, in_=in_, **kw).then_inc(self.ring[k], 16)
        self.ring_cnt[k] = prev + 1
        tok = ("dma", k, 16 * (prev + 1))
        self._mark(tok, reads, writes)
        self.ninstr += 1
        return tok

    def barrier(self, engines=None):
        for e in (engines or self.eng):
            for e2 in self.eng:
                if self.seq[e2] > 0 and not (e2 == e and e == "pe"):
                    self._wait(e, ("eng", e2, self.seq[e2]))
            for k in range(NRING):
                if self.ring_cnt[k] > 0:
                    self._wait(e, ("dma", k, 16 * self.ring_cnt[k]))

    def finish(self):
        self.barrier(["sp"])


class Ctx:
    pass


def build_program(cfg):
    nlayers = cfg.get("nlayers", DEPTH)
    stop_after = cfg.get("stop_after", None)
    dbg = cfg.get("debug", [])
    nc = bass.Bass("TRN2", target_bir_lowering=False)
    K = Ctx()
    K.nc = nc

    def din(name, shape, dt=F32):
        return nc.dram_tensor(name, list(shape), dt, kind="ExternalInput").ap()

    def dscr(name, shape, dt=F32):
        return nc.dram_tensor(name, list(shape), dt, kind="Internal").ap()

    I = {}
    I["xin"] = din("xin", [T, D])
    I["cvecT"] = din("cvecT", [D, 2])
    for nm, shp in WEIGHT_SHAPES.items():
        I[nm] = din(nm, shp)
    for nm, shp in CONST_SHAPES.items():
        I[nm] = din(nm, shp)
    for nm, shp in DERIVED_SHAPES.items():
        I[nm] = din(nm, shp)
    out_d = nc.dram_tensor("out", [LAT, D], F32, kind="ExternalOutput").ap()
    DBG = {}
    for nm, shp in dbg:
        DBG[nm] = nc.dram_tensor("dbg_" + nm, list(shp), F32, kind="ExternalOutput").ap()

    modv_d = dscr("modv_d", [DEPTH, 2, 6 * D])
    xres_d = dscr("xres_d", [T, D])
    mla_o_d = dscr("mla_o_d", [8, 64, T], BF16)
    hg_o_d = dscr("hg_o_d", [4, 128, T], BF16)
    gdn_o_d = dscr("gdn_o_d", [4, 128, T], BF16)
    gdn_raw_d = dscr("gdn_raw_d", [2, T, 512])

    es = contextlib.ExitStack()
    with es:
        S = Sched(nc, es, self_sync=cfg.get('self_sync', True))
        K.S = S

        tlc = [0]

        def tl(st, name, shape, dt):
            tlc[0] += 1
            t = st.enter_context(nc.sbuf_tensor("sb%d_%s" % (tlc[0], name), list(shape), dt))
            return t, Buf(name)

        PS = []
        PB = []
        for i in range(8):
            PS.append(es.enter_context(nc.psum_tensor("ps%d" % i, [128, 512], F32)))
            PB.append(Buf("ps%d" % i, ex=True))

        identf, b_identf = tl(es, "identf", [128, 128], F32)
        identb, b_identb = tl(es, "identb", [128, 128], BF16)
        onesb, b_onesb = tl(es, "onesb", [128, 128], BF16)
        onesf, b_onesf = tl(es, "onesf", [128, 128], F32)
        S.dma(identf[:], I["ident"][:, :], writes=[b_identf])
        S.dma(identb[:], I["ident"][:, :], writes=[b_identb], q="pool")
        S.op("dve", lambda: nc.vector.memset(onesb[:], 1.0), writes=[b_onesb])
        S.op("dve", lambda: nc.vector.memset(onesf[:], 1.0), writes=[b_onesf])
        h_fm, b_hfm = tl(es, "h_fm", [128, KC, T], BF16)
        epsb, b_eps = tl(es, "epsb", [128, 1], F32)
        S.op("dve", lambda: nc.vector.memset(epsb[:], 1e-6), writes=[b_eps])

        def dbg_out(name, ap_sb, buf, dram_ap=None):
            if name in DBG:
                S.dma(dram_ap if dram_ap is not None else DBG[name], ap_sb, reads=[buf])

        with contextlib.ExitStack() as st:
            cv, b_cv = tl(st, "cv", [128, KC, 2], F32)
            scv, b_scv = tl(st, "scv", [128, KC, 2], F32)
            S.dma(cv[:], I["cvecT"].rearrange("(kc p) s -> p kc s", p=128), writes=[b_cv])
            S.op("act", lambda: nc.scalar.activation(out=scv[:], in_=cv[:], func=AF.Silu), reads=[b_cv], writes=[b_scv])
            wm = [tl(st, "wm%d" % i, [128, KC, 512], F32) for i in range(2)]
            bm, b_bm = tl(st, "bm", [1, 6 * D], F32)
            mv = [tl(st, "mv%d" % i, [2, 6 * D], F32) for i in range(2)]
            ones2, b_ones2 = tl(st, "ones2", [1, 2], F32)
            S.op("dve", lambda: nc.vector.memset(ones2[:], 1.0), writes=[b_ones2])
            cnt = 0
            for l in range(nlayers):
                mvt, b_mv = mv[l % 2]
                S.dma(bm[:], I["b_mod"][l:l + 1, :], writes=[b_bm])
                for cg in range(12):
                    wt, b_wt = wm[cnt % 2]
                    cnt += 1
                    S.dma(wt[:], I["w_mod"][l].rearrange("(kc p) n -> p kc n", p=128)[:, :, cg * 512:(cg + 1) * 512], writes=[b_wt])
                    pb = cnt % 2
                    for kc in range(KC):
                        S.op("pe", lambda kc=kc, wt=wt, pb=pb: nc.tensor.matmul(PS[pb][0:2, :], lhsT=scv[:, kc, :], rhs=wt[:, kc, :], start=(kc == 0), stop=False),
                             reads=[b_scv, b_wt], writes=[PB[pb]])
                    S.op("pe", lambda cg=cg, pb=pb: nc.tensor.matmul(PS[pb][0:2, :], lhsT=ones2[:], rhs=bm[:, cg * 512:(cg + 1) * 512], start=False, stop=True),
                         reads=[b_ones2, b_bm], writes=[PB[pb]])
                    S.op("act", lambda cg=cg, pb=pb, mvt=mvt: nc.scalar.copy(out=mvt[:, cg * 512:(cg + 1) * 512], in_=PS[pb][0:2, :]), reads=[PB[pb]], writes=[b_mv])
                S.dma(modv_d[l], mvt[:], reads=[b_mv], writes=[])
                K.modv_tok = None
            S.barrier()
        b_modv = Buf("modv_d")
        b_modv.w = None

        def load_bc(st, name, l, stream, j, plus_one=False):
            t, b = tl(st, name, [128, D], F32)
            S.dma(t[:], modv_d[l, stream:stream + 1, j * D:(j + 1) * D].partition_broadcast(128), writes=[b])
            if plus_one:
                S.op("pool", lambda: nc.gpsimd.tensor_scalar_add(out=t[:], in0=t[:], scalar1=1.0), reads=[b], writes=[b])
            return t, b

        def load_vec_bc(st, name, dram_row_ap, n=D):
            t, b = tl(st, name, [128, n], F32)
            S.dma(t[:], dram_row_ap.partition_broadcast(128), writes=[b])
            return t, b

        def to_fm(src_bf, b_src, i, psb):
            pv = PS[psb][:].bitcast(BF16)
            for kc in range(KC):
                S.op("pe", lambda kc=kc: nc.tensor.transpose(out=pv[:, kc * 128:(kc + 1) * 128], in_=src_bf[:, kc * 128:(kc + 1) * 128], identity=identb[:]),
                     reads=[b_src, b_identb], writes=[PB[psb]])
            S.op("act", lambda: nc.scalar.copy(out=h_fm[:, :, i * 128:(i + 1) * 128], in_=pv.rearrange("p (k t) -> p k t", k=KC)),
                 reads=[PB[psb]], writes=[b_hfm])

        def stage_entry(l):
            with contextlib.ExitStack() as st:
                bc = {}
                for s in range(2):
                    bc[(s, 0)] = load_bc(st, "bsh%d" % s, l, s, 0)
                    bc[(s, 1)] = load_bc(st, "bsc%d" % s, l, s, 1, plus_one=True)
                xt = [tl(st, "xt%d" % i, [128, D], F32) for i in range(2)]
                ht = [tl(st, "ht%d" % i, [128, D], BF16) for i in range(2)]
                for i in range(NT):
                    s = 1 if i < 2 else 0
                    x_t, b_x = xt[i % 2]
                    h_t, b_h = ht[i % 2]
                    S.dma(x_t[:], I["xin"][i * 128:(i + 1) * 128, :], writes=[b_x])
                    S.dma(xres_d[i * 128:(i + 1) * 128, :], x_t[:], reads=[b_x])
                    S.op("dve", lambda x_t=x_t, s=s: nc.vector.tensor_tensor(out=x_t[:], in0=x_t[:], in1=bc[(s, 1)][0][:], op=ALU.mult),
                         reads=[b_x, bc[(s, 1)][1]], writes=[b_x])
                    S.op("dve", lambda x_t=x_t, h_t=h_t, s=s: nc.vector.tensor_tensor(out=h_t[:], in0=x_t[:], in1=bc[(s, 0)][0][:], op=ALU.add),
                         reads=[b_x, bc[(s, 0)][1]], writes=[b_h])
                    to_fm(h_t, b_h, i, i % 2)
                S.barrier()


        lbT, b_lbT = tl(es, "lbT", [128, DEPTH, 8], F32)
        omlbT, b_omlbT = tl(es, "omlbT", [128, DEPTH, 8], F32)
        rmask, b_rmask = tl(es, "rmask", [128, T], BF16)
        triu, b_triu = tl(es, "triu", [64, 64], F32)
        tril, b_tril = tl(es, "tril", [64, 64], F32)
        S.dma(rmask[:], I["rmask"][:, :], writes=[b_rmask], q="pool")
        S.dma(triu[:], I["triu"][:, :], writes=[b_triu])
        S.dma(tril[:], I["tril"][:, :], writes=[b_tril])
        with contextlib.ExitStack() as st:
            lg, b_lg = tl(st, "lg", [32, 128], F32)
            eT, b_eT = tl(st, "eT", [128, DEPTH, 8], F32)
            tot, b_tot = tl(st, "lbtot", [128, 8], F32)
            S.dma(lg[:], I["hg_lb_logits"].rearrange("l s (h p) -> (l s h) p", p=128), writes=[b_lg])
            S.op("act", lambda: nc.scalar.activation(out=lg[:], in_=lg[:], func=AF.Exp), reads=[b_lg], writes=[b_lg])
            S.op("pe", lambda: nc.tensor.transpose(out=PS[0][:, 0:32], in_=lg[:], identity=identf[0:32, 0:32]), reads=[b_lg, b_identf], writes=[PB[0]])
            S.op("act", lambda: nc.scalar.copy(out=eT[:].rearrange("p l x -> p (l x)"), in_=PS[0][:, 0:32]), reads=[PB[0]], writes=[b_eT])
            S.op("dve", lambda: nc.vector.tensor_tensor(out=tot[:], in0=eT[:, 0, :], in1=eT[:, 1, :], op=ALU.add), reads=[b_eT], writes=[b_tot])
            S.op("dve", lambda: nc.vector.tensor_tensor(out=tot[:], in0=tot[:], in1=eT[:, 2, :], op=ALU.add), reads=[b_eT, b_tot], writes=[b_tot])
            S.op("dve", lambda: nc.vector.tensor_tensor(out=tot[:], in0=tot[:], in1=eT[:, 3, :], op=ALU.add), reads=[b_eT, b_tot], writes=[b_tot])
            S.op("dve", lambda: nc.vector.reciprocal(out=tot[:], in_=tot[:]), reads=[b_tot], writes=[b_tot])
            S.op("dve", lambda: nc.vector.memset(lbT[:, 0, :], 0.0), writes=[b_lbT])
            S.op("dve", lambda: nc.vector.tensor_copy(out=lbT[:, 1, :], in_=eT[:, 1, :]), reads=[b_eT, b_lbT], writes=[b_lbT])
            S.op("dve", lambda: nc.vector.tensor_tensor(out=lbT[:, 2, :], in0=lbT[:, 1, :], in1=eT[:, 2, :], op=ALU.add), reads=[b_eT, b_lbT], writes=[b_lbT])
            S.op("dve", lambda: nc.vector.tensor_tensor(out=lbT[:, 3, :], in0=lbT[:, 2, :], in1=eT[:, 3, :], op=ALU.add), reads=[b_eT, b_lbT], writes=[b_lbT])
            for l in range(1, DEPTH):
                S.op("dve", lambda l=l: nc.vector.tensor_tensor(out=lbT[:, l, :], in0=lbT[:, l, :], in1=tot[:], op=ALU.mult), reads=[b_tot, b_lbT], writes=[b_lbT])
            S.op("dve", lambda: nc.vector.tensor_scalar(out=omlbT[:], in0=lbT[:], scalar1=-1.0, scalar2=1.0, op0=ALU.mult, op1=ALU.add), reads=[b_lbT], writes=[b_omlbT])
            S.barrier()

        def stage_hgrn(l):
            winv = I["w_in"][l].rearrange("(kc p) n -> p kc n", p=128)
            with contextlib.ExitStack() as st:
                wh = [tl(st, "wh%d" % i, [128, KC, 5, 128], BF16) for i in range(2)]
                hgn4, b_hgn = tl(st, "hgn", [128, DEPTH], F32)
                S.dma(hgn4[:], I["hg_norm_t"][:, :], writes=[b_hgn])
                hgn = hgn4[:, l:l + 1]
                q_bf, b_q = tl(st, "hq_bf", [128, T], BF16)
                gate_sb, b_gate = tl(st, "hgate", [128, T], BF16)
                v_tm, b_v = tl(st, "hv_tm", [64, NCH, 128], BF16)
                A, b_A = tl(st, "hA", [128, T], F32)
                B, b_B = tl(st, "hB", [128, T], F32)
                Cc, b_C = tl(st, "hC", [128, T], F32)
                qt, b_qt = tl(st, "hqt", [128, T], BF16)
                kt, b_kt = tl(st, "hkt", [128, T], BF16)
                qh, b_qh = tl(st, "hqh", [128, T], BF16)
                kh, b_kh = tl(st, "hkh", [128, T], BF16)
                khT, b_khT = tl(st, "hkhT", [64, NCH, 128], BF16)
                aT, b_aT = tl(st, "haT", [64, NCH, 64], BF16)
                o_d = [tl(st, "ho%d" % i, [128, T], F32) for i in range(2)]
                tot, b_tot = tl(st, "htot", [128, NCH], F32)
                rmid, b_rmid = tl(st, "hrmid", [128, NCH], F32)
                egl, b_egl = tl(st, "hegl", [128, NCH], F32)
                Sst, b_S = tl(st, "hS", [128, 128], F32)
                Sb = [tl(st, "hSb%d" % i, [128, 128], BF16) for i in range(2)]
                rs0, b_rs0 = tl(st, "hrs0", [128, 512], F32)
                rs1, b_rs1 = tl(st, "hrs1", [128, 512], F32)
                og = [tl(st, "hog%d" % i, [128, 512], BF16) for i in range(2)]
                b_ho = Buf("hg_o")
                C3 = Cc[:].rearrange("p (c k) -> p c k", k=64)
                B3 = B[:].rearrange("p (c k) -> p c k", k=64)
                pcnt = [0]

                def proj(col, wt, b_wt, fn_evac):
                    for (t0, n) in GROUPS:
                        pb = pcnt[0] % 2
                        pcnt[0] += 1
                        for kc in range(KC):
                            S.op("pe", lambda kc=kc, pb=pb: nc.tensor.matmul(PS[pb][:, 0:n], lhsT=wt[:, kc, col, :], rhs=h_fm[:, kc, t0:t0 + n], start=(kc == 0), stop=(kc == KC - 1)),
                                 reads=[b_wt, b_hfm], writes=[PB[pb]])
                        fn_evac(pb, t0, n)

                ocnt = 0
                for hd in range(4):
                    wt, b_wt = wh[hd % 2]
                    for ci in range(5):
                        c0 = O_HG + ci * 512 + hd * 128
                        S.dma(wt[:, :, ci, :], winv[:, :, c0:c0 + 128], writes=[b_wt], q="pool")
                    proj(0, wt, b_wt, lambda pb, t0, n: S.op("act", lambda: nc.scalar.activation(out=q_bf[:, t0:t0 + n], in_=PS[pb][:, 0:n], func=AF.Silu), reads=[PB[pb]], writes=[b_q]))
                    proj(4, wt, b_wt, lambda pb, t0, n: S.op("act", lambda: nc.scalar.activation(out=gate_sb[:, t0:t0 + n], in_=PS[pb][:, 0:n], func=AF.Silu), reads=[PB[pb]], writes=[b_gate]))
                    for c4 in range(NCH // 4):
                        pb = pcnt[0] % 2
                        pcnt[0] += 1
                        for j in range(4):
                            c = c4 * 4 + j
                            for kc in range(KC):
                                S.op("pe", lambda kc=kc, c=c, j=j, pb=pb: nc.tensor.matmul(PS[pb][0:64, j * 128:(j + 1) * 128], lhsT=h_fm[:, kc, c * 64:(c + 1) * 64], rhs=wt[:, kc, 1, :],
                                                                                         start=(kc == 0), stop=(kc == KC - 1)), reads=[b_wt, b_hfm], writes=[PB[pb]])
                        S.op("act", lambda c4=c4, pb=pb: nc.scalar.copy(out=v_tm[:, c4 * 4:(c4 + 1) * 4, :], in_=PS[pb][0:64, :].rearrange("p (j x) -> p j x", j=4)),
                             reads=[PB[pb]], writes=[b_v])
                    for s in range(2):
                        o_t, b_o = o_d[s]
                        lbc = lbT[:, l, s * 4 + hd:s * 4 + hd + 1]
                        omc = omlbT[:, l, s * 4 + hd:s * 4 + hd + 1]
                        proj(2 + s, wt, b_wt, lambda pb, t0, n: S.op("act", lambda: nc.scalar.activation(out=A[:, t0:t0 + n], in_=PS[pb][:, 0:n], func=AF.Sigmoid), reads=[PB[pb]], writes=[b_A]))
                        S.op("dve", lambda: nc.vector.tensor_scalar(out=A[:], in0=A[:], scalar1=omc, scalar2=lbc, op0=ALU.mult, op1=ALU.add), reads=[b_A, b_lbT, b_omlbT], writes=[b_A])
                        S.op("act", lambda: nc.scalar.activation(out=B[:], in_=A[:], func=AF.Ln), reads=[b_A], writes=[b_B])
                        S.op("pool", lambda: nc.gpsimd.tensor_scalar(out=A[:], in0=A[:], scalar1=-1.0, scalar2=1.0, op0=ALU.mult, op1=ALU.add), reads=[b_A, b_B], writes=[b_A])
                        S.op("dve", lambda: nc.vector.tensor_tensor_scan(out=Cc[:], data0=rmask[:], data1=B[:], initial=0.0, op0=ALU.mult, op1=ALU.add), reads=[b_rmask, b_B], writes=[b_C])
                        S.op("dve", lambda: nc.vector.tensor_copy(out=tot[:], in_=C3[:, :, 63]), reads=[b_C], writes=[b_tot])
                        totb = tot[:].unsqueeze(2).to_broadcast([128, NCH, 64])
                        if s == 1:
                            S.op("dve", lambda: nc.vector.scalar_tensor_tensor(out=C3, in0=C3, scalar=-1.0, in1=totb, op0=ALU.mult, op1=ALU.add), reads=[b_C, b_tot], writes=[b_C])
                            S.op("dve", lambda: nc.vector.tensor_tensor(out=Cc[:], in0=Cc[:], in1=B[:], op=ALU.add), reads=[b_C, b_B], writes=[b_C])
                        S.op("dve", lambda: nc.vector.tensor_copy(out=rmid[:], in_=C3[:, :, 31 + s]), reads=[b_C], writes=[b_rmid])
                        S.op("act", lambda: nc.scalar.activation(out=egl[:], in_=tot[:], func=AF.Exp), reads=[b_tot], writes=[b_egl])
                        S.op("dve", lambda: nc.vector.tensor_tensor(out=B3, in0=C3, in1=rmid[:].unsqueeze(2).to_broadcast([128, NCH, 64]), op=ALU.subtract), reads=[b_C, b_rmid, b_B], writes=[b_B])
                        S.op("act", lambda: nc.scalar.activation(out=B[:], in_=B[:], func=AF.Exp), reads=[b_B], writes=[b_B])
                        S.op("pool", lambda: nc.gpsimd.tensor_tensor(out=qt[:], in0=q_bf[:], in1=B[:], op=ALU.mult), reads=[b_q, b_B], writes=[b_qt])
                        S.op("dve", lambda: nc.vector.reciprocal(out=B[:], in_=B[:]), reads=[b_B, b_qt], writes=[b_B])
                        S.op("dve", lambda: nc.vector.tensor_tensor(out=kt[:], in0=A[:], in1=B[:], op=ALU.mult), reads=[b_A, b_B], writes=[b_kt])
                        S.op("act", lambda: nc.scalar.activation(out=B[:], in_=Cc[:], func=AF.Exp), reads=[b_C, b_kt], writes=[b_B])
                        S.op("pool", lambda: nc.gpsimd.tensor_tensor(out=qh[:], in0=q_bf[:], in1=B[:], op=ALU.mult), reads=[b_q, b_B], writes=[b_qh])
                        S.op("dve", lambda: nc.vector.scalar_tensor_tensor(out=B3, in0=C3, scalar=-1.0, in1=totb, op0=ALU.mult, op1=ALU.add), reads=[b_C, b_tot, b_qh], writes=[b_B])
                        S.op("act", lambda: nc.scalar.activation(out=B[:], in_=B[:], func=AF.Exp), reads=[b_B], writes=[b_B])
                        S.op("dve", lambda: nc.vector.tensor_tensor(out=kh[:], in0=A[:], in1=B[:], op=ALU.mult), reads=[b_A, b_B], writes=[b_kh])
                        pvb = PS[2][:].bitcast(BF16)
                        msk = triu if s == 0 else tril
                        b_msk = b_triu if s == 0 else b_tril
                        fr = 0 if s == 0 else 32
                        dr = 32 - fr
                        S.op("dve", lambda: nc.vector.memset(aT[dr:dr + 32, :, fr:fr + 32], 0.0), writes=[b_aT])
                        for c8 in range(0, NCH, 8):
                            nb = min(8, NCH - c8)
                            for j in range(nb):
                                c = c8 + j
                                S.op("pe", lambda c=c, j=j: nc.tensor.transpose(out=pvb[0:64, j * 128:(j + 1) * 128], in_=kh[:, c * 64:(c + 1) * 64], identity=identb[:]),
                                     reads=[b_kh, b_identb], writes=[PB[2]])
                            S.op("act", lambda c8=c8, nb=nb: nc.scalar.copy(out=khT[:, c8:c8 + nb, :], in_=pvb[0:64, 0:nb * 128].rearrange("p (j x) -> p j x", j=nb)),
                                 reads=[PB[2]], writes=[b_khT])
                            for j in range(nb):
                                c = c8 + j
                                S.op("pe", lambda c=c, j=j: nc.tensor.matmul(PS[3][fr:fr + 32, j * 64:(j + 1) * 64], lhsT=kt[:, c * 64 + fr:c * 64 + fr + 32], rhs=qt[:, c * 64:(c + 1) * 64], start=True, stop=True),
                                     reads=[b_kt, b_qt], writes=[PB[3]])
                                S.op("pe", lambda c=c, j=j: nc.tensor.matmul(PS[3][dr:dr + 32, j * 64 + dr:j * 64 + dr + 32], lhsT=kt[:, c * 64 + dr:c * 64 + dr + 32], rhs=qt[:, c * 64 + dr:c * 64 + dr + 32], start=True, stop=True),
                                     reads=[b_kt, b_qt], writes=[PB[3]])
                            pv3 = PS[3][:, 0:nb * 64].rearrange("p (j x) -> p j x", j=nb)
                            S.op("dve", lambda c8=c8, nb=nb, pv3=pv3: nc.vector.tensor_tensor(out=aT[fr:fr + 32, c8:c8 + nb, :], in0=pv3[fr:fr + 32, :, :],
                                                                                     in1=msk[fr:fr + 32, :].unsqueeze(1).to_broadcast([32, nb, 64]), op=ALU.mult),
                                 reads=[PB[3], b_msk], writes=[b_aT])
                            S.op("dve", lambda c8=c8, nb=nb, pv3=pv3: nc.vector.tensor_tensor(out=aT[dr:dr + 32, c8:c8 + nb, dr:dr + 32], in0=pv3[dr:dr + 32, :, dr:dr + 32],
                                                                                     in1=msk[dr:dr + 32, dr:dr + 32].unsqueeze(1).to_broadcast([32, nb, 32]), op=ALU.mult),
                                 reads=[PB[3], b_msk], writes=[b_aT])
                        order = list(range(NCH)) if s == 0 else [3, 2, 1, 0] + list(range(NCH - 1, 3, -1))
                        for idx, c in enumerate(order):
                            po = 4 + idx % 2
                            pS = 6 + idx % 2
                            if idx > 0:
                                sb_t, b_sb = Sb[idx % 2]
                                S.op("pe", lambda c=c, po=po, sb_t=sb_t: nc.tensor.matmul(PS[po][:, 0:64], lhsT=sb_t[:], rhs=qh[:, c * 64:(c + 1) * 64], start=True, stop=False),
                                     reads=[b_sb, b_qh], writes=[PB[po]])
                            S.op("pe", lambda c=c, po=po, idx=idx: nc.tensor.matmul(PS[po][:, 0:64], lhsT=v_tm[:, c, :], rhs=aT[:, c, :], start=(idx == 0), stop=True),
                                 reads=[b_v, b_aT], writes=[PB[po]])
                            S.op("act", lambda c=c, po=po: nc.scalar.copy(out=o_t[:, c * 64:(c + 1) * 64], in_=PS[po][:, 0:64]), reads=[PB[po]], writes=[b_o])
                            if idx < NCH - 1:
                                S.op("pe", lambda c=c, pS=pS: nc.tensor.matmul(PS[pS][:, 0:128], lhsT=khT[:, c, :], rhs=v_tm[:, c, :], start=True, stop=True),
                                     reads=[b_khT, b_v], writes=[PB[pS]])
                                if idx == 0:
                                    S.op("dve", lambda pS=pS: nc.vector.tensor_copy(out=Sst[:], in_=PS[pS][:, 0:128]), reads=[PB[pS]], writes=[b_S])
                                else:
                                    S.op("dve", lambda c=c, pS=pS: nc.vector.scalar_tensor_tensor(out=Sst[:], in0=Sst[:], scalar=egl[:, c:c + 1], in1=PS[pS][:, 0:128], op0=ALU.mult, op1=ALU.add),
                                         reads=[b_S, b_egl, PB[pS]], writes=[b_S])
                                nsb, b_nsb = Sb[(idx + 1) % 2]
                                S.op("act", lambda nsb=nsb: nc.scalar.copy(out=nsb[:], in_=Sst[:]), reads=[b_S], writes=[b_nsb])
                    o_f, b_of = o_d[0]
                    o_b, b_ob = o_d[1]
                    S.op("dve", lambda: nc.vector.tensor_tensor(out=o_f[:], in0=o_f[:], in1=o_b[:], op=ALU.add), reads=[b_of, b_ob], writes=[b_of])
                    S.op("act", lambda: nc.scalar.activation(out=qt[:], in_=o_f[:], func=AF.Square), reads=[b_of, b_qt], writes=[b_qt])
                    for (t0, n) in GROUPS:
                        pb = pcnt[0] % 2
                        pcnt[0] += 1
                        og_t, b_og = og[ocnt % 2]
                        ocnt += 1
                        S.op("pe", lambda pb=pb: nc.tensor.matmul(PS[pb][:, 0:n], lhsT=onesb[:], rhs=qt[:, t0:t0 + n], start=True, stop=True), reads=[b_onesb, b_qt], writes=[PB[pb]])
                        S.op("act", lambda pb=pb: nc.scalar.activation(out=rs0[:, 0:n], in_=PS[pb][:, 0:n], func=AF.Sqrt, scale=1.0 / 128, bias=epsb[:, 0:1]), reads=[PB[pb], b_eps], writes=[b_rs0])
                        S.op("dve", lambda: nc.vector.reciprocal(out=rs1[:, 0:n], in_=rs0[:, 0:n]), reads=[b_rs0], writes=[b_rs1])
                        S.op("dve", lambda: nc.vector.scalar_tensor_tensor(out=rs0[:, 0:n], in0=o_f[:, t0:t0 + n], scalar=hgn, in1=rs1[:, 0:n], op0=ALU.mult, op1=ALU.mult),
                             reads=[b_of, b_hgn, b_rs1, b_rs0], writes=[b_rs0])
                        S.op("dve", lambda og_t=og_t: nc.vector.tensor_tensor(out=og_t[:, 0:n], in0=rs0[:, 0:n], in1=gate_sb[:, t0:t0 + n], op=ALU.mult), reads=[b_rs0, b_gate], writes=[b_og])
                        S.dma(hg_o_d[hd, :, t0:t0 + n], og_t[:, 0:n], reads=[b_og], writes=[b_ho])
                S.barrier()


        def stage_gdn(l):
            winv = I["w_in"][l].rearrange("(kc p) n -> p kc n", p=128)
            with contextlib.ExitStack() as st0:
                mist, b_mist = tl(st0, "mist", [64, 2, 64], F32)
                mast, b_mast = tl(st0, "mast", [64, 2, 64], F32)
                S.dma(mist[:], I["mist"][:, :, :], writes=[b_mist])
                S.dma(mast[:], I["mast"][:, :, :], writes=[b_mast])
                g_t, b_g = tl(st0, "g_g", [64, NCH, 8], F32)
                beta, b_beta = tl(st0, "g_beta", [64, NCH, 8], F32)
                nbeta, b_nbeta = tl(st0, "g_nbeta", [64, NCH, 8], F32)
                egc, b_egc = tl(st0, "g_egc", [64, NCH, 8], F32)
                negc, b_negc = tl(st0, "g_negc", [64, NCH, 8], F32)
                ekd, b_ekd = tl(st0, "g_ekd", [64, NCH, 8], F32)
                egl, b_egl = tl(st0, "g_egl", [128, NCH, 8], F32)
                cw, b_cw = tl(st0, "g_cw", [128, 12, 5], F32)
                S.dma(cw[:], I["gdn_conv_t"][l], writes=[b_cw])
                with contextlib.ExitStack() as st:
                    wg, b_wg = tl(st, "g_wg", [128, KC, 16], BF16)
                    S.dma(wg[:], winv[:, :, O_GA:O_GA + 16], writes=[b_wg], q="pool")
                    gab, b_gab = tl(st, "g_gab", [64, NCH, 16], F32)
                    alog, b_alog = tl(st, "g_alog", [64, 8], F32)
                    dtb, b_dtb = tl(st, "g_dtb", [64, 8], F32)
                    S.dma(alog[:], I["gdn_a_log"][l:l + 1].rearrange("o s h -> o (s h)").partition_broadcast(64), writes=[b_alog])
                    S.dma(dtb[:], I["gdn_dt_bias"][l:l + 1].rearrange("o s h -> o (s h)").partition_broadcast(64), writes=[b_dtb])
                    for c in range(NCH):
                        pb = c // 32
                        cc = c % 32
                        for kc in range(KC):
                            S.op("pe", lambda c=c, kc=kc, pb=pb, cc=cc: nc.tensor.matmul(PS[pb][0:64, cc * 16:(cc + 1) * 16], lhsT=h_fm[:, kc, c * 64:(c + 1) * 64], rhs=wg[:, kc, :],
                                                                                     start=(kc == 0), stop=(kc == KC - 1)), reads=[b_hfm, b_wg], writes=[PB[pb]])
                    S.op("act", lambda: nc.scalar.copy(out=gab[:, 0:32, :], in_=PS[0][0:64, :].rearrange("p (c x) -> p c x", x=16)), reads=[PB[0]], writes=[b_gab])
                    S.op("act", lambda: nc.scalar.copy(out=gab[:, 32:36, :], in_=PS[1][0:64, 0:64].rearrange("p (c x) -> p c x", x=16)), reads=[PB[1]], writes=[b_gab])
                    S.op("act", lambda: nc.scalar.activation(out=alog[:], in_=alog[:], func=AF.Exp), reads=[b_alog], writes=[b_alog])
                    S.op("dve", lambda: nc.vector.tensor_tensor(out=g_t[:], in0=gab[:, :, 0:8], in1=dtb[:].unsqueeze(1).to_broadcast([64, NCH, 8]), op=ALU.add), reads=[b_gab, b_dtb], writes=[b_g])
                    S.op("act", lambda: nc.scalar.activation(out=g_t[:], in_=g_t[:], func=AF.Exp), reads=[b_g], writes=[b_g])
                    S.op("act", lambda: nc.scalar.activation(out=g_t[:], in_=g_t[:], func=AF.Ln, bias=onesf[0:64, 0:1]), reads=[b_g, b_onesf], writes=[b_g])
                    S.op("dve", lambda: nc.vector.scalar_tensor_tensor(out=g_t[:], in0=g_t[:], scalar=-1.0, in1=alog[:].unsqueeze(1).to_broadcast([64, NCH, 8]), op0=ALU.mult, op1=ALU.mult),
                         reads=[b_g, b_alog], writes=[b_g])
                    S.op("act", lambda: nc.scalar.activation(out=beta[:], in_=gab[:, :, 8:16], func=AF.Sigmoid), reads=[b_gab], writes=[b_beta])
                    S.op("dve", lambda: nc.vector.tensor_scalar(out=nbeta[:], in0=beta[:], scalar1=-1.0, scalar2=None, op0=ALU.mult), reads=[b_beta], writes=[b_nbeta])
                    for c in range(NCH):
                        for sd in range(2):
                            S.op("pe", lambda c=c, sd=sd: nc.tensor.matmul(PS[2][0:64, c * 8 + sd * 4:c * 8 + sd * 4 + 4], lhsT=mist[:, sd, :], rhs=g_t[:, c, sd * 4:sd * 4 + 4], start=True, stop=True),
                                 reads=[b_mist, b_g], writes=[PB[2]])
                            S.op("pe", lambda c=c, sd=sd: nc.tensor.matmul(PS[3][0:64, c * 8 + sd * 4:c * 8 + sd * 4 + 4], lhsT=mast[:, sd, :], rhs=g_t[:, c, sd * 4:sd * 4 + 4], start=True, stop=True),
                                 reads=[b_mast, b_g], writes=[PB[3]])
                        S.op("pe", lambda c=c: nc.tensor.matmul(PS[4][:, c * 8:c * 8 + 8], lhsT=onesf[0:64, :], rhs=g_t[:, c, :], start=True, stop=True), reads=[b_onesf, b_g], writes=[PB[4]])
                    S.op("act", lambda: nc.scalar.activation(out=egc[:].rearrange("p c x -> p (c x)"), in_=PS[2][0:64, 0:NCH * 8], func=AF.Exp), reads=[PB[2]], writes=[b_egc])
                    S.op("act", lambda: nc.scalar.activation(out=ekd[:].rearrange("p c x -> p (c x)"), in_=PS[3][0:64, 0:NCH * 8], func=AF.Exp), reads=[PB[3]], writes=[b_ekd])
                    S.op("act", lambda: nc.scalar.activation(out=egl[:].rearrange("p c x -> p (c x)"), in_=PS[4][:, 0:NCH * 8], func=AF.Exp), reads=[PB[4]], writes=[b_egl])
                    S.op("dve", lambda: nc.vector.tensor_scalar(out=negc[:], in0=egc[:], scalar1=-1.0, scalar2=None, op0=ALU.mult), reads=[b_egc], writes=[b_negc])
                    S.barrier()
                if cfg.get("gdn_stop") == 1:
                    S.barrier()
                    return

                for pr in range(2):
                    with contextlib.ExitStack() as st:
                        q_fm = [tl(st, "g_q%d" % i, [128, T], BF16) for i in range(2)]
                        k_fm = [tl(st, "g_k%d" % i, [128, T], BF16) for i in range(2)]
                        k_tm = [tl(st, "g_ktm%d" % i, [64, NCH, 128], BF16) for i in range(2)]
                        v_tm = [tl(st, "g_vtm%d" % i, [64, NCH, 128], BF16) for i in range(2)]
                        aqkT, b_aqkT = tl(st, "g_aqkT", [64, NCH, 4, 64], BF16)
                        R5b, b_R5b = tl(st, "g_R5b", [64, NCH, 4, 64], BF16)
                        with contextlib.ExitStack() as st2:
                            wc = [tl(st2, "g_wc%d" % i, [128, KC, 128], BF16) for i in range(2)]
                            zpad, b_zp = tl(st2, "g_zpad", [128, T + 8], F32)
                            acc, b_acc = tl(st2, "g_acc", [128, T + 8], F32)
                            xs, b_xs = tl(st2, "g_xs", [128, T], F32)
                            sqb, b_sqb = tl(st2, "g_sq", [128, T], BF16)
                            vfm, b_vfm = tl(st2, "g_vfm", [128, T], BF16)
                            r0, b_r0 = tl(st2, "g_r0", [128, 512], F32)
                            r1, b_r1 = tl(st2, "g_r1", [128, 512], F32)
                            S.op("dve", lambda: nc.vector.memset(zpad[:], 0.0), writes=[b_zp])
                            wcnt = 0
                            for hh in range(2):
                                hd = pr * 2 + hh
                                for part in range(3):
                                    ch = part * 4 + hd
                                    wct, b_wc = wc[wcnt % 2]
                                    wcnt += 1
                                    S.dma(wct[:], winv[:, :, O_GDN + ch * 128:O_GDN + (ch + 1) * 128], writes=[b_wc], q="pool")
                                    for gi, (t0, n) in enumerate(GROUPS):
                                        pb = gi % 2
                                        for kc in range(KC):
                                            S.op("pe", lambda kc=kc, pb=pb, wct=wct: nc.tensor.matmul(PS[pb][:, 0:n], lhsT=wct[:, kc, :], rhs=h_fm[:, kc, t0:t0 + n], start=(kc == 0), stop=(kc == KC - 1)),
                                                 reads=[b_wc, b_hfm], writes=[PB[pb]])
                                        z0 = 2 + t0 if gi == 0 else 6 + t0
                                        S.op("act", lambda pb=pb, z0=z0: nc.scalar.copy(out=zpad[:, z0:z0 + n], in_=PS[pb][:, 0:n]), reads=[PB[pb]], writes=[b_zp])
                                    NW = T + 4
                                    S.op("dve", lambda ch=ch: nc.vector.tensor_scalar(out=acc[:, 2:2 + NW], in0=zpad[:, 0:NW], scalar1=cw[:, ch, 0:1], scalar2=None, op0=ALU.mult),
                                         reads=[b_zp, b_cw], writes=[b_acc])
                                    for tau in range(1, 5):
                                        S.op("dve", lambda ch=ch, tau=tau: nc.vector.scalar_tensor_tensor(out=acc[:, 2:2 + NW], in0=zpad[:, tau:tau + NW], scalar=cw[:, ch, tau:tau + 1], in1=acc[:, 2:2 + NW],
                                                                                                        op0=ALU.mult, op1=ALU.add), reads=[b_zp, b_cw, b_acc], writes=[b_acc])
                                    if part == 2:
                                        S.op("act", lambda: nc.scalar.activation(out=vfm[:, 0:CTX], in_=acc[:, 2:2 + CTX], func=AF.Silu), reads=[b_acc], writes=[b_vfm])
                                        S.op("act", lambda: nc.scalar.activation(out=vfm[:, CTX:T], in_=acc[:, 6 + CTX:6 + T], func=AF.Silu), reads=[b_acc], writes=[b_vfm])
                                        srcs = [(vfm, b_vfm, v_tm[hh])]
                                    else:
                                        S.op("act", lambda: nc.scalar.activation(out=xs[:, 0:CTX], in_=acc[:, 2:2 + CTX], func=AF.Silu), reads=[b_acc], writes=[b_xs])
                                        S.op("act", lambda: nc.scalar.activation(out=xs[:, CTX:T], in_=acc[:, 6 + CTX:6 + T], func=AF.Silu), reads=[b_acc], writes=[b_xs])
                                        S.op("act", lambda: nc.scalar.activation(out=sqb[:], in_=xs[:], func=AF.Square), reads=[b_xs], writes=[b_sqb])
                                        dst, b_dst = (q_fm if part == 0 else k_fm)[hh]
                                        for gi, (t0, n) in enumerate(GROUPS):
                                            pb = 2 + gi % 2
                                            S.op("pe", lambda pb=pb: nc.tensor.matmul(PS[pb][:, 0:n], lhsT=onesb[:], rhs=sqb[:, t0:t0 + n], start=True, stop=True), reads=[b_onesb, b_sqb], writes=[PB[pb]])
                                            S.op("act", lambda pb=pb: nc.scalar.activation(out=r0[:, 0:n], in_=PS[pb][:, 0:n], func=AF.Sqrt, bias=epsb[:, 0:1]), reads=[PB[pb], b_eps], writes=[b_r0])
                                            S.op("dve", lambda: nc.vector.reciprocal(out=r1[:, 0:n], in_=r0[:, 0:n]), reads=[b_r0], writes=[b_r1])
                                            S.op("dve", lambda dst=dst: nc.vector.scalar_tensor_tensor(out=dst[:, t0:t0 + n], in0=xs[:, t0:t0 + n], scalar=(128 ** -0.5 if part == 0 else 1.0), in1=r1[:, 0:n],
                                                                                                   op0=ALU.mult, op1=ALU.mult), reads=[b_xs, b_r1], writes=[b_dst])
                                        srcs = [(dst, b_dst, k_tm[hh])] if part == 1 else []
                                    for (src, b_src, (dtm, b_dtm)) in srcs:
                                        pvb = PS[4][:].bitcast(BF16)
                                        pvb2 = PS[5][:].bitcast(BF16)
                                        for c8 in range(0, NCH, 8):
                                            nb = min(8, NCH - c8)
                                            pv = pvb if (c8 // 8) % 2 == 0 else pvb2
                                            pbi = 4 + (c8 // 8) % 2
                                            for j in range(nb):
                                                c = c8 + j
                                                S.op("pe", lambda c=c, j=j, pv=pv, src=src: nc.tensor.transpose(out=pv[0:64, j * 128:(j + 1) * 128], in_=src[:, c * 64:(c + 1) * 64], identity=identb[:]),
                                                     reads=[b_src, b_identb], writes=[PB[pbi]])
                                            S.op("act", lambda c8=c8, nb=nb, pv=pv, dtm=dtm: nc.scalar.copy(out=dtm[:, c8:c8 + nb, :], in_=pv[0:64, 0:nb * 128].rearrange("p (j x) -> p j x", j=nb)),
                                                 reads=[PB[pbi]], writes=[b_dtm])
                            S.barrier()
                        if cfg.get("gdn_stop") == 2:
                            S.barrier()
                            return
                        with contextlib.ExitStack() as st2:
                            NCB = 2
                            NB = NCB * 4
                            mistF, b_mistF = tl(st2, "g_mistF", [64, NCB, 2, 2, 64], F32)
                            mastF, b_mastF = tl(st2, "g_mastF", [64, NCB, 2, 2, 64], F32)
                            for cj in range(NCB):
                                for hh in range(2):
                                    S.op("dve", lambda cj=cj, hh=hh: nc.vector.tensor_copy(out=mistF[:, cj, :, hh, :], in_=mist[:]), reads=[b_mist], writes=[b_mistF])
                                    S.op("dve", lambda cj=cj, hh=hh: nc.vector.tensor_copy(out=mastF[:, cj, :, hh, :], in_=mast[:]), reads=[b_mast], writes=[b_mastF])
                            fl = lambda t_: t_[:].rearrange("p b x -> p (b x)")
                            v3 = lambda t_: t_[:].rearrange("p (cs h) x -> p cs h x", h=2)
                            mI3 = mistF[:].rearrange("p c s h x -> p (c s) h x")
                            mA3 = mastF[:].rearrange("p c s h x -> p (c s) h x")
                            lI, b_lI = tl(st2, "g_lI", [64, NB, 64], F32)
                            lA, b_lA = tl(st2, "g_lA", [64, NB, 64], F32)
                            Dec, b_Dec = tl(st2, "g_Dec", [64, NB, 64], F32)
                            DecT, b_DecT = tl(st2, "g_DecT", [64, NB, 64], F32)
                            t1, b_t1 = tl(st2, "g_t1", [64, NB, 64], F32)
                            Pm = [tl(st2, "g_P%d" % i, [64, NB, 64], F32) for i in range(2)]
                            Qm = [tl(st2, "g_Q%d" % i, [64, NB, 64], F32) for i in range(2)]
                            Rm = [tl(st2, "g_R%d" % i, [64, NB, 64], F32) for i in range(2)]

                            def gcols(tile_, c0):
                                return tile_[:, c0:c0 + NCB, :].rearrange("p c (s h) -> p (c s) h", s=2)[:, :, pr * 2:pr * 2 + 2]

                            for c0 in range(0, NCH, NCB):
                                for cj in range(NCB):
                                    c = c0 + cj
                                    cs = slice(c * 64, (c + 1) * 64)
                                    for b in range(4):
                                        hh = b % 2
                                        bb = cj * 4 + b
                                        kf, b_kf = k_fm[hh]
                                        qf, b_qf = q_fm[hh]
                                        S.op("pe", lambda bb=bb, kf=kf, cs=cs: nc.tensor.matmul(PS[0][0:64, bb * 64:(bb + 1) * 64], lhsT=kf[:, cs], rhs=kf[:, cs], start=True, stop=True), reads=[b_kf], writes=[PB[0]])
                                        S.op("pe", lambda bb=bb, kf=kf, qf=qf, cs=cs: nc.tensor.matmul(PS[7][0:64, bb * 64:(bb + 1) * 64], lhsT=kf[:, cs], rhs=qf[:, cs], start=True, stop=True),
                                             reads=[b_kf, b_qf], writes=[PB[7]])
                                g3 = gcols(g_t, c0).unsqueeze(3).to_broadcast([64, 2 * NCB, 2, 64])
                                S.op("dve", lambda g3=g3: nc.vector.tensor_tensor(out=v3(lI), in0=mI3, in1=g3, op=ALU.mult), reads=[b_mistF, b_g], writes=[b_lI])
                                S.op("dve", lambda g3=g3: nc.vector.tensor_tensor(out=v3(lA), in0=mA3, in1=g3, op=ALU.mult), reads=[b_mastF, b_g], writes=[b_lA])
                                for bb in range(NB):
                                    sd = (bb % 4) // 2
                                    S.op("pe", lambda bb=bb, sd=sd: nc.tensor.matmul(PS[1][0:64, bb * 64:(bb + 1) * 64], lhsT=lI[:, bb, :], rhs=mast[:, sd, :], start=True, stop=True), reads=[b_lI, b_mast], writes=[PB[1]])
                                    S.op("pe", lambda bb=bb, sd=sd: nc.tensor.matmul(PS[2][0:64, bb * 64:(bb + 1) * 64], lhsT=lA[:, bb, :], rhs=mist[:, sd, :], start=True, stop=True), reads=[b_lA, b_mist], writes=[PB[2]])
                                S.op("act", lambda: nc.scalar.activation(out=fl(Dec), in_=PS[1][0:64, :], func=AF.Exp), reads=[PB[1]], writes=[b_Dec])
                                S.op("act", lambda: nc.scalar.activation(out=fl(DecT), in_=PS[2][0:64, :], func=AF.Exp), reads=[PB[2]], writes=[b_DecT])
                                P0, b_P0 = Pm[0]
                                Q0, b_Q0 = Qm[0]
                                R0, b_R0 = Rm[0]
                                S.op("dve", lambda: nc.vector.tensor_tensor(out=fl(t1), in0=PS[0][0:64, :], in1=fl(Dec), op=ALU.mult), reads=[PB[0], b_Dec], writes=[b_t1])
                                S.op("dve", lambda: nc.vector.tensor_tensor(out=fl(t1), in0=fl(t1), in1=mastF[:].rearrange("p c s h x -> p (c s h x)"), op=ALU.mult), reads=[b_t1, b_mastF], writes=[b_t1])
                                S.op("dve", lambda c0=c0: nc.vector.tensor_tensor(out=v3(P0), in0=v3(t1), in1=gcols(nbeta, c0).unsqueeze(3).to_broadcast([64, 2 * NCB, 2, 64]), op=ALU.mult),
                                     reads=[b_t1, b_nbeta], writes=[b_P0])
                                S.op("dve", lambda: nc.vector.tensor_tensor(out=fl(DecT), in0=PS[7][0:64, :], in1=fl(DecT), op=ALU.mult), reads=[PB[7], b_DecT], writes=[b_DecT])
                                S.op("dve", lambda c0=c0: nc.vector.tensor_tensor(out=aqkT[:, c0:c0 + NCB, :, :].rearrange("p c b x -> p (c b x)"), in0=fl(DecT), in1=mistF[:].rearrange("p c s h x -> p (c s h x)"), op=ALU.mult),
                                     reads=[b_DecT, b_mistF], writes=[b_aqkT])
                                for bb in range(NB):
                                    S.op("pe", lambda bb=bb: nc.tensor.transpose(out=PS[3][0:64, bb * 64:(bb + 1) * 64], in_=P0[:, bb, :], identity=identf[0:64, 0:64]), reads=[b_P0, b_identf], writes=[PB[3]])
                                S.op("act", lambda: nc.scalar.copy(out=fl(Q0), in_=PS[3][0:64, :]), reads=[PB[3]], writes=[b_Q0])
                                S.op("dve", lambda: nc.vector.tensor_tensor(out=R0[:], in0=Q0[:], in1=identf[0:64, 0:64].unsqueeze(1).to_broadcast([64, NB, 64]), op=ALU.add),
                                     reads=[b_Q0, b_identf], writes=[b_R0])
                                for k in range(1, 6):
                                    Pp, b_Pp = Pm[(k - 1) % 2]
                                    Qp, b_Qp = Qm[(k - 1) % 2]
                                    Rp, b_Rp = Rm[(k - 1) % 2]
                                    Pn, b_Pn = Pm[k % 2]
                                    Qn, b_Qn = Qm[k % 2]
                                    Rn, b_Rn = Rm[k % 2]
                                    for bb in range(NB):
                                        S.op("pe", lambda bb=bb: nc.tensor.matmul(PS[4][0:64, bb * 64:(bb + 1) * 64], lhsT=Qp[:, bb, :], rhs=Pp[:, bb, :], start=True, stop=True), reads=[b_Qp, b_Pp], writes=[PB[4]])
                                    if k < 5:
                                        for bb in range(NB):
                                            S.op("pe", lambda bb=bb: nc.tensor.matmul(PS[5][0:64, bb * 64:(bb + 1) * 64], lhsT=Pp[:, bb, :], rhs=Qp[:, bb, :], start=True, stop=True), reads=[b_Qp, b_Pp], writes=[PB[5]])
                                    S.op("act", lambda: nc.scalar.copy(out=fl(Pn), in_=PS[4][0:64, :]), reads=[PB[4]], writes=[b_Pn])
                                    if k < 5:
                                        S.op("dve", lambda: nc.vector.tensor_copy(out=fl(Qn), in_=PS[5][0:64, :]), reads=[PB[5]], writes=[b_Qn])
                                    for bb in range(NB):
                                        S.op("pe", lambda bb=bb: nc.tensor.matmul(PS[6][0:64, bb * 64:(bb + 1) * 64], lhsT=Pn[:, bb, :], rhs=Rp[:, bb, :], start=True, stop=True), reads=[b_Pn, b_Rp], writes=[PB[6]])
                                    S.op("dve", lambda: nc.vector.tensor_tensor(out=fl(Rn), in0=fl(Rp), in1=PS[6][0:64, :], op=ALU.add), reads=[b_Rp, PB[6]], writes=[b_Rn])
                                    if k == 5:
                                        S.op("dve", lambda c0=c0: nc.vector.tensor_tensor(out=R5b[:, c0:c0 + NCB, :, :].rearrange("p c (s h) x -> p (c s) h x", s=2), in0=v3(Rn), in1=gcols(beta, c0).unsqueeze(3).to_broadcast([64, 2 * NCB, 2, 64]), op=ALU.mult),
                                             reads=[b_Rn, b_beta], writes=[b_R5b])
                            S.barrier()
                        if cfg.get("gdn_stop") == 3:
                            S.barrier()
                            return
                        with contextlib.ExitStack() as st2:
                            Sst = [tl(st2, "g_S%d" % i, [128, 128], F32) for i in range(4)]
                            Sbb = [[tl(st2, "g_Sb%d_%d" % (i, j), [128, 128], BF16) for j in range(2)] for i in range(4)]
                            Xs = [tl(st2, "g_X%d" % i, [64, 128], BF16) for i in range(4)]
                            vnb = [tl(st2, "g_vn%d" % i, [64, 128], BF16) for i in range(4)]
                            vnk = [tl(st2, "g_vk%d" % i, [64, 128], BF16) for i in range(4)]
                            tmpo = [tl(st2, "g_to%d" % i, [64, 128], F32) for i in range(4)]
                            oo = [[tl(st2, "g_oo%d_%d" % (i, j), [64, 128], F32) for j in range(2)] for i in range(4)]
                            b_raw = Buf("gdn_raw")
                            orders = [list(range(NCH)), [3, 2, 1, 0] + list(range(NCH - 1, 3, -1))]
                            for idx in range(NCH):
                                ch = []
                                for b in range(4):
                                    sd, hh = b // 2, b % 2
                                    hd = pr * 2 + hh
                                    c = orders[sd][idx]
                                    ch.append(dict(b=b, sd=sd, hh=hh, hd=hd, col=sd * 4 + hd, c=c, cs=slice(c * 64, (c + 1) * 64), pa=2 * b, pc=2 * b + 1,
                                                   kf=k_fm[hh], qf=q_fm[hh], vt=v_tm[hh], ktm=k_tm[hh], X=Xs[b], vn=vnb[b], vk=vnk[b], to=tmpo[b], o=oo[b][idx % 2], S=Sst[b],
                                                   sb=Sbb[b][idx % 2], nsb=Sbb[b][(idx + 1) % 2]))
                                if idx > 0:
                                    for d in ch:
                                        S.op("pe", lambda d=d: nc.tensor.matmul(PS[d["pa"]][0:64, 0:128], lhsT=d["kf"][0][:, d["cs"]], rhs=d["sb"][0][:], start=True, stop=True), reads=[d["kf"][1], d["sb"][1]], writes=[PB[d["pa"]]])
                                        S.op("pe", lambda d=d: nc.tensor.matmul(PS[d["pa"]][0:64, 128:256], lhsT=d["qf"][0][:, d["cs"]], rhs=d["sb"][0][:], start=True, stop=True), reads=[d["qf"][1], d["sb"][1]], writes=[PB[d["pa"]]])
                                    for d in ch:
                                        S.op("dve", lambda d=d: nc.vector.scalar_tensor_tensor(out=d["X"][0][:], in0=PS[d["pa"]][0:64, 0:128], scalar=negc[:, d["c"], d["col"]:d["col"] + 1], in1=d["vt"][0][:, d["c"], :], op0=ALU.mult, op1=ALU.add),
                                             reads=[PB[d["pa"]], b_negc, d["vt"][1]], writes=[d["X"][1]])
                                for d in ch:
                                    if idx > 0:
                                        xin_ap, xr = d["X"][0][:], [d["X"][1]]
                                    else:
                                        xin_ap, xr = d["vt"][0][:, d["c"], :], [d["vt"][1]]
                                    S.op("pe", lambda d=d, xin_ap=xin_ap: nc.tensor.matmul(PS[d["pa"]][0:64, 256:384], lhsT=R5b[:, d["c"], d["b"], :], rhs=xin_ap, start=True, stop=True), reads=[b_R5b] + xr, writes=[PB[d["pa"]]])
                                for d in ch:
                                    S.op("act", lambda d=d: nc.scalar.copy(out=d["vn"][0][:], in_=PS[d["pa"]][0:64, 256:384]), reads=[PB[d["pa"]]], writes=[d["vn"][1]])
                                    S.op("dve", lambda d=d: nc.vector.tensor_scalar(out=d["vk"][0][:], in0=PS[d["pa"]][0:64, 256:384], scalar1=ekd[:, d["c"], d["col"]:d["col"] + 1], scalar2=None, op0=ALU.mult), reads=[PB[d["pa"]], b_ekd], writes=[d["vk"][1]])
                                for d in ch:
                                    S.op("pe", lambda d=d: nc.tensor.matmul(PS[d["pa"]][0:64, 384:512], lhsT=aqkT[:, d["c"], d["b"], :], rhs=d["vn"][0][:], start=True, stop=True), reads=[b_aqkT, d["vn"][1]], writes=[PB[d["pa"]]])
                                    if idx < NCH - 1:
                                        S.op("pe", lambda d=d: nc.tensor.matmul(PS[d["pc"]][:, 0:128], lhsT=d["ktm"][0][:, d["c"], :], rhs=d["vk"][0][:], start=True, stop=True), reads=[d["ktm"][1], d["vk"][1]], writes=[PB[d["pc"]]])
                                if idx < NCH - 1:
                                    for d in ch:
                                        if idx == 0:
                                            S.op("dve", lambda d=d: nc.vector.tensor_copy(out=d["S"][0][:], in_=PS[d["pc"]][:, 0:128]), reads=[PB[d["pc"]]], writes=[d["S"][1]])
                                        else:
                                            S.op("dve", lambda d=d: nc.vector.scalar_tensor_tensor(out=d["S"][0][:], in0=d["S"][0][:], scalar=egl[:, d["c"], d["col"]:d["col"] + 1], in1=PS[d["pc"]][:, 0:128], op0=ALU.mult, op1=ALU.add),
                                                 reads=[d["S"][1], b_egl, PB[d["pc"]]], writes=[d["S"][1]])
                                    for d in ch:
                                        S.op("act", lambda d=d: nc.scalar.copy(out=d["nsb"][0][:], in_=d["S"][0][:]), reads=[d["S"][1]], writes=[d["nsb"][1]])
                                for d in ch:
                                    if idx > 0:
                                        S.op("act", lambda d=d: nc.scalar.copy(out=d["to"][0][:], in_=PS[d["pa"]][0:64, 384:512]), reads=[PB[d["pa"]]], writes=[d["to"][1]])
                                        S.op("dve", lambda d=d: nc.vector.scalar_tensor_tensor(out=d["o"][0][:], in0=PS[d["pa"]][0:64, 128:256], scalar=egc[:, d["c"], d["col"]:d["col"] + 1], in1=d["to"][0][:], op0=ALU.mult, op1=ALU.add),
                                             reads=[PB[d["pa"]], b_egc, d["to"][1]], writes=[d["o"][1]])
                                    else:
                                        S.op("act", lambda d=d: nc.scalar.copy(out=d["o"][0][:], in_=PS[d["pa"]][0:64, 384:512]), reads=[PB[d["pa"]]], writes=[d["o"][1]])
                                    S.dma(gdn_raw_d[d["sd"], d["c"] * 64:(d["c"] + 1) * 64, d["hd"] * 128:(d["hd"] + 1) * 128], d["o"][0][:], reads=[d["o"][1]], writes=[b_raw])
                            S.barrier()
                if cfg.get("gdn_stop") == 4:
                    S.barrier()
                    return
                with contextlib.ExitStack() as st:
                    wgg, b_wgg = tl(st, "g_wgg", [128, KC, 512], BF16)
                    S.dma(wgg[:], winv[:, :, O_GG:O_GG + 512], writes=[b_wgg], q="pool")
                    gnw, b_gnw = tl(st, "g_gnw", [128, 128], F32)
                    S.dma(gnw[:], I["gdn_norm"][l:l + 1, :].partition_broadcast(128), writes=[b_gnw])
                    of_ = [tl(st, "g_of%d" % i, [128, 512], F32) for i in range(2)]
                    ob_ = [tl(st, "g_ob%d" % i, [128, 512], F32) for i in range(2)]
                    sqt = [tl(st, "g_sqt%d" % i, [128, 512], F32) for i in range(2)]
                    gt_ = [tl(st, "g_gt%d" % i, [128, 512], F32) for i in range(2)]
                    ms = [tl(st, "g_ms%d" % i, [128, 4], F32) for i in range(2)]
                    obf = [tl(st, "g_obf%d" % i, [128, 512], BF16) for i in range(2)]
                    ofm = [tl(st, "g_ofm%d" % i, [128, 4, 128], BF16) for i in range(2)]
                    b_go = Buf("gdn_o")
                    hsub = cfg.get("gdn_hsub", 99)
                    for i in range(cfg.get("gdn_hnt", NT)):
                        a, b_a = of_[i % 2]
                        bb, b_bb = ob_[i % 2]
                        sq_t, b_sq = sqt[i % 2]
                        g_tl, b_gt = gt_[i % 2]
                        ms_t, b_ms = ms[i % 2]
                        obf_t, b_obf = obf[i % 2]
                        ofm_t, b_ofm = ofm[i % 2]
                        ts_ = slice(i * 128, (i + 1) * 128)
                        S.dma(a[:], gdn_raw_d[0, ts_, :], writes=[b_a])
                        S.dma(bb[:], gdn_raw_d[1, ts_, :], writes=[b_bb])
                        pb = i % 2
                        for kc in range(KC):
                            S.op("pe", lambda kc=kc, pb=pb: nc.tensor.matmul(PS[pb][:, :], lhsT=h_fm[:, kc, ts_], rhs=wgg[:, kc, :], start=(kc == 0), stop=(kc == KC - 1)), reads=[b_hfm, b_wgg], writes=[PB[pb]])
                        S.op("act", lambda: nc.scalar.activation(out=g_tl[:], in_=PS[pb][:, :], func=AF.Silu), reads=[PB[pb]], writes=[b_gt])
                        if hsub < 1:
                            continue
                        S.op("dve", lambda: nc.vector.tensor_tensor(out=a[:], in0=a[:], in1=bb[:], op=ALU.add), reads=[b_a, b_bb], writes=[b_a])
                        S.op("act", lambda: nc.scalar.activation(out=sq_t[:], in_=a[:], func=AF.Square), reads=[b_a], writes=[b_sq])
                        S.op("dve", lambda: nc.vector.tensor_reduce(out=ms_t[:], in_=sq_t[:].rearrange("p (h x) -> p h x", h=4), axis=AX.X, op=ALU.add), reads=[b_sq], writes=[b_ms])
                        S.op("act", lambda: nc.scalar.activation(out=ms_t[:], in_=ms_t[:], func=AF.Sqrt, scale=1.0 / 128, bias=epsb[:, 0:1]), reads=[b_ms, b_eps], writes=[b_ms])
                        S.op("dve", lambda: nc.vector.reciprocal(out=ms_t[:], in_=ms_t[:]), reads=[b_ms], writes=[b_ms])
                        if hsub < 2:
                            continue
                        a3 = a[:].rearrange("p (h x) -> p h x", h=4)
                        S.op("dve", lambda: nc.vector.tensor_tensor(out=a3, in0=a3, in1=ms_t[:].unsqueeze(2).to_broadcast([128, 4, 128]), op=ALU.mult), reads=[b_a, b_ms], writes=[b_a])
                        S.op("dve", lambda: nc.vector.tensor_tensor(out=a3, in0=a3, in1=gnw[:].unsqueeze(1).to_broadcast([128, 4, 128]), op=ALU.mult), reads=[b_a, b_gnw], writes=[b_a])
                        S.op("dve", lambda: nc.vector.tensor_tensor(out=obf_t[:], in0=a[:], in1=g_tl[:], op=ALU.mult), reads=[b_a, b_gt], writes=[b_obf])
                        if hsub < 3:
                            continue
                        pv = PS[2 + pb][:].bitcast(BF16)
                        for hd in range(4):
                            S.op("pe", lambda hd=hd: nc.tensor.transpose(out=pv[:, hd * 128:(hd + 1) * 128], in_=obf_t[:, hd * 128:(hd + 1) * 128], identity=identb[:]), reads=[b_obf, b_identb], writes=[PB[2 + pb]])
                        S.op("act", lambda: nc.scalar.copy(out=ofm_t[:], in_=pv[:, 0:512].rearrange("p (h x) -> p h x", h=4)), reads=[PB[2 + pb]], writes=[b_ofm])
                        if hsub < 4:
                            continue
                        S.dma(gdn_o_d[:, :, ts_].rearrange("h d t -> d h t"), ofm_t[:], reads=[b_ofm], writes=[b_go])
                    S.barrier()


        def ln_tile(st_tiles, i, f_halves, f_bufs, prm, out_final):
            s = 1 if i < 2 else 0
            x_t, b_x = st_tiles["x"][i % 2]
            t_t, b_t = st_tiles["t"][i % 2]
            h_t, b_h = st_tiles["h"][i % 2]
            stt, b_st = st_tiles["st"][i % 2]
            mv, b_mv = st_tiles["mv"][i % 2]
            ts_ = slice(i * 128, (i + 1) * 128)
            S.dma(x_t[:], xres_d[ts_, :], reads=[b_xres[i]], writes=[b_x])
            gate_t, b_gate = prm["gate"][s]
            for hf in range(2):
                hs = slice(hf * 512, (hf + 1) * 512)
                S.op("dve", lambda hf=hf, hs=hs: nc.vector.tensor_tensor(out=t_t[:, hs], in0=f_halves[hf], in1=gate_t[:, hs], op=ALU.mult), reads=[f_bufs[hf], b_gate], writes=[b_t])
            S.op("dve", lambda: nc.vector.scalar_tensor_tensor(out=x_t[:], in0=x_t[:], scalar=ALPHA, in1=t_t[:], op0=ALU.mult, op1=ALU.add), reads=[b_x, b_t], writes=[b_x])
            for hf in range(2):
                S.op("dve", lambda hf=hf: nc.vector.bn_stats(out=stt[:, hf, :], in_=x_t[:, hf * 512:(hf + 1) * 512]), reads=[b_x], writes=[b_st])
            S.op("dve", lambda: nc.vector.bn_aggr(out=mv[:, 0:2], in_=stt[:].rearrange("p a b -> p (a b)")), reads=[b_st], writes=[b_mv])
            S.op("act", lambda: nc.scalar.activation(out=mv[:, 2:3], in_=mv[:, 1:2], func=AF.Sqrt, bias=epsb[:, 0:1]), reads=[b_mv, b_eps], writes=[b_mv])
            S.op("dve", lambda: nc.vector.reciprocal(out=mv[:, 3:4], in_=mv[:, 2:3]), reads=[b_mv], writes=[b_mv])
            S.op("dve", lambda: nc.vector.tensor_scalar(out=x_t[:], in0=x_t[:], scalar1=mv[:, 0:1], scalar2=mv[:, 3:4], op0=ALU.subtract, op1=ALU.mult), reads=[b_x, b_mv], writes=[b_x])
            S.op("pool", lambda: nc.gpsimd.tensor_tensor(out=x_t[:], in0=x_t[:], in1=prm["g"][0][:], op=ALU.mult), reads=[b_x, prm["g"][1]], writes=[b_x])
            S.op("pool", lambda: nc.gpsimd.tensor_tensor(out=x_t[:], in0=x_t[:], in1=prm["b"][0][:], op=ALU.add), reads=[b_x, prm["b"][1]], writes=[b_x])
            if out_final:
                S.dma(out_d[(i - 2) * 128:(i - 1) * 128, :], x_t[:], reads=[b_x])
                return
            S.dma(xres_d[ts_, :], x_t[:], reads=[b_x], writes=[b_xres[i]])
            sc_t, b_sc = prm["sc"][s]
            sh_t, b_sh = prm["sh"][s]
            S.op("dve", lambda: nc.vector.tensor_tensor(out=t_t[:], in0=x_t[:], in1=sc_t[:], op=ALU.mult), reads=[b_x, b_sc], writes=[b_t])
            S.op("pool", lambda: nc.gpsimd.tensor_tensor(out=h_t[:], in0=t_t[:], in1=sh_t[:], op=ALU.add), reads=[b_t, b_sh], writes=[b_h])
            to_fm(h_t, b_h, i, 6 + i % 2)

        def ln_setup(st, l_mod, jgate, ln_g, ln_b, l, jsh, jsc, need_mod):
            tiles = {
                "x": [tl(st, "ln_x%d" % i, [128, D], F32) for i in range(2)],
                "t": [tl(st, "ln_t%d" % i, [128, D], F32) for i in range(2)],
                "h": [tl(st, "ln_h%d" % i, [128, D], BF16) for i in range(2)],
                "st": [tl(st, "ln_st%d" % i, [128, 2, 6], F32) for i in range(2)],
                "mv": [tl(st, "ln_mv%d" % i, [128, 4], F32) for i in range(2)],
            }
            prm = {"gate": [load_bc(st, "ln_gate%d" % s_, l, s_, jgate) for s_ in range(2)],
                   "g": load_vec_bc(st, "ln_g", I[ln_g][l:l + 1, :]),
                   "b": load_vec_bc(st, "ln_b", I[ln_b][l:l + 1, :])}
            if need_mod:
                prm["sh"] = [load_bc(st, "ln_sh%d" % s_, l_mod, s_, jsh) for s_ in range(2)]
                prm["sc"] = [load_bc(st, "ln_sc%d" % s_, l_mod, s_, jsc, plus_one=True) for s_ in range(2)]
            return tiles, prm

        def stage_merge(l, last):
            winv = I["w_in"][l].rearrange("(kc p) n -> p kc n", p=128)
            groups = GROUPS[1:] if last else GROUPS
            tiles_i = range(2, NT) if last else range(NT)
            with contextlib.ExitStack() as st:
                y_fm, b_y = tl(st, "y_fm", [128, KC, T], BF16)
                with contextlib.ExitStack() as st2:
                    mo, b_mo = tl(st2, "m_mo", [64, 8, T], BF16)
                    ho, b_ho = tl(st2, "m_ho", [128, 4, T], BF16)
                    go, b_go = tl(st2, "m_go", [128, 4, T], BF16)
                    S.dma(mo[:], mla_o_d.rearrange("h d t -> d h t"), writes=[b_mo])
                    S.dma(ho[:], hg_o_d.rearrange("h d t -> d h t"), writes=[b_ho])
                    S.dma(go[:], gdn_o_d.rearrange("h d t -> d h t"), writes=[b_go])
                    wgt = [tl(st2, "m_wg%d" % i, [128, KC, 3, 128], BF16) for i in range(2)]
                    wbr = [tl(st2, "m_wbr%d" % i, [128, 2, 4, 128], BF16) for i in range(2)]
                    wbm = [tl(st2, "m_wbm%d" % i, [64, 8, 128], BF16) for i in range(2)]
                    sg = [tl(st2, "m_sg%d" % i, [128, 512], F32) for i in range(3)]
                    ta, b_ta = tl(st2, "m_ta", [128, 512], F32)
                    tb, b_tb = tl(st2, "m_tb", [128, 512], F32)
                    for dc in range(KC):
                        wg_t, b_wg = wgt[dc % 2]
                        wbr_t, b_wbr = wbr[dc % 2]
                        wbm_t, b_wbm = wbm[dc % 2]
                        for n_ in range(3):
                            c0 = O_GATES + n_ * D + dc * 128
                            S.dma(wg_t[:, :, n_, :], winv[:, :, c0:c0 + 128], writes=[b_wg], q="pool")
                        for n_ in range(2):
                            S.dma(wbr_t[:, n_, :, :], I["w_branch"][l, n_ + 1].rearrange("(kc p) n -> p kc n", p=128)[:, :, dc * 128:(dc + 1) * 128], writes=[b_wbr], q="pool")
                        S.dma(wbm_t[:], I["w_branch"][l, 0].rearrange("(h p) n -> p h n", p=64)[:, :, dc * 128:(dc + 1) * 128], writes=[b_wbm], q="pool")
                        for (t0, n) in groups:
                            for n_ in range(3):
                                for kc in range(KC):
                                    S.op("pe", lambda kc=kc, n_=n_: nc.tensor.matmul(PS[n_][:, 0:n], lhsT=wg_t[:, kc, n_, :], rhs=h_fm[:, kc, t0:t0 + n], start=(kc == 0), stop=(kc == KC - 1)),
                                         reads=[b_wg, b_hfm], writes=[PB[n_]])
                                S.op("act", lambda n_=n_: nc.scalar.activation(out=sg[n_][0][:, 0:n], in_=PS[n_][:, 0:n], func=AF.Sigmoid), reads=[PB[n_]], writes=[sg[n_][1]])
                            for h in range(8):
                                S.op("pe", lambda h=h: nc.tensor.matmul(PS[3][:, 0:n], lhsT=wbm_t[:, h, :], rhs=mo[:, h, t0:t0 + n], start=(h == 0), stop=(h == 7)), reads=[b_wbm, b_mo], writes=[PB[3]])
                            for kc in range(4):
                                S.op("pe", lambda kc=kc: nc.tensor.matmul(PS[4][:, 0:n], lhsT=wbr_t[:, 0, kc, :], rhs=ho[:, kc, t0:t0 + n], start=(kc == 0), stop=(kc == 3)), reads=[b_wbr, b_ho], writes=[PB[4]])
                            for kc in range(4):
                                S.op("pe", lambda kc=kc: nc.tensor.matmul(PS[5][:, 0:n], lhsT=wbr_t[:, 1, kc, :], rhs=go[:, kc, t0:t0 + n], start=(kc == 0), stop=(kc == 3)), reads=[b_wbr, b_go], writes=[PB[5]])
                            S.op("dve", lambda: nc.vector.tensor_tensor(out=ta[:, 0:n], in0=sg[0][0][:, 0:n], in1=PS[3][:, 0:n], op=ALU.mult), reads=[sg[0][1], PB[3]], writes=[b_ta])
                            S.op("dve", lambda: nc.vector.tensor_tensor(out=tb[:, 0:n], in0=sg[1][0][:, 0:n], in1=PS[4][:, 0:n], op=ALU.mult), reads=[sg[1][1], PB[4]], writes=[b_tb])
                            S.op("pool", lambda: nc.gpsimd.tensor_tensor(out=ta[:, 0:n], in0=ta[:, 0:n], in1=tb[:, 0:n], op=ALU.add), reads=[b_ta, b_tb], writes=[b_ta])
                            S.op("dve", lambda: nc.vector.tensor_tensor(out=tb[:, 0:n], in0=sg[2][0][:, 0:n], in1=PS[5][:, 0:n], op=ALU.mult), reads=[sg[2][1], PB[5], b_ta], writes=[b_tb])
                            S.op("dve", lambda dc=dc: nc.vector.tensor_tensor(out=y_fm[:, dc, t0:t0 + n], in0=ta[:, 0:n], in1=tb[:, 0:n], op=ALU.add), reads=[b_ta, b_tb], writes=[b_y])
                    S.barrier()
                if "y_fm" in DBG:
                    S.dma(DBG["y_fm"].rearrange("(kc p) t -> p kc t", p=128), y_fm[:], reads=[b_y], q="pool")
                wo, b_wo = tl(st, "m_wo", [128, KC, D], BF16)
                S.dma(wo[:], I["w_out"][l].rearrange("(kc p) n -> p kc n", p=128), writes=[b_wo], q="pool")
                tiles, prm = ln_setup(st, l, 2, "ln1_g", "ln1_b", l, 3, 4, True)
                for i in tiles_i:
                    ts_ = slice(i * 128, (i + 1) * 128)
                    for hf in range(2):
                        pb = 4 + hf
                        for kc in range(KC):
                            S.op("pe", lambda kc=kc, hf=hf, pb=pb: nc.tensor.matmul(PS[pb][:, :], lhsT=y_fm[:, kc, ts_], rhs=wo[:, kc, hf * 512:(hf + 1) * 512], start=(kc == 0), stop=(kc == KC - 1)),
                                 reads=[b_y, b_wo], writes=[PB[pb]])
                    ln_tile(tiles, i, [PS[4][:, :], PS[5][:, :]], [PB[4], PB[5]], prm, False)
                S.barrier()

        def stage_moe(l, last):
            groups = GROUPS[1:] if last else GROUPS
            tiles_i = list(range(2, NT)) if last else list(range(NT))
            with contextlib.ExitStack() as st:
                acc, b_acc = tl(st, "acc", [128, NT, D], F32)
                comb, b_comb = tl(st, "comb", [128, NT, 65], F32)
                S.op("dve", lambda: nc.vector.memset(comb[:, :, 64:65], 1.0), writes=[b_comb])
                with contextlib.ExitStack() as st2:
                    wr, b_wr = tl(st2, "wr", [128, KC, 64], BF16)
                    S.dma(wr[:], I["w_router"][l].rearrange("(kc p) n -> p kc n", p=128), writes=[b_wr], q="pool")
                    rb, b_rb = load_vec_bc(st2, "rb", I["router_bias"][l:l + 1, :], n=64)
                    sc_ = [tl(st2, "r_sc%d" % i, [128, 64], F32) for i in range(2)]
                    sel = [tl(st2, "r_sel%d" % i, [128, 64], F32) for i in range(2)]
                    selm = [tl(st2, "r_selm%d" % i, [128, 64], F32) for i in range(2)]
                    m8 = [tl(st2, "r_m8%d" % i, [128, 8, 8], F32) for i in range(2)]
                    sm = [tl(st2, "r_sm%d" % i, [128, 40], F32) for i in range(2)]
                    for i in tiles_i:
                        ts_ = slice(i * 128, (i + 1) * 128)
                        pb = i % 2
                        sc_t, b_sc = sc_[i % 2]
                        sel_t, b_sel = sel[i % 2]
                        selm_t, b_selm = selm[i % 2]
                        m8_t, b_m8 = m8[i % 2]
                        sm_t, b_sm = sm[i % 2]
                        for kc in range(KC):
                            S.op("pe", lambda kc=kc: nc.tensor.matmul(PS[pb][:, 0:64], lhsT=h_fm[:, kc, ts_], rhs=wr[:, kc, :], start=(kc == 0), stop=(kc == KC - 1)), reads=[b_hfm, b_wr], writes=[PB[pb]])
                        S.op("act", lambda: nc.scalar.activation(out=sc_t[:], in_=PS[pb][:, 0:64], func=AF.Sigmoid), reads=[PB[pb]], writes=[b_sc])
                        S.op("dve", lambda: nc.vector.tensor_tensor(out=sel_t[:], in0=sc_t[:], in1=rb[:], op=ALU.add), reads=[b_sc, b_rb], writes=[b_sel])
                        for g8 in range(8):
                            S.op("dve", lambda g8=g8: nc.vector.max(out=m8_t[:, g8, :], in_=sel_t[:, g8 * 8:(g8 + 1) * 8]), reads=[b_sel], writes=[b_m8])
                        gs = sm_t[:, 0:8]
                        gm8 = sm_t[:, 8:16]
                        gmask = sm_t[:, 16:24]
                        pen = sm_t[:, 24:32]
                        t8 = sm_t[:, 32:40]
                        S.op("dve", lambda: nc.vector.tensor_tensor(out=gs, in0=m8_t[:, :, 0], in1=m8_t[:, :, 1], op=ALU.add), reads=[b_m8], writes=[b_sm])
                        S.op("dve", lambda: nc.vector.max(out=gm8, in_=gs), reads=[b_sm], writes=[b_sm])
                        S.op("dve", lambda: nc.vector.tensor_scalar(out=gmask, in0=gs, scalar1=sm_t[:, 11:12], scalar2=None, op0=ALU.is_ge), reads=[b_sm], writes=[b_sm])
                        S.op("dve", lambda: nc.vector.tensor_scalar(out=pen, in0=gmask, scalar1=10.0, scalar2=-10.0, op0=ALU.mult, op1=ALU.add), reads=[b_sm], writes=[b_sm])
                        sel3 = sel_t[:].rearrange("p (g x) -> p g x", g=8)
                        selm3 = selm_t[:].rearrange("p (g x) -> p g x", g=8)
                        S.op("dve", lambda: nc.vector.tensor_tensor(out=selm3, in0=sel3, in1=gmask.unsqueeze(2).to_broadcast([128, 8, 8]), op=ALU.mult), reads=[b_sel, b_sm], writes=[b_selm])
                        S.op("dve", lambda: nc.vector.tensor_tensor(out=selm3, in0=selm3, in1=pen.unsqueeze(2).to_broadcast([128, 8, 8]), op=ALU.add), reads=[b_selm, b_sm], writes=[b_selm])
                        S.op("dve", lambda: nc.vector.max(out=t8, in_=selm_t[:]), reads=[b_selm], writes=[b_sm])
                        S.op("dve", lambda: nc.vector.tensor_scalar(out=selm_t[:], in0=selm_t[:], scalar1=sm_t[:, 39:40], scalar2=None, op0=ALU.is_ge), reads=[b_selm, b_sm], writes=[b_selm])
                        S.op("dve", lambda: nc.vector.tensor_tensor(out=sel_t[:], in0=sc_t[:], in1=selm_t[:], op=ALU.mult), reads=[b_sc, b_selm], writes=[b_sel])
                        S.op("dve", lambda: nc.vector.tensor_reduce(out=sm_t[:, 0:1], in_=sel_t[:], axis=AX.X, op=ALU.add), reads=[b_sel], writes=[b_sm])
                        S.op("dve", lambda: nc.vector.reciprocal(out=sm_t[:, 1:2], in_=sm_t[:, 0:1]), reads=[b_sm], writes=[b_sm])
                        S.op("dve", lambda i=i: nc.vector.tensor_scalar(out=comb[:, i, 0:64], in0=sel_t[:], scalar1=sm_t[:, 1:2], scalar2=2.5, op0=ALU.mult, op1=ALU.mult), reads=[b_sel, b_sm], writes=[b_comb])
                    S.barrier()
                if "comb" in DBG:
                    S.dma(DBG["comb"].rearrange("(i p) e -> p i e", p=128), comb[:], reads=[b_comb])
                with contextlib.ExitStack() as st2:
                    wgu = [tl(st2, "wgu%d" % i, [128, KC, 512], BF16) for i in range(3)]
                    wdn = [tl(st2, "wdn%d" % i, [128, 2, D], BF16) for i in range(3)]
                    sgt = [tl(st2, "e_sg%d" % i, [128, 512], F32) for i in range(2)]
                    act = [tl(st2, "e_act%d" % i, [128, 2, 512], BF16) for i in range(2)]
                    ne = cfg.get("n_experts", 65)

                    def load_e(e):
                        wg_t, b_wg = wgu[e % 3]
                        wd_t, b_wd = wdn[e % 3]
                        if e < 64:
                            S.dma(wg_t[:], I["w_gu"][l, e].rearrange("(kc p) n -> p kc n", p=128), writes=[b_wg], q="pool")
                            S.dma(wd_t[:], I["w_down"][l, e].rearrange("(kc p) n -> p kc n", p=128), writes=[b_wd], q="pool")
                        else:
                            S.dma(wg_t[:], I["w_sh_gu"][l].rearrange("(kc p) n -> p kc n", p=128), writes=[b_wg], q="pool")
                            S.dma(wd_t[:], I["w_sh_down"][l].rearrange("(kc p) n -> p kc n", p=128), writes=[b_wd], q="pool")

                    elist = list(range(64 - (ne - 1), 65)) if ne < 65 else list(range(65))
                    load_e(elist[0])
                    if len(elist) > 1:
                        load_e(elist[1])
                    gcnt = 0
                    dcnt_ = [0]
                    pend_down = [None]
                    for ei, e in enumerate(elist):
                        need_load = ei + 2 < len(elist)
                        wg_t, b_wg = wgu[e % 3]
                        wd_t, b_wd = wdn[e % 3]
                        for (t0, n) in groups:
                            act_t, b_act = act[gcnt % 2]
                            gcnt += 1
                            for c in range(2):
                                pg, pu = 2 * c, 2 * c + 1
                                for kc in range(KC):
                                    S.op("pe", lambda kc=kc, c=c, pg=pg: nc.tensor.matmul(PS[pg][:, 0:n], lhsT=wg_t[:, kc, c * 128:(c + 1) * 128], rhs=h_fm[:, kc, t0:t0 + n], start=(kc == 0), stop=(kc == KC - 1)),
                                         reads=[b_wg, b_hfm], writes=[PB[pg]])
                                for kc in range(KC):
                                    S.op("pe", lambda kc=kc, c=c, pu=pu: nc.tensor.matmul(PS[pu][:, 0:n], lhsT=wg_t[:, kc, 256 + c * 128:256 + (c + 1) * 128], rhs=h_fm[:, kc, t0:t0 + n], start=(kc == 0), stop=(kc == KC - 1)),
                                         reads=[b_wg, b_hfm], writes=[PB[pu]])
                                sg_t, b_sg = sgt[c]
                                S.op("act", lambda pg=pg, sg_t=sg_t: nc.scalar.activation(out=sg_t[:, 0:n], in_=PS[pg][:, 0:n], func=AF.Silu), reads=[PB[pg]], writes=[b_sg])
                                S.op("dve", lambda c=c, pu=pu, sg_t=sg_t, act_t=act_t: nc.vector.tensor_tensor(out=act_t[:, c, 0:n], in0=sg_t[:, 0:n], in1=PS[pu][:, 0:n], op=ALU.mult), reads=[b_sg, PB[pu]], writes=[b_act])
                            if pend_down[0] is not None:
                                pend_down[0]()
                            if need_load:
                                load_e(elist[ei + 2])
                                need_load = False

                            def down(t0=t0, n=n, act_t=act_t, b_act=b_act, wd_t=wd_t, b_wd=b_wd, ei=ei, e=e):
                                for tt in range(n // 128):
                                    i = t0 // 128 + tt
                                    for hf in range(2):
                                        pd = 4 + dcnt_[0] % 4
                                        dcnt_[0] += 1
                                        for c in range(2):
                                            S.op("pe", lambda c=c: nc.tensor.matmul(PS[pd][:, :], lhsT=act_t[:, c, tt * 128:(tt + 1) * 128], rhs=wd_t[:, c, hf * 512:(hf + 1) * 512], start=(c == 0), stop=(c == 1)),
                                                 reads=[b_act, b_wd], writes=[PB[pd]])
                                        if ei == 0:
                                            S.op("dve", lambda: nc.vector.tensor_scalar(out=acc[:, i, hf * 512:(hf + 1) * 512], in0=PS[pd][:, :], scalar1=comb[:, i, e:e + 1], scalar2=None, op0=ALU.mult),
                                                 reads=[PB[pd], b_comb], writes=[b_acc])
                                        else:
                                            S.op("dve", lambda: nc.vector.scalar_tensor_tensor(out=acc[:, i, hf * 512:(hf + 1) * 512], in0=PS[pd][:, :], scalar=comb[:, i, e:e + 1], in1=acc[:, i, hf * 512:(hf + 1) * 512], op0=ALU.mult, op1=ALU.add),
                                                 reads=[PB[pd], b_comb, b_acc], writes=[b_acc])
                            pend_down[0] = down
                    if pend_down[0] is not None:
                        pend_down[0]()
                    S.barrier()
                if "ff" in DBG:
                    S.dma(DBG["ff"].rearrange("(i p) d -> p i d", p=128), acc[:], reads=[b_acc])
                final = (l == nlayers - 1)
                tiles, prm = ln_setup(st, l + 1, 5, "ln2_g", "ln2_b", l, 0, 1, not final)
                for i in tiles_i:
                    ln_tile(tiles, i, [acc[:, i, 0:512], acc[:, i, 512:1024]], [b_acc, b_acc], prm, final)
                S.barrier()

        def stage_mla(l, last):
            with contextlib.ExitStack() as st:
                winv = I["w_in"][l].rearrange("(kc p) n -> p kc n", p=128)
                wA, b_wA = tl(st, "wA", [128, KC, 672], BF16)
                S.dma(wA[:], winv[:, :, 0:672], writes=[b_wA], q="pool")
                wKs, b_wKs = tl(st, "wKs", [128, KC, 32], BF16)
                S.dma(wKs[:], I["w_kr_sw"][l].rearrange("(kc p) n -> p kc n", p=128), writes=[b_wKs], q="pool")
                wQ, b_wQ = tl(st, "wQ", [128, 3, 768], BF16)
                S.dma(wQ[:], I["w_q_b"][l].rearrange("(kc p) n -> p kc n", p=128), writes=[b_wQ], q="pool")
                wQs, b_wQs = tl(st, "wQs", [128, 3, 256], BF16)
                S.dma(wQs[:], I["w_qr_sw"][l].rearrange("(kc p) n -> p kc n", p=128), writes=[b_wQs], q="pool")
                wKV, b_wKV = tl(st, "wKV", [128, 2, 1024], BF16)
                S.dma(wKV[:], I["w_kv_b"][l].rearrange("(kc p) n -> p kc n", p=128), writes=[b_wKV], q="pool")
                gq, b_gq = tl(st, "gq", [128, 3], F32)
                S.dma(gq[:], I["q_a_norm_t"][l], writes=[b_gq])
                gkv, b_gkv = tl(st, "gkv", [128, 2], F32)
                S.dma(gkv[:], I["kv_a_norm_t"][l], writes=[b_gkv])
                ropeC, b_rC = tl(st, "ropeC", [32, LAT], F32)
                ropeS, b_rS = tl(st, "ropeS", [32, LAT], F32)
                S.dma(ropeC[:], I["ropeC"][:, :], writes=[b_rC])
                S.dma(ropeS[:], I["ropeS"][:, :], writes=[b_rS])
                qan, b_qan = tl(st, "qan", [128, 3, T], BF16)
                kvan, b_kvan = tl(st, "kvan", [128, 2, T], BF16)
                kr, b_kr = tl(st, "kr", [32, T], BF16)
                raw = [tl(st, "raw%d" % i, [128, 512], F32) for i in range(5)]
                sq = [tl(st, "sq%d" % i, [128, 512], BF16) for i in range(5)]
                rs = [tl(st, "rs%d" % i, [128, 512], F32) for i in range(2)]
                rt = [tl(st, "rt%d" % i, [32, 512], F32) for i in range(2)]

                def rope_or_copy(dst, b_dst, t0, n, pa, pb, isctx):
                    if isctx:
                        S.op("act", lambda: nc.scalar.copy(out=dst[:, t0:t0 + n], in_=PS[pa][0:32, 0:n]), reads=[PB[pa]], writes=[b_dst])
                        return
                    l0 = t0 - CTX
                    S.op("dve", lambda: nc.vector.tensor_tensor(out=rt[0][0][:, 0:n], in0=PS[pa][0:32, 0:n], in1=ropeC[:, l0:l0 + n], op=ALU.mult),
                         reads=[PB[pa], b_rC], writes=[rt[0][1]])
                    S.op("dve", lambda: nc.vector.tensor_tensor(out=rt[1][0][:, 0:n], in0=PS[pb][0:32, 0:n], in1=ropeS[:, l0:l0 + n], op=ALU.mult),
                         reads=[PB[pb], b_rS], writes=[rt[1][1]])
                    S.op("dve", lambda: nc.vector.tensor_tensor(out=dst[:, t0:t0 + n], in0=rt[0][0][:, 0:n], in1=rt[1][0][:, 0:n], op=ALU.add),
                         reads=[rt[0][1], rt[1][1]], writes=[b_dst])

                def rmsnorm_group(col0, nchunk, gvec, b_gvec, dst, b_dst, t0, n, pbase, ri):
                    for c in range(nchunk):
                        pb = pbase + c
                        for kc in range(KC):
                            S.op("pe", lambda kc=kc, c=c, pb=pb: nc.tensor.matmul(PS[pb][:, 0:n], lhsT=wA[:, kc, col0 + c * 128:col0 + (c + 1) * 128],
                                                                                  rhs=h_fm[:, kc, t0:t0 + n], start=(kc == 0), stop=(kc == KC - 1)),
                                 reads=[b_wA, b_hfm], writes=[PB[pb]])
                        rw, b_rw = raw[ri + c]
                        sqt, b_sq = sq[ri + c]
                        S.op("act", lambda rw=rw, pb=pb: nc.scalar.copy(out=rw[:, 0:n], in_=PS[pb][:, 0:n]), reads=[PB[pb]], writes=[b_rw])
                        S.op("act", lambda sqt=sqt, pb=pb: nc.scalar.activation(out=sqt[:, 0:n], in_=PS[pb][:, 0:n], func=AF.Square), reads=[PB[pb]], writes=[b_sq])
                    pss = pbase + nchunk
                    for c in range(nchunk):
                        S.op("pe", lambda c=c: nc.tensor.matmul(PS[pss][:, 0:n], lhsT=onesb[:], rhs=sq[ri + c][0][:, 0:n], start=(c == 0), stop=(c == nchunk - 1)),
                             reads=[b_onesb, sq[ri + c][1]], writes=[PB[pss]])
                    r0, b_r0 = rs[0]
                    r1, b_r1 = rs[1]
                    S.op("act", lambda: nc.scalar.activation(out=r0[:, 0:n], in_=PS[pss][:, 0:n], func=AF.Sqrt, scale=1.0 / (128 * nchunk), bias=epsb[:, 0:1]),
                         reads=[PB[pss], b_eps], writes=[b_r0])
                    S.op("dve", lambda: nc.vector.reciprocal(out=r1[:, 0:n], in_=r0[:, 0:n]), reads=[b_r0], writes=[b_r1])
                    for c in range(nchunk):
                        S.op("dve", lambda c=c: nc.vector.scalar_tensor_tensor(out=dst[:, c, t0:t0 + n], in0=raw[ri + c][0][:, 0:n], scalar=gvec[:, c:c + 1],
                                                                                 in1=r1[:, 0:n], op0=ALU.mult, op1=ALU.mult),
                             reads=[raw[ri + c][1], b_gvec, b_r1], writes=[b_dst])

                for gi, (t0, n) in enumerate(GROUPS):
                    rmsnorm_group(0, 3, gq, b_gq, qan, b_qan, t0, n, 0, 0)
                    rmsnorm_group(384, 2, gkv, b_gkv, kvan, b_kvan, t0, n, 4, 3)
                    for kc in range(KC):
                        S.op("pe", lambda kc=kc: nc.tensor.matmul(PS[7][0:32, 0:n], lhsT=wA[:, kc, 640:672], rhs=h_fm[:, kc, t0:t0 + n], start=(kc == 0), stop=(kc == KC - 1)),
                             reads=[b_wA, b_hfm], writes=[PB[7]])
                    for kc in range(KC):
                        S.op("pe", lambda kc=kc: nc.tensor.matmul(PS[3][0:32, 0:n], lhsT=wKs[:, kc, :], rhs=h_fm[:, kc, t0:t0 + n], start=(kc == 0), stop=(kc == KC - 1)),
                             reads=[b_wKs, b_hfm], writes=[PB[3]])
                    rope_or_copy(kr, b_kr, t0, n, 7, 3, gi == 0)

                v_aug, b_va = tl(st, "v_aug", [128, NT, 8, 65], BF16)
                S.op("dve", lambda: nc.vector.memset(v_aug[:, :, :, 64:65], 1.0), writes=[b_va])
                wKVh = wKV[:].rearrange("p c (h x) -> p c h x", h=8)
                for i in range(NT):
                    pb = i % 2
                    for c in range(2):
                        S.op("pe", lambda c=c, i=i, pb=pb: nc.tensor.matmul(PS[pb][:, :].rearrange("p (h x) -> p h x", h=8), lhsT=kvan[:, c, i * 128:(i + 1) * 128],
                                                                            rhs=wKVh[:, c, :, 64:128], start=(c == 0), stop=(c == 1)),
                             reads=[b_kvan, b_wKV], writes=[PB[pb]])
                    S.op("act", lambda i=i, pb=pb: nc.scalar.copy(out=v_aug[:, i, :, 0:64], in_=PS[pb][:, :].rearrange("p (h x) -> p h x", h=8)),
                         reads=[PB[pb]], writes=[b_va])

                qn = [tl(st, "qn%d" % i, [64, T], BF16) for i in range(2)]
                qr = [tl(st, "qr%d" % i, [32, T], BF16) for i in range(2)]
                kn = [tl(st, "kn%d" % i, [64, T], BF16) for i in range(2)]
                Et = [tl(st, "Et%d" % i, [128, 512], BF16) for i in range(3)]
                rc, b_rc = tl(st, "rc", [65, 512], F32)
                numt = [tl(st, "numt%d" % i, [64, 512], F32) for i in range(2)]
                ot = [tl(st, "ot%d" % i, [64, 512], BF16) for i in range(2)]
                b_mo = Buf("mla_o")
                ecnt = 0
                ocnt = 0
                for h in range(8):
                    qn_t, b_qn = qn[h % 2]
                    qr_t, b_qr = qr[h % 2]
                    kn_t, b_kn = kn[h % 2]
                    for gi, (t0, n) in enumerate(GROUPS):
                        if not (last and gi == 0):
                            for c in range(3):
                                S.op("pe", lambda c=c: nc.tensor.matmul(PS[0][0:64, 0:n], lhsT=wQ[:, c, 96 * h:96 * h + 64], rhs=qan[:, c, t0:t0 + n], start=(c == 0), stop=(c == 2)),
                                     reads=[b_wQ, b_qan], writes=[PB[0]])
                            S.op("act", lambda: nc.scalar.copy(out=qn_t[:, t0:t0 + n], in_=PS[0][0:64, 0:n]), reads=[PB[0]], writes=[b_qn])
                            for c in range(3):
                                S.op("pe", lambda c=c: nc.tensor.matmul(PS[1][0:32, 0:n], lhsT=wQ[:, c, 96 * h + 64:96 * h + 96], rhs=qan[:, c, t0:t0 + n], start=(c == 0), stop=(c == 2)),
                                     reads=[b_wQ, b_qan], writes=[PB[1]])
                            for c in range(3):
                                S.op("pe", lambda c=c: nc.tensor.matmul(PS[2][0:32, 0:n], lhsT=wQs[:, c, 32 * h:32 * h + 32], rhs=qan[:, c, t0:t0 + n], start=(c == 0), stop=(c == 2)),
                                     reads=[b_wQs, b_qan], writes=[PB[2]])
                            rope_or_copy(qr_t, b_qr, t0, n, 1, 2, gi == 0)
                        for c in range(2):
                            S.op("pe", lambda c=c: nc.tensor.matmul(PS[3][0:64, 0:n], lhsT=wKV[:, c, 128 * h:128 * h + 64], rhs=kvan[:, c, t0:t0 + n], start=(c == 0), stop=(c == 1)),
                                 reads=[b_wKV, b_kvan], writes=[PB[3]])
                        S.op("act", lambda: nc.scalar.copy(out=kn_t[:, t0:t0 + n], in_=PS[3][0:64, 0:n]), reads=[PB[3]], writes=[b_kn])
                    for gi, (t0, n) in enumerate(GROUPS):
                        if last and gi == 0:
                            continue
                        kts = list(range(2)) if gi == 0 else list(range(NT))
                        pend = None
                        for ki, kt in enumerate(kts):
                            psb = 4 + (ecnt % 2)
                            E_t, b_E = Et[ecnt % 3]
                            ecnt += 1
                            S.op("pe", lambda kt=kt, psb=psb: nc.tensor.matmul(PS[psb][:, 0:n], lhsT=kn_t[:, kt * 128:(kt + 1) * 128], rhs=qn_t[:, t0:t0 + n], start=True, stop=False),
                                 reads=[b_kn, b_qn], writes=[PB[psb]])
                            S.op("pe", lambda kt=kt, psb=psb: nc.tensor.matmul(PS[psb][:, 0:n], lhsT=kr[:, kt * 128:(kt + 1) * 128], rhs=qr_t[:, t0:t0 + n], start=False, stop=True),
                                 reads=[b_kr, b_qr], writes=[PB[psb]])
                            S.op("act", lambda psb=psb, E_t=E_t: nc.scalar.activation(out=E_t[:, 0:n], in_=PS[psb][:, 0:n], func=AF.Exp, scale=MLA_SCALE),
                                 reads=[PB[psb]], writes=[b_E])
                            if pend is not None:
                                pend()
                            pend = (lambda kt=kt, E_t=E_t, b_E=b_E, ki=ki: S.op("pe", lambda: nc.tensor.matmul(PS[6][0:65, 0:n], lhsT=v_aug[:, kt, h, :], rhs=E_t[:, 0:n], start=(ki == 0), stop=(ki == len(kts) - 1)),
                                 reads=[b_va, b_E], writes=[PB[6]]))
                        pend()
                        nm_t, b_nm = numt[ocnt % 2]
                        o_t, b_o = ot[ocnt % 2]
                        ocnt += 1
                        S.op("dve", lambda: nc.vector.reciprocal(out=rc[64:65, 0:n], in_=PS[6][64:65, 0:n]), reads=[PB[6]], writes=[b_rc])
                        S.op("act", lambda nm_t=nm_t: nc.scalar.copy(out=nm_t[:, 0:n], in_=PS[6][0:64, 0:n]), reads=[PB[6]], writes=[b_nm])
                        S.op("pe", lambda: nc.tensor.matmul(PS[7][0:64, 0:n], lhsT=onesf[64:65, 0:64], rhs=rc[64:65, 0:n], start=True, stop=True),
                             reads=[b_onesf, b_rc], writes=[PB[7]])
                        S.op("dve", lambda nm_t=nm_t, o_t=o_t: nc.vector.tensor_tensor(out=o_t[:, 0:n], in0=nm_t[:, 0:n], in1=PS[7][0:64, 0:n], op=ALU.mult),
                             reads=[b_nm, PB[7]], writes=[b_o])
                        S.dma(mla_o_d[h, :, t0:t0 + n], o_t[:, 0:n], reads=[b_o], writes=[b_mo])
                S.barrier()

        b_xres = [Buf("xres%d" % i) for i in range(NT)]
        stage_entry(0)
        for l in range(nlayers):
            last = (l == nlayers - 1)
            if not cfg.get("skip_mla"):
                stage_mla(l, last)
            if not cfg.get("skip_hg"):
                stage_hgrn(l)
            if not cfg.get("skip_gdn"):
                stage_gdn(l)
            if cfg.get("stop_after") == "mixers":
                break
            stage_merge(l, last)
            if cfg.get("stop_after") == "merge":
                break
            stage_moe(l, last)
        if "h_fm" in DBG:
            S.dma(DBG["h_fm"].rearrange("(kc p) t -> p kc t", p=128), h_fm[:], reads=[b_hfm], q="pool")
        if "xres" in DBG:
            S.dma(DBG["xres"], xres_d[:, :])
        if "mla_o" in DBG:
            S.dma(DBG["mla_o"], mla_o_d.rearrange("h d t -> (h d) t"), q="pool")
        if "hg_o" in DBG:
            S.dma(DBG["hg_o"], hg_o_d.rearrange("h d t -> (h d) t"), q="pool")
        if "gdn_o" in DBG:
            S.dma(DBG["gdn_o"], gdn_o_d.rearrange("h d t -> (h d) t"), q="pool")
        K.PS, K.PB = PS, PB

        S.finish()
    K.ninstr = S.ninstr
    return nc, K


WEIGHT_SHAPES = {
    "w_mod": [DEPTH, D, 6 * D], "b_mod": [DEPTH, 6 * D], "w_in": [DEPTH, D, IN_W],
    "w_q_b": [DEPTH, 384, 768], "w_kv_b": [DEPTH, 256, 1024],
    "hg_lb_logits": [DEPTH, 2, 512],
    "gdn_a_log": [DEPTH, 2, 4], "gdn_dt_bias": [DEPTH, 2, 4], "gdn_norm": [DEPTH, 128],
    "w_branch": [DEPTH, 3, 512, D], "w_out": [DEPTH, D, D],
    "ln1_g": [DEPTH, D], "ln1_b": [DEPTH, D], "ln2_g": [DEPTH, D], "ln2_b": [DEPTH, D],
    "w_router": [DEPTH, D, 64], "router_bias": [DEPTH, 64],
    "w_gu": [DEPTH, 64, D, 512], "w_down": [DEPTH, 64, 256, D], "w_sh_gu": [DEPTH, D, 512], "w_sh_down": [DEPTH, 256, D],
}
DERIVED_SHAPES = {
    "w_kr_sw": [DEPTH, D, 32], "w_qr_sw": [DEPTH, 384, 256],
    "q_a_norm_t": [DEPTH, 128, 3], "kv_a_norm_t": [DEPTH, 128, 2],
    "hg_norm_t": [128, DEPTH], "gdn_conv_t": [DEPTH, 128, 12, 5],
}
CONST_SHAPES = {
    "ident": [128, 128], "ropeC": [32, LAT], "ropeS": [32, LAT],
    "rmask": [128, T], "triu": [64, 64], "tril": [64, 64],
    "mist": [64, 2, 64], "mast": [64, 2, 64],
}


def host_consts():
    c = {}
    c["ident"] = np.eye(128, dtype=np.float32)
    pos = np.arange(LAT)
    row = (pos // 64).astype(np.float32)
    col = (pos % 64).astype(np.float32)
    inv = (np.float32(10000.0) ** (-np.arange(8, dtype=np.float32) / np.float32(8))).astype(np.float32)
    C = np.zeros((32, LAT), np.float32)
    Sg = np.zeros((32, LAT), np.float32)
    for ax, p in enumerate((row, col)):
        ang = (p[None, :] * inv[:, None]).astype(np.float32)
        for half in range(2):
            r0 = ax * 16 + half * 8
            C[r0:r0 + 8] = np.cos(ang)
            Sg[r0:r0 + 8] = np.sin(ang) * (-1.0 if half == 0 else 1.0)
    c["ropeC"] = C
    rm = np.ones((128, T), np.float32)
    rm[:, ::64] = 0.0
    c["rmask"] = rm
    c["triu"] = np.triu(np.ones((64, 64), np.float32))
    c["tril"] = np.tril(np.ones((64, 64), np.float32))
    c["mist"] = np.ascontiguousarray(np.stack([c["triu"], c["tril"]], axis=1))
    c["mast"] = np.ascontiguousarray(np.stack([c["tril"] - np.eye(64, dtype=np.float32), c["triu"] - np.eye(64, dtype=np.float32)], axis=1))
    c["ropeS"] = Sg
    return c


def prep_inputs(inputs):
    x = np.asarray(inputs["x"], np.float32)
    ctx = np.asarray(inputs["ctx"], np.float32)
    c = np.asarray(inputs["c"], np.float32)
    c_ctx = np.asarray(inputs["c_ctx"], np.float32)
    shared = {}
    for nm in WEIGHT_SHAPES:
        shared[nm] = np.ascontiguousarray(np.asarray(inputs[nm], np.float32)).reshape(WEIGHT_SHAPES[nm])
    shared.update(host_consts())
    perm = np.arange(32) ^ 8
    w_in = shared["w_in"]
    shared["w_kr_sw"] = np.ascontiguousarray(w_in[:, :, 640:672][:, :, perm])
    wqb = shared["w_q_b"].reshape(DEPTH, 384, 8, 96)
    shared["w_qr_sw"] = np.ascontiguousarray(wqb[:, :, :, 64:96][:, :, :, perm].reshape(DEPTH, 384, 256))
    shared["q_a_norm_t"] = np.ascontiguousarray(np.asarray(inputs["q_a_norm"], np.float32).reshape(DEPTH, 3, 128).transpose(0, 2, 1))
    shared["gdn_conv_t"] = np.ascontiguousarray(np.asarray(inputs["gdn_conv"], np.float32).reshape(DEPTH, 5, 12, 128).transpose(0, 3, 2, 1))
    shared["hg_norm_t"] = np.ascontiguousarray(np.asarray(inputs["hg_norm"], np.float32).T)
    shared["kv_a_norm_t"] = np.ascontiguousarray(np.asarray(inputs["kv_a_norm"], np.float32).reshape(DEPTH, 2, 128).transpose(0, 2, 1))
    maps = []
    for b in range(x.shape[0]):
        m = dict(shared)
        m["xin"] = np.ascontiguousarray(np.concatenate([ctx[b], x[b]], axis=0))
        m["cvecT"] = np.ascontiguousarray(np.stack([c[b], c_ctx], axis=1))
        maps.append(m)
    return maps


def kernel(**inputs):
    maps = prep_inputs(inputs)
    nc, K = build_program({})
    res = run_bass_kernel_spmd(nc, maps, core_ids=list(range(8)))
    out = np.stack([np.asarray(r["out"], np.float32) for r in res.results], axis=0)
    return out
```

```python
import contextlib
import numpy as np
import concourse.bass as bass
import concourse.mybir as mybir
from concourse.bass_utils import run_bass_kernel_spmd

F32 = mybir.dt.float32
BF16 = mybir.dt.bfloat16
AF = mybir.ActivationFunctionType
ALU = mybir.AluOpType
AX = mybir.AxisListType

EPOCH = 30000
NRING = 40

DEPTH = 4
D = 1024
KC = 8
LAT = 2048
CTX = 256
T = LAT + CTX
NT = T // 128
GROUPS = [(0, 256), (256, 512), (768, 512), (1280, 512), (1792, 512)]
NCH = T // 64
IN_W = 8368
MLA_SCALE = 96 ** -0.5
ALPHA = (2 * DEPTH) ** 0.25
O_HG = 672
O_GDN = O_HG + 2560
O_GG = O_GDN + 1536
O_GA = O_GG + 512
O_GB = O_GA + 8
O_GATES = O_GB + 8


class Buf:
    __slots__ = ("name", "w", "r", "ex")

    def __init__(self, name="", ex=False):
        self.name = name
        self.w = None
        self.r = []
        self.ex = ex


class Sched:
    def __init__(self, nc, es, self_sync=True):
        self.nc = nc
        self.es = es
        self.eng = {"pe": nc.tensor, "act": nc.scalar, "dve": nc.vector, "pool": nc.gpsimd, "sp": nc.sync}
        self.seq = {e: 0 for e in self.eng}
        self.sems = {e: [] for e in self.eng}
        self.known = {e: {} for e in self.eng}
        self.known_dma = {e: set() for e in self.eng}
        self.ring = [es.enter_context(nc.semaphore("dr%d" % i)) for i in range(NRING)]
        self.ring_cnt = [0] * NRING
        self.ring_next = 0
        self.self_sync = self_sync
        self.ninstr = 0

    def _sem(self, e, ep):
        while len(self.sems[e]) <= ep:
            self.sems[e].append(self.es.enter_context(self.nc.semaphore("s_%s%d" % (e, len(self.sems[e])))))
        return self.sems[e][ep]

    def _wait(self, e, tok):
        if tok is None:
            return
        if tok[0] == "dma":
            _, k, val = tok
            key = (k, val)
            if key in self.known_dma[e]:
                return
            self.eng[e].wait_ge(self.ring[k], val)
            self.known_dma[e].add(key)
            return
        _, e2, s = tok
        if e2 == e and (e == "pe" or not self.self_sync):
            return
        if self.known[e].get(e2, 0) >= s:
            return
        ep = (s - 1) // EPOCH
        self.eng[e].wait_ge(self._sem(e2, ep), s - ep * EPOCH)
        self.known[e][e2] = s

    def _deps(self, e, reads, writes):
        for b in reads:
            if b.w is not None:
                self._wait(e, b.w)
            if b.ex:
                for t in b.r:
                    if t[0] == "eng" and t[1] != e:
                        self._wait(e, t)
        for b in writes:
            if b.w is not None:
                self._wait(e, b.w)
            for t in b.r:
                self._wait(e, t)

    def _mark(self, tok, reads, writes):
        for b in reads:
            b.r.append(tok)
            if len(b.r) > 24:
                b.r = self._prune(b.r)
        for b in writes:
            b.w = tok
            b.r = []

    def _prune(self, r):
        best = {}
        out = []
        for t in r:
            if t[0] == "dma":
                out.append(t)
            elif t[1] not in best or best[t[1]][2] < t[2]:
                best[t[1]] = t
        return out + list(best.values())

    def op(self, e, fn, reads=(), writes=()):
        self._deps(e, reads, writes)
        ins = fn()
        self.seq[e] += 1
        s = self.seq[e]
        ep = (s - 1) // EPOCH
        ins.then_inc(self._sem(e, ep), 1)
        tok = ("eng", e, s)
        self._mark(tok, reads, writes)
        self.ninstr += 1
        return tok

    def dma(self, out, in_, reads=(), writes=(), q="sp", **kw):
        k = self.ring_next
        self.ring_next = (self.ring_next + 1) % NRING
        prev = self.ring_cnt[k]
        if prev > 0:
            self._wait(q, ("dma", k, 16 * prev))
        self._deps(q, reads, writes)
        self.eng[q].dma_start(out=out, in_=in_, **kw).then_inc(self.ring[k], 16)
        self.ring_cnt[k] = prev + 1
        tok = ("dma", k, 16 * (prev + 1))
        self._mark(tok, reads, writes)
        self.ninstr += 1
        return tok

    def barrier(self, engines=None):
        for e in (engines or self.eng):
            for e2 in self.eng:
                if self.seq[e2] > 0 and not (e2 == e and e == "pe"):
                    self._wait(e, ("eng", e2, self.seq[e2]))
            for k in range(NRING):
                if self.ring_cnt[k] > 0:
                    self._wait(e, ("dma", k, 16 * self.ring_cnt[k]))

    def finish(self):
        self.barrier(["sp"])


class Ctx:
    pass


def build_program(cfg):
    nlayers = cfg.get("nlayers", DEPTH)
    stop_after = cfg.get("stop_after", None)
    dbg = cfg.get("debug", [])
    nc = bass.Bass("TRN2", target_bir_lowering=False)
    K = Ctx()
    K.nc = nc

    def din(name, shape, dt=F32):
        return nc.dram_tensor(name, list(shape), dt, kind="ExternalInput").ap()

    def dscr(name, shape, dt=F32):
        return nc.dram_tensor(name, list(shape), dt, kind="Internal").ap()

    I = {}
    I["xin"] = din("xin", [T, D])
    I["cvecT"] = din("cvecT", [D, 2])
    for nm, shp in WEIGHT_SHAPES.items():
        I[nm] = din(nm, shp)
    for nm, shp in CONST_SHAPES.items():
        I[nm] = din(nm, shp)
    for nm, shp in DERIVED_SHAPES.items():
        I[nm] = din(nm, shp)
    out_d = nc.dram_tensor("out", [LAT, D], F32, kind="ExternalOutput").ap()
    DBG = {}
    for nm, shp in dbg:
        DBG[nm] = nc.dram_tensor("dbg_" + nm, list(shp), F32, kind="ExternalOutput").ap()

    modv_d = dscr("modv_d", [DEPTH, 2, 6 * D])
    xres_d = dscr("xres_d", [T, D])
    mla_o_d = dscr("mla_o_d", [8, 64, T], BF16)
    hg_o_d = dscr("hg_o_d", [4, 128, T], BF16)
    gdn_o_d = dscr("gdn_o_d", [4, 128, T], BF16)
    gdn_raw_d = dscr("gdn_raw_d", [2, T, 512])

    es = contextlib.ExitStack()
    with es:
        S = Sched(nc, es, self_sync=cfg.get('self_sync', True))
        K.S = S

        tlc = [0]

        def tl(st, name, shape, dt):
            tlc[0] += 1
            t = st.enter_context(nc.sbuf_tensor("sb%d_%s" % (tlc[0], name), list(shape), dt))
            return t, Buf(name)

        PS = []
        PB = []
        for i in range(8):
            PS.append(es.enter_context(nc.psum_tensor("ps%d" % i, [128, 512], F32)))
            PB.append(Buf("ps%d" % i, ex=True))

        identf, b_identf = tl(es, "identf", [128, 128], F32)
        identb, b_identb = tl(es, "identb", [128, 128], BF16)
        onesb, b_onesb = tl(es, "onesb", [128, 128], BF16)
        onesf, b_onesf = tl(es, "onesf", [128, 128], F32)
        S.dma(identf[:], I["ident"][:, :], writes=[b_identf])
        S.dma(identb[:], I["ident"][:, :], writes=[b_identb], q="pool")
        S.op("dve", lambda: nc.vector.memset(onesb[:], 1.0), writes=[b_onesb])
        S.op("dve", lambda: nc.vector.memset(onesf[:], 1.0), writes=[b_onesf])
        h_fm, b_hfm = tl(es, "h_fm", [128, KC, T], BF16)
        epsb, b_eps = tl(es, "epsb", [128, 1], F32)
        S.op("dve", lambda: nc.vector.memset(epsb[:], 1e-6), writes=[b_eps])

        def dbg_out(name, ap_sb, buf, dram_ap=None):
            if name in DBG:
                S.dma(dram_ap if dram_ap is not None else DBG[name], ap_sb, reads=[buf])

        with contextlib.ExitStack() as st:
            cv, b_cv = tl(st, "cv", [128, KC, 2], F32)
            scv, b_scv = tl(st, "scv", [128, KC, 2], F32)
            S.dma(cv[:], I["cvecT"].rearrange("(kc p) s -> p kc s", p=128), writes=[b_cv])
            S.op("act", lambda: nc.scalar.activation(out=scv[:], in_=cv[:], func=AF.Silu), reads=[b_cv], writes=[b_scv])
            wm = [tl(st, "wm%d" % i, [128, KC, 512], F32) for i in range(2)]
            bm, b_bm = tl(st, "bm", [1, 6 * D], F32)
            mv = [tl(st, "mv%d" % i, [2, 6 * D], F32) for i in range(2)]
            ones2, b_ones2 = tl(st, "ones2", [1, 2], F32)
            S.op("dve", lambda: nc.vector.memset(ones2[:], 1.0), writes=[b_ones2])
            cnt = 0
            for l in range(nlayers):
                mvt, b_mv = mv[l % 2]
                S.dma(bm[:], I["b_mod"][l:l + 1, :], writes=[b_bm])
                for cg in range(12):
                    wt, b_wt = wm[cnt % 2]
                    cnt += 1
                    S.dma(wt[:], I["w_mod"][l].rearrange("(kc p) n -> p kc n", p=128)[:, :, cg * 512:(cg + 1) * 512], writes=[b_wt])
                    pb = cnt % 2
                    for kc in range(KC):
                        S.op("pe", lambda kc=kc, wt=wt, pb=pb: nc.tensor.matmul(PS[pb][0:2, :], lhsT=scv[:, kc, :], rhs=wt[:, kc, :], start=(kc == 0), stop=False),
                             reads=[b_scv, b_wt], writes=[PB[pb]])
                    S.op("pe", lambda cg=cg, pb=pb: nc.tensor.matmul(PS[pb][0:2, :], lhsT=ones2[:], rhs=bm[:, cg * 512:(cg + 1) * 512], start=False, stop=True),
                         reads=[b_ones2, b_bm], writes=[PB[pb]])
                    S.op("act", lambda cg=cg, pb=pb, mvt=mvt: nc.scalar.copy(out=mvt[:, cg * 512:(cg + 1) * 512], in_=PS[pb][0:2, :]), reads=[PB[pb]], writes=[b_mv])
                S.dma(modv_d[l], mvt[:], reads=[b_mv], writes=[])
                K.modv_tok = None
            S.barrier()
        b_modv = Buf("modv_d")
        b_modv.w = None

        def load_bc(st, name, l, stream, j, plus_one=False):
            t, b = tl(st, name, [128, D], F32)
            S.dma(t[:], modv_d[l, stream:stream + 1, j * D:(j + 1) * D].partition_broadcast(128), writes=[b])
            if plus_one:
                S.op("pool", lambda: nc.gpsimd.tensor_scalar_add(out=t[:], in0=t[:], scalar1=1.0), reads=[b], writes=[b])
            return t, b

        def load_vec_bc(st, name, dram_row_ap, n=D):
            t, b = tl(st, name, [128, n], F32)
            S.dma(t[:], dram_row_ap.partition_broadcast(128), writes=[b])
            return t, b

        def to_fm(src_bf, b_src, i, psb):
            pv = PS[psb][:].bitcast(BF16)
            for kc in range(KC):
                S.op("pe", lambda kc=kc: nc.tensor.transpose(out=pv[:, kc * 128:(kc + 1) * 128], in_=src_bf[:, kc * 128:(kc + 1) * 128], identity=identb[:]),
                     reads=[b_src, b_identb], writes=[PB[psb]])
            S.op("act", lambda: nc.scalar.copy(out=h_fm[:, :, i * 128:(i + 1) * 128], in_=pv.rearrange("p (k t) -> p k t", k=KC)),
                 reads=[PB[psb]], writes=[b_hfm])

        def stage_entry(l):
            with contextlib.ExitStack() as st:
                bc = {}
                for s in range(2):
                    bc[(s, 0)] = load_bc(st, "bsh%d" % s, l, s, 0)
                    bc[(s, 1)] = load_bc(st, "bsc%d" % s, l, s, 1, plus_one=True)
                xt = [tl(st, "xt%d" % i, [128, D], F32) for i in range(2)]
                ht = [tl(st, "ht%d" % i, [128, D], BF16) for i in range(2)]
                for i in range(NT):
                    s = 1 if i < 2 else 0
                    x_t, b_x = xt[i % 2]
                    h_t, b_h = ht[i % 2]
                    S.dma(x_t[:], I["xin"][i * 128:(i + 1) * 128, :], writes=[b_x])
                    S.dma(xres_d[i * 128:(i + 1) * 128, :], x_t[:], reads=[b_x])
                    S.op("dve", lambda x_t=x_t, s=s: nc.vector.tensor_tensor(out=x_t[:], in0=x_t[:], in1=bc[(s, 1)][0][:], op=ALU.mult),
                         reads=[b_x, bc[(s, 1)][1]], writes=[b_x])
                    S.op("dve", lambda x_t=x_t, h_t=h_t, s=s: nc.vector.tensor_tensor(out=h_t[:], in0=x_t[:], in1=bc[(s, 0)][0][:], op=ALU.add),
                         reads=[b_x, bc[(s, 0)][1]], writes=[b_h])
                    to_fm(h_t, b_h, i, i % 2)
                S.barrier()


        lbT, b_lbT = tl(es, "lbT", [128, DEPTH, 8], F32)
        omlbT, b_omlbT = tl(es, "omlbT", [128, DEPTH, 8], F32)
        rmask, b_rmask = tl(es, "rmask", [128, T], BF16)
        triu, b_triu = tl(es, "triu", [64, 64], F32)
        tril, b_tril = tl(es, "tril", [64, 64], F32)
        S.dma(rmask[:], I["rmask"][:, :], writes=[b_rmask], q="pool")
        S.dma(triu[:], I["triu"][:, :], writes=[b_triu])
        S.dma(tril[:], I["tril"][:, :], writes=[b_tril])
        with contextlib.ExitStack() as st:
            lg, b_lg = tl(st, "lg", [32, 128], F32)
            eT, b_eT = tl(st, "eT", [128, DEPTH, 8], F32)
            tot, b_tot = tl(st, "lbtot", [128, 8], F32)
            S.dma(lg[:], I["hg_lb_logits"].rearrange("l s (h p) -> (l s h) p", p=128), writes=[b_lg])
            S.op("act", lambda: nc.scalar.activation(out=lg[:], in_=lg[:], func=AF.Exp), reads=[b_lg], writes=[b_lg])
            S.op("pe", lambda: nc.tensor.transpose(out=PS[0][:, 0:32], in_=lg[:], identity=identf[0:32, 0:32]), reads=[b_lg, b_identf], writes=[PB[0]])
            S.op("act", lambda: nc.scalar.copy(out=eT[:].rearrange("p l x -> p (l x)"), in_=PS[0][:, 0:32]), reads=[PB[0]], writes=[b_eT])
            S.op("dve", lambda: nc.vector.tensor_tensor(out=tot[:], in0=eT[:, 0, :], in1=eT[:, 1, :], op=ALU.add), reads=[b_eT], writes=[b_tot])
            S.op("dve", lambda: nc.vector.tensor_tensor(out=tot[:], in0=tot[:], in1=eT[:, 2, :], op=ALU.add), reads=[b_eT, b_tot], writes=[b_tot])
            S.op("dve", lambda: nc.vector.tensor_tensor(out=tot[:], in0=tot[:], in1=eT[:, 3, :], op=ALU.add), reads=[b_eT, b_tot], writes=[b_tot])
            S.op("dve", lambda: nc.vector.reciprocal(out=tot[:], in_=tot[:]), reads=[b_tot], writes=[b_tot])
            S.op("dve", lambda: nc.vector.memset(lbT[:, 0, :], 0.0), writes=[b_lbT])
            S.op("dve", lambda: nc.vector.tensor_copy(out=lbT[:, 1, :], in_=eT[:, 1, :]), reads=[b_eT, b_lbT], writes=[b_lbT])
            S.op("dve", lambda: nc.vector.tensor_tensor(out=lbT[:, 2, :], in0=lbT[:, 1, :], in1=eT[:, 2, :], op=ALU.add), reads=[b_eT, b_lbT], writes=[b_lbT])
            S.op("dve", lambda: nc.vector.tensor_tensor(out=lbT[:, 3, :], in0=lbT[:, 2, :], in1=eT[:, 3, :], op=ALU.add), reads=[b_eT, b_lbT], writes=[b_lbT])
            for l in range(1, DEPTH):
                S.op("dve", lambda l=l: nc.vector.tensor_tensor(out=lbT[:, l, :], in0=lbT[:, l, :], in1=tot[:], op=ALU.mult), reads=[b_tot, b_lbT], writes=[b_lbT])
            S.op("dve", lambda: nc.vector.tensor_scalar(out=omlbT[:], in0=lbT[:], scalar1=-1.0, scalar2=1.0, op0=ALU.mult, op1=ALU.add), reads=[b_lbT], writes=[b_omlbT])
            S.barrier()

        def stage_hgrn(l):
            winv = I["w_in"][l].rearrange("(kc p) n -> p kc n", p=128)
            with contextlib.ExitStack() as st:
                wh = [tl(st, "wh%d" % i, [128, KC, 5, 128], BF16) for i in range(2)]
                hgn4, b_hgn = tl(st, "hgn", [128, DEPTH], F32)
                S.dma(hgn4[:], I["hg_norm_t"][:, :], writes=[b_hgn])
                hgn = hgn4[:, l:l + 1]
                q_bf, b_q = tl(st, "hq_bf", [128, T], BF16)
                gate_sb, b_gate = tl(st, "hgate", [128, T], BF16)
                v_tm, b_v = tl(st, "hv_tm", [64, NCH, 128], BF16)
                A, b_A = tl(st, "hA", [128, T], F32)
                B, b_B = tl(st, "hB", [128, T], F32)
                Cc, b_C = tl(st, "hC", [128, T], F32)
                qt, b_qt = tl(st, "hqt", [128, T], BF16)
                kt, b_kt = tl(st, "hkt", [128, T], BF16)
                qh, b_qh = tl(st, "hqh", [128, T], BF16)
                kh, b_kh = tl(st, "hkh", [128, T], BF16)
                khT, b_khT = tl(st, "hkhT", [64, NCH, 128], BF16)
                aT, b_aT = tl(st, "haT", [64, NCH, 64], BF16)
                o_d = [tl(st, "ho%d" % i, [128, T], F32) for i in range(2)]
                tot, b_tot = tl(st, "htot", [128, NCH], F32)
                rmid, b_rmid = tl(st, "hrmid", [128, NCH], F32)
                egl, b_egl = tl(st, "hegl", [128, NCH], F32)
                Sst, b_S = tl(st, "hS", [128, 128], F32)
                Sb = [tl(st, "hSb%d" % i, [128, 128], BF16) for i in range(2)]
                rs0, b_rs0 = tl(st, "hrs0", [128, 512], F32)
                rs1, b_rs1 = tl(st, "hrs1", [128, 512], F32)
                og = [tl(st, "hog%d" % i, [128, 512], BF16) for i in range(2)]
                b_ho = Buf("hg_o")
                C3 = Cc[:].rearrange("p (c k) -> p c k", k=64)
                B3 = B[:].rearrange("p (c k) -> p c k", k=64)
                pcnt = [0]

                def proj(col, wt, b_wt, fn_evac):
                    for (t0, n) in GROUPS:
                        pb = pcnt[0] % 2
                        pcnt[0] += 1
                        for kc in range(KC):
                            S.op("pe", lambda kc=kc, pb=pb: nc.tensor.matmul(PS[pb][:, 0:n], lhsT=wt[:, kc, col, :], rhs=h_fm[:, kc, t0:t0 + n], start=(kc == 0), stop=(kc == KC - 1)),
                                 reads=[b_wt, b_hfm], writes=[PB[pb]])
                        fn_evac(pb, t0, n)

                ocnt = 0
                for hd in range(4):
                    wt, b_wt = wh[hd % 2]
                    for ci in range(5):
                        c0 = O_HG + ci * 512 + hd * 128
                        S.dma(wt[:, :, ci, :], winv[:, :, c0:c0 + 128], writes=[b_wt], q="pool")
                    proj(0, wt, b_wt, lambda pb, t0, n: S.op("act", lambda: nc.scalar.activation(out=q_bf[:, t0:t0 + n], in_=PS[pb][:, 0:n], func=AF.Silu), reads=[PB[pb]], writes=[b_q]))
                    proj(4, wt, b_wt, lambda pb, t0, n: S.op("act", lambda: nc.scalar.activation(out=gate_sb[:, t0:t0 + n], in_=PS[pb][:, 0:n], func=AF.Silu), reads=[PB[pb]], writes=[b_gate]))
                    for c4 in range(NCH // 4):
                        pb = pcnt[0] % 2
                        pcnt[0] += 1
                        for j in range(4):
                            c = c4 * 4 + j
                            for kc in range(KC):
                                S.op("pe", lambda kc=kc, c=c, j=j, pb=pb: nc.tensor.matmul(PS[pb][0:64, j * 128:(j + 1) * 128], lhsT=h_fm[:, kc, c * 64:(c + 1) * 64], rhs=wt[:, kc, 1, :],
                                                                                         start=(kc == 0), stop=(kc == KC - 1)), reads=[b_wt, b_hfm], writes=[PB[pb]])
                        S.op("act", lambda c4=c4, pb=pb: nc.scalar.copy(out=v_tm[:, c4 * 4:(c4 + 1) * 4, :], in_=PS[pb][0:64, :].rearrange("p (j x) -> p j x", j=4)),
                             reads=[PB[pb]], writes=[b_v])
                    for s in range(2):
                        o_t, b_o = o_d[s]
                        lbc = lbT[:, l, s * 4 + hd:s * 4 + hd + 1]
                        omc = omlbT[:, l, s * 4 + hd:s * 4 + hd + 1]
                        proj(2 + s, wt, b_wt, lambda pb, t0, n: S.op("act", lambda: nc.scalar.activation(out=A[:, t0:t0 + n], in_=PS[pb][:, 0:n], func=AF.Sigmoid), reads=[PB[pb]], writes=[b_A]))
                        S.op("dve", lambda: nc.vector.tensor_scalar(out=A[:], in0=A[:], scalar1=omc, scalar2=lbc, op0=ALU.mult, op1=ALU.add), reads=[b_A, b_lbT, b_omlbT], writes=[b_A])
                        S.op("act", lambda: nc.scalar.activation(out=B[:], in_=A[:], func=AF.Ln), reads=[b_A], writes=[b_B])
                        S.op("pool", lambda: nc.gpsimd.tensor_scalar(out=A[:], in0=A[:], scalar1=-1.0, scalar2=1.0, op0=ALU.mult, op1=ALU.add), reads=[b_A, b_B], writes=[b_A])
                        S.op("dve", lambda: nc.vector.tensor_tensor_scan(out=Cc[:], data0=rmask[:], data1=B[:], initial=0.0, op0=ALU.mult, op1=ALU.add), reads=[b_rmask, b_B], writes=[b_C])
                        S.op("dve", lambda: nc.vector.tensor_copy(out=tot[:], in_=C3[:, :, 63]), reads=[b_C], writes=[b_tot])
                        totb = tot[:].unsqueeze(2).to_broadcast([128, NCH, 64])
                        if s == 1:
                            S.op("dve", lambda: nc.vector.scalar_tensor_tensor(out=C3, in0=C3, scalar=-1.0, in1=totb, op0=ALU.mult, op1=ALU.add), reads=[b_C, b_tot], writes=[b_C])
                            S.op("dve", lambda: nc.vector.tensor_tensor(out=Cc[:], in0=Cc[:], in1=B[:], op=ALU.add), reads=[b_C, b_B], writes=[b_C])
                        S.op("dve", lambda: nc.vector.tensor_copy(out=rmid[:], in_=C3[:, :, 31 + s]), reads=[b_C], writes=[b_rmid])
                        S.op("act", lambda: nc.scalar.activation(out=egl[:], in_=tot[:], func=AF.Exp), reads=[b_tot], writes=[b_egl])
                        S.op("dve", lambda: nc.vector.tensor_tensor(out=B3, in0=C3, in1=rmid[:].unsqueeze(2).to_broadcast([128, NCH, 64]), op=ALU.subtract), reads=[b_C, b_rmid, b_B], writes=[b_B])
                        S.op("act", lambda: nc.scalar.activation(out=B[:], in_=B[:], func=AF.Exp), reads=[b_B], writes=[b_B])
                        S.op("pool", lambda: nc.gpsimd.tensor_tensor(out=qt[:], in0=q_bf[:], in1=B[:], op=ALU.mult), reads=[b_q, b_B], writes=[b_qt])
                        S.op("dve", lambda: nc.vector.reciprocal(out=B[:], in_=B[:]), reads=[b_B, b_qt], writes=[b_B])
                        S.op("dve", lambda: nc.vector.tensor_tensor(out=kt[:], in0=A[:], in1=B[:], op=ALU.mult), reads=[b_A, b_B], writes=[b_kt])
                        S.op("act", lambda: nc.scalar.activation(out=B[:], in_=Cc[:], func=AF.Exp), reads=[b_C, b_kt], writes=[b_B])
                        S.op("pool", lambda: nc.gpsimd.tensor_tensor(out=qh[:], in0=q_bf[:], in1=B[:], op=ALU.mult), reads=[b_q, b_B], writes=[b_qh])
                        S.op("dve", lambda: nc.vector.scalar_tensor_tensor(out=B3, in0=C3, scalar=-1.0, in1=totb, op0=ALU.mult, op1=ALU.add), reads=[b_C, b_tot, b_qh], writes=[b_B])
                        S.op("act", lambda: nc.scalar.activation(out=B[:], in_=B[:], func=AF.Exp), reads=[b_B], writes=[b_B])
                        S.op("dve", lambda: nc.vector.tensor_tensor(out=kh[:], in0=A[:], in1=B[:], op=ALU.mult), reads=[b_A, b_B], writes=[b_kh])
                        pvb = PS[2][:].bitcast(BF16)
                        msk = triu if s == 0 else tril
                        b_msk = b_triu if s == 0 else b_tril
                        fr = 0 if s == 0 else 32
                        dr = 32 - fr
                        S.op("dve", lambda: nc.vector.memset(aT[dr:dr + 32, :, fr:fr + 32], 0.0), writes=[b_aT])
                        for c8 in range(0, NCH, 8):
                            nb = min(8, NCH - c8)
                            for j in range(nb):
                                c = c8 + j
                                S.op("pe", lambda c=c, j=j: nc.tensor.transpose(out=pvb[0:64, j * 128:(j + 1) * 128], in_=kh[:, c * 64:(c + 1) * 64], identity=identb[:]),
                                     reads=[b_kh, b_identb], writes=[PB[2]])
                            S.op("act", lambda c8=c8, nb=nb: nc.scalar.copy(out=khT[:, c8:c8 + nb, :], in_=pvb[0:64, 0:nb * 128].rearrange("p (j x) -> p j x", j=nb)),
                                 reads=[PB[2]], writes=[b_khT])
                            for j in range(nb):
                                c = c8 + j
                                S.op("pe", lambda c=c, j=j: nc.tensor.matmul(PS[3][fr:fr + 32, j * 64:(j + 1) * 64], lhsT=kt[:, c * 64 + fr:c * 64 + fr + 32], rhs=qt[:, c * 64:(c + 1) * 64], start=True, stop=True),
                                     reads=[b_kt, b_qt], writes=[PB[3]])
                                S.op("pe", lambda c=c, j=j: nc.tensor.matmul(PS[3][dr:dr + 32, j * 64 + dr:j * 64 + dr + 32], lhsT=kt[:, c * 64 + dr:c * 64 + dr + 32], rhs=qt[:, c * 64 + dr:c * 64 + dr + 32], start=True, stop=True),
                                     reads=[b_kt, b_qt], writes=[PB[3]])
                            pv3 = PS[3][:, 0:nb * 64].rearrange("p (j x) -> p j x", j=nb)
                            S.op("dve", lambda c8=c8, nb=nb, pv3=pv3: nc.vector.tensor_tensor(out=aT[fr:fr + 32, c8:c8 + nb, :], in0=pv3[fr:fr + 32, :, :],
                                                                                     in1=msk[fr:fr + 32, :].unsqueeze(1).to_broadcast([32, nb, 64]), op=ALU.mult),
                                 reads=[PB[3], b_msk], writes=[b_aT])
                            S.op("dve", lambda c8=c8, nb=nb, pv3=pv3: nc.vector.tensor_tensor(out=aT[dr:dr + 32, c8:c8 + nb, dr:dr + 32], in0=pv3[dr:dr + 32, :, dr:dr + 32],
                                                                                     in1=msk[dr:dr + 32, dr:dr + 32].unsqueeze(1).to_broadcast([32, nb, 32]), op=ALU.mult),
                                 reads=[PB[3], b_msk], writes=[b_aT])
                        order = list(range(NCH)) if s == 0 else [3, 2, 1, 0] + list(range(NCH - 1, 3, -1))
                        def emit_pS(idx):
                            c = order[idx]
                            pS = 6 + idx % 2
                            S.op("pe", lambda: nc.tensor.matmul(PS[pS][:, 0:128], lhsT=khT[:, c, :], rhs=v_tm[:, c, :], start=True, stop=True), reads=[b_khT, b_v], writes=[PB[pS]])
                        emit_pS(0)
                        for idx, c in enumerate(order):
                            po = 4 + idx % 2
                            pS = 6 + idx % 2
                            if idx + 1 < NCH - 1:
                                emit_pS(idx + 1)
                            if idx < NCH - 1:
                                if idx == 0:
                                    S.op("dve", lambda pS=pS: nc.vector.tensor_copy(out=Sst[:], in_=PS[pS][:, 0:128]), reads=[PB[pS]], writes=[b_S])
                                else:
                                    S.op("dve", lambda c=c, pS=pS: nc.vector.scalar_tensor_tensor(out=Sst[:], in0=Sst[:], scalar=egl[:, c:c + 1], in1=PS[pS][:, 0:128], op0=ALU.mult, op1=ALU.add),
                                         reads=[b_S, b_egl, PB[pS]], writes=[b_S])
                            if idx > 0:
                                sb_t, b_sb = Sb[idx % 2]
                                S.op("pe", lambda c=c, po=po, sb_t=sb_t: nc.tensor.matmul(PS[po][:, 0:64], lhsT=sb_t[:], rhs=qh[:, c * 64:(c + 1) * 64], start=True, stop=False),
                                     reads=[b_sb, b_qh], writes=[PB[po]])
                            S.op("pe", lambda c=c, po=po, idx=idx: nc.tensor.matmul(PS[po][:, 0:64], lhsT=v_tm[:, c, :], rhs=aT[:, c, :], start=(idx == 0), stop=True),
                                 reads=[b_v, b_aT], writes=[PB[po]])
                            S.op("act", lambda c=c, po=po: nc.scalar.copy(out=o_t[:, c * 64:(c + 1) * 64], in_=PS[po][:, 0:64]), reads=[PB[po]], writes=[b_o])
                            if idx < NCH - 1:
                                nsb, b_nsb = Sb[(idx + 1) % 2]
                                S.op("act", lambda nsb=nsb: nc.scalar.copy(out=nsb[:], in_=Sst[:]), reads=[b_S], writes=[b_nsb])
                    o_f, b_of = o_d[0]
                    o_b, b_ob = o_d[1]
                    S.op("dve", lambda: nc.vector.tensor_tensor(out=o_f[:], in0=o_f[:], in1=o_b[:], op=ALU.add), reads=[b_of, b_ob], writes=[b_of])
                    S.op("act", lambda: nc.scalar.activation(out=qt[:], in_=o_f[:], func=AF.Square), reads=[b_of, b_qt], writes=[b_qt])
                    for (t0, n) in GROUPS:
                        pb = pcnt[0] % 2
                        pcnt[0] += 1
                        og_t, b_og = og[ocnt % 2]
                        ocnt += 1
                        S.op("pe", lambda pb=pb: nc.tensor.matmul(PS[pb][:, 0:n], lhsT=onesb[:], rhs=qt[:, t0:t0 + n], start=True, stop=True), reads=[b_onesb, b_qt], writes=[PB[pb]])
                        S.op("act", lambda pb=pb: nc.scalar.activation(out=rs0[:, 0:n], in_=PS[pb][:, 0:n], func=AF.Sqrt, scale=1.0 / 128, bias=epsb[:, 0:1]), reads=[PB[pb], b_eps], writes=[b_rs0])
                        S.op("dve", lambda: nc.vector.reciprocal(out=rs1[:, 0:n], in_=rs0[:, 0:n]), reads=[b_rs0], writes=[b_rs1])
                        S.op("dve", lambda: nc.vector.scalar_tensor_tensor(out=rs0[:, 0:n], in0=o_f[:, t0:t0 + n], scalar=hgn, in1=rs1[:, 0:n], op0=ALU.mult, op1=ALU.mult),
                             reads=[b_of, b_hgn, b_rs1, b_rs0], writes=[b_rs0])
                        S.op("dve", lambda og_t=og_t: nc.vector.tensor_tensor(out=og_t[:, 0:n], in0=rs0[:, 0:n], in1=gate_sb[:, t0:t0 + n], op=ALU.mult), reads=[b_rs0, b_gate], writes=[b_og])
                        S.dma(hg_o_d[hd, :, t0:t0 + n], og_t[:, 0:n], reads=[b_og], writes=[b_ho])
                S.barrier()


        def stage_gdn(l):
            winv = I["w_in"][l].rearrange("(kc p) n -> p kc n", p=128)
            with contextlib.ExitStack() as st0:
                mist, b_mist = tl(st0, "mist", [64, 2, 64], F32)
                mast, b_mast = tl(st0, "mast", [64, 2, 64], F32)
                S.dma(mist[:], I["mist"][:, :, :], writes=[b_mist])
                S.dma(mast[:], I["mast"][:, :, :], writes=[b_mast])
                g_t, b_g = tl(st0, "g_g", [64, NCH, 8], F32)
                beta, b_beta = tl(st0, "g_beta", [64, NCH, 8], F32)
                nbeta, b_nbeta = tl(st0, "g_nbeta", [64, NCH, 8], F32)
                egc, b_egc = tl(st0, "g_egc", [64, NCH, 8], F32)
                negc, b_negc = tl(st0, "g_negc", [64, NCH, 8], F32)
                ekd, b_ekd = tl(st0, "g_ekd", [64, NCH, 8], F32)
                egl, b_egl = tl(st0, "g_egl", [128, NCH, 8], F32)
                cw, b_cw = tl(st0, "g_cw", [128, 12, 5], F32)
                S.dma(cw[:], I["gdn_conv_t"][l], writes=[b_cw])
                with contextlib.ExitStack() as st:
                    wg, b_wg = tl(st, "g_wg", [128, KC, 16], BF16)
                    S.dma(wg[:], winv[:, :, O_GA:O_GA + 16], writes=[b_wg], q="pool")
                    gab, b_gab = tl(st, "g_gab", [64, NCH, 16], F32)
                    alog, b_alog = tl(st, "g_alog", [64, 8], F32)
                    dtb, b_dtb = tl(st, "g_dtb", [64, 8], F32)
                    S.dma(alog[:], I["gdn_a_log"][l:l + 1].rearrange("o s h -> o (s h)").partition_broadcast(64), writes=[b_alog])
                    S.dma(dtb[:], I["gdn_dt_bias"][l:l + 1].rearrange("o s h -> o (s h)").partition_broadcast(64), writes=[b_dtb])
                    for c in range(NCH):
                        pb = c // 32
                        cc = c % 32
                        for kc in range(KC):
                            S.op("pe", lambda c=c, kc=kc, pb=pb, cc=cc: nc.tensor.matmul(PS[pb][0:64, cc * 16:(cc + 1) * 16], lhsT=h_fm[:, kc, c * 64:(c + 1) * 64], rhs=wg[:, kc, :],
                                                                                     start=(kc == 0), stop=(kc == KC - 1)), reads=[b_hfm, b_wg], writes=[PB[pb]])
                    S.op("act", lambda: nc.scalar.copy(out=gab[:, 0:32, :], in_=PS[0][0:64, :].rearrange("p (c x) -> p c x", x=16)), reads=[PB[0]], writes=[b_gab])
                    S.op("act", lambda: nc.scalar.copy(out=gab[:, 32:36, :], in_=PS[1][0:64, 0:64].rearrange("p (c x) -> p c x", x=16)), reads=[PB[1]], writes=[b_gab])
                    S.op("act", lambda: nc.scalar.activation(out=alog[:], in_=alog[:], func=AF.Exp), reads=[b_alog], writes=[b_alog])
                    S.op("dve", lambda: nc.vector.tensor_tensor(out=g_t[:], in0=gab[:, :, 0:8], in1=dtb[:].unsqueeze(1).to_broadcast([64, NCH, 8]), op=ALU.add), reads=[b_gab, b_dtb], writes=[b_g])
                    S.op("act", lambda: nc.scalar.activation(out=g_t[:], in_=g_t[:], func=AF.Exp), reads=[b_g], writes=[b_g])
                    S.op("act", lambda: nc.scalar.activation(out=g_t[:], in_=g_t[:], func=AF.Ln, bias=onesf[0:64, 0:1]), reads=[b_g, b_onesf], writes=[b_g])
                    S.op("dve", lambda: nc.vector.scalar_tensor_tensor(out=g_t[:], in0=g_t[:], scalar=-1.0, in1=alog[:].unsqueeze(1).to_broadcast([64, NCH, 8]), op0=ALU.mult, op1=ALU.mult),
                         reads=[b_g, b_alog], writes=[b_g])
                    S.op("act", lambda: nc.scalar.activation(out=beta[:], in_=gab[:, :, 8:16], func=AF.Sigmoid), reads=[b_gab], writes=[b_beta])
                    S.op("dve", lambda: nc.vector.tensor_scalar(out=nbeta[:], in0=beta[:], scalar1=-1.0, scalar2=None, op0=ALU.mult), reads=[b_beta], writes=[b_nbeta])
                    for c in range(NCH):
                        for sd in range(2):
                            S.op("pe", lambda c=c, sd=sd: nc.tensor.matmul(PS[2][0:64, c * 8 + sd * 4:c * 8 + sd * 4 + 4], lhsT=mist[:, sd, :], rhs=g_t[:, c, sd * 4:sd * 4 + 4], start=True, stop=True),
                                 reads=[b_mist, b_g], writes=[PB[2]])
                            S.op("pe", lambda c=c, sd=sd: nc.tensor.matmul(PS[3][0:64, c * 8 + sd * 4:c * 8 + sd * 4 + 4], lhsT=mast[:, sd, :], rhs=g_t[:, c, sd * 4:sd * 4 + 4], start=True, stop=True),
                                 reads=[b_mast, b_g], writes=[PB[3]])
                        S.op("pe", lambda c=c: nc.tensor.matmul(PS[4][:, c * 8:c * 8 + 8], lhsT=onesf[0:64, :], rhs=g_t[:, c, :], start=True, stop=True), reads=[b_onesf, b_g], writes=[PB[4]])
                    S.op("act", lambda: nc.scalar.activation(out=egc[:].rearrange("p c x -> p (c x)"), in_=PS[2][0:64, 0:NCH * 8], func=AF.Exp), reads=[PB[2]], writes=[b_egc])
                    S.op("act", lambda: nc.scalar.activation(out=ekd[:].rearrange("p c x -> p (c x)"), in_=PS[3][0:64, 0:NCH * 8], func=AF.Exp), reads=[PB[3]], writes=[b_ekd])
                    S.op("act", lambda: nc.scalar.activation(out=egl[:].rearrange("p c x -> p (c x)"), in_=PS[4][:, 0:NCH * 8], func=AF.Exp), reads=[PB[4]], writes=[b_egl])
                    S.op("dve", lambda: nc.vector.tensor_scalar(out=negc[:], in0=egc[:], scalar1=-1.0, scalar2=None, op0=ALU.mult), reads=[b_egc], writes=[b_negc])
                    S.barrier()
                if cfg.get("gdn_stop") == 1:
                    S.barrier()
                    return

                for pr in range(2):
                    with contextlib.ExitStack() as st:
                        q_fm = [tl(st, "g_q%d" % i, [128, T], BF16) for i in range(2)]
                        k_fm = [tl(st, "g_k%d" % i, [128, T], BF16) for i in range(2)]
                        k_tm = [tl(st, "g_ktm%d" % i, [64, NCH, 128], BF16) for i in range(2)]
                        v_tm = [tl(st, "g_vtm%d" % i, [64, NCH, 128], BF16) for i in range(2)]
                        aqkT, b_aqkT = tl(st, "g_aqkT", [64, NCH, 4, 64], BF16)
                        R5b, b_R5b = tl(st, "g_R5b", [64, NCH, 4, 64], BF16)
                        with contextlib.ExitStack() as st2:
                            wc = [tl(st2, "g_wc%d" % i, [128, KC, 128], BF16) for i in range(2)]
                            zpad, b_zp = tl(st2, "g_zpad", [128, T + 8], F32)
                            acc, b_acc = tl(st2, "g_acc", [128, T + 8], F32)
                            xs, b_xs = tl(st2, "g_xs", [128, T], F32)
                            sqb, b_sqb = tl(st2, "g_sq", [128, T], BF16)
                            vfm, b_vfm = tl(st2, "g_vfm", [128, T], BF16)
                            r0, b_r0 = tl(st2, "g_r0", [128, 512], F32)
                            r1, b_r1 = tl(st2, "g_r1", [128, 512], F32)
                            S.op("dve", lambda: nc.vector.memset(zpad[:], 0.0), writes=[b_zp])
                            wcnt = 0
                            for hh in range(2):
                                hd = pr * 2 + hh
                                for part in range(3):
                                    ch = part * 4 + hd
                                    wct, b_wc = wc[wcnt % 2]
                                    wcnt += 1
                                    S.dma(wct[:], winv[:, :, O_GDN + ch * 128:O_GDN + (ch + 1) * 128], writes=[b_wc], q="pool")
                                    for gi, (t0, n) in enumerate(GROUPS):
                                        pb = gi % 2
                                        for kc in range(KC):
                                            S.op("pe", lambda kc=kc, pb=pb, wct=wct: nc.tensor.matmul(PS[pb][:, 0:n], lhsT=wct[:, kc, :], rhs=h_fm[:, kc, t0:t0 + n], start=(kc == 0), stop=(kc == KC - 1)),
                                                 reads=[b_wc, b_hfm], writes=[PB[pb]])
                                        z0 = 2 + t0 if gi == 0 else 6 + t0
                                        S.op("act", lambda pb=pb, z0=z0: nc.scalar.copy(out=zpad[:, z0:z0 + n], in_=PS[pb][:, 0:n]), reads=[PB[pb]], writes=[b_zp])
                                    NW = T + 4
                                    S.op("dve", lambda ch=ch: nc.vector.tensor_scalar(out=acc[:, 2:2 + NW], in0=zpad[:, 0:NW], scalar1=cw[:, ch, 0:1], scalar2=None, op0=ALU.mult),
                                         reads=[b_zp, b_cw], writes=[b_acc])
                                    for tau in range(1, 5):
                                        S.op("dve", lambda ch=ch, tau=tau: nc.vector.scalar_tensor_tensor(out=acc[:, 2:2 + NW], in0=zpad[:, tau:tau + NW], scalar=cw[:, ch, tau:tau + 1], in1=acc[:, 2:2 + NW],
                                                                                                        op0=ALU.mult, op1=ALU.add), reads=[b_zp, b_cw, b_acc], writes=[b_acc])
                                    if part == 2:
                                        S.op("act", lambda: nc.scalar.activation(out=vfm[:, 0:CTX], in_=acc[:, 2:2 + CTX], func=AF.Silu), reads=[b_acc], writes=[b_vfm])
                                        S.op("act", lambda: nc.scalar.activation(out=vfm[:, CTX:T], in_=acc[:, 6 + CTX:6 + T], func=AF.Silu), reads=[b_acc], writes=[b_vfm])
                                        srcs = [(vfm, b_vfm, v_tm[hh])]
                                    else:
                                        S.op("act", lambda: nc.scalar.activation(out=xs[:, 0:CTX], in_=acc[:, 2:2 + CTX], func=AF.Silu), reads=[b_acc], writes=[b_xs])
                                        S.op("act", lambda: nc.scalar.activation(out=xs[:, CTX:T], in_=acc[:, 6 + CTX:6 + T], func=AF.Silu), reads=[b_acc], writes=[b_xs])
                                        S.op("act", lambda: nc.scalar.activation(out=sqb[:], in_=xs[:], func=AF.Square), reads=[b_xs], writes=[b_sqb])
                                        dst, b_dst = (q_fm if part == 0 else k_fm)[hh]
                                        for gi, (t0, n) in enumerate(GROUPS):
                                            pb = 2 + gi % 2
                                            S.op("pe", lambda pb=pb: nc.tensor.matmul(PS[pb][:, 0:n], lhsT=onesb[:], rhs=sqb[:, t0:t0 + n], start=True, stop=True), reads=[b_onesb, b_sqb], writes=[PB[pb]])
                                            S.op("act", lambda pb=pb: nc.scalar.activation(out=r0[:, 0:n], in_=PS[pb][:, 0:n], func=AF.Sqrt, bias=epsb[:, 0:1]), reads=[PB[pb], b_eps], writes=[b_r0])
                                            S.op("dve", lambda: nc.vector.reciprocal(out=r1[:, 0:n], in_=r0[:, 0:n]), reads=[b_r0], writes=[b_r1])
                                            S.op("dve", lambda dst=dst: nc.vector.scalar_tensor_tensor(out=dst[:, t0:t0 + n], in0=xs[:, t0:t0 + n], scalar=(128 ** -0.5 if part == 0 else 1.0), in1=r1[:, 0:n],
                                                                                                   op0=ALU.mult, op1=ALU.mult), reads=[b_xs, b_r1], writes=[b_dst])
                                        srcs = [(dst, b_dst, k_tm[hh])] if part == 1 else []
                                    for (src, b_src, (dtm, b_dtm)) in srcs:
                                        pvb = PS[4][:].bitcast(BF16)
                                        pvb2 = PS[5][:].bitcast(BF16)
                                        for c8 in range(0, NCH, 8):
                                            nb = min(8, NCH - c8)
                                            pv = pvb if (c8 // 8) % 2 == 0 else pvb2
                                            pbi = 4 + (c8 // 8) % 2
                                            for j in range(nb):
                                                c = c8 + j
                                                S.op("pe", lambda c=c, j=j, pv=pv, src=src: nc.tensor.transpose(out=pv[0:64, j * 128:(j + 1) * 128], in_=src[:, c * 64:(c + 1) * 64], identity=identb[:]),
                                                     reads=[b_src, b_identb], writes=[PB[pbi]])
                                            S.op("act", lambda c8=c8, nb=nb, pv=pv, dtm=dtm: nc.scalar.copy(out=dtm[:, c8:c8 + nb, :], in_=pv[0:64, 0:nb * 128].rearrange("p (j x) -> p j x", j=nb)),
                                                 reads=[PB[pbi]], writes=[b_dtm])
                            S.barrier()
                        if cfg.get("gdn_stop") == 2:
                            S.barrier()
                            return
                        with contextlib.ExitStack() as st2:
                            NCB = 2
                            NB = NCB * 4
                            mistF, b_mistF = tl(st2, "g_mistF", [64, NCB, 2, 2, 64], F32)
                            mastF, b_mastF = tl(st2, "g_mastF", [64, NCB, 2, 2, 64], F32)
                            for cj in range(NCB):
                                for hh in range(2):
                                    S.op("dve", lambda cj=cj, hh=hh: nc.vector.tensor_copy(out=mistF[:, cj, :, hh, :], in_=mist[:]), reads=[b_mist], writes=[b_mistF])
                                    S.op("dve", lambda cj=cj, hh=hh: nc.vector.tensor_copy(out=mastF[:, cj, :, hh, :], in_=mast[:]), reads=[b_mast], writes=[b_mastF])
                            fl = lambda t_: t_[:].rearrange("p b x -> p (b x)")
                            v3 = lambda t_: t_[:].rearrange("p (cs h) x -> p cs h x", h=2)
                            mI3 = mistF[:].rearrange("p c s h x -> p (c s) h x")
                            mA3 = mastF[:].rearrange("p c s h x -> p (c s) h x")
                            lI, b_lI = tl(st2, "g_lI", [64, NB, 64], F32)
                            lA, b_lA = tl(st2, "g_lA", [64, NB, 64], F32)
                            Dec, b_Dec = tl(st2, "g_Dec", [64, NB, 64], F32)
                            DecT, b_DecT = tl(st2, "g_DecT", [64, NB, 64], F32)
                            t1, b_t1 = tl(st2, "g_t1", [64, NB, 64], F32)
                            Pm = [tl(st2, "g_P%d" % i, [64, NB, 64], F32) for i in range(2)]
                            Qm = [tl(st2, "g_Q%d" % i, [64, NB, 64], F32) for i in range(2)]
                            Rm = [tl(st2, "g_R%d" % i, [64, NB, 64], F32) for i in range(2)]

                            def gcols(tile_, c0):
                                return tile_[:, c0:c0 + NCB, :].rearrange("p c (s h) -> p (c s) h", s=2)[:, :, pr * 2:pr * 2 + 2]

                            for c0 in range(0, NCH, NCB):
                                for cj in range(NCB):
                                    c = c0 + cj
                                    cs = slice(c * 64, (c + 1) * 64)
                                    for b in range(4):
                                        hh = b % 2
                                        bb = cj * 4 + b
                                        kf, b_kf = k_fm[hh]
                                        qf, b_qf = q_fm[hh]
                                        S.op("pe", lambda bb=bb, kf=kf, cs=cs: nc.tensor.matmul(PS[0][0:64, bb * 64:(bb + 1) * 64], lhsT=kf[:, cs], rhs=kf[:, cs], start=True, stop=True), reads=[b_kf], writes=[PB[0]])
                                        S.op("pe", lambda bb=bb, kf=kf, qf=qf, cs=cs: nc.tensor.matmul(PS[7][0:64, bb * 64:(bb + 1) * 64], lhsT=kf[:, cs], rhs=qf[:, cs], start=True, stop=True),
                                             reads=[b_kf, b_qf], writes=[PB[7]])
                                g3 = gcols(g_t, c0).unsqueeze(3).to_broadcast([64, 2 * NCB, 2, 64])
                                S.op("dve", lambda g3=g3: nc.vector.tensor_tensor(out=v3(lI), in0=mI3, in1=g3, op=ALU.mult), reads=[b_mistF, b_g], writes=[b_lI])
                                S.op("dve", lambda g3=g3: nc.vector.tensor_tensor(out=v3(lA), in0=mA3, in1=g3, op=ALU.mult), reads=[b_mastF, b_g], writes=[b_lA])
                                for bb in range(NB):
                                    sd = (bb % 4) // 2
                                    S.op("pe", lambda bb=bb, sd=sd: nc.tensor.matmul(PS[1][0:64, bb * 64:(bb + 1) * 64], lhsT=lI[:, bb, :], rhs=mast[:, sd, :], start=True, stop=True), reads=[b_lI, b_mast], writes=[PB[1]])
                                    S.op("pe", lambda bb=bb, sd=sd: nc.tensor.matmul(PS[2][0:64, bb * 64:(bb + 1) * 64], lhsT=lA[:, bb, :], rhs=mist[:, sd, :], start=True, stop=True), reads=[b_lA, b_mist], writes=[PB[2]])
                                S.op("act", lambda: nc.scalar.activation(out=fl(Dec), in_=PS[1][0:64, :], func=AF.Exp), reads=[PB[1]], writes=[b_Dec])
                                S.op("act", lambda: nc.scalar.activation(out=fl(DecT), in_=PS[2][0:64, :], func=AF.Exp), reads=[PB[2]], writes=[b_DecT])
                                P0, b_P0 = Pm[0]
                                Q0, b_Q0 = Qm[0]
                                R0, b_R0 = Rm[0]
                                S.op("dve", lambda: nc.vector.tensor_tensor(out=fl(t1), in0=PS[0][0:64, :], in1=fl(Dec), op=ALU.mult), reads=[PB[0], b_Dec], writes=[b_t1])
                                S.op("dve", lambda: nc.vector.tensor_tensor(out=fl(t1), in0=fl(t1), in1=mastF[:].rearrange("p c s h x -> p (c s h x)"), op=ALU.mult), reads=[b_t1, b_mastF], writes=[b_t1])
                                S.op("dve", lambda c0=c0: nc.vector.tensor_tensor(out=v3(P0), in0=v3(t1), in1=gcols(nbeta, c0).unsqueeze(3).to_broadcast([64, 2 * NCB, 2, 64]), op=ALU.mult),
                                     reads=[b_t1, b_nbeta], writes=[b_P0])
                                S.op("dve", lambda: nc.vector.tensor_tensor(out=fl(DecT), in0=PS[7][0:64, :], in1=fl(DecT), op=ALU.mult), reads=[PB[7], b_DecT], writes=[b_DecT])
                                S.op("dve", lambda c0=c0: nc.vector.tensor_tensor(out=aqkT[:, c0:c0 + NCB, :, :].rearrange("p c b x -> p (c b x)"), in0=fl(DecT), in1=mistF[:].rearrange("p c s h x -> p (c s h x)"), op=ALU.mult),
                                     reads=[b_DecT, b_mistF], writes=[b_aqkT])
                                for bb in range(NB):
                                    S.op("pe", lambda bb=bb: nc.tensor.transpose(out=PS[3][0:64, bb * 64:(bb + 1) * 64], in_=P0[:, bb, :], identity=identf[0:64, 0:64]), reads=[b_P0, b_identf], writes=[PB[3]])
                                S.op("act", lambda: nc.scalar.copy(out=fl(Q0), in_=PS[3][0:64, :]), reads=[PB[3]], writes=[b_Q0])
                                S.op("dve", lambda: nc.vector.tensor_tensor(out=R0[:], in0=Q0[:], in1=identf[0:64, 0:64].unsqueeze(1).to_broadcast([64, NB, 64]), op=ALU.add),
                                     reads=[b_Q0, b_identf], writes=[b_R0])
                                for k in range(1, 6):
                                    Pp, b_Pp = Pm[(k - 1) % 2]
                                    Qp, b_Qp = Qm[(k - 1) % 2]
                                    Rp, b_Rp = Rm[(k - 1) % 2]
                                    Pn, b_Pn = Pm[k % 2]
                                    Qn, b_Qn = Qm[k % 2]
                                    Rn, b_Rn = Rm[k % 2]
                                    for bb in range(NB):
                                        S.op("pe", lambda bb=bb: nc.tensor.matmul(PS[4][0:64, bb * 64:(bb + 1) * 64], lhsT=Qp[:, bb, :], rhs=Pp[:, bb, :], start=True, stop=True), reads=[b_Qp, b_Pp], writes=[PB[4]])
                                    if k < 5:
                                        for bb in range(NB):
                                            S.op("pe", lambda bb=bb: nc.tensor.matmul(PS[5][0:64, bb * 64:(bb + 1) * 64], lhsT=Pp[:, bb, :], rhs=Qp[:, bb, :], start=True, stop=True), reads=[b_Qp, b_Pp], writes=[PB[5]])
                                    S.op("act", lambda: nc.scalar.copy(out=fl(Pn), in_=PS[4][0:64, :]), reads=[PB[4]], writes=[b_Pn])
                                    if k < 5:
                                        S.op("dve", lambda: nc.vector.tensor_copy(out=fl(Qn), in_=PS[5][0:64, :]), reads=[PB[5]], writes=[b_Qn])
                                    for bb in range(NB):
                                        S.op("pe", lambda bb=bb: nc.tensor.matmul(PS[6][0:64, bb * 64:(bb + 1) * 64], lhsT=Pn[:, bb, :], rhs=Rp[:, bb, :], start=True, stop=True), reads=[b_Pn, b_Rp], writes=[PB[6]])
                                    S.op("dve", lambda: nc.vector.tensor_tensor(out=fl(Rn), in0=fl(Rp), in1=PS[6][0:64, :], op=ALU.add), reads=[b_Rp, PB[6]], writes=[b_Rn])
                                    if k == 5:
                                        S.op("dve", lambda c0=c0: nc.vector.tensor_tensor(out=R5b[:, c0:c0 + NCB, :, :].rearrange("p c (s h) x -> p (c s) h x", s=2), in0=v3(Rn), in1=gcols(beta, c0).unsqueeze(3).to_broadcast([64, 2 * NCB, 2, 64]), op=ALU.mult),
                                             reads=[b_Rn, b_beta], writes=[b_R5b])
                            S.barrier()
                        if cfg.get("gdn_stop") == 3:
                            S.barrier()
                            return
                        with contextlib.ExitStack() as st2:
                            Sst = [tl(st2, "g_S%d" % i, [128, 128], F32) for i in range(4)]
                            Sbb = [[tl(st2, "g_Sb%d_%d" % (i, j), [128, 128], BF16) for j in range(2)] for i in range(4)]
                            Xs = [tl(st2, "g_X%d" % i, [64, 128], BF16) for i in range(4)]
                            vnb = [tl(st2, "g_vn%d" % i, [64, 128], BF16) for i in range(4)]
                            vnk = [tl(st2, "g_vk%d" % i, [64, 128], BF16) for i in range(4)]
                            tmpo = [tl(st2, "g_to%d" % i, [64, 128], F32) for i in range(4)]
                            oo = [[tl(st2, "g_oo%d_%d" % (i, j), [64, 128], F32) for j in range(2)] for i in range(4)]
                            b_raw = Buf("gdn_raw")
                            orders = [list(range(NCH)), [3, 2, 1, 0] + list(range(NCH - 1, 3, -1))]
                            for idx in range(NCH):
                                ch = []
                                for b in range(4):
                                    sd, hh = b // 2, b % 2
                                    hd = pr * 2 + hh
                                    c = orders[sd][idx]
                                    ch.append(dict(b=b, sd=sd, hh=hh, hd=hd, col=sd * 4 + hd, c=c, cs=slice(c * 64, (c + 1) * 64), pa=2 * b, pc=2 * b + 1,
                                                   kf=k_fm[hh], qf=q_fm[hh], vt=v_tm[hh], ktm=k_tm[hh], X=Xs[b], vn=vnb[b], vk=vnk[b], to=tmpo[b], o=oo[b][idx % 2], S=Sst[b],
                                                   sb=Sbb[b][idx % 2], nsb=Sbb[b][(idx + 1) % 2]))
                                if idx > 0:
                                    for d in ch:
                                        S.op("pe", lambda d=d: nc.tensor.matmul(PS[d["pa"]][0:64, 0:128], lhsT=d["kf"][0][:, d["cs"]], rhs=d["sb"][0][:], start=True, stop=True), reads=[d["kf"][1], d["sb"][1]], writes=[PB[d["pa"]]])
                                        S.op("pe", lambda d=d: nc.tensor.matmul(PS[d["pa"]][0:64, 128:256], lhsT=d["qf"][0][:, d["cs"]], rhs=d["sb"][0][:], start=True, stop=True), reads=[d["qf"][1], d["sb"][1]], writes=[PB[d["pa"]]])
                                    for d in ch:
                                        S.op("dve", lambda d=d: nc.vector.scalar_tensor_tensor(out=d["X"][0][:], in0=PS[d["pa"]][0:64, 0:128], scalar=negc[:, d["c"], d["col"]:d["col"] + 1], in1=d["vt"][0][:, d["c"], :], op0=ALU.mult, op1=ALU.add),
                                             reads=[PB[d["pa"]], b_negc, d["vt"][1]], writes=[d["X"][1]])
                                for d in ch:
                                    if idx > 0:
                                        xin_ap, xr = d["X"][0][:], [d["X"][1]]
                                    else:
                                        xin_ap, xr = d["vt"][0][:, d["c"], :], [d["vt"][1]]
                                    S.op("pe", lambda d=d, xin_ap=xin_ap: nc.tensor.matmul(PS[d["pa"]][0:64, 256:384], lhsT=R5b[:, d["c"], d["b"], :], rhs=xin_ap, start=True, stop=True), reads=[b_R5b] + xr, writes=[PB[d["pa"]]])
                                for d in ch:
                                    S.op("act", lambda d=d: nc.scalar.copy(out=d["vn"][0][:], in_=PS[d["pa"]][0:64, 256:384]), reads=[PB[d["pa"]]], writes=[d["vn"][1]])
                                    S.op("dve", lambda d=d: nc.vector.tensor_scalar(out=d["vk"][0][:], in0=PS[d["pa"]][0:64, 256:384], scalar1=ekd[:, d["c"], d["col"]:d["col"] + 1], scalar2=None, op0=ALU.mult), reads=[PB[d["pa"]], b_ekd], writes=[d["vk"][1]])
                                for d in ch:
                                    S.op("pe", lambda d=d: nc.tensor.matmul(PS[d["pa"]][0:64, 384:512], lhsT=aqkT[:, d["c"], d["b"], :], rhs=d["vn"][0][:], start=True, stop=True), reads=[b_aqkT, d["vn"][1]], writes=[PB[d["pa"]]])
                                    if idx < NCH - 1:
                                        S.op("pe", lambda d=d: nc.tensor.matmul(PS[d["pc"]][:, 0:128], lhsT=d["ktm"][0][:, d["c"], :], rhs=d["vk"][0][:], start=True, stop=True), reads=[d["ktm"][1], d["vk"][1]], writes=[PB[d["pc"]]])
                                if idx < NCH - 1:
                                    for d in ch:
                                        if idx == 0:
                                            S.op("dve", lambda d=d: nc.vector.tensor_copy(out=d["S"][0][:], in_=PS[d["pc"]][:, 0:128]), reads=[PB[d["pc"]]], writes=[d["S"][1]])
                                        else:
                                            S.op("dve", lambda d=d: nc.vector.scalar_tensor_tensor(out=d["S"][0][:], in0=d["S"][0][:], scalar=egl[:, d["c"], d["col"]:d["col"] + 1], in1=PS[d["pc"]][:, 0:128], op0=ALU.mult, op1=ALU.add),
                                                 reads=[d["S"][1], b_egl, PB[d["pc"]]], writes=[d["S"][1]])
                                    for d in ch:
                                        S.op("act", lambda d=d: nc.scalar.copy(out=d["nsb"][0][:], in_=d["S"][0][:]), reads=[d["S"][1]], writes=[d["nsb"][1]])
                                for d in ch:
                                    if idx > 0:
                                        S.op("act", lambda d=d: nc.scalar.copy(out=d["to"][0][:], in_=PS[d["pa"]][0:64, 384:512]), reads=[PB[d["pa"]]], writes=[d["to"][1]])
                                        S.op("dve", lambda d=d: nc.vector.scalar_tensor_tensor(out=d["o"][0][:], in0=PS[d["pa"]][0:64, 128:256], scalar=egc[:, d["c"], d["col"]:d["col"] + 1], in1=d["to"][0][:], op0=ALU.mult, op1=ALU.add),
                                             reads=[PB[d["pa"]], b_egc, d["to"][1]], writes=[d["o"][1]])
                                    else:
                                        S.op("act", lambda d=d: nc.scalar.copy(out=d["o"][0][:], in_=PS[d["pa"]][0:64, 384:512]), reads=[PB[d["pa"]]], writes=[d["o"][1]])
                                    S.dma(gdn_raw_d[d["sd"], d["c"] * 64:(d["c"] + 1) * 64, d["hd"] * 128:(d["hd"] + 1) * 128], d["o"][0][:], reads=[d["o"][1]], writes=[b_raw])
                            S.barrier()
                if cfg.get("gdn_stop") == 4:
                    S.barrier()
                    return
                with contextlib.ExitStack() as st:
                    wgg, b_wgg = tl(st, "g_wgg", [128, KC, 512], BF16)
                    S.dma(wgg[:], winv[:, :, O_GG:O_GG + 512], writes=[b_wgg], q="pool")
                    gnw, b_gnw = tl(st, "g_gnw", [128, 128], F32)
                    S.dma(gnw[:], I["gdn_norm"][l:l + 1, :].partition_broadcast(128), writes=[b_gnw])
                    of_ = [tl(st, "g_of%d" % i, [128, 512], F32) for i in range(2)]
                    ob_ = [tl(st, "g_ob%d" % i, [128, 512], F32) for i in range(2)]
                    sqt = [tl(st, "g_sqt%d" % i, [128, 512], F32) for i in range(2)]
                    gt_ = [tl(st, "g_gt%d" % i, [128, 512], F32) for i in range(2)]
                    ms = [tl(st, "g_ms%d" % i, [128, 4], F32) for i in range(2)]
                    obf = [tl(st, "g_obf%d" % i, [128, 512], BF16) for i in range(2)]
                    ofm = [tl(st, "g_ofm%d" % i, [128, 4, 128], BF16) for i in range(2)]
                    b_go = Buf("gdn_o")
                    hsub = cfg.get("gdn_hsub", 99)
                    for i in range(cfg.get("gdn_hnt", NT)):
                        a, b_a = of_[i % 2]
                        bb, b_bb = ob_[i % 2]
                        sq_t, b_sq = sqt[i % 2]
                        g_tl, b_gt = gt_[i % 2]
                        ms_t, b_ms = ms[i % 2]
                        obf_t, b_obf = obf[i % 2]
                        ofm_t, b_ofm = ofm[i % 2]
                        ts_ = slice(i * 128, (i + 1) * 128)
                        S.dma(a[:], gdn_raw_d[0, ts_, :], writes=[b_a])
                        S.dma(bb[:], gdn_raw_d[1, ts_, :], writes=[b_bb])
                        pb = i % 2
                        for kc in range(KC):
                            S.op("pe", lambda kc=kc, pb=pb: nc.tensor.matmul(PS[pb][:, :], lhsT=h_fm[:, kc, ts_], rhs=wgg[:, kc, :], start=(kc == 0), stop=(kc == KC - 1)), reads=[b_hfm, b_wgg], writes=[PB[pb]])
                        S.op("act", lambda: nc.scalar.activation(out=g_tl[:], in_=PS[pb][:, :], func=AF.Silu), reads=[PB[pb]], writes=[b_gt])
                        if hsub < 1:
                            continue
                        S.op("dve", lambda: nc.vector.tensor_tensor(out=a[:], in0=a[:], in1=bb[:], op=ALU.add), reads=[b_a, b_bb], writes=[b_a])
                        S.op("act", lambda: nc.scalar.activation(out=sq_t[:], in_=a[:], func=AF.Square), reads=[b_a], writes=[b_sq])
                        S.op("dve", lambda: nc.vector.tensor_reduce(out=ms_t[:], in_=sq_t[:].rearrange("p (h x) -> p h x", h=4), axis=AX.X, op=ALU.add), reads=[b_sq], writes=[b_ms])
                        S.op("act", lambda: nc.scalar.activation(out=ms_t[:], in_=ms_t[:], func=AF.Sqrt, scale=1.0 / 128, bias=epsb[:, 0:1]), reads=[b_ms, b_eps], writes=[b_ms])
                        S.op("dve", lambda: nc.vector.reciprocal(out=ms_t[:], in_=ms_t[:]), reads=[b_ms], writes=[b_ms])
                        if hsub < 2:
                            continue
                        a3 = a[:].rearrange("p (h x) -> p h x", h=4)
                        S.op("dve", lambda: nc.vector.tensor_tensor(out=a3, in0=a3, in1=ms_t[:].unsqueeze(2).to_broadcast([128, 4, 128]), op=ALU.mult), reads=[b_a, b_ms], writes=[b_a])
                        S.op("dve", lambda: nc.vector.tensor_tensor(out=a3, in0=a3, in1=gnw[:].unsqueeze(1).to_broadcast([128, 4, 128]), op=ALU.mult), reads=[b_a, b_gnw], writes=[b_a])
                        S.op("dve", lambda: nc.vector.tensor_tensor(out=obf_t[:], in0=a[:], in1=g_tl[:], op=ALU.mult), reads=[b_a, b_gt], writes=[b_obf])
                        if hsub < 3:
                            continue
                        pv = PS[2 + pb][:].bitcast(BF16)
                        for hd in range(4):
                            S.op("pe", lambda hd=hd: nc.tensor.transpose(out=pv[:, hd * 128:(hd + 1) * 128], in_=obf_t[:, hd * 128:(hd + 1) * 128], identity=identb[:]), reads=[b_obf, b_identb], writes=[PB[2 + pb]])
                        S.op("act", lambda: nc.scalar.copy(out=ofm_t[:], in_=pv[:, 0:512].rearrange("p (h x) -> p h x", h=4)), reads=[PB[2 + pb]], writes=[b_ofm])
                        if hsub < 4:
                            continue
                        S.dma(gdn_o_d[:, :, ts_].rearrange("h d t -> d h t"), ofm_t[:], reads=[b_ofm], writes=[b_go])
                    S.barrier()


        def ln_tile(st_tiles, i, f_halves, f_bufs, prm, out_final):
            s = 1 if i < 2 else 0
            x_t, b_x = st_tiles["x"][i % 2]
            t_t, b_t = st_tiles["t"][i % 2]
            h_t, b_h = st_tiles["h"][i % 2]
            stt, b_st = st_tiles["st"][i % 2]
            mv, b_mv = st_tiles["mv"][i % 2]
            ts_ = slice(i * 128, (i + 1) * 128)
            S.dma(x_t[:], xres_d[ts_, :], reads=[b_xres[i]], writes=[b_x])
            gate_t, b_gate = prm["gate"][s]
            for hf in range(2):
                hs = slice(hf * 512, (hf + 1) * 512)
                S.op("dve", lambda hf=hf, hs=hs: nc.vector.tensor_tensor(out=t_t[:, hs], in0=f_halves[hf], in1=gate_t[:, hs], op=ALU.mult), reads=[f_bufs[hf], b_gate], writes=[b_t])
            S.op("dve", lambda: nc.vector.scalar_tensor_tensor(out=x_t[:], in0=x_t[:], scalar=ALPHA, in1=t_t[:], op0=ALU.mult, op1=ALU.add), reads=[b_x, b_t], writes=[b_x])
            for hf in range(2):
                S.op("dve", lambda hf=hf: nc.vector.bn_stats(out=stt[:, hf, :], in_=x_t[:, hf * 512:(hf + 1) * 512]), reads=[b_x], writes=[b_st])
            S.op("dve", lambda: nc.vector.bn_aggr(out=mv[:, 0:2], in_=stt[:].rearrange("p a b -> p (a b)")), reads=[b_st], writes=[b_mv])
            S.op("act", lambda: nc.scalar.activation(out=mv[:, 2:3], in_=mv[:, 1:2], func=AF.Sqrt, bias=epsb[:, 0:1]), reads=[b_mv, b_eps], writes=[b_mv])
            S.op("dve", lambda: nc.vector.reciprocal(out=mv[:, 3:4], in_=mv[:, 2:3]), reads=[b_mv], writes=[b_mv])
            S.op("dve", lambda: nc.vector.tensor_scalar(out=x_t[:], in0=x_t[:], scalar1=mv[:, 0:1], scalar2=mv[:, 3:4], op0=ALU.subtract, op1=ALU.mult), reads=[b_x, b_mv], writes=[b_x])
            S.op("pool", lambda: nc.gpsimd.tensor_tensor(out=x_t[:], in0=x_t[:], in1=prm["g"][0][:], op=ALU.mult), reads=[b_x, prm["g"][1]], writes=[b_x])
            S.op("pool", lambda: nc.gpsimd.tensor_tensor(out=x_t[:], in0=x_t[:], in1=prm["b"][0][:], op=ALU.add), reads=[b_x, prm["b"][1]], writes=[b_x])
            if out_final:
                S.dma(out_d[(i - 2) * 128:(i - 1) * 128, :], x_t[:], reads=[b_x])
                return
            S.dma(xres_d[ts_, :], x_t[:], reads=[b_x], writes=[b_xres[i]])
            sc_t, b_sc = prm["sc"][s]
            sh_t, b_sh = prm["sh"][s]
            S.op("dve", lambda: nc.vector.tensor_tensor(out=t_t[:], in0=x_t[:], in1=sc_t[:], op=ALU.mult), reads=[b_x, b_sc], writes=[b_t])
            S.op("pool", lambda: nc.gpsimd.tensor_tensor(out=h_t[:], in0=t_t[:], in1=sh_t[:], op=ALU.add), reads=[b_t, b_sh], writes=[b_h])
            to_fm(h_t, b_h, i, 6 + i % 2)

        def ln_setup(st, l_mod, jgate, ln_g, ln_b, l, jsh, jsc, need_mod):
            tiles = {
                "x": [tl(st, "ln_x%d" % i, [128, D], F32) for i in range(2)],
                "t": [tl(st, "ln_t%d" % i, [128, D], F32) for i in range(2)],
                "h": [tl(st, "ln_h%d" % i, [128, D], BF16) for i in range(2)],
                "st": [tl(st, "ln_st%d" % i, [128, 2, 6], F32) for i in range(2)],
                "mv": [tl(st, "ln_mv%d" % i, [128, 4], F32) for i in range(2)],
            }
            prm = {"gate": [load_bc(st, "ln_gate%d" % s_, l, s_, jgate) for s_ in range(2)],
                   "g": load_vec_bc(st, "ln_g", I[ln_g][l:l + 1, :]),
                   "b": load_vec_bc(st, "ln_b", I[ln_b][l:l + 1, :])}
            if need_mod:
                prm["sh"] = [load_bc(st, "ln_sh%d" % s_, l_mod, s_, jsh) for s_ in range(2)]
                prm["sc"] = [load_bc(st, "ln_sc%d" % s_, l_mod, s_, jsc, plus_one=True) for s_ in range(2)]
            return tiles, prm

        def stage_merge(l, last):
            winv = I["w_in"][l].rearrange("(kc p) n -> p kc n", p=128)
            groups = GROUPS[1:] if last else GROUPS
            tiles_i = range(2, NT) if last else range(NT)
            with contextlib.ExitStack() as st:
                y_fm, b_y = tl(st, "y_fm", [128, KC, T], BF16)
                with contextlib.ExitStack() as st2:
                    mo, b_mo = tl(st2, "m_mo", [64, 8, T], BF16)
                    ho, b_ho = tl(st2, "m_ho", [128, 4, T], BF16)
                    go, b_go = tl(st2, "m_go", [128, 4, T], BF16)
                    S.dma(mo[:], mla_o_d.rearrange("h d t -> d h t"), writes=[b_mo])
                    S.dma(ho[:], hg_o_d.rearrange("h d t -> d h t"), writes=[b_ho])
                    S.dma(go[:], gdn_o_d.rearrange("h d t -> d h t"), writes=[b_go])
                    wgt = [tl(st2, "m_wg%d" % i, [128, KC, 3, 128], BF16) for i in range(2)]
                    wbr = [tl(st2, "m_wbr%d" % i, [128, 2, 4, 128], BF16) for i in range(2)]
                    wbm = [tl(st2, "m_wbm%d" % i, [64, 8, 128], BF16) for i in range(2)]
                    sg = [tl(st2, "m_sg%d" % i, [128, 512], F32) for i in range(3)]
                    ta, b_ta = tl(st2, "m_ta", [128, 512], F32)
                    tb, b_tb = tl(st2, "m_tb", [128, 512], F32)
                    mcnt = [0]
                    for dc in range(KC):
                        wg_t, b_wg = wgt[dc % 2]
                        wbr_t, b_wbr = wbr[dc % 2]
                        wbm_t, b_wbm = wbm[dc % 2]
                        for n_ in range(3):
                            c0 = O_GATES + n_ * D + dc * 128
                            S.dma(wg_t[:, :, n_, :], winv[:, :, c0:c0 + 128], writes=[b_wg], q="pool")
                        for n_ in range(2):
                            S.dma(wbr_t[:, n_, :, :], I["w_branch"][l, n_ + 1].rearrange("(kc p) n -> p kc n", p=128)[:, :, dc * 128:(dc + 1) * 128], writes=[b_wbr], q="pool")
                        S.dma(wbm_t[:], I["w_branch"][l, 0].rearrange("(h p) n -> p h n", p=64)[:, :, dc * 128:(dc + 1) * 128], writes=[b_wbm], q="pool")
                        for (t0, n) in groups:
                            for n_ in range(3):
                                pg = mcnt[0] % 4
                                pp = 4 + mcnt[0] % 4
                                mcnt[0] += 1
                                for kc in range(KC):
                                    S.op("pe", lambda kc=kc, n_=n_, pg=pg: nc.tensor.matmul(PS[pg][:, 0:n], lhsT=wg_t[:, kc, n_, :], rhs=h_fm[:, kc, t0:t0 + n], start=(kc == 0), stop=(kc == KC - 1)),
                                         reads=[b_wg, b_hfm], writes=[PB[pg]])
                                S.op("act", lambda n_=n_, pg=pg: nc.scalar.activation(out=sg[n_][0][:, 0:n], in_=PS[pg][:, 0:n], func=AF.Sigmoid), reads=[PB[pg]], writes=[sg[n_][1]])
                                if n_ == 0:
                                    for h in range(8):
                                        S.op("pe", lambda h=h, pp=pp: nc.tensor.matmul(PS[pp][:, 0:n], lhsT=wbm_t[:, h, :], rhs=mo[:, h, t0:t0 + n], start=(h == 0), stop=(h == 7)), reads=[b_wbm, b_mo], writes=[PB[pp]])
                                else:
                                    src, b_src = (ho, b_ho) if n_ == 1 else (go, b_go)
                                    for kc in range(4):
                                        S.op("pe", lambda kc=kc, pp=pp, src=src, n_=n_: nc.tensor.matmul(PS[pp][:, 0:n], lhsT=wbr_t[:, n_ - 1, kc, :], rhs=src[:, kc, t0:t0 + n], start=(kc == 0), stop=(kc == 3)), reads=[b_wbr, b_src], writes=[PB[pp]])
                                if n_ == 0:
                                    S.op("dve", lambda pp=pp: nc.vector.tensor_tensor(out=ta[:, 0:n], in0=sg[0][0][:, 0:n], in1=PS[pp][:, 0:n], op=ALU.mult), reads=[sg[0][1], PB[pp]], writes=[b_ta])
                                elif n_ == 1:
                                    S.op("dve", lambda pp=pp: nc.vector.tensor_tensor(out=tb[:, 0:n], in0=sg[1][0][:, 0:n], in1=PS[pp][:, 0:n], op=ALU.mult), reads=[sg[1][1], PB[pp]], writes=[b_tb])
                                    S.op("pool", lambda: nc.gpsimd.tensor_tensor(out=ta[:, 0:n], in0=ta[:, 0:n], in1=tb[:, 0:n], op=ALU.add), reads=[b_ta, b_tb], writes=[b_ta])
                                else:
                                    S.op("dve", lambda pp=pp: nc.vector.tensor_tensor(out=tb[:, 0:n], in0=sg[2][0][:, 0:n], in1=PS[pp][:, 0:n], op=ALU.mult), reads=[sg[2][1], PB[pp], b_ta], writes=[b_tb])
                                    S.op("dve", lambda dc=dc: nc.vector.tensor_tensor(out=y_fm[:, dc, t0:t0 + n], in0=ta[:, 0:n], in1=tb[:, 0:n], op=ALU.add), reads=[b_ta, b_tb], writes=[b_y])
                    S.barrier()
                if "y_fm" in DBG:
                    S.dma(DBG["y_fm"].rearrange("(kc p) t -> p kc t", p=128), y_fm[:], reads=[b_y], q="pool")
                wo, b_wo = tl(st, "m_wo", [128, KC, D], BF16)
                S.dma(wo[:], I["w_out"][l].rearrange("(kc p) n -> p kc n", p=128), writes=[b_wo], q="pool")
                tiles, prm = ln_setup(st, l, 2, "ln1_g", "ln1_b", l, 3, 4, True)
                for i in tiles_i:
                    ts_ = slice(i * 128, (i + 1) * 128)
                    for hf in range(2):
                        pb = 4 + hf
                        for kc in range(KC):
                            S.op("pe", lambda kc=kc, hf=hf, pb=pb: nc.tensor.matmul(PS[pb][:, :], lhsT=y_fm[:, kc, ts_], rhs=wo[:, kc, hf * 512:(hf + 1) * 512], start=(kc == 0), stop=(kc == KC - 1)),
                                 reads=[b_y, b_wo], writes=[PB[pb]])
                    ln_tile(tiles, i, [PS[4][:, :], PS[5][:, :]], [PB[4], PB[5]], prm, False)
                S.barrier()

        def stage_moe(l, last):
            groups = GROUPS[1:] if last else GROUPS
            tiles_i = list(range(2, NT)) if last else list(range(NT))
            with contextlib.ExitStack() as st:
                acc, b_acc = tl(st, "acc", [128, NT, D], F32)
                comb, b_comb = tl(st, "comb", [128, NT, 65], F32)
                S.op("dve", lambda: nc.vector.memset(comb[:, :, 64:65], 1.0), writes=[b_comb])
                with contextlib.ExitStack() as st2:
                    wr, b_wr = tl(st2, "wr", [128, KC, 64], BF16)
                    S.dma(wr[:], I["w_router"][l].rearrange("(kc p) n -> p kc n", p=128), writes=[b_wr], q="pool")
                    rb, b_rb = load_vec_bc(st2, "rb", I["router_bias"][l:l + 1, :], n=64)
                    sc_ = [tl(st2, "r_sc%d" % i, [128, 64], F32) for i in range(2)]
                    sel = [tl(st2, "r_sel%d" % i, [128, 64], F32) for i in range(2)]
                    selm = [tl(st2, "r_selm%d" % i, [128, 64], F32) for i in range(2)]
                    m8 = [tl(st2, "r_m8%d" % i, [128, 8, 8], F32) for i in range(2)]
                    sm = [tl(st2, "r_sm%d" % i, [128, 40], F32) for i in range(2)]
                    for i in tiles_i:
                        ts_ = slice(i * 128, (i + 1) * 128)
                        pb = i % 2
                        sc_t, b_sc = sc_[i % 2]
                        sel_t, b_sel = sel[i % 2]
                        selm_t, b_selm = selm[i % 2]
                        m8_t, b_m8 = m8[i % 2]
                        sm_t, b_sm = sm[i % 2]
                        for kc in range(KC):
                            S.op("pe", lambda kc=kc: nc.tensor.matmul(PS[pb][:, 0:64], lhsT=h_fm[:, kc, ts_], rhs=wr[:, kc, :], start=(kc == 0), stop=(kc == KC - 1)), reads=[b_hfm, b_wr], writes=[PB[pb]])
                        S.op("act", lambda: nc.scalar.activation(out=sc_t[:], in_=PS[pb][:, 0:64], func=AF.Sigmoid), reads=[PB[pb]], writes=[b_sc])
                        S.op("dve", lambda: nc.vector.tensor_tensor(out=sel_t[:], in0=sc_t[:], in1=rb[:], op=ALU.add), reads=[b_sc, b_rb], writes=[b_sel])
                        for g8 in range(8):
                            S.op("dve", lambda g8=g8: nc.vector.max(out=m8_t[:, g8, :], in_=sel_t[:, g8 * 8:(g8 + 1) * 8]), reads=[b_sel], writes=[b_m8])
                        gs = sm_t[:, 0:8]
                        gm8 = sm_t[:, 8:16]
                        gmask = sm_t[:, 16:24]
                        pen = sm_t[:, 24:32]
                        t8 = sm_t[:, 32:40]
                        S.op("dve", lambda: nc.vector.tensor_tensor(out=gs, in0=m8_t[:, :, 0], in1=m8_t[:, :, 1], op=ALU.add), reads=[b_m8], writes=[b_sm])
                        S.op("dve", lambda: nc.vector.max(out=gm8, in_=gs), reads=[b_sm], writes=[b_sm])
                        S.op("dve", lambda: nc.vector.tensor_scalar(out=gmask, in0=gs, scalar1=sm_t[:, 11:12], scalar2=None, op0=ALU.is_ge), reads=[b_sm], writes=[b_sm])
                        S.op("dve", lambda: nc.vector.tensor_scalar(out=pen, in0=gmask, scalar1=10.0, scalar2=-10.0, op0=ALU.mult, op1=ALU.add), reads=[b_sm], writes=[b_sm])
                        sel3 = sel_t[:].rearrange("p (g x) -> p g x", g=8)
                        selm3 = selm_t[:].rearrange("p (g x) -> p g x", g=8)
                        S.op("dve", lambda: nc.vector.tensor_tensor(out=selm3, in0=sel3, in1=gmask.unsqueeze(2).to_broadcast([128, 8, 8]), op=ALU.mult), reads=[b_sel, b_sm], writes=[b_selm])
                        S.op("dve", lambda: nc.vector.tensor_tensor(out=selm3, in0=selm3, in1=pen.unsqueeze(2).to_broadcast([128, 8, 8]), op=ALU.add), reads=[b_selm, b_sm], writes=[b_selm])
                        S.op("dve", lambda: nc.vector.max(out=t8, in_=selm_t[:]), reads=[b_selm], writes=[b_sm])
                        S.op("dve", lambda: nc.vector.tensor_scalar(out=selm_t[:], in0=selm_t[:], scalar1=sm_t[:, 39:40], scalar2=None, op0=ALU.is_ge), reads=[b_selm, b_sm], writes=[b_selm])
                        S.op("dve", lambda: nc.vector.tensor_tensor(out=sel_t[:], in0=sc_t[:], in1=selm_t[:], op=ALU.mult), reads=[b_sc, b_selm], writes=[b_sel])
                        S.op("dve", lambda: nc.vector.tensor_reduce(out=sm_t[:, 0:1], in_=sel_t[:], axis=AX.X, op=ALU.add), reads=[b_sel], writes=[b_sm])
                        S.op("dve", lambda: nc.vector.reciprocal(out=sm_t[:, 1:2], in_=sm_t[:, 0:1]), reads=[b_sm], writes=[b_sm])
                        S.op("dve", lambda i=i: nc.vector.tensor_scalar(out=comb[:, i, 0:64], in0=sel_t[:], scalar1=sm_t[:, 1:2], scalar2=2.5, op0=ALU.mult, op1=ALU.mult), reads=[b_sel, b_sm], writes=[b_comb])
                    S.barrier()
                if "comb" in DBG:
                    S.dma(DBG["comb"].rearrange("(i p) e -> p i e", p=128), comb[:], reads=[b_comb])
                with contextlib.ExitStack() as st2:
                    wgu = [tl(st2, "wgu%d" % i, [128, KC, 512], BF16) for i in range(3)]
                    wdn = [tl(st2, "wdn%d" % i, [128, 2, D], BF16) for i in range(4)]
                    sgt = [tl(st2, "e_sg%d" % i, [128, 512], F32) for i in range(2)]
                    act = [tl(st2, "e_act%d" % i, [128, 2, 512], BF16) for i in range(2)]
                    ne = cfg.get("n_experts", 65)

                    def load_e(e):
                        wg_t, b_wg = wgu[e % 3]
                        wd_t, b_wd = wdn[e % 4]
                        if e < 64:
                            S.dma(wg_t[:], I["w_gu"][l, e].rearrange("(kc p) n -> p kc n", p=128), writes=[b_wg], q="pool")
                            S.dma(wd_t[:], I["w_down"][l, e].rearrange("(kc p) n -> p kc n", p=128), writes=[b_wd], q="pool")
                        else:
                            S.dma(wg_t[:], I["w_sh_gu"][l].rearrange("(kc p) n -> p kc n", p=128), writes=[b_wg], q="pool")
                            S.dma(wd_t[:], I["w_sh_down"][l].rearrange("(kc p) n -> p kc n", p=128), writes=[b_wd], q="pool")

                    elist = list(range(64 - (ne - 1), 65)) if ne < 65 else list(range(65))
                    load_e(elist[0])
                    if len(elist) > 1:
                        load_e(elist[1])
                    gcnt = 0
                    dcnt_ = [0]
                    pend_down = [None]
                    for ei, e in enumerate(elist):
                        need_load = ei + 2 < len(elist)
                        wg_t, b_wg = wgu[e % 3]
                        wd_t, b_wd = wdn[e % 4]
                        for (t0, n) in groups:
                            act_t, b_act = act[gcnt % 2]
                            gcnt += 1
                            pend_tiles = pend_down[0] if pend_down[0] is not None else []
                            pend_down[0] = None
                            if need_load:
                                load_e(elist[ei + 2])
                                need_load = False

                            def flush(k):
                                for _ in range(k):
                                    if pend_tiles:
                                        pend_tiles.pop(0)()
                            for c in range(2):
                                pg, pu = 2 * c, 2 * c + 1
                                for kc in range(KC):
                                    S.op("pe", lambda kc=kc, c=c, pg=pg: nc.tensor.matmul(PS[pg][:, 0:n], lhsT=wg_t[:, kc, c * 128:(c + 1) * 128], rhs=h_fm[:, kc, t0:t0 + n], start=(kc == 0), stop=(kc == KC - 1)),
                                         reads=[b_wg, b_hfm], writes=[PB[pg]])
                                flush(2)
                                for kc in range(KC):
                                    S.op("pe", lambda kc=kc, c=c, pu=pu: nc.tensor.matmul(PS[pu][:, 0:n], lhsT=wg_t[:, kc, 256 + c * 128:256 + (c + 1) * 128], rhs=h_fm[:, kc, t0:t0 + n], start=(kc == 0), stop=(kc == KC - 1)),
                                         reads=[b_wg, b_hfm], writes=[PB[pu]])
                                sg_t, b_sg = sgt[c]
                                S.op("act", lambda pg=pg, sg_t=sg_t: nc.scalar.activation(out=sg_t[:, 0:n], in_=PS[pg][:, 0:n], func=AF.Silu), reads=[PB[pg]], writes=[b_sg])
                                S.op("dve", lambda c=c, pu=pu, sg_t=sg_t, act_t=act_t: nc.vector.tensor_tensor(out=act_t[:, c, 0:n], in0=sg_t[:, 0:n], in1=PS[pu][:, 0:n], op=ALU.mult), reads=[b_sg, PB[pu]], writes=[b_act])
                                flush(2)
                            flush(99)

                            def mk_tiles(t0=t0, n=n, act_t=act_t, b_act=b_act, wd_t=wd_t, b_wd=b_wd, ei=ei, e=e):
                                fs = []
                                for tt in range(n // 128):
                                    for hf in range(2):
                                        def one(tt=tt, hf=hf):
                                            i = t0 // 128 + tt
                                            pd = 4 + dcnt_[0] % 4
                                            dcnt_[0] += 1
                                            for c in range(2):
                                                S.op("pe", lambda c=c: nc.tensor.matmul(PS[pd][:, :], lhsT=act_t[:, c, tt * 128:(tt + 1) * 128], rhs=wd_t[:, c, hf * 512:(hf + 1) * 512], start=(c == 0), stop=(c == 1)),
                                                     reads=[b_act, b_wd], writes=[PB[pd]])
                                            if ei == 0:
                                                S.op("dve", lambda: nc.vector.tensor_scalar(out=acc[:, i, hf * 512:(hf + 1) * 512], in0=PS[pd][:, :], scalar1=comb[:, i, e:e + 1], scalar2=None, op0=ALU.mult),
                                                     reads=[PB[pd], b_comb], writes=[b_acc])
                                            else:
                                                S.op("dve", lambda: nc.vector.scalar_tensor_tensor(out=acc[:, i, hf * 512:(hf + 1) * 512], in0=PS[pd][:, :], scalar=comb[:, i, e:e + 1], in1=acc[:, i, hf * 512:(hf + 1) * 512], op0=ALU.mult, op1=ALU.add),
                                                     reads=[PB[pd], b_comb, b_acc], writes=[b_acc])
                                        fs.append(one)
                                return fs
                            pend_down[0] = mk_tiles()
                    if pend_down[0] is not None:
                        for f in pend_down[0]:
                            f()
                    S.barrier()
                if "ff" in DBG:
                    S.dma(DBG["ff"].rearrange("(i p) d -> p i d", p=128), acc[:], reads=[b_acc])
                final = (l == nlayers - 1)
                tiles, prm = ln_setup(st, l + 1, 5, "ln2_g", "ln2_b", l, 0, 1, not final)
                for i in tiles_i:
                    ln_tile(tiles, i, [acc[:, i, 0:512], acc[:, i, 512:1024]], [b_acc, b_acc], prm, final)
                S.barrier()

        def stage_mla(l, last):
            with contextlib.ExitStack() as st:
                winv = I["w_in"][l].rearrange("(kc p) n -> p kc n", p=128)
                wA, b_wA = tl(st, "wA", [128, KC, 672], BF16)
                S.dma(wA[:], winv[:, :, 0:672], writes=[b_wA], q="pool")
                wKs, b_wKs = tl(st, "wKs", [128, KC, 32], BF16)
                S.dma(wKs[:], I["w_kr_sw"][l].rearrange("(kc p) n -> p kc n", p=128), writes=[b_wKs], q="pool")
                wQ, b_wQ = tl(st, "wQ", [128, 3, 768], BF16)
                S.dma(wQ[:], I["w_q_b"][l].rearrange("(kc p) n -> p kc n", p=128), writes=[b_wQ], q="pool")
                wQs, b_wQs = tl(st, "wQs", [128, 3, 256], BF16)
                S.dma(wQs[:], I["w_qr_sw"][l].rearrange("(kc p) n -> p kc n", p=128), writes=[b_wQs], q="pool")
                wKV, b_wKV = tl(st, "wKV", [128, 2, 1024], BF16)
                S.dma(wKV[:], I["w_kv_b"][l].rearrange("(kc p) n -> p kc n", p=128), writes=[b_wKV], q="pool")
                gq, b_gq = tl(st, "gq", [128, 3], F32)
                S.dma(gq[:], I["q_a_norm_t"][l], writes=[b_gq])
                gkv, b_gkv = tl(st, "gkv", [128, 2], F32)
                S.dma(gkv[:], I["kv_a_norm_t"][l], writes=[b_gkv])
                ropeC, b_rC = tl(st, "ropeC", [96, LAT], F32)
                ropeS, b_rS = tl(st, "ropeS", [96, LAT], F32)
                S.dma(ropeC[64:96, :], I["ropeC"][:, :], writes=[b_rC])
                S.dma(ropeS[64:96, :], I["ropeS"][:, :], writes=[b_rS])
                qan, b_qan = tl(st, "qan", [128, 3, T], BF16)
                kvan, b_kvan = tl(st, "kvan", [128, 2, T], BF16)
                kr, b_kr = tl(st, "kr", [96, T], BF16)
                raw = [tl(st, "raw%d" % i, [128, 512], F32) for i in range(5)]
                sq = [tl(st, "sq%d" % i, [128, 512], BF16) for i in range(5)]
                rs = [tl(st, "rs%d" % i, [128, 512], F32) for i in range(2)]
                rt = [tl(st, "rt%d" % i, [96, 512], F32) for i in range(2)]

                def rope_or_copy(dst, b_dst, t0, n, pa, pb, isctx):
                    if isctx:
                        S.op("act", lambda: nc.scalar.copy(out=dst[64:96, t0:t0 + n], in_=PS[pa][64:96, 0:n]), reads=[PB[pa]], writes=[b_dst])
                        return
                    l0 = t0 - CTX
                    S.op("dve", lambda: nc.vector.tensor_tensor(out=rt[0][0][64:96, 0:n], in0=PS[pa][64:96, 0:n], in1=ropeC[64:96, l0:l0 + n], op=ALU.mult),
                         reads=[PB[pa], b_rC], writes=[rt[0][1]])
                    S.op("dve", lambda: nc.vector.tensor_tensor(out=rt[1][0][64:96, 0:n], in0=PS[pb][64:96, 0:n], in1=ropeS[64:96, l0:l0 + n], op=ALU.mult),
                         reads=[PB[pb], b_rS], writes=[rt[1][1]])
                    S.op("dve", lambda: nc.vector.tensor_tensor(out=dst[64:96, t0:t0 + n], in0=rt[0][0][64:96, 0:n], in1=rt[1][0][64:96, 0:n], op=ALU.add),
                         reads=[rt[0][1], rt[1][1]], writes=[b_dst])

                def rmsnorm_group(col0, nchunk, gvec, b_gvec, dst, b_dst, t0, n, pbase, ri):
                    for c in range(nchunk):
                        pb = pbase + c
                        for kc in range(KC):
                            S.op("pe", lambda kc=kc, c=c, pb=pb: nc.tensor.matmul(PS[pb][:, 0:n], lhsT=wA[:, kc, col0 + c * 128:col0 + (c + 1) * 128],
                                                                                  rhs=h_fm[:, kc, t0:t0 + n], start=(kc == 0), stop=(kc == KC - 1)),
                                 reads=[b_wA, b_hfm], writes=[PB[pb]])
                        rw, b_rw = raw[ri + c]
                        sqt, b_sq = sq[ri + c]
                        S.op("act", lambda rw=rw, pb=pb: nc.scalar.copy(out=rw[:, 0:n], in_=PS[pb][:, 0:n]), reads=[PB[pb]], writes=[b_rw])
                        S.op("act", lambda sqt=sqt, pb=pb: nc.scalar.activation(out=sqt[:, 0:n], in_=PS[pb][:, 0:n], func=AF.Square), reads=[PB[pb]], writes=[b_sq])
                    pss = pbase + nchunk
                    for c in range(nchunk):
                        S.op("pe", lambda c=c: nc.tensor.matmul(PS[pss][:, 0:n], lhsT=onesb[:], rhs=sq[ri + c][0][:, 0:n], start=(c == 0), stop=(c == nchunk - 1)),
                             reads=[b_onesb, sq[ri + c][1]], writes=[PB[pss]])
                    r0, b_r0 = rs[0]
                    r1, b_r1 = rs[1]
                    S.op("act", lambda: nc.scalar.activation(out=r0[:, 0:n], in_=PS[pss][:, 0:n], func=AF.Sqrt, scale=1.0 / (128 * nchunk), bias=epsb[:, 0:1]),
                         reads=[PB[pss], b_eps], writes=[b_r0])
                    S.op("dve", lambda: nc.vector.reciprocal(out=r1[:, 0:n], in_=r0[:, 0:n]), reads=[b_r0], writes=[b_r1])
                    for c in range(nchunk):
                        S.op("dve", lambda c=c: nc.vector.scalar_tensor_tensor(out=dst[:, c, t0:t0 + n], in0=raw[ri + c][0][:, 0:n], scalar=gvec[:, c:c + 1],
                                                                                 in1=r1[:, 0:n], op0=ALU.mult, op1=ALU.mult),
                             reads=[raw[ri + c][1], b_gvec, b_r1], writes=[b_dst])

                for gi, (t0, n) in enumerate(GROUPS):
                    rmsnorm_group(0, 3, gq, b_gq, qan, b_qan, t0, n, 0, 0)
                    rmsnorm_group(384, 2, gkv, b_gkv, kvan, b_kvan, t0, n, 4, 3)
                    for kc in range(KC):
                        S.op("pe", lambda kc=kc: nc.tensor.matmul(PS[7][64:96, 0:n], lhsT=wA[:, kc, 640:672], rhs=h_fm[:, kc, t0:t0 + n], start=(kc == 0), stop=(kc == KC - 1)),
                             reads=[b_wA, b_hfm], writes=[PB[7]])
                    for kc in range(KC):
                        S.op("pe", lambda kc=kc: nc.tensor.matmul(PS[3][64:96, 0:n], lhsT=wKs[:, kc, :], rhs=h_fm[:, kc, t0:t0 + n], start=(kc == 0), stop=(kc == KC - 1)),
                             reads=[b_wKs, b_hfm], writes=[PB[3]])
                    rope_or_copy(kr, b_kr, t0, n, 7, 3, gi == 0)

                v_aug, b_va = tl(st, "v_aug", [128, NT, 8, 65], BF16)
                S.op("dve", lambda: nc.vector.memset(v_aug[:, :, :, 64:65], 1.0), writes=[b_va])
                wKVh = wKV[:].rearrange("p c (h x) -> p c h x", h=8)
                for i in range(NT):
                    pb = i % 2
                    for c in range(2):
                        S.op("pe", lambda c=c, i=i, pb=pb: nc.tensor.matmul(PS[pb][:, :].rearrange("p (h x) -> p h x", h=8), lhsT=kvan[:, c, i * 128:(i + 1) * 128],
                                                                            rhs=wKVh[:, c, :, 64:128], start=(c == 0), stop=(c == 1)),
                             reads=[b_kvan, b_wKV], writes=[PB[pb]])
                    S.op("act", lambda i=i, pb=pb: nc.scalar.copy(out=v_aug[:, i, :, 0:64], in_=PS[pb][:, :].rearrange("p (h x) -> p h x", h=8)),
                         reads=[PB[pb]], writes=[b_va])

                qn = [tl(st, "qn%d" % i, [96, T], BF16) for i in range(2)]
                kn = [tl(st, "kn%d" % i, [96, T], BF16) for i in range(2)]
                Et = [tl(st, "Et%d" % i, [128, 512], BF16) for i in range(5)]
                rc, b_rc = tl(st, "rc", [65, 512], F32)
                numt = [tl(st, "numt%d" % i, [64, 512], F32) for i in range(2)]
                ot = [tl(st, "ot%d" % i, [64, 512], BF16) for i in range(2)]
                b_mo = Buf("mla_o")
                ecnt = 0
                ocnt = 0
                for h in range(8):
                    qn_t, b_qn = qn[h % 2]
                    kn_t, b_kn = kn[h % 2]
                    S.op("pool", lambda: nc.gpsimd.tensor_copy(out=kn_t[64:96, :], in_=kr[64:96, :]), reads=[b_kr], writes=[b_kn])
                    for gi, (t0, n) in enumerate(GROUPS):
                        if not (last and gi == 0):
                            for c in range(3):
                                S.op("pe", lambda c=c: nc.tensor.matmul(PS[0][0:64, 0:n], lhsT=wQ[:, c, 96 * h:96 * h + 64], rhs=qan[:, c, t0:t0 + n], start=(c == 0), stop=(c == 2)),
                                     reads=[b_wQ, b_qan], writes=[PB[0]])
                            S.op("act", lambda: nc.scalar.copy(out=qn_t[0:64, t0:t0 + n], in_=PS[0][0:64, 0:n]), reads=[PB[0]], writes=[b_qn])
                            for c in range(3):
                                S.op("pe", lambda c=c: nc.tensor.matmul(PS[1][64:96, 0:n], lhsT=wQ[:, c, 96 * h + 64:96 * h + 96], rhs=qan[:, c, t0:t0 + n], start=(c == 0), stop=(c == 2)),
                                     reads=[b_wQ, b_qan], writes=[PB[1]])
                            for c in range(3):
                                S.op("pe", lambda c=c: nc.tensor.matmul(PS[2][64:96, 0:n], lhsT=wQs[:, c, 32 * h:32 * h + 32], rhs=qan[:, c, t0:t0 + n], start=(c == 0), stop=(c == 2)),
                                     reads=[b_wQs, b_qan], writes=[PB[2]])
                            rope_or_copy(qn_t, b_qn, t0, n, 1, 2, gi == 0)
                        for c in range(2):
                            S.op("pe", lambda c=c: nc.tensor.matmul(PS[3][0:64, 0:n], lhsT=wKV[:, c, 128 * h:128 * h + 64], rhs=kvan[:, c, t0:t0 + n], start=(c == 0), stop=(c == 1)),
                                 reads=[b_wKV, b_kvan], writes=[PB[3]])
                        S.op("act", lambda: nc.scalar.copy(out=kn_t[0:64, t0:t0 + n], in_=PS[3][0:64, 0:n]), reads=[PB[3]], writes=[b_kn])
                    for gi, (t0, n) in enumerate(GROUPS):
                        if last and gi == 0:
                            continue
                        kts = list(range(2)) if gi == 0 else list(range(NT))
                        pend = []
                        sbanks = [4, 5, 0, 1]
                        for ki, kt in enumerate(kts):
                            psb = sbanks[ecnt % 4]
                            E_t, b_E = Et[ecnt % 5]
                            ecnt += 1
                            S.op("pe", lambda kt=kt, psb=psb: nc.tensor.matmul(PS[psb][:, 0:n], lhsT=kn_t[:, kt * 128:(kt + 1) * 128], rhs=qn_t[:, t0:t0 + n], start=True, stop=True),
                                 reads=[b_kn, b_qn], writes=[PB[psb]])
                            S.op("act", lambda psb=psb, E_t=E_t: nc.scalar.activation(out=E_t[:, 0:n], in_=PS[psb][:, 0:n], func=AF.Exp, scale=MLA_SCALE),
                                 reads=[PB[psb]], writes=[b_E])
                            pend.append(lambda kt=kt, E_t=E_t, b_E=b_E, ki=ki: S.op("pe", lambda: nc.tensor.matmul(PS[6][0:65, 0:n], lhsT=v_aug[:, kt, h, :], rhs=E_t[:, 0:n], start=(ki == 0), stop=(ki == len(kts) - 1)),
                                 reads=[b_va, b_E], writes=[PB[6]]))
                            if len(pend) > 2:
                                pend.pop(0)()
                        while pend:
                            pend.pop(0)()
                        nm_t, b_nm = numt[ocnt % 2]
                        o_t, b_o = ot[ocnt % 2]
                        ocnt += 1
                        S.op("dve", lambda: nc.vector.reciprocal(out=rc[64:65, 0:n], in_=PS[6][64:65, 0:n]), reads=[PB[6]], writes=[b_rc])
                        S.op("act", lambda nm_t=nm_t: nc.scalar.copy(out=nm_t[:, 0:n], in_=PS[6][0:64, 0:n]), reads=[PB[6]], writes=[b_nm])
                        S.op("pe", lambda: nc.tensor.matmul(PS[7][0:64, 0:n], lhsT=onesf[64:65, 0:64], rhs=rc[64:65, 0:n], start=True, stop=True),
                             reads=[b_onesf, b_rc], writes=[PB[7]])
                        S.op("dve", lambda nm_t=nm_t, o_t=o_t: nc.vector.tensor_tensor(out=o_t[:, 0:n], in0=nm_t[:, 0:n], in1=PS[7][0:64, 0:n], op=ALU.mult),
                             reads=[b_nm, PB[7]], writes=[b_o])
                        S.dma(mla_o_d[h, :, t0:t0 + n], o_t[:, 0:n], reads=[b_o], writes=[b_mo])
                S.barrier()

        b_xres = [Buf("xres%d" % i) for i in range(NT)]
        stage_entry(0)
        for l in range(nlayers):
            last = (l == nlayers - 1)
            if not cfg.get("skip_mla"):
                stage_mla(l, last)
            if not cfg.get("skip_hg"):
                stage_hgrn(l)
            if not cfg.get("skip_gdn"):
                stage_gdn(l)
            if cfg.get("stop_after") == "mixers":
                break
            stage_merge(l, last)
            if cfg.get("stop_after") == "merge":
                break
            stage_moe(l, last)
        if "h_fm" in DBG:
            S.dma(DBG["h_fm"].rearrange("(kc p) t -> p kc t", p=128), h_fm[:], reads=[b_hfm], q="pool")
        if "xres" in DBG:
            S.dma(DBG["xres"], xres_d[:, :])
        if "mla_o" in DBG:
            S.dma(DBG["mla_o"], mla_o_d.rearrange("h d t -> (h d) t"), q="pool")
        if "hg_o" in DBG:
            S.dma(DBG["hg_o"], hg_o_d.rearrange("h d t -> (h d) t"), q="pool")
        if "gdn_o" in DBG:
            S.dma(DBG["gdn_o"], gdn_o_d.rearrange("h d t -> (h d) t"), q="pool")
        K.PS, K.PB = PS, PB

        S.finish()
    K.ninstr = S.ninstr
    return nc, K


WEIGHT_SHAPES = {
    "w_mod": [DEPTH, D, 6 * D], "b_mod": [DEPTH, 6 * D], "w_in": [DEPTH, D, IN_W],
    "w_q_b": [DEPTH, 384, 768], "w_kv_b": [DEPTH, 256, 1024],
    "hg_lb_logits": [DEPTH, 2, 512],
    "gdn_a_log": [DEPTH, 2, 4], "gdn_dt_bias": [DEPTH, 2, 4], "gdn_norm": [DEPTH, 128],
    "w_branch": [DEPTH, 3, 512, D], "w_out": [DEPTH, D, D],
    "ln1_g": [DEPTH, D], "ln1_b": [DEPTH, D], "ln2_g": [DEPTH, D], "ln2_b": [DEPTH, D],
    "w_router": [DEPTH, D, 64], "router_bias": [DEPTH, 64],
    "w_gu": [DEPTH, 64, D, 512], "w_down": [DEPTH, 64, 256, D], "w_sh_gu": [DEPTH, D, 512], "w_sh_down": [DEPTH, 256, D],
}
DERIVED_SHAPES = {
    "w_kr_sw": [DEPTH, D, 32], "w_qr_sw": [DEPTH, 384, 256],
    "q_a_norm_t": [DEPTH, 128, 3], "kv_a_norm_t": [DEPTH, 128, 2],
    "hg_norm_t": [128, DEPTH], "gdn_conv_t": [DEPTH, 128, 12, 5],
}
CONST_SHAPES = {
    "ident": [128, 128], "ropeC": [32, LAT], "ropeS": [32, LAT],
    "rmask": [128, T], "triu": [64, 64], "tril": [64, 64],
    "mist": [64, 2, 64], "mast": [64, 2, 64],
}


def host_consts():
    c = {}
    c["ident"] = np.eye(128, dtype=np.float32)
    pos = np.arange(LAT)
    row = (pos // 64).astype(np.float32)
    col = (pos % 64).astype(np.float32)
    inv = (np.float32(10000.0) ** (-np.arange(8, dtype=np.float32) / np.float32(8))).astype(np.float32)
    C = np.zeros((32, LAT), np.float32)
    Sg = np.zeros((32, LAT), np.float32)
    for ax, p in enumerate((row, col)):
        ang = (p[None, :] * inv[:, None]).astype(np.float32)
        for half in range(2):
            r0 = ax * 16 + half * 8
            C[r0:r0 + 8] = np.cos(ang)
            Sg[r0:r0 + 8] = np.sin(ang) * (-1.0 if half == 0 else 1.0)
    c["ropeC"] = C
    rm = np.ones((128, T), np.float32)
    rm[:, ::64] = 0.0
    c["rmask"] = rm
    c["triu"] = np.triu(np.ones((64, 64), np.float32))
    c["tril"] = np.tril(np.ones((64, 64), np.float32))
    c["mist"] = np.ascontiguousarray(np.stack([c["triu"], c["tril"]], axis=1))
    c["mast"] = np.ascontiguousarray(np.stack([c["tril"] - np.eye(64, dtype=np.float32), c["triu"] - np.eye(64, dtype=np.float32)], axis=1))
    c["ropeS"] = Sg
    return c


def prep_inputs(inputs):
    x = np.asarray(inputs["x"], np.float32)
    ctx = np.asarray(inputs["ctx"], np.float32)
    c = np.asarray(inputs["c"], np.float32)
    c_ctx = np.asarray(inputs["c_ctx"], np.float32)
    shared = {}
    for nm in WEIGHT_SHAPES:
        shared[nm] = np.ascontiguousarray(np.asarray(inputs[nm], np.float32)).reshape(WEIGHT_SHAPES[nm])
    shared.update(host_consts())
    perm = np.arange(32) ^ 8
    w_in = shared["w_in"]
    shared["w_kr_sw"] = np.ascontiguousarray(w_in[:, :, 640:672][:, :, perm])
    wqb = shared["w_q_b"].reshape(DEPTH, 384, 8, 96)
    shared["w_qr_sw"] = np.ascontiguousarray(wqb[:, :, :, 64:96][:, :, :, perm].reshape(DEPTH, 384, 256))
    shared["q_a_norm_t"] = np.ascontiguousarray(np.asarray(inputs["q_a_norm"], np.float32).reshape(DEPTH, 3, 128).transpose(0, 2, 1))
    shared["gdn_conv_t"] = np.ascontiguousarray(np.asarray(inputs["gdn_conv"], np.float32).reshape(DEPTH, 5, 12, 128).transpose(0, 3, 2, 1))
    shared["hg_norm_t"] = np.ascontiguousarray(np.asarray(inputs["hg_norm"], np.float32).T)
    shared["kv_a_norm_t"] = np.ascontiguousarray(np.asarray(inputs["kv_a_norm"], np.float32).reshape(DEPTH, 2, 128).transpose(0, 2, 1))
    maps = []
    for b in range(x.shape[0]):
        m = dict(shared)
        m["xin"] = np.ascontiguousarray(np.concatenate([ctx[b], x[b]], axis=0))
        m["cvecT"] = np.ascontiguousarray(np.stack([c[b], c_ctx], axis=1))
        maps.append(m)
    return maps


def kernel(**inputs):
    maps = prep_inputs(inputs)
    nc, K = build_program({})
    res = run_bass_kernel_spmd(nc, maps, core_ids=list(range(8)))
    out = np.stack([np.asarray(r["out"], np.float32) for r in res.results], axis=0)
    return out
```

```python
import contextlib
import numpy as np
import concourse.bass as bass
import concourse.mybir as mybir
from concourse.bass_utils import run_bass_kernel_spmd

F32 = mybir.dt.float32
BF16 = mybir.dt.bfloat16
AF = mybir.ActivationFunctionType
ALU = mybir.AluOpType
AX = mybir.AxisListType

EPOCH = 30000
NRING = 40

DEPTH = 4
D = 1024
KC = 8
LAT = 2048
CTX = 256
T = LAT + CTX
NT = T // 128
GROUPS = [(0, 256), (256, 512), (768, 512), (1280, 512), (1792, 512)]
NCH = T // 64
IN_W = 8368
MLA_SCALE = 96 ** -0.5
ALPHA = (2 * DEPTH) ** 0.25
O_HG = 672
O_GDN = O_HG + 2560
O_GG = O_GDN + 1536
O_GA = O_GG + 512
O_GB = O_GA + 8
O_GATES = O_GB + 8


class Buf:
    __slots__ = ("name", "w", "r", "ex")

    def __init__(self, name="", ex=False):
        self.name = name
        self.w = None
        self.r = []
        self.ex = ex


class Sched:
    def __init__(self, nc, es, self_sync=True):
        self.nc = nc
        self.es = es
        self.eng = {"pe": nc.tensor, "act": nc.scalar, "dve": nc.vector, "pool": nc.gpsimd, "sp": nc.sync}
        self.seq = {e: 0 for e in self.eng}
        self.sems = {e: [] for e in self.eng}
        self.known = {e: {} for e in self.eng}
        self.known_dma = {e: set() for e in self.eng}
        self.ring = [es.enter_context(nc.semaphore("dr%d" % i)) for i in range(NRING)]
        self.ring_cnt = [0] * NRING
        self.ring_next = 0
        self.self_sync = self_sync
        self.ninstr = 0

    def _sem(self, e, ep):
        while len(self.sems[e]) <= ep:
            self.sems[e].append(self.es.enter_context(self.nc.semaphore("s_%s%d" % (e, len(self.sems[e])))))
        return self.sems[e][ep]

    def _wait(self, e, tok):
        if tok is None:
            return
        if tok[0] == "dma":
            _, k, val = tok
            key = (k, val)
            if key in self.known_dma[e]:
                return
            self.eng[e].wait_ge(self.ring[k], val)
            self.known_dma[e].add(key)
            return
        _, e2, s = tok
        if e2 == e and (e == "pe" or not self.self_sync):
            return
        if self.known[e].get(e2, 0) >= s:
            return
        ep = (s - 1) // EPOCH
        self.eng[e].wait_ge(self._sem(e2, ep), s - ep * EPOCH)
        self.known[e][e2] = s

    def _deps(self, e, reads, writes):
        for b in reads:
            if b.w is not None:
                self._wait(e, b.w)
            if b.ex:
                for t in b.r:
                    if t[0] == "eng" and t[1] != e:
                        self._wait(e, t)
        for b in writes:
            if b.w is not None:
                self._wait(e, b.w)
            for t in b.r:
                self._wait(e, t)

    def _mark(self, tok, reads, writes):
        for b in reads:
            b.r.append(tok)
            if len(b.r) > 24:
                b.r = self._prune(b.r)
        for b in writes:
            b.w = tok
            b.r = []

    def _prune(self, r):
        best = {}
        out = []
        for t in r:
            if t[0] == "dma":
                out.append(t)
            elif t[1] not in best or best[t[1]][2] < t[2]:
                best[t[1]] = t
        return out + list(best.values())

    def op(self, e, fn, reads=(), writes=()):
        self._deps(e, reads, writes)
        ins = fn()
        self.seq[e] += 1
        s = self.seq[e]
        ep = (s - 1) // EPOCH
        ins.then_inc(self._sem(e, ep), 1)
        tok = ("eng", e, s)
        self._mark(tok, reads, writes)
        self.ninstr += 1
        return tok

    def dma(self, out, in_, reads=(), writes=(), q="sp", **kw):
        k = self.ring_next
        self.ring_next = (self.ring_next + 1) % NRING
        prev = self.ring_cnt[k]
        if prev > 0:
            self._wait(q, ("dma", k, 16 * prev))
        self._deps(q, reads, writes)
        self.eng[q].dma_start(out=out, in_=in_, **kw).then_inc(self.ring[k], 16)
        self.ring_cnt[k] = prev + 1
        tok = ("dma", k, 16 * (prev + 1))
        self._mark(tok, reads, writes)
        self.ninstr += 1
        return tok

    def barrier(self, engines=None):
        for e in (engines or self.eng):
            for e2 in self.eng:
                if self.seq[e2] > 0 and not (e2 == e and e == "pe"):
                    self._wait(e, ("eng", e2, self.seq[e2]))
            for k in range(NRING):
                if self.ring_cnt[k] > 0:
                    self._wait(e, ("dma", k, 16 * self.ring_cnt[k]))

    def finish(self):
        self.barrier(["sp"])


class Ctx:
    pass


def build_program(cfg):
    nlayers = cfg.get("nlayers", DEPTH)
    stop_after = cfg.get("stop_after", None)
    dbg = cfg.get("debug", [])
    nc = bass.Bass("TRN2", target_bir_lowering=False)
    K = Ctx()
    K.nc = nc

    def din(name, shape, dt=F32):
        return nc.dram_tensor(name, list(shape), dt, kind="ExternalInput").ap()

    def dscr(name, shape, dt=F32):
        return nc.dram_tensor(name, list(shape), dt, kind="Internal").ap()

    I = {}
    I["xin"] = din("xin", [T, D])
    I["cvecT"] = din("cvecT", [D, 2])
    for nm, shp in WEIGHT_SHAPES.items():
        I[nm] = din(nm, shp)
    for nm, shp in CONST_SHAPES.items():
        I[nm] = din(nm, shp)
    for nm, shp in DERIVED_SHAPES.items():
        I[nm] = din(nm, shp)
    out_d = nc.dram_tensor("out", [LAT, D], F32, kind="ExternalOutput").ap()
    DBG = {}
    for nm, shp in dbg:
        DBG[nm] = nc.dram_tensor("dbg_" + nm, list(shp), F32, kind="ExternalOutput").ap()

    modv_d = dscr("modv_d", [DEPTH, 2, 6 * D])
    xres_d = dscr("xres_d", [T, D])
    mla_o_d = dscr("mla_o_d", [8, 64, T], BF16)
    hg_o_d = dscr("hg_o_d", [4, 128, T], BF16)
    gdn_o_d = dscr("gdn_o_d", [4, 128, T], BF16)
    gdn_raw_d = dscr("gdn_raw_d", [2, T, 512])

    es = contextlib.ExitStack()
    with es:
        S = Sched(nc, es, self_sync=cfg.get('self_sync', True))
        K.S = S

        tlc = [0]

        def tl(st, name, shape, dt):
            tlc[0] += 1
            t = st.enter_context(nc.sbuf_tensor("sb%d_%s" % (tlc[0], name), list(shape), dt))
            return t, Buf(name)

        PS = []
        PB = []
        for i in range(8):
            PS.append(es.enter_context(nc.psum_tensor("ps%d" % i, [128, 512], F32)))
            PB.append(Buf("ps%d" % i, ex=True))

        identf, b_identf = tl(es, "identf", [128, 128], F32)
        identb, b_identb = tl(es, "identb", [128, 128], BF16)
        onesb, b_onesb = tl(es, "onesb", [128, 128], BF16)
        onesf, b_onesf = tl(es, "onesf", [128, 128], F32)
        S.dma(identf[:], I["ident"][:, :], writes=[b_identf])
        S.dma(identb[:], I["ident"][:, :], writes=[b_identb], q="pool")
        S.op("dve", lambda: nc.vector.memset(onesb[:], 1.0), writes=[b_onesb])
        S.op("dve", lambda: nc.vector.memset(onesf[:], 1.0), writes=[b_onesf])
        h_fm, b_hfm = tl(es, "h_fm", [128, KC, T], BF16)
        epsb, b_eps = tl(es, "epsb", [128, 1], F32)
        S.op("dve", lambda: nc.vector.memset(epsb[:], 1e-6), writes=[b_eps])

        def dbg_out(name, ap_sb, buf, dram_ap=None):
            if name in DBG:
                S.dma(dram_ap if dram_ap is not None else DBG[name], ap_sb, reads=[buf])

        with contextlib.ExitStack() as st:
            cv, b_cv = tl(st, "cv", [128, KC, 2], F32)
            scv, b_scv = tl(st, "scv", [128, KC, 2], F32)
            S.dma(cv[:], I["cvecT"].rearrange("(kc p) s -> p kc s", p=128), writes=[b_cv])
            S.op("act", lambda: nc.scalar.activation(out=scv[:], in_=cv[:], func=AF.Silu), reads=[b_cv], writes=[b_scv])
            wm = [tl(st, "wm%d" % i, [128, KC, 512], F32) for i in range(2)]
            bm, b_bm = tl(st, "bm", [1, 6 * D], F32)
            mv = [tl(st, "mv%d" % i, [2, 6 * D], F32) for i in range(2)]
            ones2, b_ones2 = tl(st, "ones2", [1, 2], F32)
            S.op("dve", lambda: nc.vector.memset(ones2[:], 1.0), writes=[b_ones2])
            cnt = 0
            for l in range(nlayers):
                mvt, b_mv = mv[l % 2]
                S.dma(bm[:], I["b_mod"][l:l + 1, :], writes=[b_bm])
                for cg in range(12):
                    wt, b_wt = wm[cnt % 2]
                    cnt += 1
                    S.dma(wt[:], I["w_mod"][l].rearrange("(kc p) n -> p kc n", p=128)[:, :, cg * 512:(cg + 1) * 512], writes=[b_wt])
                    pb = cnt % 2
                    for kc in range(KC):
                        S.op("pe", lambda kc=kc, wt=wt, pb=pb: nc.tensor.matmul(PS[pb][0:2, :], lhsT=scv[:, kc, :], rhs=wt[:, kc, :], start=(kc == 0), stop=False),
                             reads=[b_scv, b_wt], writes=[PB[pb]])
                    S.op("pe", lambda cg=cg, pb=pb: nc.tensor.matmul(PS[pb][0:2, :], lhsT=ones2[:], rhs=bm[:, cg * 512:(cg + 1) * 512], start=False, stop=True),
                         reads=[b_ones2, b_bm], writes=[PB[pb]])
                    S.op("act", lambda cg=cg, pb=pb, mvt=mvt: nc.scalar.copy(out=mvt[:, cg * 512:(cg + 1) * 512], in_=PS[pb][0:2, :]), reads=[PB[pb]], writes=[b_mv])
                S.dma(modv_d[l], mvt[:], reads=[b_mv], writes=[])
                K.modv_tok = None
            S.barrier()
        b_modv = Buf("modv_d")
        b_modv.w = None

        def load_bc(st, name, l, stream, j, plus_one=False):
            t, b = tl(st, name, [128, D], F32)
            S.dma(t[:], modv_d[l, stream:stream + 1, j * D:(j + 1) * D].partition_broadcast(128), writes=[b])
            if plus_one:
                S.op("pool", lambda: nc.gpsimd.tensor_scalar_add(out=t[:], in0=t[:], scalar1=1.0), reads=[b], writes=[b])
            return t, b

        def load_vec_bc(st, name, dram_row_ap, n=D):
            t, b = tl(st, name, [128, n], F32)
            S.dma(t[:], dram_row_ap.partition_broadcast(128), writes=[b])
            return t, b

        def to_fm(src_bf, b_src, i, psb):
            pv = PS[psb][:].bitcast(BF16)
            for kc in range(KC):
                S.op("pe", lambda kc=kc: nc.tensor.transpose(out=pv[:, kc * 128:(kc + 1) * 128], in_=src_bf[:, kc * 128:(kc + 1) * 128], identity=identb[:]),
                     reads=[b_src, b_identb], writes=[PB[psb]])
            S.op("act", lambda: nc.scalar.copy(out=h_fm[:, :, i * 128:(i + 1) * 128], in_=pv.rearrange("p (k t) -> p k t", k=KC)),
                 reads=[PB[psb]], writes=[b_hfm])

        def stage_entry(l):
            with contextlib.ExitStack() as st:
                bc = {}
                for s in range(2):
                    bc[(s, 0)] = load_bc(st, "bsh%d" % s, l, s, 0)
                    bc[(s, 1)] = load_bc(st, "bsc%d" % s, l, s, 1, plus_one=True)
                xt = [tl(st, "xt%d" % i, [128, D], F32) for i in range(2)]
                ht = [tl(st, "ht%d" % i, [128, D], BF16) for i in range(2)]
                for i in range(NT):
                    s = 1 if i < 2 else 0
                    x_t, b_x = xt[i % 2]
                    h_t, b_h = ht[i % 2]
                    S.dma(x_t[:], I["xin"][i * 128:(i + 1) * 128, :], writes=[b_x])
                    S.dma(xres_d[i * 128:(i + 1) * 128, :], x_t[:], reads=[b_x])
                    S.op("dve", lambda x_t=x_t, s=s: nc.vector.tensor_tensor(out=x_t[:], in0=x_t[:], in1=bc[(s, 1)][0][:], op=ALU.mult),
                         reads=[b_x, bc[(s, 1)][1]], writes=[b_x])
                    S.op("dve", lambda x_t=x_t, h_t=h_t, s=s: nc.vector.tensor_tensor(out=h_t[:], in0=x_t[:], in1=bc[(s, 0)][0][:], op=ALU.add),
                         reads=[b_x, bc[(s, 0)][1]], writes=[b_h])
                    to_fm(h_t, b_h, i, i % 2)
                S.barrier()


        lbT, b_lbT = tl(es, "lbT", [128, DEPTH, 8], F32)
        omlbT, b_omlbT = tl(es, "omlbT", [128, DEPTH, 8], F32)
        rmask, b_rmask = tl(es, "rmask", [128, T], BF16)
        triu, b_triu = tl(es, "triu", [64, 64], F32)
        tril, b_tril = tl(es, "tril", [64, 64], F32)
        S.dma(rmask[:], I["rmask"][:, :], writes=[b_rmask], q="pool")
        S.dma(triu[:], I["triu"][:, :], writes=[b_triu])
        S.dma(tril[:], I["tril"][:, :], writes=[b_tril])
        with contextlib.ExitStack() as st:
            lg, b_lg = tl(st, "lg", [32, 128], F32)
            eT, b_eT = tl(st, "eT", [128, DEPTH, 8], F32)
            tot, b_tot = tl(st, "lbtot", [128, 8], F32)
            S.dma(lg[:], I["hg_lb_logits"].rearrange("l s (h p) -> (l s h) p", p=128), writes=[b_lg])
            S.op("act", lambda: nc.scalar.activation(out=lg[:], in_=lg[:], func=AF.Exp), reads=[b_lg], writes=[b_lg])
            S.op("pe", lambda: nc.tensor.transpose(out=PS[0][:, 0:32], in_=lg[:], identity=identf[0:32, 0:32]), reads=[b_lg, b_identf], writes=[PB[0]])
            S.op("act", lambda: nc.scalar.copy(out=eT[:].rearrange("p l x -> p (l x)"), in_=PS[0][:, 0:32]), reads=[PB[0]], writes=[b_eT])
            S.op("dve", lambda: nc.vector.tensor_tensor(out=tot[:], in0=eT[:, 0, :], in1=eT[:, 1, :], op=ALU.add), reads=[b_eT], writes=[b_tot])
            S.op("dve", lambda: nc.vector.tensor_tensor(out=tot[:], in0=tot[:], in1=eT[:, 2, :], op=ALU.add), reads=[b_eT, b_tot], writes=[b_tot])
            S.op("dve", lambda: nc.vector.tensor_tensor(out=tot[:], in0=tot[:], in1=eT[:, 3, :], op=ALU.add), reads=[b_eT, b_tot], writes=[b_tot])
            S.op("dve", lambda: nc.vector.reciprocal(out=tot[:], in_=tot[:]), reads=[b_tot], writes=[b_tot])
            S.op("dve", lambda: nc.vector.memset(lbT[:, 0, :], 0.0), writes=[b_lbT])
            S.op("dve", lambda: nc.vector.tensor_copy(out=lbT[:, 1, :], in_=eT[:, 1, :]), reads=[b_eT, b_lbT], writes=[b_lbT])
            S.op("dve", lambda: nc.vector.tensor_tensor(out=lbT[:, 2, :], in0=lbT[:, 1, :], in1=eT[:, 2, :], op=ALU.add), reads=[b_eT, b_lbT], writes=[b_lbT])
            S.op("dve", lambda: nc.vector.tensor_tensor(out=lbT[:, 3, :], in0=lbT[:, 2, :], in1=eT[:, 3, :], op=ALU.add), reads=[b_eT, b_lbT], writes=[b_lbT])
            for l in range(1, DEPTH):
                S.op("dve", lambda l=l: nc.vector.tensor_tensor(out=lbT[:, l, :], in0=lbT[:, l, :], in1=tot[:], op=ALU.mult), reads=[b_tot, b_lbT], writes=[b_lbT])
            S.op("dve", lambda: nc.vector.tensor_scalar(out=omlbT[:], in0=lbT[:], scalar1=-1.0, scalar2=1.0, op0=ALU.mult, op1=ALU.add), reads=[b_lbT], writes=[b_omlbT])
            S.barrier()

        def stage_hgrn(l):
            winv = I["w_in"][l].rearrange("(kc p) n -> p kc n", p=128)
            with contextlib.ExitStack() as st:
                wh = [tl(st, "wh%d" % i, [128, KC, 5, 128], BF16) for i in range(2)]
                hgn4, b_hgn = tl(st, "hgn", [128, DEPTH], F32)
                S.dma(hgn4[:], I["hg_norm_t"][:, :], writes=[b_hgn])
                hgn = hgn4[:, l:l + 1]
                q_bf, b_q = tl(st, "hq_bf", [128, T], BF16)
                gate_sb, b_gate = tl(st, "hgate", [128, T], BF16)
                v_tm, b_v = tl(st, "hv_tm", [64, NCH, 128], BF16)
                A, b_A = tl(st, "hA", [128, T], F32)
                B, b_B = tl(st, "hB", [128, T], F32)
                Cc, b_C = tl(st, "hC", [128, T], F32)
                qt, b_qt = tl(st, "hqt", [128, T], BF16)
                kt, b_kt = tl(st, "hkt", [128, T], BF16)
                qh, b_qh = tl(st, "hqh", [128, T], BF16)
                kh, b_kh = tl(st, "hkh", [128, T], BF16)
                khT, b_khT = tl(st, "hkhT", [64, NCH, 128], BF16)
                aT, b_aT = tl(st, "haT", [64, NCH, 64], BF16)
                o_d = [tl(st, "ho%d" % i, [128, T], F32) for i in range(2)]
                tot, b_tot = tl(st, "htot", [128, NCH], F32)
                rmid, b_rmid = tl(st, "hrmid", [128, NCH], F32)
                egl, b_egl = tl(st, "hegl", [128, NCH], F32)
                Sst, b_S = tl(st, "hS", [128, 128], F32)
                Sb = [tl(st, "hSb%d" % i, [128, 128], BF16) for i in range(2)]
                rs0, b_rs0 = tl(st, "hrs0", [128, 512], F32)
                rs1, b_rs1 = tl(st, "hrs1", [128, 512], F32)
                og = [tl(st, "hog%d" % i, [128, 512], BF16) for i in range(2)]
                b_ho = Buf("hg_o")
                C3 = Cc[:].rearrange("p (c k) -> p c k", k=64)
                B3 = B[:].rearrange("p (c k) -> p c k", k=64)
                pcnt = [0]

                def proj(col, wt, b_wt, fn_evac):
                    for (t0, n) in GROUPS:
                        pb = pcnt[0] % 2
                        pcnt[0] += 1
                        for kc in range(KC):
                            S.op("pe", lambda kc=kc, pb=pb: nc.tensor.matmul(PS[pb][:, 0:n], lhsT=wt[:, kc, col, :], rhs=h_fm[:, kc, t0:t0 + n], start=(kc == 0), stop=(kc == KC - 1)),
                                 reads=[b_wt, b_hfm], writes=[PB[pb]])
                        fn_evac(pb, t0, n)

                ocnt = 0
                def load_head(hd):
                    wt, b_wt = wh[hd % 2]
                    for ci in range(5):
                        c0 = O_HG + ci * 512 + hd * 128
                        S.dma(wt[:, :, ci, :], winv[:, :, c0:c0 + 128], writes=[b_wt], q="pool")
                load_head(0)
                for hd in range(4):
                    wt, b_wt = wh[hd % 2]
                    if hd + 1 < 4:
                        load_head(hd + 1)
                    proj(0, wt, b_wt, lambda pb, t0, n: S.op("act", lambda: nc.scalar.activation(out=q_bf[:, t0:t0 + n], in_=PS[pb][:, 0:n], func=AF.Silu), reads=[PB[pb]], writes=[b_q]))
                    proj(4, wt, b_wt, lambda pb, t0, n: S.op("act", lambda: nc.scalar.activation(out=gate_sb[:, t0:t0 + n], in_=PS[pb][:, 0:n], func=AF.Silu), reads=[PB[pb]], writes=[b_gate]))
                    for c4 in range(NCH // 4):
                        pb = pcnt[0] % 2
                        pcnt[0] += 1
                        for j in range(4):
                            c = c4 * 4 + j
                            for kc in range(KC):
                                S.op("pe", lambda kc=kc, c=c, j=j, pb=pb: nc.tensor.matmul(PS[pb][0:64, j * 128:(j + 1) * 128], lhsT=h_fm[:, kc, c * 64:(c + 1) * 64], rhs=wt[:, kc, 1, :],
                                                                                         start=(kc == 0), stop=(kc == KC - 1)), reads=[b_wt, b_hfm], writes=[PB[pb]])
                        S.op("act", lambda c4=c4, pb=pb: nc.scalar.copy(out=v_tm[:, c4 * 4:(c4 + 1) * 4, :], in_=PS[pb][0:64, :].rearrange("p (j x) -> p j x", j=4)),
                             reads=[PB[pb]], writes=[b_v])
                    for s in range(2):
                        o_t, b_o = o_d[s]
                        lbc = lbT[:, l, s * 4 + hd:s * 4 + hd + 1]
                        omc = omlbT[:, l, s * 4 + hd:s * 4 + hd + 1]
                        proj(2 + s, wt, b_wt, lambda pb, t0, n: S.op("act", lambda: nc.scalar.activation(out=A[:, t0:t0 + n], in_=PS[pb][:, 0:n], func=AF.Sigmoid), reads=[PB[pb]], writes=[b_A]))
                        S.op("dve", lambda: nc.vector.tensor_scalar(out=A[:], in0=A[:], scalar1=omc, scalar2=lbc, op0=ALU.mult, op1=ALU.add), reads=[b_A, b_lbT, b_omlbT], writes=[b_A])
                        S.op("act", lambda: nc.scalar.activation(out=B[:], in_=A[:], func=AF.Ln), reads=[b_A], writes=[b_B])
                        S.op("pool", lambda: nc.gpsimd.tensor_scalar(out=A[:], in0=A[:], scalar1=-1.0, scalar2=1.0, op0=ALU.mult, op1=ALU.add), reads=[b_A, b_B], writes=[b_A])
                        S.op("dve", lambda: nc.vector.tensor_tensor_scan(out=Cc[:], data0=rmask[:], data1=B[:], initial=0.0, op0=ALU.mult, op1=ALU.add), reads=[b_rmask, b_B], writes=[b_C])
                        S.op("dve", lambda: nc.vector.tensor_copy(out=tot[:], in_=C3[:, :, 63]), reads=[b_C], writes=[b_tot])
                        totb = tot[:].unsqueeze(2).to_broadcast([128, NCH, 64])
                        if s == 1:
                            S.op("dve", lambda: nc.vector.scalar_tensor_tensor(out=C3, in0=C3, scalar=-1.0, in1=totb, op0=ALU.mult, op1=ALU.add), reads=[b_C, b_tot], writes=[b_C])
                            S.op("dve", lambda: nc.vector.tensor_tensor(out=Cc[:], in0=Cc[:], in1=B[:], op=ALU.add), reads=[b_C, b_B], writes=[b_C])
                        S.op("dve", lambda: nc.vector.tensor_copy(out=rmid[:], in_=C3[:, :, 31 + s]), reads=[b_C], writes=[b_rmid])
                        S.op("act", lambda: nc.scalar.activation(out=egl[:], in_=tot[:], func=AF.Exp), reads=[b_tot], writes=[b_egl])
                        S.op("dve", lambda: nc.vector.tensor_tensor(out=B3, in0=C3, in1=rmid[:].unsqueeze(2).to_broadcast([128, NCH, 64]), op=ALU.subtract), reads=[b_C, b_rmid, b_B], writes=[b_B])
                        S.op("act", lambda: nc.scalar.activation(out=B[:], in_=B[:], func=AF.Exp), reads=[b_B], writes=[b_B])
                        S.op("pool", lambda: nc.gpsimd.tensor_tensor(out=qt[:], in0=q_bf[:], in1=B[:], op=ALU.mult), reads=[b_q, b_B], writes=[b_qt])
                        S.op("dve", lambda: nc.vector.reciprocal(out=B[:], in_=B[:]), reads=[b_B, b_qt], writes=[b_B])
                        S.op("dve", lambda: nc.vector.tensor_tensor(out=kt[:], in0=A[:], in1=B[:], op=ALU.mult), reads=[b_A, b_B], writes=[b_kt])
                        S.op("act", lambda: nc.scalar.activation(out=B[:], in_=Cc[:], func=AF.Exp), reads=[b_C, b_kt], writes=[b_B])
                        S.op("pool", lambda: nc.gpsimd.tensor_tensor(out=qh[:], in0=q_bf[:], in1=B[:], op=ALU.mult), reads=[b_q, b_B], writes=[b_qh])
                        S.op("dve", lambda: nc.vector.scalar_tensor_tensor(out=B3, in0=C3, scalar=-1.0, in1=totb, op0=ALU.mult, op1=ALU.add), reads=[b_C, b_tot, b_qh], writes=[b_B])
                        S.op("act", lambda: nc.scalar.activation(out=B[:], in_=B[:], func=AF.Exp), reads=[b_B], writes=[b_B])
                        S.op("dve", lambda: nc.vector.tensor_tensor(out=kh[:], in0=A[:], in1=B[:], op=ALU.mult), reads=[b_A, b_B], writes=[b_kh])
                        pvb = PS[2][:].bitcast(BF16)
                        msk = triu if s == 0 else tril
                        b_msk = b_triu if s == 0 else b_tril
                        fr = 0 if s == 0 else 32
                        dr = 32 - fr
                        S.op("dve", lambda: nc.vector.memset(aT[dr:dr + 32, :, fr:fr + 32], 0.0), writes=[b_aT])
                        for c8 in range(0, NCH, 8):
                            nb = min(8, NCH - c8)
                            for j in range(nb):
                                c = c8 + j
                                S.op("pe", lambda c=c, j=j: nc.tensor.transpose(out=pvb[0:64, j * 128:(j + 1) * 128], in_=kh[:, c * 64:(c + 1) * 64], identity=identb[:]),
                                     reads=[b_kh, b_identb], writes=[PB[2]])
                            S.op("act", lambda c8=c8, nb=nb: nc.scalar.copy(out=khT[:, c8:c8 + nb, :], in_=pvb[0:64, 0:nb * 128].rearrange("p (j x) -> p j x", j=nb)),
                                 reads=[PB[2]], writes=[b_khT])
                            for j in range(nb):
                                c = c8 + j
                                S.op("pe", lambda c=c, j=j: nc.tensor.matmul(PS[3][fr:fr + 32, j * 64:(j + 1) * 64], lhsT=kt[:, c * 64 + fr:c * 64 + fr + 32], rhs=qt[:, c * 64:(c + 1) * 64], start=True, stop=True),
                                     reads=[b_kt, b_qt], writes=[PB[3]])
                                S.op("pe", lambda c=c, j=j: nc.tensor.matmul(PS[3][dr:dr + 32, j * 64 + dr:j * 64 + dr + 32], lhsT=kt[:, c * 64 + dr:c * 64 + dr + 32], rhs=qt[:, c * 64 + dr:c * 64 + dr + 32], start=True, stop=True),
                                     reads=[b_kt, b_qt], writes=[PB[3]])
                            pv3 = PS[3][:, 0:nb * 64].rearrange("p (j x) -> p j x", j=nb)
                            S.op("dve", lambda c8=c8, nb=nb, pv3=pv3: nc.vector.tensor_tensor(out=aT[fr:fr + 32, c8:c8 + nb, :], in0=pv3[fr:fr + 32, :, :],
                                                                                     in1=msk[fr:fr + 32, :].unsqueeze(1).to_broadcast([32, nb, 64]), op=ALU.mult),
                                 reads=[PB[3], b_msk], writes=[b_aT])
                            S.op("dve", lambda c8=c8, nb=nb, pv3=pv3: nc.vector.tensor_tensor(out=aT[dr:dr + 32, c8:c8 + nb, dr:dr + 32], in0=pv3[dr:dr + 32, :, dr:dr + 32],
                                                                                     in1=msk[dr:dr + 32, dr:dr + 32].unsqueeze(1).to_broadcast([32, nb, 32]), op=ALU.mult),
                                 reads=[PB[3], b_msk], writes=[b_aT])
                        order = list(range(NCH)) if s == 0 else [3, 2, 1, 0] + list(range(NCH - 1, 3, -1))
                        def emit_pS(idx):
                            c = order[idx]
                            pS = 6 + idx % 2
                            S.op("pe", lambda: nc.tensor.matmul(PS[pS][:, 0:128], lhsT=khT[:, c, :], rhs=v_tm[:, c, :], start=True, stop=True), reads=[b_khT, b_v], writes=[PB[pS]])
                        emit_pS(0)
                        for idx, c in enumerate(order):
                            po = 4 + idx % 2
                            pS = 6 + idx % 2
                            if idx + 1 < NCH - 1:
                                emit_pS(idx + 1)
                            if idx < NCH - 1:
                                if idx == 0:
                                    S.op("dve", lambda pS=pS: nc.vector.tensor_copy(out=Sst[:], in_=PS[pS][:, 0:128]), reads=[PB[pS]], writes=[b_S])
                                else:
                                    S.op("dve", lambda c=c, pS=pS: nc.vector.scalar_tensor_tensor(out=Sst[:], in0=Sst[:], scalar=egl[:, c:c + 1], in1=PS[pS][:, 0:128], op0=ALU.mult, op1=ALU.add),
                                         reads=[b_S, b_egl, PB[pS]], writes=[b_S])
                            if idx > 0:
                                sb_t, b_sb = Sb[idx % 2]
                                S.op("pe", lambda c=c, po=po, sb_t=sb_t: nc.tensor.matmul(PS[po][:, 0:64], lhsT=sb_t[:], rhs=qh[:, c * 64:(c + 1) * 64], start=True, stop=False),
                                     reads=[b_sb, b_qh], writes=[PB[po]])
                            S.op("pe", lambda c=c, po=po, idx=idx: nc.tensor.matmul(PS[po][:, 0:64], lhsT=v_tm[:, c, :], rhs=aT[:, c, :], start=(idx == 0), stop=True),
                                 reads=[b_v, b_aT], writes=[PB[po]])
                            S.op("act", lambda c=c, po=po: nc.scalar.copy(out=o_t[:, c * 64:(c + 1) * 64], in_=PS[po][:, 0:64]), reads=[PB[po]], writes=[b_o])
                            if idx < NCH - 1:
                                nsb, b_nsb = Sb[(idx + 1) % 2]
                                S.op("act", lambda nsb=nsb: nc.scalar.copy(out=nsb[:], in_=Sst[:]), reads=[b_S], writes=[b_nsb])
                    o_f, b_of = o_d[0]
                    o_b, b_ob = o_d[1]
                    S.op("dve", lambda: nc.vector.tensor_tensor(out=o_f[:], in0=o_f[:], in1=o_b[:], op=ALU.add), reads=[b_of, b_ob], writes=[b_of])
                    S.op("act", lambda: nc.scalar.activation(out=qt[:], in_=o_f[:], func=AF.Square), reads=[b_of, b_qt], writes=[b_qt])
                    for (t0, n) in GROUPS:
                        pb = pcnt[0] % 2
                        pcnt[0] += 1
                        og_t, b_og = og[ocnt % 2]
                        ocnt += 1
                        S.op("pe", lambda pb=pb: nc.tensor.matmul(PS[pb][:, 0:n], lhsT=onesb[:], rhs=qt[:, t0:t0 + n], start=True, stop=True), reads=[b_onesb, b_qt], writes=[PB[pb]])
                        S.op("act", lambda pb=pb: nc.scalar.activation(out=rs0[:, 0:n], in_=PS[pb][:, 0:n], func=AF.Sqrt, scale=1.0 / 128, bias=epsb[:, 0:1]), reads=[PB[pb], b_eps], writes=[b_rs0])
                        S.op("dve", lambda: nc.vector.reciprocal(out=rs1[:, 0:n], in_=rs0[:, 0:n]), reads=[b_rs0], writes=[b_rs1])
                        S.op("dve", lambda: nc.vector.scalar_tensor_tensor(out=rs0[:, 0:n], in0=o_f[:, t0:t0 + n], scalar=hgn, in1=rs1[:, 0:n], op0=ALU.mult, op1=ALU.mult),
                             reads=[b_of, b_hgn, b_rs1, b_rs0], writes=[b_rs0])
                        S.op("dve", lambda og_t=og_t: nc.vector.tensor_tensor(out=og_t[:, 0:n], in0=rs0[:, 0:n], in1=gate_sb[:, t0:t0 + n], op=ALU.mult), reads=[b_rs0, b_gate], writes=[b_og])
                        S.dma(hg_o_d[hd, :, t0:t0 + n], og_t[:, 0:n], reads=[b_og], writes=[b_ho])
                S.barrier()


        def stage_gdn(l):
            winv = I["w_in"][l].rearrange("(kc p) n -> p kc n", p=128)
            with contextlib.ExitStack() as st0:
                mist, b_mist = tl(st0, "mist", [64, 2, 64], F32)
                mast, b_mast = tl(st0, "mast", [64, 2, 64], F32)
                S.dma(mist[:], I["mist"][:, :, :], writes=[b_mist])
                S.dma(mast[:], I["mast"][:, :, :], writes=[b_mast])
                g_t, b_g = tl(st0, "g_g", [64, NCH, 8], F32)
                beta, b_beta = tl(st0, "g_beta", [64, NCH, 8], F32)
                nbeta, b_nbeta = tl(st0, "g_nbeta", [64, NCH, 8], F32)
                egc, b_egc = tl(st0, "g_egc", [64, NCH, 8], F32)
                negc, b_negc = tl(st0, "g_negc", [64, NCH, 8], F32)
                ekd, b_ekd = tl(st0, "g_ekd", [64, NCH, 8], F32)
                egl, b_egl = tl(st0, "g_egl", [128, NCH, 8], F32)
                cw, b_cw = tl(st0, "g_cw", [128, 12, 5], F32)
                S.dma(cw[:], I["gdn_conv_t"][l], writes=[b_cw])
                with contextlib.ExitStack() as st:
                    wg, b_wg = tl(st, "g_wg", [128, KC, 16], BF16)
                    S.dma(wg[:], winv[:, :, O_GA:O_GA + 16], writes=[b_wg], q="pool")
                    gab, b_gab = tl(st, "g_gab", [64, NCH, 16], F32)
                    alog, b_alog = tl(st, "g_alog", [64, 8], F32)
                    dtb, b_dtb = tl(st, "g_dtb", [64, 8], F32)
                    S.dma(alog[:], I["gdn_a_log"][l:l + 1].rearrange("o s h -> o (s h)").partition_broadcast(64), writes=[b_alog])
                    S.dma(dtb[:], I["gdn_dt_bias"][l:l + 1].rearrange("o s h -> o (s h)").partition_broadcast(64), writes=[b_dtb])
                    for c in range(NCH):
                        pb = c // 32
                        cc = c % 32
                        for kc in range(KC):
                            S.op("pe", lambda c=c, kc=kc, pb=pb, cc=cc: nc.tensor.matmul(PS[pb][0:64, cc * 16:(cc + 1) * 16], lhsT=h_fm[:, kc, c * 64:(c + 1) * 64], rhs=wg[:, kc, :],
                                                                                     start=(kc == 0), stop=(kc == KC - 1)), reads=[b_hfm, b_wg], writes=[PB[pb]])
                    S.op("act", lambda: nc.scalar.copy(out=gab[:, 0:32, :], in_=PS[0][0:64, :].rearrange("p (c x) -> p c x", x=16)), reads=[PB[0]], writes=[b_gab])
                    S.op("act", lambda: nc.scalar.copy(out=gab[:, 32:36, :], in_=PS[1][0:64, 0:64].rearrange("p (c x) -> p c x", x=16)), reads=[PB[1]], writes=[b_gab])
                    S.op("act", lambda: nc.scalar.activation(out=alog[:], in_=alog[:], func=AF.Exp), reads=[b_alog], writes=[b_alog])
                    S.op("dve", lambda: nc.vector.tensor_tensor(out=g_t[:], in0=gab[:, :, 0:8], in1=dtb[:].unsqueeze(1).to_broadcast([64, NCH, 8]), op=ALU.add), reads=[b_gab, b_dtb], writes=[b_g])
                    S.op("act", lambda: nc.scalar.activation(out=g_t[:], in_=g_t[:], func=AF.Exp), reads=[b_g], writes=[b_g])
                    S.op("act", lambda: nc.scalar.activation(out=g_t[:], in_=g_t[:], func=AF.Ln, bias=onesf[0:64, 0:1]), reads=[b_g, b_onesf], writes=[b_g])
                    S.op("dve", lambda: nc.vector.scalar_tensor_tensor(out=g_t[:], in0=g_t[:], scalar=-1.0, in1=alog[:].unsqueeze(1).to_broadcast([64, NCH, 8]), op0=ALU.mult, op1=ALU.mult),
                         reads=[b_g, b_alog], writes=[b_g])
                    S.op("act", lambda: nc.scalar.activation(out=beta[:], in_=gab[:, :, 8:16], func=AF.Sigmoid), reads=[b_gab], writes=[b_beta])
                    S.op("dve", lambda: nc.vector.tensor_scalar(out=nbeta[:], in0=beta[:], scalar1=-1.0, scalar2=None, op0=ALU.mult), reads=[b_beta], writes=[b_nbeta])
                    for c in range(NCH):
                        for sd in range(2):
                            S.op("pe", lambda c=c, sd=sd: nc.tensor.matmul(PS[2][0:64, c * 8 + sd * 4:c * 8 + sd * 4 + 4], lhsT=mist[:, sd, :], rhs=g_t[:, c, sd * 4:sd * 4 + 4], start=True, stop=True),
                                 reads=[b_mist, b_g], writes=[PB[2]])
                            S.op("pe", lambda c=c, sd=sd: nc.tensor.matmul(PS[3][0:64, c * 8 + sd * 4:c * 8 + sd * 4 + 4], lhsT=mast[:, sd, :], rhs=g_t[:, c, sd * 4:sd * 4 + 4], start=True, stop=True),
                                 reads=[b_mast, b_g], writes=[PB[3]])
                        S.op("pe", lambda c=c: nc.tensor.matmul(PS[4][:, c * 8:c * 8 + 8], lhsT=onesf[0:64, :], rhs=g_t[:, c, :], start=True, stop=True), reads=[b_onesf, b_g], writes=[PB[4]])
                    S.op("act", lambda: nc.scalar.activation(out=egc[:].rearrange("p c x -> p (c x)"), in_=PS[2][0:64, 0:NCH * 8], func=AF.Exp), reads=[PB[2]], writes=[b_egc])
                    S.op("act", lambda: nc.scalar.activation(out=ekd[:].rearrange("p c x -> p (c x)"), in_=PS[3][0:64, 0:NCH * 8], func=AF.Exp), reads=[PB[3]], writes=[b_ekd])
                    S.op("act", lambda: nc.scalar.activation(out=egl[:].rearrange("p c x -> p (c x)"), in_=PS[4][:, 0:NCH * 8], func=AF.Exp), reads=[PB[4]], writes=[b_egl])
                    S.op("dve", lambda: nc.vector.tensor_scalar(out=negc[:], in0=egc[:], scalar1=-1.0, scalar2=None, op0=ALU.mult), reads=[b_egc], writes=[b_negc])
                    S.barrier()
                if cfg.get("gdn_stop") == 1:
                    S.barrier()
                    return

                for pr in range(2):
                    with contextlib.ExitStack() as st:
                        q_fm = [tl(st, "g_q%d" % i, [128, T], BF16) for i in range(2)]
                        k_fm = [tl(st, "g_k%d" % i, [128, T], BF16) for i in range(2)]
                        k_tm = [tl(st, "g_ktm%d" % i, [64, NCH, 128], BF16) for i in range(2)]
                        v_tm = [tl(st, "g_vtm%d" % i, [64, NCH, 128], BF16) for i in range(2)]
                        aqkT, b_aqkT = tl(st, "g_aqkT", [64, NCH, 4, 64], BF16)
                        R5b, b_R5b = tl(st, "g_R5b", [64, NCH, 4, 64], BF16)
                        with contextlib.ExitStack() as st2:
                            wc = [tl(st2, "g_wc%d" % i, [128, KC, 128], BF16) for i in range(2)]
                            zpad, b_zp = tl(st2, "g_zpad", [128, T + 8], F32)
                            acc, b_acc = tl(st2, "g_acc", [128, T + 8], F32)
                            xs, b_xs = tl(st2, "g_xs", [128, T], F32)
                            sqb, b_sqb = tl(st2, "g_sq", [128, T], BF16)
                            vfm, b_vfm = tl(st2, "g_vfm", [128, T], BF16)
                            r0, b_r0 = tl(st2, "g_r0", [128, 512], F32)
                            r1, b_r1 = tl(st2, "g_r1", [128, 512], F32)
                            S.op("dve", lambda: nc.vector.memset(zpad[:], 0.0), writes=[b_zp])
                            wcnt = 0
                            items = [(hh, part) for hh in range(2) for part in range(3)]

                            def load_wc(k):
                                hh_, part_ = items[k]
                                ch_ = part_ * 4 + pr * 2 + hh_
                                wct_, b_wc_ = wc[k % 2]
                                S.dma(wct_[:], winv[:, :, O_GDN + ch_ * 128:O_GDN + (ch_ + 1) * 128], writes=[b_wc_], q="pool")
                            load_wc(0)
                            for hh in range(2):
                                hd = pr * 2 + hh
                                for part in range(3):
                                    ch = part * 4 + hd
                                    wct, b_wc = wc[wcnt % 2]
                                    wcnt += 1
                                    if wcnt < len(items):
                                        load_wc(wcnt)
                                    for gi, (t0, n) in enumerate(GROUPS):
                                        pb = gi % 2
                                        for kc in range(KC):
                                            S.op("pe", lambda kc=kc, pb=pb, wct=wct: nc.tensor.matmul(PS[pb][:, 0:n], lhsT=wct[:, kc, :], rhs=h_fm[:, kc, t0:t0 + n], start=(kc == 0), stop=(kc == KC - 1)),
                                                 reads=[b_wc, b_hfm], writes=[PB[pb]])
                                        z0 = 2 + t0 if gi == 0 else 6 + t0
                                        S.op("act", lambda pb=pb, z0=z0: nc.scalar.copy(out=zpad[:, z0:z0 + n], in_=PS[pb][:, 0:n]), reads=[PB[pb]], writes=[b_zp])
                                    NW = T + 4
                                    S.op("dve", lambda ch=ch: nc.vector.tensor_scalar(out=acc[:, 2:2 + NW], in0=zpad[:, 0:NW], scalar1=cw[:, ch, 0:1], scalar2=None, op0=ALU.mult),
                                         reads=[b_zp, b_cw], writes=[b_acc])
                                    for tau in range(1, 5):
                                        S.op("dve", lambda ch=ch, tau=tau: nc.vector.scalar_tensor_tensor(out=acc[:, 2:2 + NW], in0=zpad[:, tau:tau + NW], scalar=cw[:, ch, tau:tau + 1], in1=acc[:, 2:2 + NW],
                                                                                                        op0=ALU.mult, op1=ALU.add), reads=[b_zp, b_cw, b_acc], writes=[b_acc])
                                    if part == 2:
                                        S.op("act", lambda: nc.scalar.activation(out=vfm[:, 0:CTX], in_=acc[:, 2:2 + CTX], func=AF.Silu), reads=[b_acc], writes=[b_vfm])
                                        S.op("act", lambda: nc.scalar.activation(out=vfm[:, CTX:T], in_=acc[:, 6 + CTX:6 + T], func=AF.Silu), reads=[b_acc], writes=[b_vfm])
                                        srcs = [(vfm, b_vfm, v_tm[hh])]
                                    else:
                                        S.op("act", lambda: nc.scalar.activation(out=xs[:, 0:CTX], in_=acc[:, 2:2 + CTX], func=AF.Silu), reads=[b_acc], writes=[b_xs])
                                        S.op("act", lambda: nc.scalar.activation(out=xs[:, CTX:T], in_=acc[:, 6 + CTX:6 + T], func=AF.Silu), reads=[b_acc], writes=[b_xs])
                                        S.op("act", lambda: nc.scalar.activation(out=sqb[:], in_=xs[:], func=AF.Square), reads=[b_xs], writes=[b_sqb])
                                        dst, b_dst = (q_fm if part == 0 else k_fm)[hh]
                                        for gi, (t0, n) in enumerate(GROUPS):
                                            pb = 2 + gi % 2
                                            S.op("pe", lambda pb=pb: nc.tensor.matmul(PS[pb][:, 0:n], lhsT=onesb[:], rhs=sqb[:, t0:t0 + n], start=True, stop=True), reads=[b_onesb, b_sqb], writes=[PB[pb]])
                                            S.op("act", lambda pb=pb: nc.scalar.activation(out=r0[:, 0:n], in_=PS[pb][:, 0:n], func=AF.Sqrt, bias=epsb[:, 0:1]), reads=[PB[pb], b_eps], writes=[b_r0])
                                            S.op("dve", lambda: nc.vector.reciprocal(out=r1[:, 0:n], in_=r0[:, 0:n]), reads=[b_r0], writes=[b_r1])
                                            S.op("dve", lambda dst=dst: nc.vector.scalar_tensor_tensor(out=dst[:, t0:t0 + n], in0=xs[:, t0:t0 + n], scalar=(128 ** -0.5 if part == 0 else 1.0), in1=r1[:, 0:n],
                                                                                                   op0=ALU.mult, op1=ALU.mult), reads=[b_xs, b_r1], writes=[b_dst])
                                        srcs = [(dst, b_dst, k_tm[hh])] if part == 1 else []
                                    for (src, b_src, (dtm, b_dtm)) in srcs:
                                        pvb = PS[4][:].bitcast(BF16)
                                        pvb2 = PS[5][:].bitcast(BF16)
                                        for c8 in range(0, NCH, 8):
                                            nb = min(8, NCH - c8)
                                            pv = pvb if (c8 // 8) % 2 == 0 else pvb2
                                            pbi = 4 + (c8 // 8) % 2
                                            for j in range(nb):
                                                c = c8 + j
                                                S.op("pe", lambda c=c, j=j, pv=pv, src=src: nc.tensor.transpose(out=pv[0:64, j * 128:(j + 1) * 128], in_=src[:, c * 64:(c + 1) * 64], identity=identb[:]),
                                                     reads=[b_src, b_identb], writes=[PB[pbi]])
                                            S.op("act", lambda c8=c8, nb=nb, pv=pv, dtm=dtm: nc.scalar.copy(out=dtm[:, c8:c8 + nb, :], in_=pv[0:64, 0:nb * 128].rearrange("p (j x) -> p j x", j=nb)),
                                                 reads=[PB[pbi]], writes=[b_dtm])
                            S.barrier()
                        if cfg.get("gdn_stop") == 2:
                            S.barrier()
                            return
                        with contextlib.ExitStack() as st2:
                            NCB = 2
                            NB = NCB * 4
                            mistF, b_mistF = tl(st2, "g_mistF", [64, NCB, 2, 2, 64], F32)
                            mastF, b_mastF = tl(st2, "g_mastF", [64, NCB, 2, 2, 64], F32)
                            for cj in range(NCB):
                                for hh in range(2):
                                    S.op("dve", lambda cj=cj, hh=hh: nc.vector.tensor_copy(out=mistF[:, cj, :, hh, :], in_=mist[:]), reads=[b_mist], writes=[b_mistF])
                                    S.op("dve", lambda cj=cj, hh=hh: nc.vector.tensor_copy(out=mastF[:, cj, :, hh, :], in_=mast[:]), reads=[b_mast], writes=[b_mastF])
                            fl = lambda t_: t_[:].rearrange("p b x -> p (b x)")
                            v3 = lambda t_: t_[:].rearrange("p (cs h) x -> p cs h x", h=2)
                            mI3 = mistF[:].rearrange("p c s h x -> p (c s) h x")
                            mA3 = mastF[:].rearrange("p c s h x -> p (c s) h x")
                            lI, b_lI = tl(st2, "g_lI", [64, NB, 64], F32)
                            lA, b_lA = tl(st2, "g_lA", [64, NB, 64], F32)
                            Dec, b_Dec = tl(st2, "g_Dec", [64, NB, 64], F32)
                            DecT, b_DecT = tl(st2, "g_DecT", [64, NB, 64], F32)
                            t1, b_t1 = tl(st2, "g_t1", [64, NB, 64], F32)
                            Pm = [tl(st2, "g_P%d" % i, [64, NB, 64], BF16) for i in range(2)]
                            Qm = [tl(st2, "g_Q%d" % i, [64, NB, 64], BF16) for i in range(2)]
                            Rbm = [tl(st2, "g_Rb%d" % i, [64, NB, 64], BF16) for i in range(2)]
                            Rm = [tl(st2, "g_R%d" % i, [64, NB, 64], F32) for i in range(2)]

                            def gcols(tile_, c0):
                                return tile_[:, c0:c0 + NCB, :].rearrange("p c (s h) -> p (c s) h", s=2)[:, :, pr * 2:pr * 2 + 2]

                            for c0 in range(0, NCH, NCB):
                                for cj in range(NCB):
                                    c = c0 + cj
                                    cs = slice(c * 64, (c + 1) * 64)
                                    for b in range(4):
                                        hh = b % 2
                                        bb = cj * 4 + b
                                        kf, b_kf = k_fm[hh]
                                        qf, b_qf = q_fm[hh]
                                        S.op("pe", lambda bb=bb, kf=kf, cs=cs: nc.tensor.matmul(PS[0][0:64, bb * 64:(bb + 1) * 64], lhsT=kf[:, cs], rhs=kf[:, cs], start=True, stop=True), reads=[b_kf], writes=[PB[0]])
                                        S.op("pe", lambda bb=bb, kf=kf, qf=qf, cs=cs: nc.tensor.matmul(PS[7][0:64, bb * 64:(bb + 1) * 64], lhsT=kf[:, cs], rhs=qf[:, cs], start=True, stop=True),
                                             reads=[b_kf, b_qf], writes=[PB[7]])
                                g3 = gcols(g_t, c0).unsqueeze(3).to_broadcast([64, 2 * NCB, 2, 64])
                                S.op("dve", lambda g3=g3: nc.vector.tensor_tensor(out=v3(lI), in0=mI3, in1=g3, op=ALU.mult), reads=[b_mistF, b_g], writes=[b_lI])
                                S.op("dve", lambda g3=g3: nc.vector.tensor_tensor(out=v3(lA), in0=mA3, in1=g3, op=ALU.mult), reads=[b_mastF, b_g], writes=[b_lA])
                                for bb in range(NB):
                                    sd = (bb % 4) // 2
                                    S.op("pe", lambda bb=bb, sd=sd: nc.tensor.matmul(PS[1][0:64, bb * 64:(bb + 1) * 64], lhsT=lI[:, bb, :], rhs=mast[:, sd, :], start=True, stop=True), reads=[b_lI, b_mast], writes=[PB[1]])
                                    S.op("pe", lambda bb=bb, sd=sd: nc.tensor.matmul(PS[2][0:64, bb * 64:(bb + 1) * 64], lhsT=lA[:, bb, :], rhs=mist[:, sd, :], start=True, stop=True), reads=[b_lA, b_mist], writes=[PB[2]])
                                S.op("act", lambda: nc.scalar.activation(out=fl(Dec), in_=PS[1][0:64, :], func=AF.Exp), reads=[PB[1]], writes=[b_Dec])
                                S.op("act", lambda: nc.scalar.activation(out=fl(DecT), in_=PS[2][0:64, :], func=AF.Exp), reads=[PB[2]], writes=[b_DecT])
                                P0, b_P0 = Pm[0]
                                Q0, b_Q0 = Qm[0]
                                R0, b_R0 = Rm[0]
                                S.op("dve", lambda: nc.vector.tensor_tensor(out=fl(t1), in0=PS[0][0:64, :], in1=fl(Dec), op=ALU.mult), reads=[PB[0], b_Dec], writes=[b_t1])
                                S.op("dve", lambda: nc.vector.tensor_tensor(out=fl(t1), in0=fl(t1), in1=mastF[:].rearrange("p c s h x -> p (c s h x)"), op=ALU.mult), reads=[b_t1, b_mastF], writes=[b_t1])
                                S.op("dve", lambda c0=c0: nc.vector.tensor_tensor(out=v3(P0), in0=v3(t1), in1=gcols(nbeta, c0).unsqueeze(3).to_broadcast([64, 2 * NCB, 2, 64]), op=ALU.mult),
                                     reads=[b_t1, b_nbeta], writes=[b_P0])
                                S.op("dve", lambda: nc.vector.tensor_tensor(out=fl(DecT), in0=PS[7][0:64, :], in1=fl(DecT), op=ALU.mult), reads=[PB[7], b_DecT], writes=[b_DecT])
                                S.op("dve", lambda c0=c0: nc.vector.tensor_tensor(out=aqkT[:, c0:c0 + NCB, :, :].rearrange("p c b x -> p (c b x)"), in0=fl(DecT), in1=mistF[:].rearrange("p c s h x -> p (c s h x)"), op=ALU.mult),
                                     reads=[b_DecT, b_mistF], writes=[b_aqkT])
                                pv3b = PS[3][:].bitcast(BF16)
                                for bb in range(NB):
                                    S.op("pe", lambda bb=bb: nc.tensor.transpose(out=pv3b[0:64, bb * 64:(bb + 1) * 64], in_=P0[:, bb, :], identity=identb[0:64, 0:64]), reads=[b_P0, b_identb], writes=[PB[3]])
                                S.op("act", lambda: nc.scalar.copy(out=fl(Q0), in_=pv3b[0:64, 0:NB * 64]), reads=[PB[3]], writes=[b_Q0])
                                S.op("dve", lambda: nc.vector.tensor_tensor(out=R0[:], in0=Q0[:], in1=identf[0:64, 0:64].unsqueeze(1).to_broadcast([64, NB, 64]), op=ALU.add),
                                     reads=[b_Q0, b_identf], writes=[b_R0])
                                S.op("act", lambda: nc.scalar.copy(out=fl(Rbm[0][0]), in_=fl(R0)), reads=[b_R0], writes=[Rbm[0][1]])
                                for k in range(1, 6):
                                    Pp, b_Pp = Pm[(k - 1) % 2]
                                    Qp, b_Qp = Qm[(k - 1) % 2]
                                    Rp, b_Rp = Rm[(k - 1) % 2]
                                    Rbp, b_Rbp = Rbm[(k - 1) % 2]
                                    Pn, b_Pn = Pm[k % 2]
                                    Qn, b_Qn = Qm[k % 2]
                                    Rn, b_Rn = Rm[k % 2]
                                    Rbn, b_Rbn = Rbm[k % 2]
                                    for bb in range(NB):
                                        S.op("pe", lambda bb=bb: nc.tensor.matmul(PS[4][0:64, bb * 64:(bb + 1) * 64], lhsT=Qp[:, bb, :], rhs=Pp[:, bb, :], start=True, stop=True), reads=[b_Qp, b_Pp], writes=[PB[4]])
                                    if k < 5:
                                        for bb in range(NB):
                                            S.op("pe", lambda bb=bb: nc.tensor.matmul(PS[5][0:64, bb * 64:(bb + 1) * 64], lhsT=Pp[:, bb, :], rhs=Qp[:, bb, :], start=True, stop=True), reads=[b_Qp, b_Pp], writes=[PB[5]])
                                    S.op("act", lambda: nc.scalar.copy(out=fl(Pn), in_=PS[4][0:64, :]), reads=[PB[4]], writes=[b_Pn])
                                    if k < 5:
                                        S.op("dve", lambda: nc.vector.tensor_copy(out=fl(Qn), in_=PS[5][0:64, :]), reads=[PB[5]], writes=[b_Qn])
                                    for bb in range(NB):
                                        S.op("pe", lambda bb=bb: nc.tensor.matmul(PS[6][0:64, bb * 64:(bb + 1) * 64], lhsT=Pn[:, bb, :], rhs=Rbp[:, bb, :], start=True, stop=True), reads=[b_Pn, b_Rbp], writes=[PB[6]])
                                    S.op("dve", lambda: nc.vector.tensor_tensor(out=fl(Rn), in0=fl(Rp), in1=PS[6][0:64, :], op=ALU.add), reads=[b_Rp, PB[6]], writes=[b_Rn])
                                    if k < 5:
                                        S.op("act", lambda: nc.scalar.copy(out=fl(Rbn), in_=fl(Rn)), reads=[b_Rn], writes=[b_Rbn])
                                    if k == 5:
                                        S.op("dve", lambda c0=c0: nc.vector.tensor_tensor(out=R5b[:, c0:c0 + NCB, :, :].rearrange("p c (s h) x -> p (c s) h x", s=2), in0=v3(Rn), in1=gcols(beta, c0).unsqueeze(3).to_broadcast([64, 2 * NCB, 2, 64]), op=ALU.mult),
                                             reads=[b_Rn, b_beta], writes=[b_R5b])
                            S.barrier()
                        if cfg.get("gdn_stop") == 3:
                            S.barrier()
                            return
                        with contextlib.ExitStack() as st2:
                            Sst = [tl(st2, "g_S%d" % i, [128, 128], F32) for i in range(4)]
                            Sbb = [[tl(st2, "g_Sb%d_%d" % (i, j), [128, 128], BF16) for j in range(2)] for i in range(4)]
                            Xs = [tl(st2, "g_X%d" % i, [64, 128], BF16) for i in range(4)]
                            vnb = [tl(st2, "g_vn%d" % i, [64, 128], BF16) for i in range(4)]
                            vnk = [tl(st2, "g_vk%d" % i, [64, 128], BF16) for i in range(4)]
                            tmpo = [tl(st2, "g_to%d" % i, [64, 128], F32) for i in range(4)]
                            oo = [[tl(st2, "g_oo%d_%d" % (i, j), [64, 128], F32) for j in range(2)] for i in range(4)]
                            b_raw = Buf("gdn_raw")
                            orders = [list(range(NCH)), [3, 2, 1, 0] + list(range(NCH - 1, 3, -1))]
                            for idx in range(NCH):
                                ch = []
                                for b in range(4):
                                    sd, hh = b // 2, b % 2
                                    hd = pr * 2 + hh
                                    c = orders[sd][idx]
                                    ch.append(dict(b=b, sd=sd, hh=hh, hd=hd, col=sd * 4 + hd, c=c, cs=slice(c * 64, (c + 1) * 64), pa=2 * b, pc=2 * b + 1,
                                                   kf=k_fm[hh], qf=q_fm[hh], vt=v_tm[hh], ktm=k_tm[hh], X=Xs[b], vn=vnb[b], vk=vnk[b], to=tmpo[b], o=oo[b][idx % 2], S=Sst[b],
                                                   sb=Sbb[b][idx % 2], nsb=Sbb[b][(idx + 1) % 2]))
                                if idx > 0:
                                    for d in ch:
                                        S.op("pe", lambda d=d: nc.tensor.matmul(PS[d["pa"]][0:64, 0:128], lhsT=d["kf"][0][:, d["cs"]], rhs=d["sb"][0][:], start=True, stop=True), reads=[d["kf"][1], d["sb"][1]], writes=[PB[d["pa"]]])
                                        S.op("pe", lambda d=d: nc.tensor.matmul(PS[d["pa"]][0:64, 128:256], lhsT=d["qf"][0][:, d["cs"]], rhs=d["sb"][0][:], start=True, stop=True), reads=[d["qf"][1], d["sb"][1]], writes=[PB[d["pa"]]])
                                    for d in ch:
                                        S.op("dve", lambda d=d: nc.vector.scalar_tensor_tensor(out=d["X"][0][:], in0=PS[d["pa"]][0:64, 0:128], scalar=negc[:, d["c"], d["col"]:d["col"] + 1], in1=d["vt"][0][:, d["c"], :], op0=ALU.mult, op1=ALU.add),
                                             reads=[PB[d["pa"]], b_negc, d["vt"][1]], writes=[d["X"][1]])
                                for d in ch:
                                    if idx > 0:
                                        xin_ap, xr = d["X"][0][:], [d["X"][1]]
                                    else:
                                        xin_ap, xr = d["vt"][0][:, d["c"], :], [d["vt"][1]]
                                    S.op("pe", lambda d=d, xin_ap=xin_ap: nc.tensor.matmul(PS[d["pa"]][0:64, 256:384], lhsT=R5b[:, d["c"], d["b"], :], rhs=xin_ap, start=True, stop=True), reads=[b_R5b] + xr, writes=[PB[d["pa"]]])
                                for d in ch:
                                    S.op("act", lambda d=d: nc.scalar.copy(out=d["vn"][0][:], in_=PS[d["pa"]][0:64, 256:384]), reads=[PB[d["pa"]]], writes=[d["vn"][1]])
                                    S.op("dve", lambda d=d: nc.vector.tensor_scalar(out=d["vk"][0][:], in0=PS[d["pa"]][0:64, 256:384], scalar1=ekd[:, d["c"], d["col"]:d["col"] + 1], scalar2=None, op0=ALU.mult), reads=[PB[d["pa"]], b_ekd], writes=[d["vk"][1]])
                                for d in ch:
                                    S.op("pe", lambda d=d: nc.tensor.matmul(PS[d["pa"]][0:64, 384:512], lhsT=aqkT[:, d["c"], d["b"], :], rhs=d["vn"][0][:], start=True, stop=True), reads=[b_aqkT, d["vn"][1]], writes=[PB[d["pa"]]])
                                    if idx < NCH - 1:
                                        S.op("pe", lambda d=d: nc.tensor.matmul(PS[d["pc"]][:, 0:128], lhsT=d["ktm"][0][:, d["c"], :], rhs=d["vk"][0][:], start=True, stop=True), reads=[d["ktm"][1], d["vk"][1]], writes=[PB[d["pc"]]])
                                if idx < NCH - 1:
                                    for d in ch:
                                        if idx == 0:
                                            S.op("dve", lambda d=d: nc.vector.tensor_copy(out=d["S"][0][:], in_=PS[d["pc"]][:, 0:128]), reads=[PB[d["pc"]]], writes=[d["S"][1]])
                                        else:
                                            S.op("dve", lambda d=d: nc.vector.scalar_tensor_tensor(out=d["S"][0][:], in0=d["S"][0][:], scalar=egl[:, d["c"], d["col"]:d["col"] + 1], in1=PS[d["pc"]][:, 0:128], op0=ALU.mult, op1=ALU.add),
                                                 reads=[d["S"][1], b_egl, PB[d["pc"]]], writes=[d["S"][1]])
                                    for d in ch:
                                        S.op("act", lambda d=d: nc.scalar.copy(out=d["nsb"][0][:], in_=d["S"][0][:]), reads=[d["S"][1]], writes=[d["nsb"][1]])
                                for d in ch:
                                    if idx > 0:
                                        S.op("act", lambda d=d: nc.scalar.copy(out=d["to"][0][:], in_=PS[d["pa"]][0:64, 384:512]), reads=[PB[d["pa"]]], writes=[d["to"][1]])
                                        S.op("dve", lambda d=d: nc.vector.scalar_tensor_tensor(out=d["o"][0][:], in0=PS[d["pa"]][0:64, 128:256], scalar=egc[:, d["c"], d["col"]:d["col"] + 1], in1=d["to"][0][:], op0=ALU.mult, op1=ALU.add),
                                             reads=[PB[d["pa"]], b_egc, d["to"][1]], writes=[d["o"][1]])
                                    else:
                                        S.op("act", lambda d=d: nc.scalar.copy(out=d["o"][0][:], in_=PS[d["pa"]][0:64, 384:512]), reads=[PB[d["pa"]]], writes=[d["o"][1]])
                                    S.dma(gdn_raw_d[d["sd"], d["c"] * 64:(d["c"] + 1) * 64, d["hd"] * 128:(d["hd"] + 1) * 128], d["o"][0][:], reads=[d["o"][1]], writes=[b_raw])
                            S.barrier()
                if cfg.get("gdn_stop") == 4:
                    S.barrier()
                    return
                with contextlib.ExitStack() as st:
                    wgg, b_wgg = tl(st, "g_wgg", [128, KC, 512], BF16)
                    S.dma(wgg[:], winv[:, :, O_GG:O_GG + 512], writes=[b_wgg], q="pool")
                    gnw, b_gnw = tl(st, "g_gnw", [128, 128], F32)
                    S.dma(gnw[:], I["gdn_norm"][l:l + 1, :].partition_broadcast(128), writes=[b_gnw])
                    of_ = [tl(st, "g_of%d" % i, [128, 512], F32) for i in range(2)]
                    ob_ = [tl(st, "g_ob%d" % i, [128, 512], F32) for i in range(2)]
                    sqt = [tl(st, "g_sqt%d" % i, [128, 512], F32) for i in range(2)]
                    gt_ = [tl(st, "g_gt%d" % i, [128, 512], F32) for i in range(2)]
                    ms = [tl(st, "g_ms%d" % i, [128, 4], F32) for i in range(2)]
                    obf = [tl(st, "g_obf%d" % i, [128, 512], BF16) for i in range(2)]
                    ofm = [tl(st, "g_ofm%d" % i, [128, 4, 128], BF16) for i in range(2)]
                    b_go = Buf("gdn_o")
                    hsub = cfg.get("gdn_hsub", 99)
                    for i in range(cfg.get("gdn_hnt", NT)):
                        a, b_a = of_[i % 2]
                        bb, b_bb = ob_[i % 2]
                        sq_t, b_sq = sqt[i % 2]
                        g_tl, b_gt = gt_[i % 2]
                        ms_t, b_ms = ms[i % 2]
                        obf_t, b_obf = obf[i % 2]
                        ofm_t, b_ofm = ofm[i % 2]
                        ts_ = slice(i * 128, (i + 1) * 128)
                        S.dma(a[:], gdn_raw_d[0, ts_, :], writes=[b_a])
                        S.dma(bb[:], gdn_raw_d[1, ts_, :], writes=[b_bb])
                        pb = i % 2
                        for kc in range(KC):
                            S.op("pe", lambda kc=kc, pb=pb: nc.tensor.matmul(PS[pb][:, :], lhsT=h_fm[:, kc, ts_], rhs=wgg[:, kc, :], start=(kc == 0), stop=(kc == KC - 1)), reads=[b_hfm, b_wgg], writes=[PB[pb]])
                        S.op("act", lambda: nc.scalar.activation(out=g_tl[:], in_=PS[pb][:, :], func=AF.Silu), reads=[PB[pb]], writes=[b_gt])
                        if hsub < 1:
                            continue
                        S.op("dve", lambda: nc.vector.tensor_tensor(out=a[:], in0=a[:], in1=bb[:], op=ALU.add), reads=[b_a, b_bb], writes=[b_a])
                        S.op("act", lambda: nc.scalar.activation(out=sq_t[:], in_=a[:], func=AF.Square), reads=[b_a], writes=[b_sq])
                        S.op("dve", lambda: nc.vector.tensor_reduce(out=ms_t[:], in_=sq_t[:].rearrange("p (h x) -> p h x", h=4), axis=AX.X, op=ALU.add), reads=[b_sq], writes=[b_ms])
                        S.op("act", lambda: nc.scalar.activation(out=ms_t[:], in_=ms_t[:], func=AF.Sqrt, scale=1.0 / 128, bias=epsb[:, 0:1]), reads=[b_ms, b_eps], writes=[b_ms])
                        S.op("dve", lambda: nc.vector.reciprocal(out=ms_t[:], in_=ms_t[:]), reads=[b_ms], writes=[b_ms])
                        if hsub < 2:
                            continue
                        a3 = a[:].rearrange("p (h x) -> p h x", h=4)
                        S.op("dve", lambda: nc.vector.tensor_tensor(out=a3, in0=a3, in1=ms_t[:].unsqueeze(2).to_broadcast([128, 4, 128]), op=ALU.mult), reads=[b_a, b_ms], writes=[b_a])
                        S.op("dve", lambda: nc.vector.tensor_tensor(out=a3, in0=a3, in1=gnw[:].unsqueeze(1).to_broadcast([128, 4, 128]), op=ALU.mult), reads=[b_a, b_gnw], writes=[b_a])
                        S.op("dve", lambda: nc.vector.tensor_tensor(out=obf_t[:], in0=a[:], in1=g_tl[:], op=ALU.mult), reads=[b_a, b_gt], writes=[b_obf])
                        if hsub < 3:
                            continue
                        pv = PS[2 + pb][:].bitcast(BF16)
                        for hd in range(4):
                            S.op("pe", lambda hd=hd: nc.tensor.transpose(out=pv[:, hd * 128:(hd + 1) * 128], in_=obf_t[:, hd * 128:(hd + 1) * 128], identity=identb[:]), reads=[b_obf, b_identb], writes=[PB[2 + pb]])
                        S.op("act", lambda: nc.scalar.copy(out=ofm_t[:], in_=pv[:, 0:512].rearrange("p (h x) -> p h x", h=4)), reads=[PB[2 + pb]], writes=[b_ofm])
                        if hsub < 4:
                            continue
                        S.dma(gdn_o_d[:, :, ts_].rearrange("h d t -> d h t"), ofm_t[:], reads=[b_ofm], writes=[b_go])
                    S.barrier()


        def ln_tile(st_tiles, i, f_halves, f_bufs, prm, out_final):
            s = 1 if i < 2 else 0
            x_t, b_x = st_tiles["x"][i % 2]
            t_t, b_t = st_tiles["t"][i % 2]
            h_t, b_h = st_tiles["h"][i % 2]
            stt, b_st = st_tiles["st"][i % 2]
            mv, b_mv = st_tiles["mv"][i % 2]
            ts_ = slice(i * 128, (i + 1) * 128)
            S.dma(x_t[:], xres_d[ts_, :], reads=[b_xres[i]], writes=[b_x])
            gate_t, b_gate = prm["gate"][s]
            for hf in range(2):
                hs = slice(hf * 512, (hf + 1) * 512)
                S.op("dve", lambda hf=hf, hs=hs: nc.vector.tensor_tensor(out=t_t[:, hs], in0=f_halves[hf], in1=gate_t[:, hs], op=ALU.mult), reads=[f_bufs[hf], b_gate], writes=[b_t])
            S.op("dve", lambda: nc.vector.scalar_tensor_tensor(out=x_t[:], in0=x_t[:], scalar=ALPHA, in1=t_t[:], op0=ALU.mult, op1=ALU.add), reads=[b_x, b_t], writes=[b_x])
            for hf in range(2):
                S.op("dve", lambda hf=hf: nc.vector.bn_stats(out=stt[:, hf, :], in_=x_t[:, hf * 512:(hf + 1) * 512]), reads=[b_x], writes=[b_st])
            S.op("dve", lambda: nc.vector.bn_aggr(out=mv[:, 0:2], in_=stt[:].rearrange("p a b -> p (a b)")), reads=[b_st], writes=[b_mv])
            S.op("act", lambda: nc.scalar.activation(out=mv[:, 2:3], in_=mv[:, 1:2], func=AF.Sqrt, bias=epsb[:, 0:1]), reads=[b_mv, b_eps], writes=[b_mv])
            S.op("dve", lambda: nc.vector.reciprocal(out=mv[:, 3:4], in_=mv[:, 2:3]), reads=[b_mv], writes=[b_mv])
            S.op("dve", lambda: nc.vector.tensor_scalar(out=x_t[:], in0=x_t[:], scalar1=mv[:, 0:1], scalar2=mv[:, 3:4], op0=ALU.subtract, op1=ALU.mult), reads=[b_x, b_mv], writes=[b_x])
            S.op("pool", lambda: nc.gpsimd.tensor_tensor(out=x_t[:], in0=x_t[:], in1=prm["g"][0][:], op=ALU.mult), reads=[b_x, prm["g"][1]], writes=[b_x])
            S.op("pool", lambda: nc.gpsimd.tensor_tensor(out=x_t[:], in0=x_t[:], in1=prm["b"][0][:], op=ALU.add), reads=[b_x, prm["b"][1]], writes=[b_x])
            if out_final:
                S.dma(out_d[(i - 2) * 128:(i - 1) * 128, :], x_t[:], reads=[b_x])
                return
            S.dma(xres_d[ts_, :], x_t[:], reads=[b_x], writes=[b_xres[i]])
            sc_t, b_sc = prm["sc"][s]
            sh_t, b_sh = prm["sh"][s]
            S.op("dve", lambda: nc.vector.tensor_tensor(out=t_t[:], in0=x_t[:], in1=sc_t[:], op=ALU.mult), reads=[b_x, b_sc], writes=[b_t])
            S.op("pool", lambda: nc.gpsimd.tensor_tensor(out=h_t[:], in0=t_t[:], in1=sh_t[:], op=ALU.add), reads=[b_t, b_sh], writes=[b_h])
            to_fm(h_t, b_h, i, 6 + i % 2)

        def ln_setup(st, l_mod, jgate, ln_g, ln_b, l, jsh, jsc, need_mod):
            tiles = {
                "x": [tl(st, "ln_x%d" % i, [128, D], F32) for i in range(2)],
                "t": [tl(st, "ln_t%d" % i, [128, D], F32) for i in range(2)],
                "h": [tl(st, "ln_h%d" % i, [128, D], BF16) for i in range(2)],
                "st": [tl(st, "ln_st%d" % i, [128, 2, 6], F32) for i in range(2)],
                "mv": [tl(st, "ln_mv%d" % i, [128, 4], F32) for i in range(2)],
            }
            prm = {"gate": [load_bc(st, "ln_gate%d" % s_, l, s_, jgate) for s_ in range(2)],
                   "g": load_vec_bc(st, "ln_g", I[ln_g][l:l + 1, :]),
                   "b": load_vec_bc(st, "ln_b", I[ln_b][l:l + 1, :])}
            if need_mod:
                prm["sh"] = [load_bc(st, "ln_sh%d" % s_, l_mod, s_, jsh) for s_ in range(2)]
                prm["sc"] = [load_bc(st, "ln_sc%d" % s_, l_mod, s_, jsc, plus_one=True) for s_ in range(2)]
            return tiles, prm

        def stage_merge(l, last):
            winv = I["w_in"][l].rearrange("(kc p) n -> p kc n", p=128)
            groups = GROUPS[1:] if last else GROUPS
            tiles_i = range(2, NT) if last else range(NT)
            with contextlib.ExitStack() as st:
                y_fm, b_y = tl(st, "y_fm", [128, KC, T], BF16)
                with contextlib.ExitStack() as st2:
                    mo, b_mo = tl(st2, "m_mo", [64, 8, T], BF16)
                    ho, b_ho = tl(st2, "m_ho", [128, 4, T], BF16)
                    go, b_go = tl(st2, "m_go", [128, 4, T], BF16)
                    S.dma(mo[:], mla_o_d.rearrange("h d t -> d h t"), writes=[b_mo])
                    S.dma(ho[:], hg_o_d.rearrange("h d t -> d h t"), writes=[b_ho])
                    S.dma(go[:], gdn_o_d.rearrange("h d t -> d h t"), writes=[b_go])
                    wgt = [tl(st2, "m_wg%d" % i, [128, KC, 3, 128], BF16) for i in range(2)]
                    wbr = [tl(st2, "m_wbr%d" % i, [128, 2, 4, 128], BF16) for i in range(2)]
                    wbm = [tl(st2, "m_wbm%d" % i, [64, 8, 128], BF16) for i in range(2)]
                    sg = [tl(st2, "m_sg%d" % i, [128, 512], F32) for i in range(3)]
                    ta, b_ta = tl(st2, "m_ta", [128, 512], F32)
                    tb, b_tb = tl(st2, "m_tb", [128, 512], F32)
                    mcnt = [0]
                    def load_dc(dc):
                        wg_t, b_wg = wgt[dc % 2]
                        wbr_t, b_wbr = wbr[dc % 2]
                        wbm_t, b_wbm = wbm[dc % 2]
                        for n_ in range(3):
                            c0 = O_GATES + n_ * D + dc * 128
                            S.dma(wg_t[:, :, n_, :], winv[:, :, c0:c0 + 128], writes=[b_wg], q="pool")
                        for n_ in range(2):
                            S.dma(wbr_t[:, n_, :, :], I["w_branch"][l, n_ + 1].rearrange("(kc p) n -> p kc n", p=128)[:, :, dc * 128:(dc + 1) * 128], writes=[b_wbr], q="pool")
                        S.dma(wbm_t[:], I["w_branch"][l, 0].rearrange("(h p) n -> p h n", p=64)[:, :, dc * 128:(dc + 1) * 128], writes=[b_wbm], q="pool")
                    load_dc(0)
                    for dc in range(KC):
                        wg_t, b_wg = wgt[dc % 2]
                        wbr_t, b_wbr = wbr[dc % 2]
                        wbm_t, b_wbm = wbm[dc % 2]
                        if dc + 1 < KC:
                            load_dc(dc + 1)
                        for (t0, n) in groups:
                            for n_ in range(3):
                                pg = mcnt[0] % 4
                                pp = 4 + mcnt[0] % 4
                                mcnt[0] += 1
                                for kc in range(KC):
                                    S.op("pe", lambda kc=kc, n_=n_, pg=pg: nc.tensor.matmul(PS[pg][:, 0:n], lhsT=wg_t[:, kc, n_, :], rhs=h_fm[:, kc, t0:t0 + n], start=(kc == 0), stop=(kc == KC - 1)),
                                         reads=[b_wg, b_hfm], writes=[PB[pg]])
                                S.op("act", lambda n_=n_, pg=pg: nc.scalar.activation(out=sg[n_][0][:, 0:n], in_=PS[pg][:, 0:n], func=AF.Sigmoid), reads=[PB[pg]], writes=[sg[n_][1]])
                                if n_ == 0:
                                    for h in range(8):
                                        S.op("pe", lambda h=h, pp=pp: nc.tensor.matmul(PS[pp][:, 0:n], lhsT=wbm_t[:, h, :], rhs=mo[:, h, t0:t0 + n], start=(h == 0), stop=(h == 7)), reads=[b_wbm, b_mo], writes=[PB[pp]])
                                else:
                                    src, b_src = (ho, b_ho) if n_ == 1 else (go, b_go)
                                    for kc in range(4):
                                        S.op("pe", lambda kc=kc, pp=pp, src=src, n_=n_: nc.tensor.matmul(PS[pp][:, 0:n], lhsT=wbr_t[:, n_ - 1, kc, :], rhs=src[:, kc, t0:t0 + n], start=(kc == 0), stop=(kc == 3)), reads=[b_wbr, b_src], writes=[PB[pp]])
                                if n_ == 0:
                                    S.op("dve", lambda pp=pp: nc.vector.tensor_tensor(out=ta[:, 0:n], in0=sg[0][0][:, 0:n], in1=PS[pp][:, 0:n], op=ALU.mult), reads=[sg[0][1], PB[pp]], writes=[b_ta])
                                elif n_ == 1:
                                    S.op("dve", lambda pp=pp: nc.vector.tensor_tensor(out=tb[:, 0:n], in0=sg[1][0][:, 0:n], in1=PS[pp][:, 0:n], op=ALU.mult), reads=[sg[1][1], PB[pp]], writes=[b_tb])
                                    S.op("pool", lambda: nc.gpsimd.tensor_tensor(out=ta[:, 0:n], in0=ta[:, 0:n], in1=tb[:, 0:n], op=ALU.add), reads=[b_ta, b_tb], writes=[b_ta])
                                else:
                                    S.op("dve", lambda pp=pp: nc.vector.tensor_tensor(out=tb[:, 0:n], in0=sg[2][0][:, 0:n], in1=PS[pp][:, 0:n], op=ALU.mult), reads=[sg[2][1], PB[pp], b_ta], writes=[b_tb])
                                    S.op("dve", lambda dc=dc: nc.vector.tensor_tensor(out=y_fm[:, dc, t0:t0 + n], in0=ta[:, 0:n], in1=tb[:, 0:n], op=ALU.add), reads=[b_ta, b_tb], writes=[b_y])
                    S.barrier()
                if "y_fm" in DBG:
                    S.dma(DBG["y_fm"].rearrange("(kc p) t -> p kc t", p=128), y_fm[:], reads=[b_y], q="pool")
                wo, b_wo = tl(st, "m_wo", [128, KC, D], BF16)
                S.dma(wo[:], I["w_out"][l].rearrange("(kc p) n -> p kc n", p=128), writes=[b_wo], q="pool")
                tiles, prm = ln_setup(st, l, 2, "ln1_g", "ln1_b", l, 3, 4, True)
                for i in tiles_i:
                    ts_ = slice(i * 128, (i + 1) * 128)
                    for hf in range(2):
                        pb = 4 + hf
                        for kc in range(KC):
                            S.op("pe", lambda kc=kc, hf=hf, pb=pb: nc.tensor.matmul(PS[pb][:, :], lhsT=y_fm[:, kc, ts_], rhs=wo[:, kc, hf * 512:(hf + 1) * 512], start=(kc == 0), stop=(kc == KC - 1)),
                                 reads=[b_y, b_wo], writes=[PB[pb]])
                    ln_tile(tiles, i, [PS[4][:, :], PS[5][:, :]], [PB[4], PB[5]], prm, False)
                S.barrier()

        def stage_moe(l, last):
            groups = GROUPS[1:] if last else GROUPS
            tiles_i = list(range(2, NT)) if last else list(range(NT))
            with contextlib.ExitStack() as st:
                acc, b_acc = tl(st, "acc", [128, NT, D], F32)
                comb, b_comb = tl(st, "comb", [128, NT, 65], F32)
                S.op("dve", lambda: nc.vector.memset(comb[:, :, 64:65], 1.0), writes=[b_comb])
                with contextlib.ExitStack() as st2:
                    wr, b_wr = tl(st2, "wr", [128, KC, 64], BF16)
                    S.dma(wr[:], I["w_router"][l].rearrange("(kc p) n -> p kc n", p=128), writes=[b_wr], q="pool")
                    rb, b_rb = load_vec_bc(st2, "rb", I["router_bias"][l:l + 1, :], n=64)
                    sc_ = [tl(st2, "r_sc%d" % i, [128, 64], F32) for i in range(2)]
                    sel = [tl(st2, "r_sel%d" % i, [128, 64], F32) for i in range(2)]
                    selm = [tl(st2, "r_selm%d" % i, [128, 64], F32) for i in range(2)]
                    m8 = [tl(st2, "r_m8%d" % i, [128, 8, 8], F32) for i in range(2)]
                    sm = [tl(st2, "r_sm%d" % i, [128, 40], F32) for i in range(2)]
                    for i in tiles_i:
                        ts_ = slice(i * 128, (i + 1) * 128)
                        pb = i % 2
                        sc_t, b_sc = sc_[i % 2]
                        sel_t, b_sel = sel[i % 2]
                        selm_t, b_selm = selm[i % 2]
                        m8_t, b_m8 = m8[i % 2]
                        sm_t, b_sm = sm[i % 2]
                        for kc in range(KC):
                            S.op("pe", lambda kc=kc: nc.tensor.matmul(PS[pb][:, 0:64], lhsT=h_fm[:, kc, ts_], rhs=wr[:, kc, :], start=(kc == 0), stop=(kc == KC - 1)), reads=[b_hfm, b_wr], writes=[PB[pb]])
                        S.op("act", lambda: nc.scalar.activation(out=sc_t[:], in_=PS[pb][:, 0:64], func=AF.Sigmoid), reads=[PB[pb]], writes=[b_sc])
                        S.op("dve", lambda: nc.vector.tensor_tensor(out=sel_t[:], in0=sc_t[:], in1=rb[:], op=ALU.add), reads=[b_sc, b_rb], writes=[b_sel])
                        for g8 in range(8):
                            S.op("dve", lambda g8=g8: nc.vector.max(out=m8_t[:, g8, :], in_=sel_t[:, g8 * 8:(g8 + 1) * 8]), reads=[b_sel], writes=[b_m8])
                        gs = sm_t[:, 0:8]
                        gm8 = sm_t[:, 8:16]
                        gmask = sm_t[:, 16:24]
                        pen = sm_t[:, 24:32]
                        t8 = sm_t[:, 32:40]
                        S.op("dve", lambda: nc.vector.tensor_tensor(out=gs, in0=m8_t[:, :, 0], in1=m8_t[:, :, 1], op=ALU.add), reads=[b_m8], writes=[b_sm])
                        S.op("dve", lambda: nc.vector.max(out=gm8, in_=gs), reads=[b_sm], writes=[b_sm])
                        S.op("dve", lambda: nc.vector.tensor_scalar(out=gmask, in0=gs, scalar1=sm_t[:, 11:12], scalar2=None, op0=ALU.is_ge), reads=[b_sm], writes=[b_sm])
                        S.op("dve", lambda: nc.vector.tensor_scalar(out=pen, in0=gmask, scalar1=10.0, scalar2=-10.0, op0=ALU.mult, op1=ALU.add), reads=[b_sm], writes=[b_sm])
                        sel3 = sel_t[:].rearrange("p (g x) -> p g x", g=8)
                        selm3 = selm_t[:].rearrange("p (g x) -> p g x", g=8)
                        S.op("dve", lambda: nc.vector.tensor_tensor(out=selm3, in0=sel3, in1=gmask.unsqueeze(2).to_broadcast([128, 8, 8]), op=ALU.mult), reads=[b_sel, b_sm], writes=[b_selm])
                        S.op("dve", lambda: nc.vector.tensor_tensor(out=selm3, in0=selm3, in1=pen.unsqueeze(2).to_broadcast([128, 8, 8]), op=ALU.add), reads=[b_selm, b_sm], writes=[b_selm])
                        S.op("dve", lambda: nc.vector.max(out=t8, in_=selm_t[:]), reads=[b_selm], writes=[b_sm])
                        S.op("dve", lambda: nc.vector.tensor_scalar(out=selm_t[:], in0=selm_t[:], scalar1=sm_t[:, 39:40], scalar2=None, op0=ALU.is_ge), reads=[b_selm, b_sm], writes=[b_selm])
                        S.op("dve", lambda: nc.vector.tensor_tensor(out=sel_t[:], in0=sc_t[:], in1=selm_t[:], op=ALU.mult), reads=[b_sc, b_selm], writes=[b_sel])
                        S.op("dve", lambda: nc.vector.tensor_reduce(out=sm_t[:, 0:1], in_=sel_t[:], axis=AX.X, op=ALU.add), reads=[b_sel], writes=[b_sm])
                        S.op("dve", lambda: nc.vector.reciprocal(out=sm_t[:, 1:2], in_=sm_t[:, 0:1]), reads=[b_sm], writes=[b_sm])
                        S.op("dve", lambda i=i: nc.vector.tensor_scalar(out=comb[:, i, 0:64], in0=sel_t[:], scalar1=sm_t[:, 1:2], scalar2=2.5, op0=ALU.mult, op1=ALU.mult), reads=[b_sel, b_sm], writes=[b_comb])
                    S.barrier()
                if "comb" in DBG:
                    S.dma(DBG["comb"].rearrange("(i p) e -> p i e", p=128), comb[:], reads=[b_comb])
                with contextlib.ExitStack() as st2:
                    wgu = [tl(st2, "wgu%d" % i, [128, KC, 512], BF16) for i in range(3)]
                    wdn = [tl(st2, "wdn%d" % i, [128, 2, D], BF16) for i in range(4)]
                    sgt = [tl(st2, "e_sg%d" % i, [128, 512], F32) for i in range(2)]
                    act = [tl(st2, "e_act%d" % i, [128, 2, 512], BF16) for i in range(2)]
                    ne = cfg.get("n_experts", 65)

                    def load_e(e):
                        wg_t, b_wg = wgu[e % 3]
                        wd_t, b_wd = wdn[e % 4]
                        if e < 64:
                            S.dma(wg_t[:], I["w_gu"][l, e].rearrange("(kc p) n -> p kc n", p=128), writes=[b_wg], q="pool")
                            S.dma(wd_t[:], I["w_down"][l, e].rearrange("(kc p) n -> p kc n", p=128), writes=[b_wd], q="pool")
                        else:
                            S.dma(wg_t[:], I["w_sh_gu"][l].rearrange("(kc p) n -> p kc n", p=128), writes=[b_wg], q="pool")
                            S.dma(wd_t[:], I["w_sh_down"][l].rearrange("(kc p) n -> p kc n", p=128), writes=[b_wd], q="pool")

                    elist = list(range(64 - (ne - 1), 65)) if ne < 65 else list(range(65))
                    load_e(elist[0])
                    if len(elist) > 1:
                        load_e(elist[1])
                    gcnt = 0
                    dcnt_ = [0]
                    pend_down = [None]
                    for ei, e in enumerate(elist):
                        need_load = ei + 2 < len(elist)
                        wg_t, b_wg = wgu[e % 3]
                        wd_t, b_wd = wdn[e % 4]
                        for (t0, n) in groups:
                            act_t, b_act = act[gcnt % 2]
                            gcnt += 1
                            pend_tiles = pend_down[0] if pend_down[0] is not None else []
                            pend_down[0] = None
                            if need_load:
                                load_e(elist[ei + 2])
                                need_load = False

                            def flush(k):
                                for _ in range(k):
                                    if pend_tiles:
                                        pend_tiles.pop(0)()
                            for c in range(2):
                                pg, pu = 2 * c, 2 * c + 1
                                for kc in range(KC):
                                    S.op("pe", lambda kc=kc, c=c, pg=pg: nc.tensor.matmul(PS[pg][:, 0:n], lhsT=wg_t[:, kc, c * 128:(c + 1) * 128], rhs=h_fm[:, kc, t0:t0 + n], start=(kc == 0), stop=(kc == KC - 1)),
                                         reads=[b_wg, b_hfm], writes=[PB[pg]])
                                flush(2)
                                for kc in range(KC):
                                    S.op("pe", lambda kc=kc, c=c, pu=pu: nc.tensor.matmul(PS[pu][:, 0:n], lhsT=wg_t[:, kc, 256 + c * 128:256 + (c + 1) * 128], rhs=h_fm[:, kc, t0:t0 + n], start=(kc == 0), stop=(kc == KC - 1)),
                                         reads=[b_wg, b_hfm], writes=[PB[pu]])
                                sg_t, b_sg = sgt[c]
                                S.op("act", lambda pg=pg, sg_t=sg_t: nc.scalar.activation(out=sg_t[:, 0:n], in_=PS[pg][:, 0:n], func=AF.Silu), reads=[PB[pg]], writes=[b_sg])
                                S.op("dve", lambda c=c, pu=pu, sg_t=sg_t, act_t=act_t: nc.vector.tensor_tensor(out=act_t[:, c, 0:n], in0=sg_t[:, 0:n], in1=PS[pu][:, 0:n], op=ALU.mult), reads=[b_sg, PB[pu]], writes=[b_act])
                                flush(2)
                            flush(99)

                            def mk_tiles(t0=t0, n=n, act_t=act_t, b_act=b_act, wd_t=wd_t, b_wd=b_wd, ei=ei, e=e):
                                fs = []
                                for tt in range(n // 128):
                                    for hf in range(2):
                                        def one(tt=tt, hf=hf):
                                            i = t0 // 128 + tt
                                            pd = 4 + dcnt_[0] % 4
                                            dcnt_[0] += 1
                                            for c in range(2):
                                                S.op("pe", lambda c=c: nc.tensor.matmul(PS[pd][:, :], lhsT=act_t[:, c, tt * 128:(tt + 1) * 128], rhs=wd_t[:, c, hf * 512:(hf + 1) * 512], start=(c == 0), stop=(c == 1)),
                                                     reads=[b_act, b_wd], writes=[PB[pd]])
                                            if ei == 0:
                                                S.op("dve", lambda: nc.vector.tensor_scalar(out=acc[:, i, hf * 512:(hf + 1) * 512], in0=PS[pd][:, :], scalar1=comb[:, i, e:e + 1], scalar2=None, op0=ALU.mult),
                                                     reads=[PB[pd], b_comb], writes=[b_acc])
                                            else:
                                                S.op("dve", lambda: nc.vector.scalar_tensor_tensor(out=acc[:, i, hf * 512:(hf + 1) * 512], in0=PS[pd][:, :], scalar=comb[:, i, e:e + 1], in1=acc[:, i, hf * 512:(hf + 1) * 512], op0=ALU.mult, op1=ALU.add),
                                                     reads=[PB[pd], b_comb, b_acc], writes=[b_acc])
                                        fs.append(one)
                                return fs
                            pend_down[0] = mk_tiles()
                    if pend_down[0] is not None:
                        for f in pend_down[0]:
                            f()
                    S.barrier()
                if "ff" in DBG:
                    S.dma(DBG["ff"].rearrange("(i p) d -> p i d", p=128), acc[:], reads=[b_acc])
                final = (l == nlayers - 1)
                tiles, prm = ln_setup(st, l + 1, 5, "ln2_g", "ln2_b", l, 0, 1, not final)
                for i in tiles_i:
                    ln_tile(tiles, i, [acc[:, i, 0:512], acc[:, i, 512:1024]], [b_acc, b_acc], prm, final)
                S.barrier()

        def stage_mla(l, last):
            with contextlib.ExitStack() as st:
                winv = I["w_in"][l].rearrange("(kc p) n -> p kc n", p=128)
                wA, b_wA = tl(st, "wA", [128, KC, 672], BF16)
                S.dma(wA[:], winv[:, :, 0:672], writes=[b_wA], q="pool")
                wKs, b_wKs = tl(st, "wKs", [128, KC, 32], BF16)
                S.dma(wKs[:], I["w_kr_sw"][l].rearrange("(kc p) n -> p kc n", p=128), writes=[b_wKs], q="pool")
                wQ, b_wQ = tl(st, "wQ", [128, 3, 768], BF16)
                S.dma(wQ[:], I["w_q_b"][l].rearrange("(kc p) n -> p kc n", p=128), writes=[b_wQ], q="pool")
                wQs, b_wQs = tl(st, "wQs", [128, 3, 256], BF16)
                S.dma(wQs[:], I["w_qr_sw"][l].rearrange("(kc p) n -> p kc n", p=128), writes=[b_wQs], q="pool")
                wKV, b_wKV = tl(st, "wKV", [128, 2, 1024], BF16)
                S.dma(wKV[:], I["w_kv_b"][l].rearrange("(kc p) n -> p kc n", p=128), writes=[b_wKV], q="pool")
                gq, b_gq = tl(st, "gq", [128, 3], F32)
                S.dma(gq[:], I["q_a_norm_t"][l], writes=[b_gq])
                gkv, b_gkv = tl(st, "gkv", [128, 2], F32)
                S.dma(gkv[:], I["kv_a_norm_t"][l], writes=[b_gkv])
                ropeC, b_rC = tl(st, "ropeC", [96, LAT], F32)
                ropeS, b_rS = tl(st, "ropeS", [96, LAT], F32)
                S.dma(ropeC[64:96, :], I["ropeC"][:, :], writes=[b_rC])
                S.dma(ropeS[64:96, :], I["ropeS"][:, :], writes=[b_rS])
                qan, b_qan = tl(st, "qan", [128, 3, T], BF16)
                kvan, b_kvan = tl(st, "kvan", [128, 2, T], BF16)
                kr, b_kr = tl(st, "kr", [96, T], BF16)
                raw = [tl(st, "raw%d" % i, [128, 512], F32) for i in range(5)]
                sq = [tl(st, "sq%d" % i, [128, 512], BF16) for i in range(5)]
                rs = [tl(st, "rs%d" % i, [128, 512], F32) for i in range(2)]
                rt = [tl(st, "rt%d" % i, [96, 512], F32) for i in range(2)]

                def rope_or_copy(dst, b_dst, t0, n, pa, pb, isctx):
                    if isctx:
                        S.op("act", lambda: nc.scalar.copy(out=dst[64:96, t0:t0 + n], in_=PS[pa][64:96, 0:n]), reads=[PB[pa]], writes=[b_dst])
                        return
                    l0 = t0 - CTX
                    S.op("dve", lambda: nc.vector.tensor_tensor(out=rt[0][0][64:96, 0:n], in0=PS[pa][64:96, 0:n], in1=ropeC[64:96, l0:l0 + n], op=ALU.mult),
                         reads=[PB[pa], b_rC], writes=[rt[0][1]])
                    S.op("dve", lambda: nc.vector.tensor_tensor(out=rt[1][0][64:96, 0:n], in0=PS[pb][64:96, 0:n], in1=ropeS[64:96, l0:l0 + n], op=ALU.mult),
                         reads=[PB[pb], b_rS], writes=[rt[1][1]])
                    S.op("dve", lambda: nc.vector.tensor_tensor(out=dst[64:96, t0:t0 + n], in0=rt[0][0][64:96, 0:n], in1=rt[1][0][64:96, 0:n], op=ALU.add),
                         reads=[rt[0][1], rt[1][1]], writes=[b_dst])

                def rmsnorm_group(col0, nchunk, gvec, b_gvec, dst, b_dst, t0, n, pbase, ri):
                    for c in range(nchunk):
                        pb = pbase + c
                        for kc in range(KC):
                            S.op("pe", lambda kc=kc, c=c, pb=pb: nc.tensor.matmul(PS[pb][:, 0:n], lhsT=wA[:, kc, col0 + c * 128:col0 + (c + 1) * 128],
                                                                                  rhs=h_fm[:, kc, t0:t0 + n], start=(kc == 0), stop=(kc == KC - 1)),
                                 reads=[b_wA, b_hfm], writes=[PB[pb]])
                        rw, b_rw = raw[ri + c]
                        sqt, b_sq = sq[ri + c]
                        S.op("act", lambda rw=rw, pb=pb: nc.scalar.copy(out=rw[:, 0:n], in_=PS[pb][:, 0:n]), reads=[PB[pb]], writes=[b_rw])
                        S.op("act", lambda sqt=sqt, pb=pb: nc.scalar.activation(out=sqt[:, 0:n], in_=PS[pb][:, 0:n], func=AF.Square), reads=[PB[pb]], writes=[b_sq])
                    pss = pbase + nchunk
                    for c in range(nchunk):
                        S.op("pe", lambda c=c: nc.tensor.matmul(PS[pss][:, 0:n], lhsT=onesb[:], rhs=sq[ri + c][0][:, 0:n], start=(c == 0), stop=(c == nchunk - 1)),
                             reads=[b_onesb, sq[ri + c][1]], writes=[PB[pss]])
                    r0, b_r0 = rs[0]
                    r1, b_r1 = rs[1]
                    S.op("act", lambda: nc.scalar.activation(out=r0[:, 0:n], in_=PS[pss][:, 0:n], func=AF.Sqrt, scale=1.0 / (128 * nchunk), bias=epsb[:, 0:1]),
                         reads=[PB[pss], b_eps], writes=[b_r0])
                    S.op("dve", lambda: nc.vector.reciprocal(out=r1[:, 0:n], in_=r0[:, 0:n]), reads=[b_r0], writes=[b_r1])
                    for c in range(nchunk):
                        S.op("dve", lambda c=c: nc.vector.scalar_tensor_tensor(out=dst[:, c, t0:t0 + n], in0=raw[ri + c][0][:, 0:n], scalar=gvec[:, c:c + 1],
                                                                                 in1=r1[:, 0:n], op0=ALU.mult, op1=ALU.mult),
                             reads=[raw[ri + c][1], b_gvec, b_r1], writes=[b_dst])

                for gi, (t0, n) in enumerate(GROUPS):
                    rmsnorm_group(0, 3, gq, b_gq, qan, b_qan, t0, n, 0, 0)
                    rmsnorm_group(384, 2, gkv, b_gkv, kvan, b_kvan, t0, n, 4, 3)
                    for kc in range(KC):
                        S.op("pe", lambda kc=kc: nc.tensor.matmul(PS[7][64:96, 0:n], lhsT=wA[:, kc, 640:672], rhs=h_fm[:, kc, t0:t0 + n], start=(kc == 0), stop=(kc == KC - 1)),
                             reads=[b_wA, b_hfm], writes=[PB[7]])
                    for kc in range(KC):
                        S.op("pe", lambda kc=kc: nc.tensor.matmul(PS[3][64:96, 0:n], lhsT=wKs[:, kc, :], rhs=h_fm[:, kc, t0:t0 + n], start=(kc == 0), stop=(kc == KC - 1)),
                             reads=[b_wKs, b_hfm], writes=[PB[3]])
                    rope_or_copy(kr, b_kr, t0, n, 7, 3, gi == 0)

                v_aug, b_va = tl(st, "v_aug", [128, NT, 8, 65], BF16)
                S.op("dve", lambda: nc.vector.memset(v_aug[:, :, :, 64:65], 1.0), writes=[b_va])
                wKVh = wKV[:].rearrange("p c (h x) -> p c h x", h=8)
                for i in range(NT):
                    pb = i % 2
                    for c in range(2):
                        S.op("pe", lambda c=c, i=i, pb=pb: nc.tensor.matmul(PS[pb][:, :].rearrange("p (h x) -> p h x", h=8), lhsT=kvan[:, c, i * 128:(i + 1) * 128],
                                                                            rhs=wKVh[:, c, :, 64:128], start=(c == 0), stop=(c == 1)),
                             reads=[b_kvan, b_wKV], writes=[PB[pb]])
                    S.op("act", lambda i=i, pb=pb: nc.scalar.copy(out=v_aug[:, i, :, 0:64], in_=PS[pb][:, :].rearrange("p (h x) -> p h x", h=8)),
                         reads=[PB[pb]], writes=[b_va])

                qn = [tl(st, "qn%d" % i, [96, T], BF16) for i in range(2)]
                kn = [tl(st, "kn%d" % i, [96, T], BF16) for i in range(2)]
                Et = [tl(st, "Et%d" % i, [128, 512], BF16) for i in range(5)]
                rc, b_rc = tl(st, "rc", [65, 512], F32)
                numt = [tl(st, "numt%d" % i, [64, 512], F32) for i in range(2)]
                ot = [tl(st, "ot%d" % i, [64, 512], BF16) for i in range(2)]
                b_mo = Buf("mla_o")
                ecnt = 0
                ocnt = 0
                for h in range(8):
                    qn_t, b_qn = qn[h % 2]
                    kn_t, b_kn = kn[h % 2]
                    S.op("pool", lambda: nc.gpsimd.tensor_copy(out=kn_t[64:96, :], in_=kr[64:96, :]), reads=[b_kr], writes=[b_kn])
                    for gi, (t0, n) in enumerate(GROUPS):
                        if not (last and gi == 0):
                            for c in range(3):
                                S.op("pe", lambda c=c: nc.tensor.matmul(PS[0][0:64, 0:n], lhsT=wQ[:, c, 96 * h:96 * h + 64], rhs=qan[:, c, t0:t0 + n], start=(c == 0), stop=(c == 2)),
                                     reads=[b_wQ, b_qan], writes=[PB[0]])
                            S.op("act", lambda: nc.scalar.copy(out=qn_t[0:64, t0:t0 + n], in_=PS[0][0:64, 0:n]), reads=[PB[0]], writes=[b_qn])
                            for c in range(3):
                                S.op("pe", lambda c=c: nc.tensor.matmul(PS[1][64:96, 0:n], lhsT=wQ[:, c, 96 * h + 64:96 * h + 96], rhs=qan[:, c, t0:t0 + n], start=(c == 0), stop=(c == 2)),
                                     reads=[b_wQ, b_qan], writes=[PB[1]])
                            for c in range(3):
                                S.op("pe", lambda c=c: nc.tensor.matmul(PS[2][64:96, 0:n], lhsT=wQs[:, c, 32 * h:32 * h + 32], rhs=qan[:, c, t0:t0 + n], start=(c == 0), stop=(c == 2)),
                                     reads=[b_wQs, b_qan], writes=[PB[2]])
                            rope_or_copy(qn_t, b_qn, t0, n, 1, 2, gi == 0)
                        for c in range(2):
                            S.op("pe", lambda c=c: nc.tensor.matmul(PS[3][0:64, 0:n], lhsT=wKV[:, c, 128 * h:128 * h + 64], rhs=kvan[:, c, t0:t0 + n], start=(c == 0), stop=(c == 1)),
                                 reads=[b_wKV, b_kvan], writes=[PB[3]])
                        S.op("act", lambda: nc.scalar.copy(out=kn_t[0:64, t0:t0 + n], in_=PS[3][0:64, 0:n]), reads=[PB[3]], writes=[b_kn])
                    for gi, (t0, n) in enumerate(GROUPS):
                        if last and gi == 0:
                            continue
                        kts = list(range(2)) if gi == 0 else list(range(NT))
                        pend = []
                        sbanks = [4, 5, 0, 1]
                        for ki, kt in enumerate(kts):
                            psb = sbanks[ecnt % 4]
                            E_t, b_E = Et[ecnt % 5]
                            ecnt += 1
                            S.op("pe", lambda kt=kt, psb=psb: nc.tensor.matmul(PS[psb][:, 0:n], lhsT=kn_t[:, kt * 128:(kt + 1) * 128], rhs=qn_t[:, t0:t0 + n], start=True, stop=True),
                                 reads=[b_kn, b_qn], writes=[PB[psb]])
                            S.op("act", lambda psb=psb, E_t=E_t: nc.scalar.activation(out=E_t[:, 0:n], in_=PS[psb][:, 0:n], func=AF.Exp, scale=MLA_SCALE),
                                 reads=[PB[psb]], writes=[b_E])
                            pend.append(lambda kt=kt, E_t=E_t, b_E=b_E, ki=ki: S.op("pe", lambda: nc.tensor.matmul(PS[6][0:65, 0:n], lhsT=v_aug[:, kt, h, :], rhs=E_t[:, 0:n], start=(ki == 0), stop=(ki == len(kts) - 1)),
                                 reads=[b_va, b_E], writes=[PB[6]]))
                            if len(pend) > 2:
                                pend.pop(0)()
                        while pend:
                            pend.pop(0)()
                        nm_t, b_nm = numt[ocnt % 2]
                        o_t, b_o = ot[ocnt % 2]
                        ocnt += 1
                        S.op("dve", lambda: nc.vector.reciprocal(out=rc[64:65, 0:n], in_=PS[6][64:65, 0:n]), reads=[PB[6]], writes=[b_rc])
                        S.op("act", lambda nm_t=nm_t: nc.scalar.copy(out=nm_t[:, 0:n], in_=PS[6][0:64, 0:n]), reads=[PB[6]], writes=[b_nm])
                        S.op("pe", lambda: nc.tensor.matmul(PS[7][0:64, 0:n], lhsT=onesf[64:65, 0:64], rhs=rc[64:65, 0:n], start=True, stop=True),
                             reads=[b_onesf, b_rc], writes=[PB[7]])
                        S.op("dve", lambda nm_t=nm_t, o_t=o_t: nc.vector.tensor_tensor(out=o_t[:, 0:n], in0=nm_t[:, 0:n], in1=PS[7][0:64, 0:n], op=ALU.mult),
                             reads=[b_nm, PB[7]], writes=[b_o])
                        S.dma(mla_o_d[h, :, t0:t0 + n], o_t[:, 0:n], reads=[b_o], writes=[b_mo])
                S.barrier()

        b_xres = [Buf("xres%d" % i) for i in range(NT)]
        stage_entry(0)
        for l in range(nlayers):
            last = (l == nlayers - 1)
            if not cfg.get("skip_mla"):
                stage_mla(l, last)
            if not cfg.get("skip_hg"):
                stage_hgrn(l)
            if not cfg.get("skip_gdn"):
                stage_gdn(l)
            if cfg.get("stop_after") == "mixers":
                break
            stage_merge(l, last)
            if cfg.get("stop_after") == "merge":
                break
            stage_moe(l, last)
        if "h_fm" in DBG:
            S.dma(DBG["h_fm"].rearrange("(kc p) t -> p kc t", p=128), h_fm[:], reads=[b_hfm], q="pool")
        if "xres" in DBG:
            S.dma(DBG["xres"], xres_d[:, :])
        if "mla_o" in DBG:
            S.dma(DBG["mla_o"], mla_o_d.rearrange("h d t -> (h d) t"), q="pool")
        if "hg_o" in DBG:
            S.dma(DBG["hg_o"], hg_o_d.rearrange("h d t -> (h d) t"), q="pool")
        if "gdn_o" in DBG:
            S.dma(DBG["gdn_o"], gdn_o_d.rearrange("h d t -> (h d) t"), q="pool")
        K.PS, K.PB = PS, PB

        S.finish()
    K.ninstr = S.ninstr
    return nc, K


WEIGHT_SHAPES = {
    "w_mod": [DEPTH, D, 6 * D], "b_mod": [DEPTH, 6 * D], "w_in": [DEPTH, D, IN_W],
    "w_q_b": [DEPTH, 384, 768], "w_kv_b": [DEPTH, 256, 1024],
    "hg_lb_logits": [DEPTH, 2, 512],
    "gdn_a_log": [DEPTH, 2, 4], "gdn_dt_bias": [DEPTH, 2, 4], "gdn_norm": [DEPTH, 128],
    "w_branch": [DEPTH, 3, 512, D], "w_out": [DEPTH, D, D],
    "ln1_g": [DEPTH, D], "ln1_b": [DEPTH, D], "ln2_g": [DEPTH, D], "ln2_b": [DEPTH, D],
    "w_router": [DEPTH, D, 64], "router_bias": [DEPTH, 64],
    "w_gu": [DEPTH, 64, D, 512], "w_down": [DEPTH, 64, 256, D], "w_sh_gu": [DEPTH, D, 512], "w_sh_down": [DEPTH, 256, D],
}
DERIVED_SHAPES = {
    "w_kr_sw": [DEPTH, D, 32], "w_qr_sw": [DEPTH, 384, 256],
    "q_a_norm_t": [DEPTH, 128, 3], "kv_a_norm_t": [DEPTH, 128, 2],
    "hg_norm_t": [128, DEPTH], "gdn_conv_t": [DEPTH, 128, 12, 5],
}
CONST_SHAPES = {
    "ident": [128, 128], "ropeC": [32, LAT], "ropeS": [32, LAT],
    "rmask": [128, T], "triu": [64, 64], "tril": [64, 64],
    "mist": [64, 2, 64], "mast": [64, 2, 64],
}


def host_consts():
    c = {}
    c["ident"] = np.eye(128, dtype=np.float32)
    pos = np.arange(LAT)
    row = (pos // 64).astype(np.float32)
    col = (pos % 64).astype(np.float32)
    inv = (np.float32(10000.0) ** (-np.arange(8, dtype=np.float32) / np.float32(8))).astype(np.float32)
    C = np.zeros((32, LAT), np.float32)
    Sg = np.zeros((32, LAT), np.float32)
    for ax, p in enumerate((row, col)):
        ang = (p[None, :] * inv[:, None]).astype(np.float32)
        for half in range(2):
            r0 = ax * 16 + half * 8
            C[r0:r0 + 8] = np.cos(ang)
            Sg[r0:r0 + 8] = np.sin(ang) * (-1.0 if half == 0 else 1.0)
    c["ropeC"] = C
    rm = np.ones((128, T), np.float32)
    rm[:, ::64] = 0.0
    c["rmask"] = rm
    c["triu"] = np.triu(np.ones((64, 64), np.float32))
    c["tril"] = np.tril(np.ones((64, 64), np.float32))
    c["mist"] = np.ascontiguousarray(np.stack([c["triu"], c["tril"]], axis=1))
    c["mast"] = np.ascontiguousarray(np.stack([c["tril"] - np.eye(64, dtype=np.float32), c["triu"] - np.eye(64, dtype=np.float32)], axis=1))
    c["ropeS"] = Sg
    return c


def prep_inputs(inputs):
    x = np.asarray(inputs["x"], np.float32)
    ctx = np.asarray(inputs["ctx"], np.float32)
    c = np.asarray(inputs["c"], np.float32)
    c_ctx = np.asarray(inputs["c_ctx"], np.float32)
    shared = {}
    for nm in WEIGHT_SHAPES:
        shared[nm] = np.ascontiguousarray(np.asarray(inputs[nm], np.float32)).reshape(WEIGHT_SHAPES[nm])
    shared.update(host_consts())
    perm = np.arange(32) ^ 8
    w_in = shared["w_in"]
    shared["w_kr_sw"] = np.ascontiguousarray(w_in[:, :, 640:672][:, :, perm])
    wqb = shared["w_q_b"].reshape(DEPTH, 384, 8, 96)
    shared["w_qr_sw"] = np.ascontiguousarray(wqb[:, :, :, 64:96][:, :, :, perm].reshape(DEPTH, 384, 256))
    shared["q_a_norm_t"] = np.ascontiguousarray(np.asarray(inputs["q_a_norm"], np.float32).reshape(DEPTH, 3, 128).transpose(0, 2, 1))
    shared["gdn_conv_t"] = np.ascontiguousarray(np.asarray(inputs["gdn_conv"], np.float32).reshape(DEPTH, 5, 12, 128).transpose(0, 3, 2, 1))
    shared["hg_norm_t"] = np.ascontiguousarray(np.asarray(inputs["hg_norm"], np.float32).T)
    shared["kv_a_norm_t"] = np.ascontiguousarray(np.asarray(inputs["kv_a_norm"], np.float32).reshape(DEPTH, 2, 128).transpose(0, 2, 1))
    maps = []
    for b in range(x.shape[0]):
        m = dict(shared)
        m["xin"] = np.ascontiguousarray(np.concatenate([ctx[b], x[b]], axis=0))
        m["cvecT"] = np.ascontiguousarray(np.stack([c[b], c_ctx], axis=1))
        maps.append(m)
    return maps


def kernel(**inputs):
    maps = prep_inputs(inputs)
    nc, K = build_program({})
    res = run_bass_kernel_spmd(nc, maps, core_ids=list(range(8)))
    out = np.stack([np.asarray(r["out"], np.float32) for r in res.results], axis=0)
    return out
```

```python
import contextlib
import numpy as np
import concourse.bass as bass
import concourse.mybir as mybir
from concourse.bass_utils import run_bass_kernel_spmd

F32 = mybir.dt.float32
BF16 = mybir.dt.bfloat16
AF = mybir.ActivationFunctionType
ALU = mybir.AluOpType
AX = mybir.AxisListType

EPOCH = 30000
NRING = 40

DEPTH = 4
D = 1024
KC = 8
LAT = 2048
CTX = 256
T = LAT + CTX
NT = T // 128
GROUPS = [(0, 256), (256, 512), (768, 512), (1280, 512), (1792, 512)]
NCH = T // 64
IN_W = 8368
MLA_SCALE = 96 ** -0.5
ALPHA = (2 * DEPTH) ** 0.25
O_HG = 672
O_GDN = O_HG + 2560
O_GG = O_GDN + 1536
O_GA = O_GG + 512
O_GB = O_GA + 8
O_GATES = O_GB + 8


class Buf:
    __slots__ = ("name", "w", "r", "ex")

    def __init__(self, name="", ex=False):
        self.name = name
        self.w = None
        self.r = []
        self.ex = ex


class Sched:
    def __init__(self, nc, es, self_sync=True):
        self.nc = nc
        self.es = es
        self.eng = {"pe": nc.tensor, "act": nc.scalar, "dve": nc.vector, "pool": nc.gpsimd, "sp": nc.sync}
        self.seq = {e: 0 for e in self.eng}
        self.sems = {e: [] for e in self.eng}
        self.known = {e: {} for e in self.eng}
        self.known_dma = {e: set() for e in self.eng}
        self.ring = [es.enter_context(nc.semaphore("dr%d" % i)) for i in range(NRING)]
        self.ring_cnt = [0] * NRING
        self.ring_next = 0
        self.self_sync = self_sync
        self.ninstr = 0

    def _sem(self, e, ep):
        while len(self.sems[e]) <= ep:
            self.sems[e].append(self.es.enter_context(self.nc.semaphore("s_%s%d" % (e, len(self.sems[e])))))
        return self.sems[e][ep]

    def _wait(self, e, tok):
        if tok is None:
            return
        if tok[0] == "dma":
            _, k, val = tok
            key = (k, val)
            if key in self.known_dma[e]:
                return
            self.eng[e].wait_ge(self.ring[k], val)
            self.known_dma[e].add(key)
            return
        _, e2, s = tok
        if e2 == e and (e == "pe" or not self.self_sync):
            return
        if self.known[e].get(e2, 0) >= s:
            return
        ep = (s - 1) // EPOCH
        self.eng[e].wait_ge(self._sem(e2, ep), s - ep * EPOCH)
        self.known[e][e2] = s

    def _deps(self, e, reads, writes):
        for b in reads:
            if b.w is not None:
                self._wait(e, b.w)
            if b.ex:
                for t in b.r:
                    if t[0] == "eng" and t[1] != e:
                        self._wait(e, t)
        for b in writes:
            if b.w is not None:
                self._wait(e, b.w)
            for t in b.r:
                self._wait(e, t)

    def _mark(self, tok, reads, writes):
        for b in reads:
            b.r.append(tok)
            if len(b.r) > 24:
                b.r = self._prune(b.r)
        for b in writes:
            b.w = tok
            b.r = []

    def _prune(self, r):
        best = {}
        out = []
        for t in r:
            if t[0] == "dma":
                out.append(t)
            elif t[1] not in best or best[t[1]][2] < t[2]:
                best[t[1]] = t
        return out + list(best.values())

    def op(self, e, fn, reads=(), writes=()):
        self._deps(e, reads, writes)
        ins = fn()
        self.seq[e] += 1
        s = self.seq[e]
        ep = (s - 1) // EPOCH
        ins.then_inc(self._sem(e, ep), 1)
        tok = ("eng", e, s)
        self._mark(tok, reads, writes)
        self.ninstr += 1
        return tok

    def dma(self, out, in_, reads=(), writes=(), q="sp", **kw):
        k = self.ring_next
        self.ring_next = (self.ring_next + 1) % NRING
        prev = self.ring_cnt[k]
        if prev > 0:
            self._wait(q, ("dma", k, 16 * prev))
        self._deps(q, reads, writes)
        self.eng[q].dma_start(out=out, in_=in_, **kw).then_inc(self.ring[k], 16)
        self.ring_cnt[k] = prev + 1
        tok = ("dma", k, 16 * (prev + 1))
        self._mark(tok, reads, writes)
        self.ninstr += 1
        return tok

    def barrier(self, engines=None):
        for e in (engines or self.eng):
            for e2 in self.eng:
                if self.seq[e2] > 0 and not (e2 == e and e == "pe"):
                    self._wait(e, ("eng", e2, self.seq[e2]))
            for k in range(NRING):
                if self.ring_cnt[k] > 0:
                    self._wait(e, ("dma", k, 16 * self.ring_cnt[k]))

    def finish(self):
        self.barrier(["sp"])


class Ctx:
    pass


def build_program(cfg):
    nlayers = cfg.get("nlayers", DEPTH)
    stop_after = cfg.get("stop_after", None)
    dbg = cfg.get("debug", [])
    nc = bass.Bass("TRN2", target_bir_lowering=False)
    K = Ctx()
    K.nc = nc

    def din(name, shape, dt=F32):
        return nc.dram_tensor(name, list(shape), dt, kind="ExternalInput").ap()

    def dscr(name, shape, dt=F32):
        return nc.dram_tensor(name, list(shape), dt, kind="Internal").ap()

    I = {}
    I["xin"] = din("xin", [T, D])
    I["cvecT"] = din("cvecT", [D, 2])
    for nm, shp in WEIGHT_SHAPES.items():
        I[nm] = din(nm, shp)
    for nm, shp in CONST_SHAPES.items():
        I[nm] = din(nm, shp)
    for nm, shp in DERIVED_SHAPES.items():
        I[nm] = din(nm, shp)
    out_d = nc.dram_tensor("out", [LAT, D], F32, kind="ExternalOutput").ap()
    DBG = {}
    for nm, shp in dbg:
        DBG[nm] = nc.dram_tensor("dbg_" + nm, list(shp), F32, kind="ExternalOutput").ap()

    modv_d = dscr("modv_d", [DEPTH, 2, 6 * D])
    xres_d = dscr("xres_d", [T, D])
    mla_o_d = dscr("mla_o_d", [8, 64, T], BF16)
    hg_o_d = dscr("hg_o_d", [4, 128, T], BF16)
    gdn_o_d = dscr("gdn_o_d", [4, 128, T], BF16)
    gdn_raw_d = dscr("gdn_raw_d", [2, T, 512])

    es = contextlib.ExitStack()
    with es:
        S = Sched(nc, es, self_sync=cfg.get('self_sync', True))
        K.S = S

        tlc = [0]

        def tl(st, name, shape, dt):
            tlc[0] += 1
            t = st.enter_context(nc.sbuf_tensor("sb%d_%s" % (tlc[0], name), list(shape), dt))
            return t, Buf(name)

        PS = []
        PB = []
        for i in range(8):
            PS.append(es.enter_context(nc.psum_tensor("ps%d" % i, [128, 512], F32)))
            PB.append(Buf("ps%d" % i, ex=True))

        identf, b_identf = tl(es, "identf", [128, 128], F32)
        identb, b_identb = tl(es, "identb", [128, 128], BF16)
        onesb, b_onesb = tl(es, "onesb", [128, 128], BF16)
        onesf, b_onesf = tl(es, "onesf", [128, 128], F32)
        S.dma(identf[:], I["ident"][:, :], writes=[b_identf])
        S.dma(identb[:], I["ident"][:, :], writes=[b_identb], q="pool")
        S.op("dve", lambda: nc.vector.memset(onesb[:], 1.0), writes=[b_onesb])
        S.op("dve", lambda: nc.vector.memset(onesf[:], 1.0), writes=[b_onesf])
        h_fm, b_hfm = tl(es, "h_fm", [128, KC, T], BF16)
        epsb, b_eps = tl(es, "epsb", [128, 1], F32)
        S.op("dve", lambda: nc.vector.memset(epsb[:], 1e-6), writes=[b_eps])

        def dbg_out(name, ap_sb, buf, dram_ap=None):
            if name in DBG:
                S.dma(dram_ap if dram_ap is not None else DBG[name], ap_sb, reads=[buf])

        with contextlib.ExitStack() as st:
            cv, b_cv = tl(st, "cv", [128, KC, 2], F32)
            scv, b_scv = tl(st, "scv", [128, KC, 2], F32)
            S.dma(cv[:], I["cvecT"].rearrange("(kc p) s -> p kc s", p=128), writes=[b_cv])
            S.op("act", lambda: nc.scalar.activation(out=scv[:], in_=cv[:], func=AF.Silu), reads=[b_cv], writes=[b_scv])
            wm = [tl(st, "wm%d" % i, [128, KC, 512], F32) for i in range(2)]
            bm, b_bm = tl(st, "bm", [1, 6 * D], F32)
            mv = [tl(st, "mv%d" % i, [2, 6 * D], F32) for i in range(2)]
            ones2, b_ones2 = tl(st, "ones2", [1, 2], F32)
            S.op("dve", lambda: nc.vector.memset(ones2[:], 1.0), writes=[b_ones2])
            cnt = 0
            for l in range(nlayers):
                mvt, b_mv = mv[l % 2]
                S.dma(bm[:], I["b_mod"][l:l + 1, :], writes=[b_bm])
                for cg in range(12):
                    wt, b_wt = wm[cnt % 2]
                    cnt += 1
                    S.dma(wt[:], I["w_mod"][l].rearrange("(kc p) n -> p kc n", p=128)[:, :, cg * 512:(cg + 1) * 512], writes=[b_wt])
                    pb = cnt % 2
                    for kc in range(KC):
                        S.op("pe", lambda kc=kc, wt=wt, pb=pb: nc.tensor.matmul(PS[pb][0:2, :], lhsT=scv[:, kc, :], rhs=wt[:, kc, :], start=(kc == 0), stop=False),
                             reads=[b_scv, b_wt], writes=[PB[pb]])
                    S.op("pe", lambda cg=cg, pb=pb: nc.tensor.matmul(PS[pb][0:2, :], lhsT=ones2[:], rhs=bm[:, cg * 512:(cg + 1) * 512], start=False, stop=True),
                         reads=[b_ones2, b_bm], writes=[PB[pb]])
                    S.op("act", lambda cg=cg, pb=pb, mvt=mvt: nc.scalar.copy(out=mvt[:, cg * 512:(cg + 1) * 512], in_=PS[pb][0:2, :]), reads=[PB[pb]], writes=[b_mv])
                S.dma(modv_d[l], mvt[:], reads=[b_mv], writes=[])
                K.modv_tok = None
            S.barrier()
        b_modv = Buf("modv_d")
        b_modv.w = None

        def load_bc(st, name, l, stream, j, plus_one=False):
            t, b = tl(st, name, [128, D], F32)
            S.dma(t[:], modv_d[l, stream:stream + 1, j * D:(j + 1) * D].partition_broadcast(128), writes=[b])
            if plus_one:
                S.op("pool", lambda: nc.gpsimd.tensor_scalar_add(out=t[:], in0=t[:], scalar1=1.0), reads=[b], writes=[b])
            return t, b

        def load_vec_bc(st, name, dram_row_ap, n=D):
            t, b = tl(st, name, [128, n], F32)
            S.dma(t[:], dram_row_ap.partition_broadcast(128), writes=[b])
            return t, b

        def to_fm(src_bf, b_src, i, psb):
            pv = PS[psb][:].bitcast(BF16)
            for kc in range(KC):
                S.op("pe", lambda kc=kc: nc.tensor.transpose(out=pv[:, kc * 128:(kc + 1) * 128], in_=src_bf[:, kc * 128:(kc + 1) * 128], identity=identb[:]),
                     reads=[b_src, b_identb], writes=[PB[psb]])
            S.op("act", lambda: nc.scalar.copy(out=h_fm[:, :, i * 128:(i + 1) * 128], in_=pv.rearrange("p (k t) -> p k t", k=KC)),
                 reads=[PB[psb]], writes=[b_hfm])

        def stage_entry(l):
            with contextlib.ExitStack() as st:
                bc = {}
                for s in range(2):
                    bc[(s, 0)] = load_bc(st, "bsh%d" % s, l, s, 0)
                    bc[(s, 1)] = load_bc(st, "bsc%d" % s, l, s, 1, plus_one=True)
                xt = [tl(st, "xt%d" % i, [128, D], F32) for i in range(2)]
                ht = [tl(st, "ht%d" % i, [128, D], BF16) for i in range(2)]
                for i in range(NT):
                    s = 1 if i < 2 else 0
                    x_t, b_x = xt[i % 2]
                    h_t, b_h = ht[i % 2]
                    S.dma(x_t[:], I["xin"][i * 128:(i + 1) * 128, :], writes=[b_x])
                    S.dma(xres_d[i * 128:(i + 1) * 128, :], x_t[:], reads=[b_x])
                    S.op("dve", lambda x_t=x_t, s=s: nc.vector.tensor_tensor(out=x_t[:], in0=x_t[:], in1=bc[(s, 1)][0][:], op=ALU.mult),
                         reads=[b_x, bc[(s, 1)][1]], writes=[b_x])
                    S.op("dve", lambda x_t=x_t, h_t=h_t, s=s: nc.vector.tensor_tensor(out=h_t[:], in0=x_t[:], in1=bc[(s, 0)][0][:], op=ALU.add),
                         reads=[b_x, bc[(s, 0)][1]], writes=[b_h])
                    to_fm(h_t, b_h, i, i % 2)
                S.barrier()


        lbT, b_lbT = tl(es, "lbT", [128, DEPTH, 8], F32)
        omlbT, b_omlbT = tl(es, "omlbT", [128, DEPTH, 8], F32)
        rmask, b_rmask = tl(es, "rmask", [128, T], BF16)
        triu, b_triu = tl(es, "triu", [64, 64], F32)
        tril, b_tril = tl(es, "tril", [64, 64], F32)
        S.dma(rmask[:], I["rmask"][:, :], writes=[b_rmask], q="pool")
        S.dma(triu[:], I["triu"][:, :], writes=[b_triu])
        S.dma(tril[:], I["tril"][:, :], writes=[b_tril])
        with contextlib.ExitStack() as st:
            lg, b_lg = tl(st, "lg", [32, 128], F32)
            eT, b_eT = tl(st, "eT", [128, DEPTH, 8], F32)
            tot, b_tot = tl(st, "lbtot", [128, 8], F32)
            S.dma(lg[:], I["hg_lb_logits"].rearrange("l s (h p) -> (l s h) p", p=128), writes=[b_lg])
            S.op("act", lambda: nc.scalar.activation(out=lg[:], in_=lg[:], func=AF.Exp), reads=[b_lg], writes=[b_lg])
            S.op("pe", lambda: nc.tensor.transpose(out=PS[0][:, 0:32], in_=lg[:], identity=identf[0:32, 0:32]), reads=[b_lg, b_identf], writes=[PB[0]])
            S.op("act", lambda: nc.scalar.copy(out=eT[:].rearrange("p l x -> p (l x)"), in_=PS[0][:, 0:32]), reads=[PB[0]], writes=[b_eT])
            S.op("dve", lambda: nc.vector.tensor_tensor(out=tot[:], in0=eT[:, 0, :], in1=eT[:, 1, :], op=ALU.add), reads=[b_eT], writes=[b_tot])
            S.op("dve", lambda: nc.vector.tensor_tensor(out=tot[:], in0=tot[:], in1=eT[:, 2, :], op=ALU.add), reads=[b_eT, b_tot], writes=[b_tot])
            S.op("dve", lambda: nc.vector.tensor_tensor(out=tot[:], in0=tot[:], in1=eT[:, 3, :], op=ALU.add), reads=[b_eT, b_tot], writes=[b_tot])
            S.op("dve", lambda: nc.vector.reciprocal(out=tot[:], in_=tot[:]), reads=[b_tot], writes=[b_tot])
            S.op("dve", lambda: nc.vector.memset(lbT[:, 0, :], 0.0), writes=[b_lbT])
            S.op("dve", lambda: nc.vector.tensor_copy(out=lbT[:, 1, :], in_=eT[:, 1, :]), reads=[b_eT, b_lbT], writes=[b_lbT])
            S.op("dve", lambda: nc.vector.tensor_tensor(out=lbT[:, 2, :], in0=lbT[:, 1, :], in1=eT[:, 2, :], op=ALU.add), reads=[b_eT, b_lbT], writes=[b_lbT])
            S.op("dve", lambda: nc.vector.tensor_tensor(out=lbT[:, 3, :], in0=lbT[:, 2, :], in1=eT[:, 3, :], op=ALU.add), reads=[b_eT, b_lbT], writes=[b_lbT])
            for l in range(1, DEPTH):
                S.op("dve", lambda l=l: nc.vector.tensor_tensor(out=lbT[:, l, :], in0=lbT[:, l, :], in1=tot[:], op=ALU.mult), reads=[b_tot, b_lbT], writes=[b_lbT])
            S.op("dve", lambda: nc.vector.tensor_scalar(out=omlbT[:], in0=lbT[:], scalar1=-1.0, scalar2=1.0, op0=ALU.mult, op1=ALU.add), reads=[b_lbT], writes=[b_omlbT])
            S.barrier()

        def stage_hgrn(l):
            winv = I["w_in"][l].rearrange("(kc p) n -> p kc n", p=128)
            with contextlib.ExitStack() as st:
                wh = [tl(st, "wh%d" % i, [128, KC, 5, 128], BF16) for i in range(2)]
                hgn4, b_hgn = tl(st, "hgn", [128, DEPTH], F32)
                S.dma(hgn4[:], I["hg_norm_t"][:, :], writes=[b_hgn])
                hgn = hgn4[:, l:l + 1]
                q_bf, b_q = tl(st, "hq_bf", [128, T], BF16)
                gate_sb, b_gate = tl(st, "hgate", [128, T], BF16)
                v_tm, b_v = tl(st, "hv_tm", [64, NCH, 128], BF16)
                A, b_A = tl(st, "hA", [128, T], F32)
                B, b_B = tl(st, "hB", [128, T], F32)
                Cc, b_C = tl(st, "hC", [128, T], F32)
                qt, b_qt = tl(st, "hqt", [128, T], BF16)
                kt, b_kt = tl(st, "hkt", [128, T], BF16)
                qh, b_qh = tl(st, "hqh", [128, T], BF16)
                kh, b_kh = tl(st, "hkh", [128, T], BF16)
                khT, b_khT = tl(st, "hkhT", [64, NCH, 128], BF16)
                aT, b_aT = tl(st, "haT", [64, NCH, 64], BF16)
                o_d = [tl(st, "ho%d" % i, [128, T], F32) for i in range(2)]
                tot, b_tot = tl(st, "htot", [128, NCH], F32)
                rmid, b_rmid = tl(st, "hrmid", [128, NCH], F32)
                egl, b_egl = tl(st, "hegl", [128, NCH], F32)
                Sst, b_S = tl(st, "hS", [128, 128], F32)
                Sb = [tl(st, "hSb%d" % i, [128, 128], BF16) for i in range(2)]
                rs0, b_rs0 = tl(st, "hrs0", [128, 512], F32)
                rs1, b_rs1 = tl(st, "hrs1", [128, 512], F32)
                og = [tl(st, "hog%d" % i, [128, 512], BF16) for i in range(2)]
                b_ho = Buf("hg_o")
                C3 = Cc[:].rearrange("p (c k) -> p c k", k=64)
                B3 = B[:].rearrange("p (c k) -> p c k", k=64)
                pcnt = [0]

                def proj(col, wt, b_wt, fn_evac):
                    for (t0, n) in GROUPS:
                        pb = pcnt[0] % 2
                        pcnt[0] += 1
                        for kc in range(KC):
                            S.op("pe", lambda kc=kc, pb=pb: nc.tensor.matmul(PS[pb][:, 0:n], lhsT=wt[:, kc, col, :], rhs=h_fm[:, kc, t0:t0 + n], start=(kc == 0), stop=(kc == KC - 1)),
                                 reads=[b_wt, b_hfm], writes=[PB[pb]])
                        fn_evac(pb, t0, n)

                ocnt = 0
                def load_head(hd):
                    wt, b_wt = wh[hd % 2]
                    for ci in range(5):
                        c0 = O_HG + ci * 512 + hd * 128
                        S.dma(wt[:, :, ci, :], winv[:, :, c0:c0 + 128], writes=[b_wt], q="pool")
                load_head(0)
                for hd in range(4):
                    wt, b_wt = wh[hd % 2]
                    if hd + 1 < 4:
                        load_head(hd + 1)
                    proj(0, wt, b_wt, lambda pb, t0, n: S.op("act", lambda: nc.scalar.activation(out=q_bf[:, t0:t0 + n], in_=PS[pb][:, 0:n], func=AF.Silu), reads=[PB[pb]], writes=[b_q]))
                    proj(4, wt, b_wt, lambda pb, t0, n: S.op("act", lambda: nc.scalar.activation(out=gate_sb[:, t0:t0 + n], in_=PS[pb][:, 0:n], func=AF.Silu), reads=[PB[pb]], writes=[b_gate]))
                    for c4 in range(NCH // 4):
                        pb = pcnt[0] % 2
                        pcnt[0] += 1
                        for j in range(4):
                            c = c4 * 4 + j
                            for kc in range(KC):
                                S.op("pe", lambda kc=kc, c=c, j=j, pb=pb: nc.tensor.matmul(PS[pb][0:64, j * 128:(j + 1) * 128], lhsT=h_fm[:, kc, c * 64:(c + 1) * 64], rhs=wt[:, kc, 1, :],
                                                                                         start=(kc == 0), stop=(kc == KC - 1)), reads=[b_wt, b_hfm], writes=[PB[pb]])
                        S.op("act", lambda c4=c4, pb=pb: nc.scalar.copy(out=v_tm[:, c4 * 4:(c4 + 1) * 4, :], in_=PS[pb][0:64, :].rearrange("p (j x) -> p j x", j=4)),
                             reads=[PB[pb]], writes=[b_v])
                    for s in range(2):
                        o_t, b_o = o_d[s]
                        lbc = lbT[:, l, s * 4 + hd:s * 4 + hd + 1]
                        omc = omlbT[:, l, s * 4 + hd:s * 4 + hd + 1]
                        proj(2 + s, wt, b_wt, lambda pb, t0, n: S.op("act", lambda: nc.scalar.activation(out=A[:, t0:t0 + n], in_=PS[pb][:, 0:n], func=AF.Sigmoid), reads=[PB[pb]], writes=[b_A]))
                        S.op("dve", lambda: nc.vector.tensor_scalar(out=A[:], in0=A[:], scalar1=omc, scalar2=lbc, op0=ALU.mult, op1=ALU.add), reads=[b_A, b_lbT, b_omlbT], writes=[b_A])
                        S.op("act", lambda: nc.scalar.activation(out=B[:], in_=A[:], func=AF.Ln), reads=[b_A], writes=[b_B])
                        S.op("pool", lambda: nc.gpsimd.tensor_scalar(out=A[:], in0=A[:], scalar1=-1.0, scalar2=1.0, op0=ALU.mult, op1=ALU.add), reads=[b_A, b_B], writes=[b_A])
                        S.op("dve", lambda: nc.vector.tensor_tensor_scan(out=Cc[:], data0=rmask[:], data1=B[:], initial=0.0, op0=ALU.mult, op1=ALU.add), reads=[b_rmask, b_B], writes=[b_C])
                        S.op("dve", lambda: nc.vector.tensor_copy(out=tot[:], in_=C3[:, :, 63]), reads=[b_C], writes=[b_tot])
                        totb = tot[:].unsqueeze(2).to_broadcast([128, NCH, 64])
                        if s == 1:
                            S.op("dve", lambda: nc.vector.scalar_tensor_tensor(out=C3, in0=C3, scalar=-1.0, in1=totb, op0=ALU.mult, op1=ALU.add), reads=[b_C, b_tot], writes=[b_C])
                            S.op("dve", lambda: nc.vector.tensor_tensor(out=Cc[:], in0=Cc[:], in1=B[:], op=ALU.add), reads=[b_C, b_B], writes=[b_C])
                        S.op("dve", lambda: nc.vector.tensor_copy(out=rmid[:], in_=C3[:, :, 31 + s]), reads=[b_C], writes=[b_rmid])
                        S.op("act", lambda: nc.scalar.activation(out=egl[:], in_=tot[:], func=AF.Exp), reads=[b_tot], writes=[b_egl])
                        S.op("dve", lambda: nc.vector.tensor_tensor(out=B3, in0=C3, in1=rmid[:].unsqueeze(2).to_broadcast([128, NCH, 64]), op=ALU.subtract), reads=[b_C, b_rmid, b_B], writes=[b_B])
                        S.op("act", lambda: nc.scalar.activation(out=B[:], in_=B[:], func=AF.Exp), reads=[b_B], writes=[b_B])
                        S.op("pool", lambda: nc.gpsimd.tensor_tensor(out=qt[:], in0=q_bf[:], in1=B[:], op=ALU.mult), reads=[b_q, b_B], writes=[b_qt])
                        S.op("dve", lambda: nc.vector.reciprocal(out=B[:], in_=B[:]), reads=[b_B, b_qt], writes=[b_B])
                        S.op("dve", lambda: nc.vector.tensor_tensor(out=kt[:], in0=A[:], in1=B[:], op=ALU.mult), reads=[b_A, b_B], writes=[b_kt])
                        S.op("act", lambda: nc.scalar.activation(out=B[:], in_=Cc[:], func=AF.Exp), reads=[b_C, b_kt], writes=[b_B])
                        S.op("pool", lambda: nc.gpsimd.tensor_tensor(out=qh[:], in0=q_bf[:], in1=B[:], op=ALU.mult), reads=[b_q, b_B], writes=[b_qh])
                        S.op("dve", lambda: nc.vector.scalar_tensor_tensor(out=B3, in0=C3, scalar=-1.0, in1=totb, op0=ALU.mult, op1=ALU.add), reads=[b_C, b_tot, b_qh], writes=[b_B])
                        S.op("act", lambda: nc.scalar.activation(out=B[:], in_=B[:], func=AF.Exp), reads=[b_B], writes=[b_B])
                        S.op("dve", lambda: nc.vector.tensor_tensor(out=kh[:], in0=A[:], in1=B[:], op=ALU.mult), reads=[b_A, b_B], writes=[b_kh])
                        pvb = PS[2][:].bitcast(BF16)
                        msk = triu if s == 0 else tril
                        b_msk = b_triu if s == 0 else b_tril
                        fr = 0 if s == 0 else 32
                        dr = 32 - fr
                        S.op("dve", lambda: nc.vector.memset(aT[dr:dr + 32, :, fr:fr + 32], 0.0), writes=[b_aT])
                        for c8 in range(0, NCH, 8):
                            nb = min(8, NCH - c8)
                            for j in range(nb):
                                c = c8 + j
                                S.op("pe", lambda c=c, j=j: nc.tensor.transpose(out=pvb[0:64, j * 128:(j + 1) * 128], in_=kh[:, c * 64:(c + 1) * 64], identity=identb[:]),
                                     reads=[b_kh, b_identb], writes=[PB[2]])
                            S.op("act", lambda c8=c8, nb=nb: nc.scalar.copy(out=khT[:, c8:c8 + nb, :], in_=pvb[0:64, 0:nb * 128].rearrange("p (j x) -> p j x", j=nb)),
                                 reads=[PB[2]], writes=[b_khT])
                            for j in range(nb):
                                c = c8 + j
                                S.op("pe", lambda c=c, j=j: nc.tensor.matmul(PS[3][fr:fr + 32, j * 64:(j + 1) * 64], lhsT=kt[:, c * 64 + fr:c * 64 + fr + 32], rhs=qt[:, c * 64:(c + 1) * 64], start=True, stop=True),
                                     reads=[b_kt, b_qt], writes=[PB[3]])
                                S.op("pe", lambda c=c, j=j: nc.tensor.matmul(PS[3][dr:dr + 32, j * 64 + dr:j * 64 + dr + 32], lhsT=kt[:, c * 64 + dr:c * 64 + dr + 32], rhs=qt[:, c * 64 + dr:c * 64 + dr + 32], start=True, stop=True),
                                     reads=[b_kt, b_qt], writes=[PB[3]])
                            pv3 = PS[3][:, 0:nb * 64].rearrange("p (j x) -> p j x", j=nb)
                            S.op("dve", lambda c8=c8, nb=nb, pv3=pv3: nc.vector.tensor_tensor(out=aT[fr:fr + 32, c8:c8 + nb, :], in0=pv3[fr:fr + 32, :, :],
                                                                                     in1=msk[fr:fr + 32, :].unsqueeze(1).to_broadcast([32, nb, 64]), op=ALU.mult),
                                 reads=[PB[3], b_msk], writes=[b_aT])
                            S.op("dve", lambda c8=c8, nb=nb, pv3=pv3: nc.vector.tensor_tensor(out=aT[dr:dr + 32, c8:c8 + nb, dr:dr + 32], in0=pv3[dr:dr + 32, :, dr:dr + 32],
                                                                                     in1=msk[dr:dr + 32, dr:dr + 32].unsqueeze(1).to_broadcast([32, nb, 32]), op=ALU.mult),
                                 reads=[PB[3], b_msk], writes=[b_aT])
                        order = list(range(NCH)) if s == 0 else [3, 2, 1, 0] + list(range(NCH - 1, 3, -1))
                        def emit_pS(idx):
                            c = order[idx]
                            pS = 6 + idx % 2
                            S.op("pe", lambda: nc.tensor.matmul(PS[pS][:, 0:128], lhsT=khT[:, c, :], rhs=v_tm[:, c, :], start=True, stop=True), reads=[b_khT, b_v], writes=[PB[pS]])
                        emit_pS(0)
                        for idx, c in enumerate(order):
                            po = 4 + idx % 2
                            pS = 6 + idx % 2
                            if idx + 1 < NCH - 1:
                                emit_pS(idx + 1)
                            if idx < NCH - 1:
                                if idx == 0:
                                    S.op("dve", lambda pS=pS: nc.vector.tensor_copy(out=Sst[:], in_=PS[pS][:, 0:128]), reads=[PB[pS]], writes=[b_S])
                                else:
                                    S.op("dve", lambda c=c, pS=pS: nc.vector.scalar_tensor_tensor(out=Sst[:], in0=Sst[:], scalar=egl[:, c:c + 1], in1=PS[pS][:, 0:128], op0=ALU.mult, op1=ALU.add),
                                         reads=[b_S, b_egl, PB[pS]], writes=[b_S])
                            if idx > 0:
                                sb_t, b_sb = Sb[idx % 2]
                                S.op("pe", lambda c=c, po=po, sb_t=sb_t: nc.tensor.matmul(PS[po][:, 0:64], lhsT=sb_t[:], rhs=qh[:, c * 64:(c + 1) * 64], start=True, stop=False),
                                     reads=[b_sb, b_qh], writes=[PB[po]])
                            S.op("pe", lambda c=c, po=po, idx=idx: nc.tensor.matmul(PS[po][:, 0:64], lhsT=v_tm[:, c, :], rhs=aT[:, c, :], start=(idx == 0), stop=True),
                                 reads=[b_v, b_aT], writes=[PB[po]])
                            S.op("act", lambda c=c, po=po: nc.scalar.copy(out=o_t[:, c * 64:(c + 1) * 64], in_=PS[po][:, 0:64]), reads=[PB[po]], writes=[b_o])
                            if idx < NCH - 1:
                                nsb, b_nsb = Sb[(idx + 1) % 2]
                                S.op("act", lambda nsb=nsb: nc.scalar.copy(out=nsb[:], in_=Sst[:]), reads=[b_S], writes=[b_nsb])
                    o_f, b_of = o_d[0]
                    o_b, b_ob = o_d[1]
                    S.op("dve", lambda: nc.vector.tensor_tensor(out=o_f[:], in0=o_f[:], in1=o_b[:], op=ALU.add), reads=[b_of, b_ob], writes=[b_of])
                    S.op("act", lambda: nc.scalar.activation(out=qt[:], in_=o_f[:], func=AF.Square), reads=[b_of, b_qt], writes=[b_qt])
                    for (t0, n) in GROUPS:
                        pb = pcnt[0] % 2
                        pcnt[0] += 1
                        og_t, b_og = og[ocnt % 2]
                        ocnt += 1
                        S.op("pe", lambda pb=pb: nc.tensor.matmul(PS[pb][:, 0:n], lhsT=onesb[:], rhs=qt[:, t0:t0 + n], start=True, stop=True), reads=[b_onesb, b_qt], writes=[PB[pb]])
                        S.op("act", lambda pb=pb: nc.scalar.activation(out=rs0[:, 0:n], in_=PS[pb][:, 0:n], func=AF.Sqrt, scale=1.0 / 128, bias=epsb[:, 0:1]), reads=[PB[pb], b_eps], writes=[b_rs0])
                        S.op("dve", lambda: nc.vector.reciprocal(out=rs1[:, 0:n], in_=rs0[:, 0:n]), reads=[b_rs0], writes=[b_rs1])
                        S.op("dve", lambda: nc.vector.scalar_tensor_tensor(out=rs0[:, 0:n], in0=o_f[:, t0:t0 + n], scalar=hgn, in1=rs1[:, 0:n], op0=ALU.mult, op1=ALU.mult),
                             reads=[b_of, b_hgn, b_rs1, b_rs0], writes=[b_rs0])
                        S.op("dve", lambda og_t=og_t: nc.vector.tensor_tensor(out=og_t[:, 0:n], in0=rs0[:, 0:n], in1=gate_sb[:, t0:t0 + n], op=ALU.mult), reads=[b_rs0, b_gate], writes=[b_og])
                        S.dma(hg_o_d[hd, :, t0:t0 + n], og_t[:, 0:n], reads=[b_og], writes=[b_ho])
                S.barrier()


        def stage_gdn(l):
            winv = I["w_in"][l].rearrange("(kc p) n -> p kc n", p=128)
            with contextlib.ExitStack() as st0:
                mist, b_mist = tl(st0, "mist", [64, 2, 64], F32)
                mast, b_mast = tl(st0, "mast", [64, 2, 64], F32)
                S.dma(mist[:], I["mist"][:, :, :], writes=[b_mist])
                S.dma(mast[:], I["mast"][:, :, :], writes=[b_mast])
                g_t, b_g = tl(st0, "g_g", [64, NCH, 8], F32)
                beta, b_beta = tl(st0, "g_beta", [64, NCH, 8], F32)
                nbeta, b_nbeta = tl(st0, "g_nbeta", [64, NCH, 8], F32)
                egc, b_egc = tl(st0, "g_egc", [64, NCH, 8], F32)
                negc, b_negc = tl(st0, "g_negc", [64, NCH, 8], F32)
                ekd, b_ekd = tl(st0, "g_ekd", [64, NCH, 8], F32)
                egl, b_egl = tl(st0, "g_egl", [128, NCH, 8], F32)
                cw, b_cw = tl(st0, "g_cw", [128, 12, 5], F32)
                S.dma(cw[:], I["gdn_conv_t"][l], writes=[b_cw])
                with contextlib.ExitStack() as st:
                    wg, b_wg = tl(st, "g_wg", [128, KC, 16], BF16)
                    S.dma(wg[:], winv[:, :, O_GA:O_GA + 16], writes=[b_wg], q="pool")
                    gab, b_gab = tl(st, "g_gab", [64, NCH, 16], F32)
                    alog, b_alog = tl(st, "g_alog", [64, 8], F32)
                    dtb, b_dtb = tl(st, "g_dtb", [64, 8], F32)
                    S.dma(alog[:], I["gdn_a_log"][l:l + 1].rearrange("o s h -> o (s h)").partition_broadcast(64), writes=[b_alog])
                    S.dma(dtb[:], I["gdn_dt_bias"][l:l + 1].rearrange("o s h -> o (s h)").partition_broadcast(64), writes=[b_dtb])
                    for c in range(NCH):
                        pb = c // 32
                        cc = c % 32
                        for kc in range(KC):
                            S.op("pe", lambda c=c, kc=kc, pb=pb, cc=cc: nc.tensor.matmul(PS[pb][0:64, cc * 16:(cc + 1) * 16], lhsT=h_fm[:, kc, c * 64:(c + 1) * 64], rhs=wg[:, kc, :],
                                                                                     start=(kc == 0), stop=(kc == KC - 1)), reads=[b_hfm, b_wg], writes=[PB[pb]])
                    S.op("act", lambda: nc.scalar.copy(out=gab[:, 0:32, :], in_=PS[0][0:64, :].rearrange("p (c x) -> p c x", x=16)), reads=[PB[0]], writes=[b_gab])
                    S.op("act", lambda: nc.scalar.copy(out=gab[:, 32:36, :], in_=PS[1][0:64, 0:64].rearrange("p (c x) -> p c x", x=16)), reads=[PB[1]], writes=[b_gab])
                    S.op("act", lambda: nc.scalar.activation(out=alog[:], in_=alog[:], func=AF.Exp), reads=[b_alog], writes=[b_alog])
                    S.op("dve", lambda: nc.vector.tensor_tensor(out=g_t[:], in0=gab[:, :, 0:8], in1=dtb[:].unsqueeze(1).to_broadcast([64, NCH, 8]), op=ALU.add), reads=[b_gab, b_dtb], writes=[b_g])
                    S.op("act", lambda: nc.scalar.activation(out=g_t[:], in_=g_t[:], func=AF.Exp), reads=[b_g], writes=[b_g])
                    S.op("act", lambda: nc.scalar.activation(out=g_t[:], in_=g_t[:], func=AF.Ln, bias=onesf[0:64, 0:1]), reads=[b_g, b_onesf], writes=[b_g])
                    S.op("dve", lambda: nc.vector.scalar_tensor_tensor(out=g_t[:], in0=g_t[:], scalar=-1.0, in1=alog[:].unsqueeze(1).to_broadcast([64, NCH, 8]), op0=ALU.mult, op1=ALU.mult),
                         reads=[b_g, b_alog], writes=[b_g])
                    S.op("act", lambda: nc.scalar.activation(out=beta[:], in_=gab[:, :, 8:16], func=AF.Sigmoid), reads=[b_gab], writes=[b_beta])
                    S.op("dve", lambda: nc.vector.tensor_scalar(out=nbeta[:], in0=beta[:], scalar1=-1.0, scalar2=None, op0=ALU.mult), reads=[b_beta], writes=[b_nbeta])
                    for c in range(NCH):
                        for sd in range(2):
                            S.op("pe", lambda c=c, sd=sd: nc.tensor.matmul(PS[2][0:64, c * 8 + sd * 4:c * 8 + sd * 4 + 4], lhsT=mist[:, sd, :], rhs=g_t[:, c, sd * 4:sd * 4 + 4], start=True, stop=True),
                                 reads=[b_mist, b_g], writes=[PB[2]])
                            S.op("pe", lambda c=c, sd=sd: nc.tensor.matmul(PS[3][0:64, c * 8 + sd * 4:c * 8 + sd * 4 + 4], lhsT=mast[:, sd, :], rhs=g_t[:, c, sd * 4:sd * 4 + 4], start=True, stop=True),
                                 reads=[b_mast, b_g], writes=[PB[3]])
                        S.op("pe", lambda c=c: nc.tensor.matmul(PS[4][:, c * 8:c * 8 + 8], lhsT=onesf[0:64, :], rhs=g_t[:, c, :], start=True, stop=True), reads=[b_onesf, b_g], writes=[PB[4]])
                    S.op("act", lambda: nc.scalar.activation(out=egc[:].rearrange("p c x -> p (c x)"), in_=PS[2][0:64, 0:NCH * 8], func=AF.Exp), reads=[PB[2]], writes=[b_egc])
                    S.op("act", lambda: nc.scalar.activation(out=ekd[:].rearrange("p c x -> p (c x)"), in_=PS[3][0:64, 0:NCH * 8], func=AF.Exp), reads=[PB[3]], writes=[b_ekd])
                    S.op("act", lambda: nc.scalar.activation(out=egl[:].rearrange("p c x -> p (c x)"), in_=PS[4][:, 0:NCH * 8], func=AF.Exp), reads=[PB[4]], writes=[b_egl])
                    S.op("dve", lambda: nc.vector.tensor_scalar(out=negc[:], in0=egc[:], scalar1=-1.0, scalar2=None, op0=ALU.mult), reads=[b_egc], writes=[b_negc])
                    S.barrier()
                if cfg.get("gdn_stop") == 1:
                    S.barrier()
                    return

                for pr in range(2):
                    with contextlib.ExitStack() as st:
                        q_fm = [tl(st, "g_q%d" % i, [128, T], BF16) for i in range(2)]
                        k_fm = [tl(st, "g_k%d" % i, [128, T], BF16) for i in range(2)]
                        k_tm = [tl(st, "g_ktm%d" % i, [64, NCH, 128], BF16) for i in range(2)]
                        v_tm = [tl(st, "g_vtm%d" % i, [64, NCH, 128], BF16) for i in range(2)]
                        aqkT, b_aqkT = tl(st, "g_aqkT", [64, NCH, 4, 64], BF16)
                        R5b, b_R5b = tl(st, "g_R5b", [64, NCH, 4, 64], BF16)
                        with contextlib.ExitStack() as st2:
                            wc = [tl(st2, "g_wc%d" % i, [128, KC, 128], BF16) for i in range(2)]
                            zpad, b_zp = tl(st2, "g_zpad", [128, T + 8], F32)
                            acc, b_acc = tl(st2, "g_acc", [128, T + 8], F32)
                            xs, b_xs = tl(st2, "g_xs", [128, T], F32)
                            sqb, b_sqb = tl(st2, "g_sq", [128, T], BF16)
                            vfm, b_vfm = tl(st2, "g_vfm", [128, T], BF16)
                            r0, b_r0 = tl(st2, "g_r0", [128, 512], F32)
                            r1, b_r1 = tl(st2, "g_r1", [128, 512], F32)
                            S.op("dve", lambda: nc.vector.memset(zpad[:], 0.0), writes=[b_zp])
                            wcnt = 0
                            items = [(hh, part) for hh in range(2) for part in range(3)]

                            def load_wc(k):
                                hh_, part_ = items[k]
                                ch_ = part_ * 4 + pr * 2 + hh_
                                wct_, b_wc_ = wc[k % 2]
                                S.dma(wct_[:], winv[:, :, O_GDN + ch_ * 128:O_GDN + (ch_ + 1) * 128], writes=[b_wc_], q="pool")
                            load_wc(0)
                            for hh in range(2):
                                hd = pr * 2 + hh
                                for part in range(3):
                                    ch = part * 4 + hd
                                    wct, b_wc = wc[wcnt % 2]
                                    wcnt += 1
                                    if wcnt < len(items):
                                        load_wc(wcnt)
                                    for gi, (t0, n) in enumerate(GROUPS):
                                        pb = gi % 2
                                        for kc in range(KC):
                                            S.op("pe", lambda kc=kc, pb=pb, wct=wct: nc.tensor.matmul(PS[pb][:, 0:n], lhsT=wct[:, kc, :], rhs=h_fm[:, kc, t0:t0 + n], start=(kc == 0), stop=(kc == KC - 1)),
                                                 reads=[b_wc, b_hfm], writes=[PB[pb]])
                                        z0 = 2 + t0 if gi == 0 else 6 + t0
                                        S.op("act", lambda pb=pb, z0=z0: nc.scalar.copy(out=zpad[:, z0:z0 + n], in_=PS[pb][:, 0:n]), reads=[PB[pb]], writes=[b_zp])
                                    NW = T + 4
                                    S.op("dve", lambda ch=ch: nc.vector.tensor_scalar(out=acc[:, 2:2 + NW], in0=zpad[:, 0:NW], scalar1=cw[:, ch, 0:1], scalar2=None, op0=ALU.mult),
                                         reads=[b_zp, b_cw], writes=[b_acc])
                                    for tau in range(1, 5):
                                        S.op("dve", lambda ch=ch, tau=tau: nc.vector.scalar_tensor_tensor(out=acc[:, 2:2 + NW], in0=zpad[:, tau:tau + NW], scalar=cw[:, ch, tau:tau + 1], in1=acc[:, 2:2 + NW],
                                                                                                        op0=ALU.mult, op1=ALU.add), reads=[b_zp, b_cw, b_acc], writes=[b_acc])
                                    if part == 2:
                                        S.op("act", lambda: nc.scalar.activation(out=vfm[:, 0:CTX], in_=acc[:, 2:2 + CTX], func=AF.Silu), reads=[b_acc], writes=[b_vfm])
                                        S.op("act", lambda: nc.scalar.activation(out=vfm[:, CTX:T], in_=acc[:, 6 + CTX:6 + T], func=AF.Silu), reads=[b_acc], writes=[b_vfm])
                                        srcs = [(vfm, b_vfm, v_tm[hh])]
                                    else:
                                        S.op("act", lambda: nc.scalar.activation(out=xs[:, 0:CTX], in_=acc[:, 2:2 + CTX], func=AF.Silu), reads=[b_acc], writes=[b_xs])
                                        S.op("act", lambda: nc.scalar.activation(out=xs[:, CTX:T], in_=acc[:, 6 + CTX:6 + T], func=AF.Silu), reads=[b_acc], writes=[b_xs])
                                        S.op("act", lambda: nc.scalar.activation(out=sqb[:], in_=xs[:], func=AF.Square), reads=[b_xs], writes=[b_sqb])
                                        dst, b_dst = (q_fm if part == 0 else k_fm)[hh]
                                        for gi, (t0, n) in enumerate(GROUPS):
                                            pb = 2 + gi % 2
                                            S.op("pe", lambda pb=pb: nc.tensor.matmul(PS[pb][:, 0:n], lhsT=onesb[:], rhs=sqb[:, t0:t0 + n], start=True, stop=True), reads=[b_onesb, b_sqb], writes=[PB[pb]])
                                            S.op("act", lambda pb=pb: nc.scalar.activation(out=r0[:, 0:n], in_=PS[pb][:, 0:n], func=AF.Sqrt, bias=epsb[:, 0:1]), reads=[PB[pb], b_eps], writes=[b_r0])
                                            S.op("dve", lambda: nc.vector.reciprocal(out=r1[:, 0:n], in_=r0[:, 0:n]), reads=[b_r0], writes=[b_r1])
                                            S.op("dve", lambda dst=dst: nc.vector.scalar_tensor_tensor(out=dst[:, t0:t0 + n], in0=xs[:, t0:t0 + n], scalar=(128 ** -0.5 if part == 0 else 1.0), in1=r1[:, 0:n],
                                                                                                   op0=ALU.mult, op1=ALU.mult), reads=[b_xs, b_r1], writes=[b_dst])
                                        srcs = [(dst, b_dst, k_tm[hh])] if part == 1 else []
                                    for (src, b_src, (dtm, b_dtm)) in srcs:
                                        pvb = PS[4][:].bitcast(BF16)
                                        pvb2 = PS[5][:].bitcast(BF16)
                                        for c8 in range(0, NCH, 8):
                                            nb = min(8, NCH - c8)
                                            pv = pvb if (c8 // 8) % 2 == 0 else pvb2
                                            pbi = 4 + (c8 // 8) % 2
                                            for j in range(nb):
                                                c = c8 + j
                                                S.op("pe", lambda c=c, j=j, pv=pv, src=src: nc.tensor.transpose(out=pv[0:64, j * 128:(j + 1) * 128], in_=src[:, c * 64:(c + 1) * 64], identity=identb[:]),
                                                     reads=[b_src, b_identb], writes=[PB[pbi]])
                                            S.op("act", lambda c8=c8, nb=nb, pv=pv, dtm=dtm: nc.scalar.copy(out=dtm[:, c8:c8 + nb, :], in_=pv[0:64, 0:nb * 128].rearrange("p (j x) -> p j x", j=nb)),
                                                 reads=[PB[pbi]], writes=[b_dtm])
                            S.barrier()
                        if cfg.get("gdn_stop") == 2:
                            S.barrier()
                            return
                        with contextlib.ExitStack() as st2:
                            NCB = 2
                            NB = NCB * 4
                            mistF, b_mistF = tl(st2, "g_mistF", [64, NCB, 2, 2, 64], F32)
                            mastF, b_mastF = tl(st2, "g_mastF", [64, NCB, 2, 2, 64], F32)
                            for cj in range(NCB):
                                for hh in range(2):
                                    S.op("dve", lambda cj=cj, hh=hh: nc.vector.tensor_copy(out=mistF[:, cj, :, hh, :], in_=mist[:]), reads=[b_mist], writes=[b_mistF])
                                    S.op("dve", lambda cj=cj, hh=hh: nc.vector.tensor_copy(out=mastF[:, cj, :, hh, :], in_=mast[:]), reads=[b_mast], writes=[b_mastF])
                            fl = lambda t_: t_[:].rearrange("p b x -> p (b x)")
                            v3 = lambda t_: t_[:].rearrange("p (cs h) x -> p cs h x", h=2)
                            mI3 = mistF[:].rearrange("p c s h x -> p (c s) h x")
                            mA3 = mastF[:].rearrange("p c s h x -> p (c s) h x")
                            lI, b_lI = tl(st2, "g_lI", [64, NB, 64], F32)
                            lA, b_lA = tl(st2, "g_lA", [64, NB, 64], F32)
                            Dec, b_Dec = tl(st2, "g_Dec", [64, NB, 64], F32)
                            DecT, b_DecT = tl(st2, "g_DecT", [64, NB, 64], F32)
                            t1, b_t1 = tl(st2, "g_t1", [64, NB, 64], F32)
                            Pm = [tl(st2, "g_P%d" % i, [64, NB, 64], BF16) for i in range(2)]
                            Qm = [tl(st2, "g_Q%d" % i, [64, NB, 64], BF16) for i in range(2)]
                            Rbm = [tl(st2, "g_Rb%d" % i, [64, NB, 64], BF16) for i in range(2)]
                            Rm = [tl(st2, "g_R%d" % i, [64, NB, 64], F32) for i in range(2)]

                            def gcols(tile_, c0):
                                return tile_[:, c0:c0 + NCB, :].rearrange("p c (s h) -> p (c s) h", s=2)[:, :, pr * 2:pr * 2 + 2]

                            for c0 in range(0, NCH, NCB):
                                for cj in range(NCB):
                                    c = c0 + cj
                                    cs = slice(c * 64, (c + 1) * 64)
                                    for b in range(4):
                                        hh = b % 2
                                        bb = cj * 4 + b
                                        kf, b_kf = k_fm[hh]
                                        qf, b_qf = q_fm[hh]
                                        S.op("pe", lambda bb=bb, kf=kf, cs=cs: nc.tensor.matmul(PS[0][0:64, bb * 64:(bb + 1) * 64], lhsT=kf[:, cs], rhs=kf[:, cs], start=True, stop=True), reads=[b_kf], writes=[PB[0]])
                                        S.op("pe", lambda bb=bb, kf=kf, qf=qf, cs=cs: nc.tensor.matmul(PS[7][0:64, bb * 64:(bb + 1) * 64], lhsT=kf[:, cs], rhs=qf[:, cs], start=True, stop=True),
                                             reads=[b_kf, b_qf], writes=[PB[7]])
                                g3 = gcols(g_t, c0).unsqueeze(3).to_broadcast([64, 2 * NCB, 2, 64])
                                S.op("dve", lambda g3=g3: nc.vector.tensor_tensor(out=v3(lI), in0=mI3, in1=g3, op=ALU.mult), reads=[b_mistF, b_g], writes=[b_lI])
                                S.op("dve", lambda g3=g3: nc.vector.tensor_tensor(out=v3(lA), in0=mA3, in1=g3, op=ALU.mult), reads=[b_mastF, b_g], writes=[b_lA])
                                for bb in range(NB):
                                    sd = (bb % 4) // 2
                                    S.op("pe", lambda bb=bb, sd=sd: nc.tensor.matmul(PS[1][0:64, bb * 64:(bb + 1) * 64], lhsT=lI[:, bb, :], rhs=mast[:, sd, :], start=True, stop=True), reads=[b_lI, b_mast], writes=[PB[1]])
                                    S.op("pe", lambda bb=bb, sd=sd: nc.tensor.matmul(PS[2][0:64, bb * 64:(bb + 1) * 64], lhsT=lA[:, bb, :], rhs=mist[:, sd, :], start=True, stop=True), reads=[b_lA, b_mist], writes=[PB[2]])
                                S.op("act", lambda: nc.scalar.activation(out=fl(Dec), in_=PS[1][0:64, :], func=AF.Exp), reads=[PB[1]], writes=[b_Dec])
                                S.op("act", lambda: nc.scalar.activation(out=fl(DecT), in_=PS[2][0:64, :], func=AF.Exp), reads=[PB[2]], writes=[b_DecT])
                                P0, b_P0 = Pm[0]
                                Q0, b_Q0 = Qm[0]
                                R0, b_R0 = Rm[0]
                                S.op("dve", lambda: nc.vector.tensor_tensor(out=fl(t1), in0=PS[0][0:64, :], in1=fl(Dec), op=ALU.mult), reads=[PB[0], b_Dec], writes=[b_t1])
                                S.op("dve", lambda: nc.vector.tensor_tensor(out=fl(t1), in0=fl(t1), in1=mastF[:].rearrange("p c s h x -> p (c s h x)"), op=ALU.mult), reads=[b_t1, b_mastF], writes=[b_t1])
                                S.op("dve", lambda c0=c0: nc.vector.tensor_tensor(out=v3(P0), in0=v3(t1), in1=gcols(nbeta, c0).unsqueeze(3).to_broadcast([64, 2 * NCB, 2, 64]), op=ALU.mult),
                                     reads=[b_t1, b_nbeta], writes=[b_P0])
                                S.op("dve", lambda: nc.vector.tensor_tensor(out=fl(DecT), in0=PS[7][0:64, :], in1=fl(DecT), op=ALU.mult), reads=[PB[7], b_DecT], writes=[b_DecT])
                                S.op("dve", lambda c0=c0: nc.vector.tensor_tensor(out=aqkT[:, c0:c0 + NCB, :, :].rearrange("p c b x -> p (c b x)"), in0=fl(DecT), in1=mistF[:].rearrange("p c s h x -> p (c s h x)"), op=ALU.mult),
                                     reads=[b_DecT, b_mistF], writes=[b_aqkT])
                                pv3b = PS[3][:].bitcast(BF16)
                                for bb in range(NB):
                                    S.op("pe", lambda bb=bb: nc.tensor.transpose(out=pv3b[0:64, bb * 64:(bb + 1) * 64], in_=P0[:, bb, :], identity=identb[0:64, 0:64]), reads=[b_P0, b_identb], writes=[PB[3]])
                                S.op("act", lambda: nc.scalar.copy(out=fl(Q0), in_=pv3b[0:64, 0:NB * 64]), reads=[PB[3]], writes=[b_Q0])
                                S.op("dve", lambda: nc.vector.tensor_tensor(out=R0[:], in0=Q0[:], in1=identf[0:64, 0:64].unsqueeze(1).to_broadcast([64, NB, 64]), op=ALU.add),
                                     reads=[b_Q0, b_identf], writes=[b_R0])
                                S.op("act", lambda: nc.scalar.copy(out=fl(Rbm[0][0]), in_=fl(R0)), reads=[b_R0], writes=[Rbm[0][1]])
                                for k in range(1, 6):
                                    Pp, b_Pp = Pm[(k - 1) % 2]
                                    Qp, b_Qp = Qm[(k - 1) % 2]
                                    Rp, b_Rp = Rm[(k - 1) % 2]
                                    Rbp, b_Rbp = Rbm[(k - 1) % 2]
                                    Pn, b_Pn = Pm[k % 2]
                                    Qn, b_Qn = Qm[k % 2]
                                    Rn, b_Rn = Rm[k % 2]
                                    Rbn, b_Rbn = Rbm[k % 2]
                                    for bb in range(NB):
                                        S.op("pe", lambda bb=bb: nc.tensor.matmul(PS[4][0:64, bb * 64:(bb + 1) * 64], lhsT=Qp[:, bb, :], rhs=Pp[:, bb, :], start=True, stop=True), reads=[b_Qp, b_Pp], writes=[PB[4]])
                                    if k < 5:
                                        for bb in range(NB):
                                            S.op("pe", lambda bb=bb: nc.tensor.matmul(PS[5][0:64, bb * 64:(bb + 1) * 64], lhsT=Pp[:, bb, :], rhs=Qp[:, bb, :], start=True, stop=True), reads=[b_Qp, b_Pp], writes=[PB[5]])
                                    S.op("act", lambda: nc.scalar.copy(out=fl(Pn), in_=PS[4][0:64, :]), reads=[PB[4]], writes=[b_Pn])
                                    if k < 5:
                                        S.op("dve", lambda: nc.vector.tensor_copy(out=fl(Qn), in_=PS[5][0:64, :]), reads=[PB[5]], writes=[b_Qn])
                                    for bb in range(NB):
                                        S.op("pe", lambda bb=bb: nc.tensor.matmul(PS[6][0:64, bb * 64:(bb + 1) * 64], lhsT=Pn[:, bb, :], rhs=Rbp[:, bb, :], start=True, stop=True), reads=[b_Pn, b_Rbp], writes=[PB[6]])
                                    S.op("dve", lambda: nc.vector.tensor_tensor(out=fl(Rn), in0=fl(Rp), in1=PS[6][0:64, :], op=ALU.add), reads=[b_Rp, PB[6]], writes=[b_Rn])
                                    if k < 5:
                                        S.op("act", lambda: nc.scalar.copy(out=fl(Rbn), in_=fl(Rn)), reads=[b_Rn], writes=[b_Rbn])
                                    if k == 5:
                                        S.op("dve", lambda c0=c0: nc.vector.tensor_tensor(out=R5b[:, c0:c0 + NCB, :, :].rearrange("p c (s h) x -> p (c s) h x", s=2), in0=v3(Rn), in1=gcols(beta, c0).unsqueeze(3).to_broadcast([64, 2 * NCB, 2, 64]), op=ALU.mult),
                                             reads=[b_Rn, b_beta], writes=[b_R5b])
                            S.barrier()
                        if cfg.get("gdn_stop") == 3:
                            S.barrier()
                            return
                        with contextlib.ExitStack() as st2:
                            Sst = [tl(st2, "g_S%d" % i, [128, 128], F32) for i in range(4)]
                            Sbb = [[tl(st2, "g_Sb%d_%d" % (i, j), [128, 128], BF16) for j in range(2)] for i in range(4)]
                            Xs = [tl(st2, "g_X%d" % i, [64, 128], BF16) for i in range(4)]
                            vnb = [tl(st2, "g_vn%d" % i, [64, 128], BF16) for i in range(4)]
                            vnk = [tl(st2, "g_vk%d" % i, [64, 128], BF16) for i in range(4)]
                            tmpo = [tl(st2, "g_to%d" % i, [64, 128], F32) for i in range(4)]
                            oo = [[tl(st2, "g_oo%d_%d" % (i, j), [64, 128], F32) for j in range(2)] for i in range(4)]
                            b_raw = Buf("gdn_raw")
                            orders = [list(range(NCH)), [3, 2, 1, 0] + list(range(NCH - 1, 3, -1))]
                            for idx in range(NCH):
                                ch = []
                                for b in range(4):
                                    sd, hh = b // 2, b % 2
                                    hd = pr * 2 + hh
                                    c = orders[sd][idx]
                                    ch.append(dict(b=b, sd=sd, hh=hh, hd=hd, col=sd * 4 + hd, c=c, cs=slice(c * 64, (c + 1) * 64), pa=2 * b, pc=2 * b + 1,
                                                   kf=k_fm[hh], qf=q_fm[hh], vt=v_tm[hh], ktm=k_tm[hh], X=Xs[b], vn=vnb[b], vk=vnk[b], to=tmpo[b], o=oo[b][idx % 2], S=Sst[b],
                                                   sb=Sbb[b][idx % 2], nsb=Sbb[b][(idx + 1) % 2]))
                                if idx > 0:
                                    for d in ch:
                                        S.op("pe", lambda d=d: nc.tensor.matmul(PS[d["pa"]][0:64, 0:128], lhsT=d["kf"][0][:, d["cs"]], rhs=d["sb"][0][:], start=True, stop=True), reads=[d["kf"][1], d["sb"][1]], writes=[PB[d["pa"]]])
                                        S.op("pe", lambda d=d: nc.tensor.matmul(PS[d["pa"]][0:64, 128:256], lhsT=d["qf"][0][:, d["cs"]], rhs=d["sb"][0][:], start=True, stop=True), reads=[d["qf"][1], d["sb"][1]], writes=[PB[d["pa"]]])
                                    for d in ch:
                                        S.op("dve", lambda d=d: nc.vector.scalar_tensor_tensor(out=d["X"][0][:], in0=PS[d["pa"]][0:64, 0:128], scalar=negc[:, d["c"], d["col"]:d["col"] + 1], in1=d["vt"][0][:, d["c"], :], op0=ALU.mult, op1=ALU.add),
                                             reads=[PB[d["pa"]], b_negc, d["vt"][1]], writes=[d["X"][1]])
                                for d in ch:
                                    if idx > 0:
                                        xin_ap, xr = d["X"][0][:], [d["X"][1]]
                                    else:
                                        xin_ap, xr = d["vt"][0][:, d["c"], :], [d["vt"][1]]
                                    S.op("pe", lambda d=d, xin_ap=xin_ap: nc.tensor.matmul(PS[d["pa"]][0:64, 256:384], lhsT=R5b[:, d["c"], d["b"], :], rhs=xin_ap, start=True, stop=True), reads=[b_R5b] + xr, writes=[PB[d["pa"]]])
                                for d in ch:
                                    S.op("act", lambda d=d: nc.scalar.copy(out=d["vn"][0][:], in_=PS[d["pa"]][0:64, 256:384]), reads=[PB[d["pa"]]], writes=[d["vn"][1]])
                                    S.op("dve", lambda d=d: nc.vector.tensor_scalar(out=d["vk"][0][:], in0=PS[d["pa"]][0:64, 256:384], scalar1=ekd[:, d["c"], d["col"]:d["col"] + 1], scalar2=None, op0=ALU.mult), reads=[PB[d["pa"]], b_ekd], writes=[d["vk"][1]])
                                for d in ch:
                                    S.op("pe", lambda d=d: nc.tensor.matmul(PS[d["pa"]][0:64, 384:512], lhsT=aqkT[:, d["c"], d["b"], :], rhs=d["vn"][0][:], start=True, stop=True), reads=[b_aqkT, d["vn"][1]], writes=[PB[d["pa"]]])
                                    if idx < NCH - 1:
                                        S.op("pe", lambda d=d: nc.tensor.matmul(PS[d["pc"]][:, 0:128], lhsT=d["ktm"][0][:, d["c"], :], rhs=d["vk"][0][:], start=True, stop=True), reads=[d["ktm"][1], d["vk"][1]], writes=[PB[d["pc"]]])
                                if idx < NCH - 1:
                                    for d in ch:
                                        if idx == 0:
                                            S.op("dve", lambda d=d: nc.vector.tensor_copy(out=d["S"][0][:], in_=PS[d["pc"]][:, 0:128]), reads=[PB[d["pc"]]], writes=[d["S"][1]])
                                        else:
                                            S.op("dve", lambda d=d: nc.vector.scalar_tensor_tensor(out=d["S"][0][:], in0=d["S"][0][:], scalar=egl[:, d["c"], d["col"]:d["col"] + 1], in1=PS[d["pc"]][:, 0:128], op0=ALU.mult, op1=ALU.add),
                                                 reads=[d["S"][1], b_egl, PB[d["pc"]]], writes=[d["S"][1]])
                                    for d in ch:
                                        S.op("act", lambda d=d: nc.scalar.copy(out=d["nsb"][0][:], in_=d["S"][0][:]), reads=[d["S"][1]], writes=[d["nsb"][1]])
                                for d in ch:
                                    if idx > 0:
                                        S.op("act", lambda d=d: nc.scalar.copy(out=d["to"][0][:], in_=PS[d["pa"]][0:64, 384:512]), reads=[PB[d["pa"]]], writes=[d["to"][1]])
                                        S.op("dve", lambda d=d: nc.vector.scalar_tensor_tensor(out=d["o"][0][:], in0=PS[d["pa"]][0:64, 128:256], scalar=egc[:, d["c"], d["col"]:d["col"] + 1], in1=d["to"][0][:], op0=ALU.mult, op1=ALU.add),
                                             reads=[PB[d["pa"]], b_egc, d["to"][1]], writes=[d["o"][1]])
                                    else:
                                        S.op("act", lambda d=d: nc.scalar.copy(out=d["o"][0][:], in_=PS[d["pa"]][0:64, 384:512]), reads=[PB[d["pa"]]], writes=[d["o"][1]])
                                    S.dma(gdn_raw_d[d["sd"], d["c"] * 64:(d["c"] + 1) * 64, d["hd"] * 128:(d["hd"] + 1) * 128], d["o"][0][:], reads=[d["o"][1]], writes=[b_raw])
                            S.barrier()
                if cfg.get("gdn_stop") == 4:
                    S.barrier()
                    return
                with contextlib.ExitStack() as st:
                    wgg, b_wgg = tl(st, "g_wgg", [128, KC, 512], BF16)
                    S.dma(wgg[:], winv[:, :, O_GG:O_GG + 512], writes=[b_wgg], q="pool")
                    gnw, b_gnw = tl(st, "g_gnw", [128, 128], F32)
                    S.dma(gnw[:], I["gdn_norm"][l:l + 1, :].partition_broadcast(128), writes=[b_gnw])
                    of_ = [tl(st, "g_of%d" % i, [128, 512], F32) for i in range(2)]
                    ob_ = [tl(st, "g_ob%d" % i, [128, 512], F32) for i in range(2)]
                    sqt = [tl(st, "g_sqt%d" % i, [128, 512], F32) for i in range(2)]
                    gt_ = [tl(st, "g_gt%d" % i, [128, 512], F32) for i in range(2)]
                    ms = [tl(st, "g_ms%d" % i, [128, 4], F32) for i in range(2)]
                    obf = [tl(st, "g_obf%d" % i, [128, 512], BF16) for i in range(2)]
                    ofm = [tl(st, "g_ofm%d" % i, [128, 4, 128], BF16) for i in range(2)]
                    b_go = Buf("gdn_o")
                    hsub = cfg.get("gdn_hsub", 99)
                    for i in range(cfg.get("gdn_hnt", NT)):
                        a, b_a = of_[i % 2]
                        bb, b_bb = ob_[i % 2]
                        sq_t, b_sq = sqt[i % 2]
                        g_tl, b_gt = gt_[i % 2]
                        ms_t, b_ms = ms[i % 2]
                        obf_t, b_obf = obf[i % 2]
                        ofm_t, b_ofm = ofm[i % 2]
                        ts_ = slice(i * 128, (i + 1) * 128)
                        S.dma(a[:], gdn_raw_d[0, ts_, :], writes=[b_a])
                        S.dma(bb[:], gdn_raw_d[1, ts_, :], writes=[b_bb])
                        pb = i % 2
                        for kc in range(KC):
                            S.op("pe", lambda kc=kc, pb=pb: nc.tensor.matmul(PS[pb][:, :], lhsT=h_fm[:, kc, ts_], rhs=wgg[:, kc, :], start=(kc == 0), stop=(kc == KC - 1)), reads=[b_hfm, b_wgg], writes=[PB[pb]])
                        S.op("act", lambda: nc.scalar.activation(out=g_tl[:], in_=PS[pb][:, :], func=AF.Silu), reads=[PB[pb]], writes=[b_gt])
                        if hsub < 1:
                            continue
                        S.op("dve", lambda: nc.vector.tensor_tensor(out=a[:], in0=a[:], in1=bb[:], op=ALU.add), reads=[b_a, b_bb], writes=[b_a])
                        S.op("act", lambda: nc.scalar.activation(out=sq_t[:], in_=a[:], func=AF.Square), reads=[b_a], writes=[b_sq])
                        S.op("dve", lambda: nc.vector.tensor_reduce(out=ms_t[:], in_=sq_t[:].rearrange("p (h x) -> p h x", h=4), axis=AX.X, op=ALU.add), reads=[b_sq], writes=[b_ms])
                        S.op("act", lambda: nc.scalar.activation(out=ms_t[:], in_=ms_t[:], func=AF.Sqrt, scale=1.0 / 128, bias=epsb[:, 0:1]), reads=[b_ms, b_eps], writes=[b_ms])
                        S.op("dve", lambda: nc.vector.reciprocal(out=ms_t[:], in_=ms_t[:]), reads=[b_ms], writes=[b_ms])
                        if hsub < 2:
                            continue
                        a3 = a[:].rearrange("p (h x) -> p h x", h=4)
                        S.op("dve", lambda: nc.vector.tensor_tensor(out=a3, in0=a3, in1=ms_t[:].unsqueeze(2).to_broadcast([128, 4, 128]), op=ALU.mult), reads=[b_a, b_ms], writes=[b_a])
                        S.op("dve", lambda: nc.vector.tensor_tensor(out=a3, in0=a3, in1=gnw[:].unsqueeze(1).to_broadcast([128, 4, 128]), op=ALU.mult), reads=[b_a, b_gnw], writes=[b_a])
                        S.op("dve", lambda: nc.vector.tensor_tensor(out=obf_t[:], in0=a[:], in1=g_tl[:], op=ALU.mult), reads=[b_a, b_gt], writes=[b_obf])
                        if hsub < 3:
                            continue
                        pv = PS[2 + pb][:].bitcast(BF16)
                        for hd in range(4):
                            S.op("pe", lambda hd=hd: nc.tensor.transpose(out=pv[:, hd * 128:(hd + 1) * 128], in_=obf_t[:, hd * 128:(hd + 1) * 128], identity=identb[:]), reads=[b_obf, b_identb], writes=[PB[2 + pb]])
                        S.op("act", lambda: nc.scalar.copy(out=ofm_t[:], in_=pv[:, 0:512].rearrange("p (h x) -> p h x", h=4)), reads=[PB[2 + pb]], writes=[b_ofm])
                        if hsub < 4:
                            continue
                        S.dma(gdn_o_d[:, :, ts_].rearrange("h d t -> d h t"), ofm_t[:], reads=[b_ofm], writes=[b_go])
                    S.barrier()


        def ln_tile(st_tiles, i, f_halves, f_bufs, prm, out_final):
            s = 1 if i < 2 else 0
            x_t, b_x = st_tiles["x"][i % 2]
            t_t, b_t = st_tiles["t"][i % 2]
            h_t, b_h = st_tiles["h"][i % 2]
            stt, b_st = st_tiles["st"][i % 2]
            mv, b_mv = st_tiles["mv"][i % 2]
            ts_ = slice(i * 128, (i + 1) * 128)
            gate_t, b_gate = prm["gate"][s]

            def A():
                S.dma(x_t[:], xres_d[ts_, :], reads=[b_xres[i]], writes=[b_x])
                for hf in range(2):
                    hs = slice(hf * 512, (hf + 1) * 512)
                    S.op("dve", lambda hf=hf, hs=hs: nc.vector.tensor_tensor(out=t_t[:, hs], in0=f_halves[hf], in1=gate_t[:, hs], op=ALU.mult), reads=[f_bufs[hf], b_gate], writes=[b_t])
                S.op("dve", lambda: nc.vector.scalar_tensor_tensor(out=x_t[:], in0=x_t[:], scalar=ALPHA, in1=t_t[:], op0=ALU.mult, op1=ALU.add), reads=[b_x, b_t], writes=[b_x])
                for hf in range(2):
                    S.op("dve", lambda hf=hf: nc.vector.bn_stats(out=stt[:, hf, :], in_=x_t[:, hf * 512:(hf + 1) * 512]), reads=[b_x], writes=[b_st])
                S.op("dve", lambda: nc.vector.bn_aggr(out=mv[:, 0:2], in_=stt[:].rearrange("p a b -> p (a b)")), reads=[b_st], writes=[b_mv])
                S.op("act", lambda: nc.scalar.activation(out=mv[:, 2:3], in_=mv[:, 1:2], func=AF.Sqrt, bias=epsb[:, 0:1]), reads=[b_mv, b_eps], writes=[b_mv])
                S.op("dve", lambda: nc.vector.reciprocal(out=mv[:, 3:4], in_=mv[:, 2:3]), reads=[b_mv], writes=[b_mv])
                S.op("dve", lambda: nc.vector.tensor_scalar(out=x_t[:], in0=x_t[:], scalar1=mv[:, 0:1], scalar2=mv[:, 3:4], op0=ALU.subtract, op1=ALU.mult), reads=[b_x, b_mv], writes=[b_x])

            def B():
                S.op("pool", lambda: nc.gpsimd.tensor_tensor(out=x_t[:], in0=x_t[:], in1=prm["g"][0][:], op=ALU.mult), reads=[b_x, prm["g"][1]], writes=[b_x])
                S.op("pool", lambda: nc.gpsimd.tensor_tensor(out=x_t[:], in0=x_t[:], in1=prm["b"][0][:], op=ALU.add), reads=[b_x, prm["b"][1]], writes=[b_x])
                if out_final:
                    S.dma(out_d[(i - 2) * 128:(i - 1) * 128, :], x_t[:], reads=[b_x])
                    return
                S.dma(xres_d[ts_, :], x_t[:], reads=[b_x], writes=[b_xres[i]])
                sc_t, b_sc = prm["sc"][s]
                sh_t, b_sh = prm["sh"][s]
                S.op("dve", lambda: nc.vector.tensor_tensor(out=t_t[:], in0=x_t[:], in1=sc_t[:], op=ALU.mult), reads=[b_x, b_sc], writes=[b_t])
                S.op("pool", lambda: nc.gpsimd.tensor_tensor(out=h_t[:], in0=t_t[:], in1=sh_t[:], op=ALU.add), reads=[b_t, b_sh], writes=[b_h])
                to_fm(h_t, b_h, i, 6 + i % 2)
            return A, B

        def ln_setup(st, l_mod, jgate, ln_g, ln_b, l, jsh, jsc, need_mod):
            tiles = {
                "x": [tl(st, "ln_x%d" % i, [128, D], F32) for i in range(2)],
                "t": [tl(st, "ln_t%d" % i, [128, D], F32) for i in range(2)],
                "h": [tl(st, "ln_h%d" % i, [128, D], BF16) for i in range(2)],
                "st": [tl(st, "ln_st%d" % i, [128, 2, 6], F32) for i in range(2)],
                "mv": [tl(st, "ln_mv%d" % i, [128, 4], F32) for i in range(2)],
            }
            prm = {"gate": [load_bc(st, "ln_gate%d" % s_, l, s_, jgate) for s_ in range(2)],
                   "g": load_vec_bc(st, "ln_g", I[ln_g][l:l + 1, :]),
                   "b": load_vec_bc(st, "ln_b", I[ln_b][l:l + 1, :])}
            if need_mod:
                prm["sh"] = [load_bc(st, "ln_sh%d" % s_, l_mod, s_, jsh) for s_ in range(2)]
                prm["sc"] = [load_bc(st, "ln_sc%d" % s_, l_mod, s_, jsc, plus_one=True) for s_ in range(2)]
            return tiles, prm

        def stage_merge(l, last):
            winv = I["w_in"][l].rearrange("(kc p) n -> p kc n", p=128)
            groups = GROUPS[1:] if last else GROUPS
            tiles_i = range(2, NT) if last else range(NT)
            with contextlib.ExitStack() as st:
                y_fm, b_y = tl(st, "y_fm", [128, KC, T], BF16)
                with contextlib.ExitStack() as st2:
                    mo, b_mo = tl(st2, "m_mo", [64, 8, T], BF16)
                    ho, b_ho = tl(st2, "m_ho", [128, 4, T], BF16)
                    go, b_go = tl(st2, "m_go", [128, 4, T], BF16)
                    S.dma(mo[:], mla_o_d.rearrange("h d t -> d h t"), writes=[b_mo])
                    S.dma(ho[:], hg_o_d.rearrange("h d t -> d h t"), writes=[b_ho])
                    S.dma(go[:], gdn_o_d.rearrange("h d t -> d h t"), writes=[b_go])
                    wgt = [tl(st2, "m_wg%d" % i, [128, KC, 3, 128], BF16) for i in range(2)]
                    wbr = [tl(st2, "m_wbr%d" % i, [128, 2, 4, 128], BF16) for i in range(2)]
                    wbm = [tl(st2, "m_wbm%d" % i, [64, 8, 128], BF16) for i in range(2)]
                    sg = [tl(st2, "m_sg%d" % i, [128, 512], F32) for i in range(3)]
                    ta, b_ta = tl(st2, "m_ta", [128, 512], F32)
                    tb, b_tb = tl(st2, "m_tb", [128, 512], F32)
                    mcnt = [0]
                    def load_dc(dc):
                        wg_t, b_wg = wgt[dc % 2]
                        wbr_t, b_wbr = wbr[dc % 2]
                        wbm_t, b_wbm = wbm[dc % 2]
                        for n_ in range(3):
                            c0 = O_GATES + n_ * D + dc * 128
                            S.dma(wg_t[:, :, n_, :], winv[:, :, c0:c0 + 128], writes=[b_wg], q="pool")
                        for n_ in range(2):
                            S.dma(wbr_t[:, n_, :, :], I["w_branch"][l, n_ + 1].rearrange("(kc p) n -> p kc n", p=128)[:, :, dc * 128:(dc + 1) * 128], writes=[b_wbr], q="pool")
                        S.dma(wbm_t[:], I["w_branch"][l, 0].rearrange("(h p) n -> p h n", p=64)[:, :, dc * 128:(dc + 1) * 128], writes=[b_wbm], q="pool")
                    load_dc(0)
                    for dc in range(KC):
                        wg_t, b_wg = wgt[dc % 2]
                        wbr_t, b_wbr = wbr[dc % 2]
                        wbm_t, b_wbm = wbm[dc % 2]
                        if dc + 1 < KC:
                            load_dc(dc + 1)
                        for (t0, n) in groups:
                            for n_ in range(3):
                                pg = mcnt[0] % 4
                                pp = 4 + mcnt[0] % 4
                                mcnt[0] += 1
                                for kc in range(KC):
                                    S.op("pe", lambda kc=kc, n_=n_, pg=pg: nc.tensor.matmul(PS[pg][:, 0:n], lhsT=wg_t[:, kc, n_, :], rhs=h_fm[:, kc, t0:t0 + n], start=(kc == 0), stop=(kc == KC - 1)),
                                         reads=[b_wg, b_hfm], writes=[PB[pg]])
                                S.op("act", lambda n_=n_, pg=pg: nc.scalar.activation(out=sg[n_][0][:, 0:n], in_=PS[pg][:, 0:n], func=AF.Sigmoid), reads=[PB[pg]], writes=[sg[n_][1]])
                                if n_ == 0:
                                    for h in range(8):
                                        S.op("pe", lambda h=h, pp=pp: nc.tensor.matmul(PS[pp][:, 0:n], lhsT=wbm_t[:, h, :], rhs=mo[:, h, t0:t0 + n], start=(h == 0), stop=(h == 7)), reads=[b_wbm, b_mo], writes=[PB[pp]])
                                else:
                                    src, b_src = (ho, b_ho) if n_ == 1 else (go, b_go)
                                    for kc in range(4):
                                        S.op("pe", lambda kc=kc, pp=pp, src=src, n_=n_: nc.tensor.matmul(PS[pp][:, 0:n], lhsT=wbr_t[:, n_ - 1, kc, :], rhs=src[:, kc, t0:t0 + n], start=(kc == 0), stop=(kc == 3)), reads=[b_wbr, b_src], writes=[PB[pp]])
                                if n_ == 0:
                                    S.op("dve", lambda pp=pp: nc.vector.tensor_tensor(out=ta[:, 0:n], in0=sg[0][0][:, 0:n], in1=PS[pp][:, 0:n], op=ALU.mult), reads=[sg[0][1], PB[pp]], writes=[b_ta])
                                elif n_ == 1:
                                    S.op("dve", lambda pp=pp: nc.vector.tensor_tensor(out=tb[:, 0:n], in0=sg[1][0][:, 0:n], in1=PS[pp][:, 0:n], op=ALU.mult), reads=[sg[1][1], PB[pp]], writes=[b_tb])
                                    S.op("pool", lambda: nc.gpsimd.tensor_tensor(out=ta[:, 0:n], in0=ta[:, 0:n], in1=tb[:, 0:n], op=ALU.add), reads=[b_ta, b_tb], writes=[b_ta])
                                else:
                                    S.op("dve", lambda pp=pp: nc.vector.tensor_tensor(out=tb[:, 0:n], in0=sg[2][0][:, 0:n], in1=PS[pp][:, 0:n], op=ALU.mult), reads=[sg[2][1], PB[pp], b_ta], writes=[b_tb])
                                    S.op("dve", lambda dc=dc: nc.vector.tensor_tensor(out=y_fm[:, dc, t0:t0 + n], in0=ta[:, 0:n], in1=tb[:, 0:n], op=ALU.add), reads=[b_ta, b_tb], writes=[b_y])
                    S.barrier()
                if "y_fm" in DBG:
                    S.dma(DBG["y_fm"].rearrange("(kc p) t -> p kc t", p=128), y_fm[:], reads=[b_y], q="pool")
                wo, b_wo = tl(st, "m_wo", [128, KC, D], BF16)
                S.dma(wo[:], I["w_out"][l].rearrange("(kc p) n -> p kc n", p=128), writes=[b_wo], q="pool")
                tiles, prm = ln_setup(st, l, 2, "ln1_g", "ln1_b", l, 3, 4, True)
                pendB = None
                for i in tiles_i:
                    ts_ = slice(i * 128, (i + 1) * 128)
                    for hf in range(2):
                        pb = 4 + hf
                        for kc in range(KC):
                            S.op("pe", lambda kc=kc, hf=hf, pb=pb: nc.tensor.matmul(PS[pb][:, :], lhsT=y_fm[:, kc, ts_], rhs=wo[:, kc, hf * 512:(hf + 1) * 512], start=(kc == 0), stop=(kc == KC - 1)),
                                 reads=[b_y, b_wo], writes=[PB[pb]])
                    A_, B_ = ln_tile(tiles, i, [PS[4][:, :], PS[5][:, :]], [PB[4], PB[5]], prm, False)
                    A_()
                    if pendB is not None:
                        pendB()
                    pendB = B_
                if pendB is not None:
                    pendB()
                S.barrier()

        def stage_moe(l, last):
            groups = GROUPS[1:] if last else GROUPS
            tiles_i = list(range(2, NT)) if last else list(range(NT))
            with contextlib.ExitStack() as st:
                acc, b_acc = tl(st, "acc", [128, NT, D], F32)
                comb, b_comb = tl(st, "comb", [128, NT, 65], F32)
                S.op("dve", lambda: nc.vector.memset(comb[:, :, 64:65], 1.0), writes=[b_comb])
                with contextlib.ExitStack() as st2:
                    wr, b_wr = tl(st2, "wr", [128, KC, 64], BF16)
                    S.dma(wr[:], I["w_router"][l].rearrange("(kc p) n -> p kc n", p=128), writes=[b_wr], q="pool")
                    rb, b_rb = load_vec_bc(st2, "rb", I["router_bias"][l:l + 1, :], n=64)
                    sc_ = [tl(st2, "r_sc%d" % i, [128, 64], F32) for i in range(2)]
                    sel = [tl(st2, "r_sel%d" % i, [128, 64], F32) for i in range(2)]
                    selm = [tl(st2, "r_selm%d" % i, [128, 64], F32) for i in range(2)]
                    m8 = [tl(st2, "r_m8%d" % i, [128, 8, 8], F32) for i in range(2)]
                    sm = [tl(st2, "r_sm%d" % i, [128, 40], F32) for i in range(2)]
                    for i in tiles_i:
                        ts_ = slice(i * 128, (i + 1) * 128)
                        pb = i % 2
                        sc_t, b_sc = sc_[i % 2]
                        sel_t, b_sel = sel[i % 2]
                        selm_t, b_selm = selm[i % 2]
                        m8_t, b_m8 = m8[i % 2]
                        sm_t, b_sm = sm[i % 2]
                        for kc in range(KC):
                            S.op("pe", lambda kc=kc: nc.tensor.matmul(PS[pb][:, 0:64], lhsT=h_fm[:, kc, ts_], rhs=wr[:, kc, :], start=(kc == 0), stop=(kc == KC - 1)), reads=[b_hfm, b_wr], writes=[PB[pb]])
                        S.op("act", lambda: nc.scalar.activation(out=sc_t[:], in_=PS[pb][:, 0:64], func=AF.Sigmoid), reads=[PB[pb]], writes=[b_sc])
                        S.op("dve", lambda: nc.vector.tensor_tensor(out=sel_t[:], in0=sc_t[:], in1=rb[:], op=ALU.add), reads=[b_sc, b_rb], writes=[b_sel])
                        for g8 in range(8):
                            S.op("dve", lambda g8=g8: nc.vector.max(out=m8_t[:, g8, :], in_=sel_t[:, g8 * 8:(g8 + 1) * 8]), reads=[b_sel], writes=[b_m8])
                        gs = sm_t[:, 0:8]
                        gm8 = sm_t[:, 8:16]
                        gmask = sm_t[:, 16:24]
                        pen = sm_t[:, 24:32]
                        t8 = sm_t[:, 32:40]
                        S.op("dve", lambda: nc.vector.tensor_tensor(out=gs, in0=m8_t[:, :, 0], in1=m8_t[:, :, 1], op=ALU.add), reads=[b_m8], writes=[b_sm])
                        S.op("dve", lambda: nc.vector.max(out=gm8, in_=gs), reads=[b_sm], writes=[b_sm])
                        S.op("dve", lambda: nc.vector.tensor_scalar(out=gmask, in0=gs, scalar1=sm_t[:, 11:12], scalar2=None, op0=ALU.is_ge), reads=[b_sm], writes=[b_sm])
                        S.op("dve", lambda: nc.vector.tensor_scalar(out=pen, in0=gmask, scalar1=10.0, scalar2=-10.0, op0=ALU.mult, op1=ALU.add), reads=[b_sm], writes=[b_sm])
                        sel3 = sel_t[:].rearrange("p (g x) -> p g x", g=8)
                        selm3 = selm_t[:].rearrange("p (g x) -> p g x", g=8)
                        S.op("dve", lambda: nc.vector.tensor_tensor(out=selm3, in0=sel3, in1=gmask.unsqueeze(2).to_broadcast([128, 8, 8]), op=ALU.mult), reads=[b_sel, b_sm], writes=[b_selm])
                        S.op("dve", lambda: nc.vector.tensor_tensor(out=selm3, in0=selm3, in1=pen.unsqueeze(2).to_broadcast([128, 8, 8]), op=ALU.add), reads=[b_selm, b_sm], writes=[b_selm])
                        S.op("dve", lambda: nc.vector.max(out=t8, in_=selm_t[:]), reads=[b_selm], writes=[b_sm])
                        S.op("dve", lambda: nc.vector.tensor_scalar(out=selm_t[:], in0=selm_t[:], scalar1=sm_t[:, 39:40], scalar2=None, op0=ALU.is_ge), reads=[b_selm, b_sm], writes=[b_selm])
                        S.op("dve", lambda: nc.vector.tensor_tensor(out=sel_t[:], in0=sc_t[:], in1=selm_t[:], op=ALU.mult), reads=[b_sc, b_selm], writes=[b_sel])
                        S.op("dve", lambda: nc.vector.tensor_reduce(out=sm_t[:, 0:1], in_=sel_t[:], axis=AX.X, op=ALU.add), reads=[b_sel], writes=[b_sm])
                        S.op("dve", lambda: nc.vector.reciprocal(out=sm_t[:, 1:2], in_=sm_t[:, 0:1]), reads=[b_sm], writes=[b_sm])
                        S.op("dve", lambda i=i: nc.vector.tensor_scalar(out=comb[:, i, 0:64], in0=sel_t[:], scalar1=sm_t[:, 1:2], scalar2=2.5, op0=ALU.mult, op1=ALU.mult), reads=[b_sel, b_sm], writes=[b_comb])
                    S.barrier()
                if "comb" in DBG:
                    S.dma(DBG["comb"].rearrange("(i p) e -> p i e", p=128), comb[:], reads=[b_comb])
                with contextlib.ExitStack() as st2:
                    wgu = [tl(st2, "wgu%d" % i, [128, KC, 512], BF16) for i in range(3)]
                    wdn = [tl(st2, "wdn%d" % i, [128, 2, D], BF16) for i in range(4)]
                    sgt = [tl(st2, "e_sg%d" % i, [128, 512], F32) for i in range(2)]
                    act = [tl(st2, "e_act%d" % i, [128, 2, 512], BF16) for i in range(2)]
                    ne = cfg.get("n_experts", 65)

                    def load_e(e):
                        wg_t, b_wg = wgu[e % 3]
                        wd_t, b_wd = wdn[e % 4]
                        if e < 64:
                            S.dma(wg_t[:], I["w_gu"][l, e].rearrange("(kc p) n -> p kc n", p=128), writes=[b_wg], q="pool")
                            S.dma(wd_t[:], I["w_down"][l, e].rearrange("(kc p) n -> p kc n", p=128), writes=[b_wd], q="pool")
                        else:
                            S.dma(wg_t[:], I["w_sh_gu"][l].rearrange("(kc p) n -> p kc n", p=128), writes=[b_wg], q="pool")
                            S.dma(wd_t[:], I["w_sh_down"][l].rearrange("(kc p) n -> p kc n", p=128), writes=[b_wd], q="pool")

                    elist = list(range(64 - (ne - 1), 65)) if ne < 65 else list(range(65))
                    load_e(elist[0])
                    if len(elist) > 1:
                        load_e(elist[1])
                    gcnt = 0
                    dcnt_ = [0]
                    pend_down = [None]
                    for ei, e in enumerate(elist):
                        need_load = ei + 2 < len(elist)
                        wg_t, b_wg = wgu[e % 3]
                        wd_t, b_wd = wdn[e % 4]
                        for (t0, n) in groups:
                            act_t, b_act = act[gcnt % 2]
                            gcnt += 1
                            pend_tiles = pend_down[0] if pend_down[0] is not None else []
                            pend_down[0] = None
                            if need_load:
                                load_e(elist[ei + 2])
                                need_load = False

                            def flush(k):
                                for _ in range(k):
                                    if pend_tiles:
                                        pend_tiles.pop(0)()
                            for c in range(2):
                                pg, pu = 2 * c, 2 * c + 1
                                for kc in range(KC):
                                    S.op("pe", lambda kc=kc, c=c, pg=pg: nc.tensor.matmul(PS[pg][:, 0:n], lhsT=wg_t[:, kc, c * 128:(c + 1) * 128], rhs=h_fm[:, kc, t0:t0 + n], start=(kc == 0), stop=(kc == KC - 1)),
                                         reads=[b_wg, b_hfm], writes=[PB[pg]])
                                flush(2)
                                for kc in range(KC):
                                    S.op("pe", lambda kc=kc, c=c, pu=pu: nc.tensor.matmul(PS[pu][:, 0:n], lhsT=wg_t[:, kc, 256 + c * 128:256 + (c + 1) * 128], rhs=h_fm[:, kc, t0:t0 + n], start=(kc == 0), stop=(kc == KC - 1)),
                                         reads=[b_wg, b_hfm], writes=[PB[pu]])
                                sg_t, b_sg = sgt[c]
                                S.op("act", lambda pg=pg, sg_t=sg_t: nc.scalar.activation(out=sg_t[:, 0:n], in_=PS[pg][:, 0:n], func=AF.Silu), reads=[PB[pg]], writes=[b_sg])
                                S.op("dve", lambda c=c, pu=pu, sg_t=sg_t, act_t=act_t: nc.vector.tensor_tensor(out=act_t[:, c, 0:n], in0=sg_t[:, 0:n], in1=PS[pu][:, 0:n], op=ALU.mult), reads=[b_sg, PB[pu]], writes=[b_act])
                                flush(2)
                            flush(99)

                            def mk_tiles(t0=t0, n=n, act_t=act_t, b_act=b_act, wd_t=wd_t, b_wd=b_wd, ei=ei, e=e):
                                fs = []
                                for tt in range(n // 128):
                                    for hf in range(2):
                                        def one(tt=tt, hf=hf):
                                            i = t0 // 128 + tt
                                            pd = 4 + dcnt_[0] % 4
                                            dcnt_[0] += 1
                                            for c in range(2):
                                                S.op("pe", lambda c=c: nc.tensor.matmul(PS[pd][:, :], lhsT=act_t[:, c, tt * 128:(tt + 1) * 128], rhs=wd_t[:, c, hf * 512:(hf + 1) * 512], start=(c == 0), stop=(c == 1)),
                                                     reads=[b_act, b_wd], writes=[PB[pd]])
                                            if ei == 0:
                                                S.op("dve", lambda: nc.vector.tensor_scalar(out=acc[:, i, hf * 512:(hf + 1) * 512], in0=PS[pd][:, :], scalar1=comb[:, i, e:e + 1], scalar2=None, op0=ALU.mult),
                                                     reads=[PB[pd], b_comb], writes=[b_acc])
                                            else:
                                                S.op("dve", lambda: nc.vector.scalar_tensor_tensor(out=acc[:, i, hf * 512:(hf + 1) * 512], in0=PS[pd][:, :], scalar=comb[:, i, e:e + 1], in1=acc[:, i, hf * 512:(hf + 1) * 512], op0=ALU.mult, op1=ALU.add),
                                                     reads=[PB[pd], b_comb, b_acc], writes=[b_acc])
                                        fs.append(one)
                                return fs
                            pend_down[0] = mk_tiles()
                    if pend_down[0] is not None:
                        for f in pend_down[0]:
                            f()
                    S.barrier()
                if "ff" in DBG:
                    S.dma(DBG["ff"].rearrange("(i p) d -> p i d", p=128), acc[:], reads=[b_acc])
                final = (l == nlayers - 1)
                tiles, prm = ln_setup(st, l + 1, 5, "ln2_g", "ln2_b", l, 0, 1, not final)
                pendB = None
                for i in tiles_i:
                    A_, B_ = ln_tile(tiles, i, [acc[:, i, 0:512], acc[:, i, 512:1024]], [b_acc, b_acc], prm, final)
                    A_()
                    if pendB is not None:
                        pendB()
                    pendB = B_
                if pendB is not None:
                    pendB()
                S.barrier()

        def stage_mla(l, last):
            with contextlib.ExitStack() as st:
                winv = I["w_in"][l].rearrange("(kc p) n -> p kc n", p=128)
                wA, b_wA = tl(st, "wA", [128, KC, 672], BF16)
                S.dma(wA[:], winv[:, :, 0:672], writes=[b_wA], q="pool")
                wKs, b_wKs = tl(st, "wKs", [128, KC, 32], BF16)
                S.dma(wKs[:], I["w_kr_sw"][l].rearrange("(kc p) n -> p kc n", p=128), writes=[b_wKs], q="pool")
                wQ, b_wQ = tl(st, "wQ", [128, 3, 768], BF16)
                S.dma(wQ[:], I["w_q_b"][l].rearrange("(kc p) n -> p kc n", p=128), writes=[b_wQ], q="pool")
                wQs, b_wQs = tl(st, "wQs", [128, 3, 256], BF16)
                S.dma(wQs[:], I["w_qr_sw"][l].rearrange("(kc p) n -> p kc n", p=128), writes=[b_wQs], q="pool")
                wKV, b_wKV = tl(st, "wKV", [128, 2, 1024], BF16)
                S.dma(wKV[:], I["w_kv_b"][l].rearrange("(kc p) n -> p kc n", p=128), writes=[b_wKV], q="pool")
                gq, b_gq = tl(st, "gq", [128, 3], F32)
                S.dma(gq[:], I["q_a_norm_t"][l], writes=[b_gq])
                gkv, b_gkv = tl(st, "gkv", [128, 2], F32)
                S.dma(gkv[:], I["kv_a_norm_t"][l], writes=[b_gkv])
                ropeC, b_rC = tl(st, "ropeC", [96, LAT], F32)
                ropeS, b_rS = tl(st, "ropeS", [96, LAT], F32)
                S.dma(ropeC[64:96, :], I["ropeC"][:, :], writes=[b_rC])
                S.dma(ropeS[64:96, :], I["ropeS"][:, :], writes=[b_rS])
                qan, b_qan = tl(st, "qan", [128, 3, T], BF16)
                kvan, b_kvan = tl(st, "kvan", [128, 2, T], BF16)
                kr, b_kr = tl(st, "kr", [96, T], BF16)
                raw = [tl(st, "raw%d" % i, [128, 512], F32) for i in range(5)]
                sq = [tl(st, "sq%d" % i, [128, 512], BF16) for i in range(5)]
                rs = [tl(st, "rs%d" % i, [128, 512], F32) for i in range(2)]
                rt = [tl(st, "rt%d" % i, [96, 512], F32) for i in range(2)]

                def rope_or_copy(dst, b_dst, t0, n, pa, pb, isctx):
                    if isctx:
                        S.op("act", lambda: nc.scalar.copy(out=dst[64:96, t0:t0 + n], in_=PS[pa][64:96, 0:n]), reads=[PB[pa]], writes=[b_dst])
                        return
                    l0 = t0 - CTX
                    S.op("dve", lambda: nc.vector.tensor_tensor(out=rt[0][0][64:96, 0:n], in0=PS[pa][64:96, 0:n], in1=ropeC[64:96, l0:l0 + n], op=ALU.mult),
                         reads=[PB[pa], b_rC], writes=[rt[0][1]])
                    S.op("dve", lambda: nc.vector.tensor_tensor(out=rt[1][0][64:96, 0:n], in0=PS[pb][64:96, 0:n], in1=ropeS[64:96, l0:l0 + n], op=ALU.mult),
                         reads=[PB[pb], b_rS], writes=[rt[1][1]])
                    S.op("dve", lambda: nc.vector.tensor_tensor(out=dst[64:96, t0:t0 + n], in0=rt[0][0][64:96, 0:n], in1=rt[1][0][64:96, 0:n], op=ALU.add),
                         reads=[rt[0][1], rt[1][1]], writes=[b_dst])

                def rmsnorm_group(col0, nchunk, gvec, b_gvec, dst, b_dst, t0, n, pbase, ri):
                    for c in range(nchunk):
                        pb = pbase + c
                        for kc in range(KC):
                            S.op("pe", lambda kc=kc, c=c, pb=pb: nc.tensor.matmul(PS[pb][:, 0:n], lhsT=wA[:, kc, col0 + c * 128:col0 + (c + 1) * 128],
                                                                                  rhs=h_fm[:, kc, t0:t0 + n], start=(kc == 0), stop=(kc == KC - 1)),
                                 reads=[b_wA, b_hfm], writes=[PB[pb]])
                        rw, b_rw = raw[ri + c]
                        sqt, b_sq = sq[ri + c]
                        S.op("act", lambda rw=rw, pb=pb: nc.scalar.copy(out=rw[:, 0:n], in_=PS[pb][:, 0:n]), reads=[PB[pb]], writes=[b_rw])
                        S.op("act", lambda sqt=sqt, pb=pb: nc.scalar.activation(out=sqt[:, 0:n], in_=PS[pb][:, 0:n], func=AF.Square), reads=[PB[pb]], writes=[b_sq])
                    pss = pbase + nchunk
                    for c in range(nchunk):
                        S.op("pe", lambda c=c: nc.tensor.matmul(PS[pss][:, 0:n], lhsT=onesb[:], rhs=sq[ri + c][0][:, 0:n], start=(c == 0), stop=(c == nchunk - 1)),
                             reads=[b_onesb, sq[ri + c][1]], writes=[PB[pss]])
                    r0, b_r0 = rs[0]
                    r1, b_r1 = rs[1]
                    S.op("act", lambda: nc.scalar.activation(out=r0[:, 0:n], in_=PS[pss][:, 0:n], func=AF.Sqrt, scale=1.0 / (128 * nchunk), bias=epsb[:, 0:1]),
                         reads=[PB[pss], b_eps], writes=[b_r0])
                    S.op("dve", lambda: nc.vector.reciprocal(out=r1[:, 0:n], in_=r0[:, 0:n]), reads=[b_r0], writes=[b_r1])
                    for c in range(nchunk):
                        S.op("dve", lambda c=c: nc.vector.scalar_tensor_tensor(out=dst[:, c, t0:t0 + n], in0=raw[ri + c][0][:, 0:n], scalar=gvec[:, c:c + 1],
                                                                                 in1=r1[:, 0:n], op0=ALU.mult, op1=ALU.mult),
                             reads=[raw[ri + c][1], b_gvec, b_r1], writes=[b_dst])

                for gi, (t0, n) in enumerate(GROUPS):
                    rmsnorm_group(0, 3, gq, b_gq, qan, b_qan, t0, n, 0, 0)
                    rmsnorm_group(384, 2, gkv, b_gkv, kvan, b_kvan, t0, n, 4, 3)
                    for kc in range(KC):
                        S.op("pe", lambda kc=kc: nc.tensor.matmul(PS[7][64:96, 0:n], lhsT=wA[:, kc, 640:672], rhs=h_fm[:, kc, t0:t0 + n], start=(kc == 0), stop=(kc == KC - 1)),
                             reads=[b_wA, b_hfm], writes=[PB[7]])
                    for kc in range(KC):
                        S.op("pe", lambda kc=kc: nc.tensor.matmul(PS[3][64:96, 0:n], lhsT=wKs[:, kc, :], rhs=h_fm[:, kc, t0:t0 + n], start=(kc == 0), stop=(kc == KC - 1)),
                             reads=[b_wKs, b_hfm], writes=[PB[3]])
                    rope_or_copy(kr, b_kr, t0, n, 7, 3, gi == 0)

                v_aug, b_va = tl(st, "v_aug", [128, NT, 8, 65], BF16)
                S.op("dve", lambda: nc.vector.memset(v_aug[:, :, :, 64:65], 1.0), writes=[b_va])
                wKVh = wKV[:].rearrange("p c (h x) -> p c h x", h=8)
                for i in range(NT):
                    pb = i % 2
                    for c in range(2):
                        S.op("pe", lambda c=c, i=i, pb=pb: nc.tensor.matmul(PS[pb][:, :].rearrange("p (h x) -> p h x", h=8), lhsT=kvan[:, c, i * 128:(i + 1) * 128],
                                                                            rhs=wKVh[:, c, :, 64:128], start=(c == 0), stop=(c == 1)),
                             reads=[b_kvan, b_wKV], writes=[PB[pb]])
                    S.op("act", lambda i=i, pb=pb: nc.scalar.copy(out=v_aug[:, i, :, 0:64], in_=PS[pb][:, :].rearrange("p (h x) -> p h x", h=8)),
                         reads=[PB[pb]], writes=[b_va])

                qn = [tl(st, "qn%d" % i, [96, T], BF16) for i in range(2)]
                kn = [tl(st, "kn%d" % i, [96, T], BF16) for i in range(2)]
                Et = [tl(st, "Et%d" % i, [128, 512], BF16) for i in range(5)]
                rc, b_rc = tl(st, "rc", [65, 512], F32)
                numt = [tl(st, "numt%d" % i, [64, 512], F32) for i in range(2)]
                ot = [tl(st, "ot%d" % i, [64, 512], BF16) for i in range(2)]
                b_mo = Buf("mla_o")
                ecnt = 0
                ocnt = 0
                for h in range(8):
                    qn_t, b_qn = qn[h % 2]
                    kn_t, b_kn = kn[h % 2]
                    S.op("pool", lambda: nc.gpsimd.tensor_copy(out=kn_t[64:96, :], in_=kr[64:96, :]), reads=[b_kr], writes=[b_kn])
                    for gi, (t0, n) in enumerate(GROUPS):
                        if not (last and gi == 0):
                            for c in range(3):
                                S.op("pe", lambda c=c: nc.tensor.matmul(PS[0][0:64, 0:n], lhsT=wQ[:, c, 96 * h:96 * h + 64], rhs=qan[:, c, t0:t0 + n], start=(c == 0), stop=(c == 2)),
                                     reads=[b_wQ, b_qan], writes=[PB[0]])
                            S.op("act", lambda: nc.scalar.copy(out=qn_t[0:64, t0:t0 + n], in_=PS[0][0:64, 0:n]), reads=[PB[0]], writes=[b_qn])
                            for c in range(3):
                                S.op("pe", lambda c=c: nc.tensor.matmul(PS[1][64:96, 0:n], lhsT=wQ[:, c, 96 * h + 64:96 * h + 96], rhs=qan[:, c, t0:t0 + n], start=(c == 0), stop=(c == 2)),
                                     reads=[b_wQ, b_qan], writes=[PB[1]])
                            for c in range(3):
                                S.op("pe", lambda c=c: nc.tensor.matmul(PS[2][64:96, 0:n], lhsT=wQs[:, c, 32 * h:32 * h + 32], rhs=qan[:, c, t0:t0 + n], start=(c == 0), stop=(c == 2)),
                                     reads=[b_wQs, b_qan], writes=[PB[2]])
                            rope_or_copy(qn_t, b_qn, t0, n, 1, 2, gi == 0)
                        for c in range(2):
                            S.op("pe", lambda c=c: nc.tensor.matmul(PS[3][0:64, 0:n], lhsT=wKV[:, c, 128 * h:128 * h + 64], rhs=kvan[:, c, t0:t0 + n], start=(c == 0), stop=(c == 1)),
                                 reads=[b_wKV, b_kvan], writes=[PB[3]])
                        S.op("act", lambda: nc.scalar.copy(out=kn_t[0:64, t0:t0 + n], in_=PS[3][0:64, 0:n]), reads=[PB[3]], writes=[b_kn])
                    for gi, (t0, n) in enumerate(GROUPS):
                        if last and gi == 0:
                            continue
                        kts = list(range(2)) if gi == 0 else list(range(NT))
                        pend = []
                        sbanks = [4, 5, 0, 1]
                        for ki, kt in enumerate(kts):
                            psb = sbanks[ecnt % 4]
                            E_t, b_E = Et[ecnt % 5]
                            ecnt += 1
                            S.op("pe", lambda kt=kt, psb=psb: nc.tensor.matmul(PS[psb][:, 0:n], lhsT=kn_t[:, kt * 128:(kt + 1) * 128], rhs=qn_t[:, t0:t0 + n], start=True, stop=True),
                                 reads=[b_kn, b_qn], writes=[PB[psb]])
                            S.op("act", lambda psb=psb, E_t=E_t: nc.scalar.activation(out=E_t[:, 0:n], in_=PS[psb][:, 0:n], func=AF.Exp, scale=MLA_SCALE),
                                 reads=[PB[psb]], writes=[b_E])
                            pend.append(lambda kt=kt, E_t=E_t, b_E=b_E, ki=ki: S.op("pe", lambda: nc.tensor.matmul(PS[6][0:65, 0:n], lhsT=v_aug[:, kt, h, :], rhs=E_t[:, 0:n], start=(ki == 0), stop=(ki == len(kts) - 1)),
                                 reads=[b_va, b_E], writes=[PB[6]]))
                            if len(pend) > 2:
                                pend.pop(0)()
                        while pend:
                            pend.pop(0)()
                        nm_t, b_nm = numt[ocnt % 2]
                        o_t, b_o = ot[ocnt % 2]
                        ocnt += 1
                        S.op("dve", lambda: nc.vector.reciprocal(out=rc[64:65, 0:n], in_=PS[6][64:65, 0:n]), reads=[PB[6]], writes=[b_rc])
                        S.op("act", lambda nm_t=nm_t: nc.scalar.copy(out=nm_t[:, 0:n], in_=PS[6][0:64, 0:n]), reads=[PB[6]], writes=[b_nm])
                        S.op("pe", lambda: nc.tensor.matmul(PS[7][0:64, 0:n], lhsT=onesf[64:65, 0:64], rhs=rc[64:65, 0:n], start=True, stop=True),
                             reads=[b_onesf, b_rc], writes=[PB[7]])
                        S.op("dve", lambda nm_t=nm_t, o_t=o_t: nc.vector.tensor_tensor(out=o_t[:, 0:n], in0=nm_t[:, 0:n], in1=PS[7][0:64, 0:n], op=ALU.mult),
                             reads=[b_nm, PB[7]], writes=[b_o])
                        S.dma(mla_o_d[h, :, t0:t0 + n], o_t[:, 0:n], reads=[b_o], writes=[b_mo])
                S.barrier()

        b_xres = [Buf("xres%d" % i) for i in range(NT)]
        stage_entry(0)
        for l in range(nlayers):
            last = (l == nlayers - 1)
            if not cfg.get("skip_mla"):
                stage_mla(l, last)
            if not cfg.get("skip_hg"):
                stage_hgrn(l)
            if not cfg.get("skip_gdn"):
                stage_gdn(l)
            if cfg.get("stop_after") == "mixers":
                break
            stage_merge(l, last)
            if cfg.get("stop_after") == "merge":
                break
            stage_moe(l, last)
        if "h_fm" in DBG:
            S.dma(DBG["h_fm"].rearrange("(kc p) t -> p kc t", p=128), h_fm[:], reads=[b_hfm], q="pool")
        if "xres" in DBG:
            S.dma(DBG["xres"], xres_d[:, :])
        if "mla_o" in DBG:
            S.dma(DBG["mla_o"], mla_o_d.rearrange("h d t -> (h d) t"), q="pool")
        if "hg_o" in DBG:
            S.dma(DBG["hg_o"], hg_o_d.rearrange("h d t -> (h d) t"), q="pool")
        if "gdn_o" in DBG:
            S.dma(DBG["gdn_o"], gdn_o_d.rearrange("h d t -> (h d) t"), q="pool")
        K.PS, K.PB = PS, PB

        S.finish()
    K.ninstr = S.ninstr
    return nc, K


WEIGHT_SHAPES = {
    "w_mod": [DEPTH, D, 6 * D], "b_mod": [DEPTH, 6 * D], "w_in": [DEPTH, D, IN_W],
    "w_q_b": [DEPTH, 384, 768], "w_kv_b": [DEPTH, 256, 1024],
    "hg_lb_logits": [DEPTH, 2, 512],
    "gdn_a_log": [DEPTH, 2, 4], "gdn_dt_bias": [DEPTH, 2, 4], "gdn_norm": [DEPTH, 128],
    "w_branch": [DEPTH, 3, 512, D], "w_out": [DEPTH, D, D],
    "ln1_g": [DEPTH, D], "ln1_b": [DEPTH, D], "ln2_g": [DEPTH, D], "ln2_b": [DEPTH, D],
    "w_router": [DEPTH, D, 64], "router_bias": [DEPTH, 64],
    "w_gu": [DEPTH, 64, D, 512], "w_down": [DEPTH, 64, 256, D], "w_sh_gu": [DEPTH, D, 512], "w_sh_down": [DEPTH, 256, D],
}
DERIVED_SHAPES = {
    "w_kr_sw": [DEPTH, D, 32], "w_qr_sw": [DEPTH, 384, 256],
    "q_a_norm_t": [DEPTH, 128, 3], "kv_a_norm_t": [DEPTH, 128, 2],
    "hg_norm_t": [128, DEPTH], "gdn_conv_t": [DEPTH, 128, 12, 5],
}
CONST_SHAPES = {
    "ident": [128, 128], "ropeC": [32, LAT], "ropeS": [32, LAT],
    "rmask": [128, T], "triu": [64, 64], "tril": [64, 64],
    "mist": [64, 2, 64], "mast": [64, 2, 64],
}


def host_consts():
    c = {}
    c["ident"] = np.eye(128, dtype=np.float32)
    pos = np.arange(LAT)
    row = (pos // 64).astype(np.float32)
    col = (pos % 64).astype(np.float32)
    inv = (np.float32(10000.0) ** (-np.arange(8, dtype=np.float32) / np.float32(8))).astype(np.float32)
    C = np.zeros((32, LAT), np.float32)
    Sg = np.zeros((32, LAT), np.float32)
    for ax, p in enumerate((row, col)):
        ang = (p[None, :] * inv[:, None]).astype(np.float32)
        for half in range(2):
            r0 = ax * 16 + half * 8
            C[r0:r0 + 8] = np.cos(ang)
            Sg[r0:r0 + 8] = np.sin(ang) * (-1.0 if half == 0 else 1.0)
    c["ropeC"] = C
    rm = np.ones((128, T), np.float32)
    rm[:, ::64] = 0.0
    c["rmask"] = rm
    c["triu"] = np.triu(np.ones((64, 64), np.float32))
    c["tril"] = np.tril(np.ones((64, 64), np.float32))
    c["mist"] = np.ascontiguousarray(np.stack([c["triu"], c["tril"]], axis=1))
    c["mast"] = np.ascontiguousarray(np.stack([c["tril"] - np.eye(64, dtype=np.float32), c["triu"] - np.eye(64, dtype=np.float32)], axis=1))
    c["ropeS"] = Sg
    return c


def prep_inputs(inputs):
    x = np.asarray(inputs["x"], np.float32)
    ctx = np.asarray(inputs["ctx"], np.float32)
    c = np.asarray(inputs["c"], np.float32)
    c_ctx = np.asarray(inputs["c_ctx"], np.float32)
    shared = {}
    for nm in WEIGHT_SHAPES:
        shared[nm] = np.ascontiguousarray(np.asarray(inputs[nm], np.float32)).reshape(WEIGHT_SHAPES[nm])
    shared.update(host_consts())
    perm = np.arange(32) ^ 8
    w_in = shared["w_in"]
    shared["w_kr_sw"] = np.ascontiguousarray(w_in[:, :, 640:672][:, :, perm])
    wqb = shared["w_q_b"].reshape(DEPTH, 384, 8, 96)
    shared["w_qr_sw"] = np.ascontiguousarray(wqb[:, :, :, 64:96][:, :, :, perm].reshape(DEPTH, 384, 256))
    shared["q_a_norm_t"] = np.ascontiguousarray(np.asarray(inputs["q_a_norm"], np.float32).reshape(DEPTH, 3, 128).transpose(0, 2, 1))
    shared["gdn_conv_t"] = np.ascontiguousarray(np.asarray(inputs["gdn_conv"], np.float32).reshape(DEPTH, 5, 12, 128).transpose(0, 3, 2, 1))
    shared["hg_norm_t"] = np.ascontiguousarray(np.asarray(inputs["hg_norm"], np.float32).T)
    shared["kv_a_norm_t"] = np.ascontiguousarray(np.asarray(inputs["kv_a_norm"], np.float32).reshape(DEPTH, 2, 128).transpose(0, 2, 1))
    maps = []
    for b in range(x.shape[0]):
        m = dict(shared)
        m["xin"] = np.ascontiguousarray(np.concatenate([ctx[b], x[b]], axis=0))
        m["cvecT"] = np.ascontiguousarray(np.stack([c[b], c_ctx], axis=1))
        maps.append(m)
    return maps


def kernel(**inputs):
    maps = prep_inputs(inputs)
    nc, K = build_program({})
    res = run_bass_kernel_spmd(nc, maps, core_ids=list(range(8)))
    out = np.stack([np.asarray(r["out"], np.float32) for r in res.results], axis=0)
    return out
```
